# Optimizing a Trainium2 kernel written in Bass

```python
import jax
import jax.numpy as jnp
from jax import lax
import numpy as np

D_MODEL = 1024
BATCH = 8
SEQ = 2048
DEPTH = 2

HEAD_DIM = 64
MIX_WIDTH = D_MODEL
GROUP_WIDTH = MIX_WIDTH // 2
N_HEADS = GROUP_WIDTH // HEAD_DIM
CONV_WIDTH = 31
MOBA_BLOCK = 256
MOBA_TOPK = 3
MOBA_Q_CHUNK = 32
NSA_KV_HEADS = 2
NSA_KV_WIDTH = NSA_KV_HEADS * HEAD_DIM
NSA_CMP_LEN = 32
NSA_CMP_STRIDE = 16
NSA_CMP_HIDDEN = 256
NSA_SEL_BLOCK = 64
NSA_TOPN = 16
NSA_WINDOW = 512
NSA_Q_CHUNK = 64
NSA_FORCE = 1e4
SWA_KV_HEADS = 2
SWA_KV_WIDTH = SWA_KV_HEADS * HEAD_DIM
SWA_WINDOW = 128
BAND_Q_BLOCK = 128
EPS = 1e-6
NEG = -1e30
TINY = 1e-30

EVEN_SIZES = (GROUP_WIDTH, GROUP_WIDTH, GROUP_WIDTH, GROUP_WIDTH, GROUP_WIDTH, GROUP_WIDTH, GROUP_WIDTH)
ODD_SIZES = (GROUP_WIDTH, NSA_KV_WIDTH, NSA_KV_WIDTH, NSA_KV_WIDTH, NSA_KV_WIDTH, NSA_KV_WIDTH,
             NSA_KV_WIDTH, 3 * N_HEADS, GROUP_WIDTH, GROUP_WIDTH, SWA_KV_WIDTH, SWA_KV_WIDTH, GROUP_WIDTH)
EVEN_IN = sum(EVEN_SIZES)
ODD_IN = sum(ODD_SIZES)

kernel_name = 'hybrid_conv_moba_nsa_swa_sink_block'


def _split(t, sizes):
    points = np.cumsum(sizes)[:-1].tolist()
    return jnp.split(t, points, axis=-1)


def _rmsnorm(x, g):
    xf = x.astype(jnp.float32)
    y = xf * lax.rsqrt(jnp.mean(xf * xf, axis=-1, keepdims=True) + EPS)
    return (y * g.astype(jnp.float32)).astype(x.dtype)


def _layernorm(x, g, b):
    xf = x.astype(jnp.float32)
    mu = jnp.mean(xf, axis=-1, keepdims=True)
    xc = xf - mu
    y = xc * lax.rsqrt(jnp.mean(xc * xc, axis=-1, keepdims=True) + EPS)
    return (y * g.astype(jnp.float32) + b.astype(jnp.float32)).astype(x.dtype)


def _split_heads(t, n):
    b, s, _ = t.shape
    return t.reshape(b, s, n, HEAD_DIM).transpose(0, 2, 1, 3)


def _merge_heads(t):
    b, n, s, d = t.shape
    return t.transpose(0, 2, 1, 3).reshape(b, s, n * d)


def _alibi_slopes(n):
    return jnp.asarray(np.power(2.0, -8.0 * np.arange(1, n + 1) / n).astype(np.float32))


def _masked_softmax(s, mask, sink=None):
    s = jnp.where(mask, s, NEG)
    m = jnp.max(s, axis=-1, keepdims=True)
    if sink is not None:
        m = jnp.maximum(m, sink)
    e = jnp.where(mask, jnp.exp(s - m), 0.0)
    den = jnp.sum(e, axis=-1, keepdims=True)
    if sink is not None:
        den = den + jnp.exp(sink - m)
    return e / jnp.maximum(den, TINY)


def _conformer_conv(u_val, u_gate, w_dw, b_dw, ln_g, ln_b):
    c = u_val.shape[-1]
    h = u_val * jax.nn.sigmoid(u_gate)
    h = jnp.pad(h, ((0, 0), (CONV_WIDTH - 1, 0), (0, 0)))
    y = lax.conv_general_dilated(h, w_dw[:, None, :].astype(h.dtype), (1,), 'VALID',
                                 dimension_numbers=('NWC', 'WIO', 'NWC'), feature_group_count=c)
    y = _layernorm(y + b_dw.astype(y.dtype), ln_g, ln_b)
    return jax.nn.silu(y)


def _moba_attention(q, k, v, slopes):
    b, h, s, d = q.shape
    nblk = -(-s // MOBA_BLOCK)
    sp = nblk * MOBA_BLOCK
    padw = ((0, 0), (0, 0), (0, sp - s), (0, 0))
    q, k, v = jnp.pad(q, padw), jnp.pad(k, padw), jnp.pad(v, padw)
    kb = k.reshape(b, h, nblk, MOBA_BLOCK, d)
    vb = v.reshape(b, h, nblk, MOBA_BLOCK, d)
    kmean = jnp.mean(kb.astype(jnp.float32), axis=3)
    n_sel = min(MOBA_TOPK, nblk)
    n_g = n_sel * MOBA_BLOCK
    scale = d ** -0.5
    bi = jnp.arange(b)[:, None, None, None]
    hi = jnp.arange(h)[None, :, None, None]
    offs = jnp.arange(MOBA_BLOCK)
    blk_ids = jnp.arange(nblk)
    sl = slopes[:, None, None]

    def chunk(c):
        t0 = c * MOBA_Q_CHUNK
        own = t0 // MOBA_BLOCK
        qc = lax.dynamic_slice_in_dim(q, t0, MOBA_Q_CHUNK, axis=2)
        tp = t0 + jnp.arange(MOBA_Q_CHUNK)
        gate = jnp.einsum('bhtd,bhnd->bhtn', qc.astype(jnp.float32), kmean)
        gate = jnp.where(blk_ids < own, gate, NEG)
        _, idx = lax.top_k(gate, n_sel)
        ks = kb[bi, hi, idx]
        vs = vb[bi, hi, idx]
        kpos = idx[..., None] * MOBA_BLOCK + offs
        dist = (tp[:, None, None] - kpos).astype(jnp.float32)
        s_sel = jnp.einsum('bhtd,bhtnld->bhtnl', qc, ks).astype(jnp.float32) * scale - sl[..., None] * dist
        m_sel = jnp.broadcast_to((idx < own)[..., None], s_sel.shape)
        k_own = lax.dynamic_index_in_dim(kb, own, axis=2, keepdims=False)
        v_own = lax.dynamic_index_in_dim(vb, own, axis=2, keepdims=False)
        kpos_own = own * MOBA_BLOCK + offs
        dist_own = (tp[:, None] - kpos_own[None, :]).astype(jnp.float32)
        s_own = jnp.einsum('bhtd,bhld->bhtl', qc, k_own).astype(jnp.float32) * scale - sl * dist_own
        m_own = jnp.broadcast_to(dist_own >= 0, s_own.shape)
        scores = jnp.concatenate([s_sel.reshape(b, h, MOBA_Q_CHUNK, n_g), s_own], axis=-1)
        mask = jnp.concatenate([m_sel.reshape(b, h, MOBA_Q_CHUNK, n_g), m_own], axis=-1)
        p = _masked_softmax(scores, mask).astype(v.dtype)
        out = jnp.einsum('bhtm,bhtmd->bhtd', p[..., :n_g], vs.reshape(b, h, MOBA_Q_CHUNK, n_g, d))
        return out + jnp.einsum('bhtl,bhld->bhtd', p[..., n_g:], v_own)

    outs = lax.map(chunk, jnp.arange(sp // MOBA_Q_CHUNK))
    outs = jnp.moveaxis(outs, 0, 2).reshape(b, h, sp, d)
    return outs[:, :, :s]


def _banded_attention(q, k, v, sl, window, sink=None):
    b, g, r, s, d = q.shape
    nq = s // BAND_Q_BLOCK
    span = window + BAND_Q_BLOCK
    padw = ((0, 0), (0, 0), (window, 0), (0, 0))
    kp, vp = jnp.pad(k, padw), jnp.pad(v, padw)
    kidx = jnp.arange(nq)[:, None] * BAND_Q_BLOCK + jnp.arange(span)[None, :]
    kband = kp[:, :, kidx]
    vband = vp[:, :, kidx]
    qb = q.reshape(b, g, r, nq, BAND_Q_BLOCK, d)
    tpos = jnp.arange(s).reshape(nq, BAND_Q_BLOCK)
    kpos = kidx - window
    dist = tpos[:, :, None] - kpos[:, None, :]
    mask = (dist >= 0) & (dist < window) & (kpos[:, None, :] >= 0)
    scores = (jnp.einsum('bgrnqd,bgnkd->bgrnqk', qb, kband).astype(jnp.float32) * d ** -0.5
              - sl[:, :, None, None, None] * dist.astype(jnp.float32))
    sk = None if sink is None else sink.astype(jnp.float32)[:, :, None, None, None]
    p = _masked_softmax(scores, mask, sk)
    out = jnp.einsum('bgrnqk,bgnkd->bgrnqd', p.astype(v.dtype), vband)
    return out.reshape(b, g, r, s, d)


def _compress(x, pos, w1, b1, w2, b2):
    s = x.shape[2]
    n_cmp = (s - NSA_CMP_LEN) // NSA_CMP_STRIDE + 1
    idx = jnp.arange(n_cmp)[:, None] * NSA_CMP_STRIDE + jnp.arange(NSA_CMP_LEN)[None, :]
    blocks = x[:, :, idx] + pos.astype(x.dtype)
    flat = blocks.reshape(blocks.shape[0], blocks.shape[1], n_cmp, NSA_CMP_LEN * x.shape[-1])
    hid = jax.nn.silu(flat @ w1 + b1)
    return hid @ w2 + b2


def _overlap_matrix(n_cmp, nsb):
    start = np.arange(n_cmp)[:, None] * NSA_CMP_STRIDE
    bs = np.arange(nsb)[None, :] * NSA_SEL_BLOCK
    return jnp.asarray(((start < bs + NSA_SEL_BLOCK) & (start + NSA_CMP_LEN > bs)).astype(np.float32))


def _nsa_attention(q, k_cmp, v_cmp, k_slc, v_slc, k_win, v_win, gates, kn_cmp,
                   pos_k, pos_v, kw1, kb1, kw2, kb2, vw1, vb1, vw2, vb2, slopes):
    b, h, s, d = q.shape
    g = k_slc.shape[1]
    r = h // g
    scale = d ** -0.5
    qg = q.reshape(b, g, r, s, d)
    sl = slopes.reshape(g, r)
    tpos = jnp.arange(s)
    kc = _rmsnorm(_compress(k_cmp, pos_k, kw1, kb1, kw2, kb2), kn_cmp)
    vc = _compress(v_cmp, pos_v, vw1, vb1, vw2, vb2)
    n_cmp = kc.shape[2]
    cend = jnp.arange(n_cmp) * NSA_CMP_STRIDE + NSA_CMP_LEN - 1
    dist_c = tpos[:, None] - cend[None, :]
    s_c = (jnp.einsum('bgrtd,bgcd->bgrtc', qg, kc).astype(jnp.float32) * scale
           - sl[:, :, None, None] * dist_c.astype(jnp.float32))
    p_c = _masked_softmax(s_c, dist_c >= 0)
    o_cmp = jnp.einsum('bgrtc,bgcd->bgrtd', p_c.astype(vc.dtype), vc)
    nsb = s // NSA_SEL_BLOCK
    imp = jnp.einsum('bgrtc,cn->bgtn', p_c, _overlap_matrix(n_cmp, nsb))
    blk = jnp.arange(nsb)[None, :]
    cur = (tpos // NSA_SEL_BLOCK)[:, None]
    forced = (blk == 0) | (blk == cur) | (blk == cur - 1)
    imp = jnp.where(forced, NSA_FORCE, imp)
    imp = jnp.where(blk <= cur, imp, NEG)
    n_top = min(NSA_TOPN, nsb)
    _, sel = lax.top_k(imp, n_top)
    kbk = k_slc.reshape(b, g, nsb, NSA_SEL_BLOCK, d)
    vbk = v_slc.reshape(b, g, nsb, NSA_SEL_BLOCK, d)
    bi = jnp.arange(b)[:, None, None, None]
    gi = jnp.arange(g)[None, :, None, None]
    offs = jnp.arange(NSA_SEL_BLOCK)
    n_g = n_top * NSA_SEL_BLOCK

    def chunk(c):
        t0 = c * NSA_Q_CHUNK
        qc = lax.dynamic_slice_in_dim(qg, t0, NSA_Q_CHUNK, axis=3)
        idx = lax.dynamic_slice_in_dim(sel, t0, NSA_Q_CHUNK, axis=2)
        tp = t0 + jnp.arange(NSA_Q_CHUNK)
        ks = kbk[bi, gi, idx]
        vs = vbk[bi, gi, idx]
        kpos = idx[..., None] * NSA_SEL_BLOCK + offs
        dist = (tp[:, None, None] - kpos).astype(jnp.float32)
        sc = (jnp.einsum('bgrtd,bgtnld->bgrtnl', qc, ks).astype(jnp.float32) * scale
              - sl[:, :, None, None, None] * dist[:, :, None])
        mask = jnp.broadcast_to((dist >= 0)[:, :, None], sc.shape)
        p = _masked_softmax(sc.reshape(b, g, r, NSA_Q_CHUNK, n_g), mask.reshape(b, g, r, NSA_Q_CHUNK, n_g))
        return jnp.einsum('bgrtm,bgtmd->bgrtd', p.astype(vs.dtype), vs.reshape(b, g, NSA_Q_CHUNK, n_g, d))

    o_slc = lax.map(chunk, jnp.arange(s // NSA_Q_CHUNK))
    o_slc = jnp.moveaxis(o_slc, 0, 3).reshape(b, g, r, s, d)
    o_win = _banded_attention(qg, k_win, v_win, sl, NSA_WINDOW)
    gt = [jnp.transpose(gates[:, :, i], (0, 2, 1)).reshape(b, g, r, s, 1) for i in range(3)]
    o = gt[0] * o_cmp + gt[1] * o_slc + gt[2] * o_win
    return o.astype(q.dtype).reshape(b, h, s, d)


def setup_inputs(seed: int = 0) -> dict:
    key = jax.random.key(seed)
    keys = iter(jax.random.split(key, 40))
    ne = (DEPTH + 1) // 2
    no = DEPTH // 2

    def nrm(shape, scale):
        return jax.random.normal(next(keys), shape, jnp.float32) * scale

    def gain(shape):
        return 1.0 + nrm(shape, 0.02)

    hd = HEAD_DIM
    flat = NSA_CMP_LEN * hd
    return {
        'x': nrm((BATCH, SEQ, D_MODEL), 1.0),
        'norm_g': gain((DEPTH, D_MODEL)),
        'w_out': nrm((DEPTH, MIX_WIDTH, D_MODEL), MIX_WIDTH ** -0.5),
        'e_w_in': nrm((ne, D_MODEL, EVEN_IN), D_MODEL ** -0.5),
        'a_conv_w': nrm((ne, CONV_WIDTH, GROUP_WIDTH), CONV_WIDTH ** -0.5),
        'a_conv_b': nrm((ne, GROUP_WIDTH), 0.02),
        'a_ln_g': gain((ne, GROUP_WIDTH)),
        'a_ln_b': nrm((ne, GROUP_WIDTH), 0.02),
        'b_qnorm_g': gain((ne, hd)),
        'b_knorm_g': gain((ne, hd)),
        'o_w_in': nrm((no, D_MODEL, ODD_IN), D_MODEL ** -0.5),
        'c_qnorm_g': gain((no, hd)),
        'c_knorm_cmp_g': gain((no, hd)),
        'c_knorm_slc_g': gain((no, hd)),
        'c_knorm_win_g': gain((no, hd)),
        'c_pos_k': nrm((no, NSA_CMP_LEN, hd), 0.02),
        'c_pos_v': nrm((no, NSA_CMP_LEN, hd), 0.02),
        'c_k_w1': nrm((no, flat, NSA_CMP_HIDDEN), flat ** -0.5),
        'c_k_b1': nrm((no, NSA_CMP_HIDDEN), 0.02),
        'c_k_w2': nrm((no, NSA_CMP_HIDDEN, hd), NSA_CMP_HIDDEN ** -0.5),
        'c_k_b2': nrm((no, hd), 0.02),
        'c_v_w1': nrm((no, flat, NSA_CMP_HIDDEN), flat ** -0.5),
        'c_v_b1': nrm((no, NSA_CMP_HIDDEN), 0.02),
        'c_v_w2': nrm((no, NSA_CMP_HIDDEN, hd), NSA_CMP_HIDDEN ** -0.5),
        'c_v_b2': nrm((no, hd), 0.02),
        'd_qnorm_g': gain((no, hd)),
        'd_knorm_g': gain((no, hd)),
        'd_sinks': nrm((no, N_HEADS), 0.5),
    }


def reference(x, norm_g, w_out, e_w_in, a_conv_w, a_conv_b, a_ln_g, a_ln_b, b_qnorm_g, b_knorm_g,
              o_w_in, c_qnorm_g, c_knorm_cmp_g, c_knorm_slc_g, c_knorm_win_g, c_pos_k, c_pos_v,
              c_k_w1, c_k_b1, c_k_w2, c_k_b2, c_v_w1, c_v_b1, c_v_w2, c_v_b2,
              d_qnorm_g, d_knorm_g, d_sinks):
    slopes = _alibi_slopes(N_HEADS)
    bsz, slen, _ = x.shape
    for layer in range(DEPTH):
        h = _rmsnorm(x, norm_g[layer])
        if layer % 2 == 0:
            i = layer // 2
            proj = jnp.einsum('bsd,de->bse', h, e_w_in[i])
            u_val, u_gate, z_a, q_b, k_b, v_b, z_b = _split(proj, EVEN_SIZES)
            y_a = _conformer_conv(u_val, u_gate, a_conv_w[i], a_conv_b[i], a_ln_g[i], a_ln_b[i]) * jax.nn.silu(z_a)
            q = _rmsnorm(_split_heads(q_b, N_HEADS), b_qnorm_g[i])
            k = _rmsnorm(_split_heads(k_b, N_HEADS), b_knorm_g[i])
            v = _split_heads(v_b, N_HEADS)
            y_b = _merge_heads(_moba_attention(q, k, v, slopes)) * jax.nn.silu(z_b)
            y = jnp.concatenate([y_a, y_b], axis=-1)
        else:
            i = layer // 2
            proj = jnp.einsum('bsd,de->bse', h, o_w_in[i])
            (q_c, kc_, vc_, ks_, vs_, kw_, vw_, g_c, z_c, q_d, k_d, v_d, z_d) = _split(proj, ODD_SIZES)
            qc = _rmsnorm(_split_heads(q_c, N_HEADS), c_qnorm_g[i])
            gates = jax.nn.sigmoid(g_c.astype(jnp.float32)).reshape(bsz, slen, 3, N_HEADS)
            o_c = _nsa_attention(
                qc, _split_heads(kc_, NSA_KV_HEADS), _split_heads(vc_, NSA_KV_HEADS),
                _rmsnorm(_split_heads(ks_, NSA_KV_HEADS), c_knorm_slc_g[i]), _split_heads(vs_, NSA_KV_HEADS),
                _rmsnorm(_split_heads(kw_, NSA_KV_HEADS), c_knorm_win_g[i]), _split_heads(vw_, NSA_KV_HEADS),
                gates, c_knorm_cmp_g[i], c_pos_k[i], c_pos_v[i],
                c_k_w1[i], c_k_b1[i], c_k_w2[i], c_k_b2[i], c_v_w1[i], c_v_b1[i], c_v_w2[i], c_v_b2[i], slopes)
            y_c = _merge_heads(o_c) * jax.nn.silu(z_c)
            r = N_HEADS // SWA_KV_HEADS
            qd = _rmsnorm(_split_heads(q_d, N_HEADS), d_qnorm_g[i]).reshape(bsz, SWA_KV_HEADS, r, slen, HEAD_DIM)
            kd = _rmsnorm(_split_heads(k_d, SWA_KV_HEADS), d_knorm_g[i])
            vd = _split_heads(v_d, SWA_KV_HEADS)
            o_d = _banded_attention(qd, kd, vd, slopes.reshape(SWA_KV_HEADS, r), SWA_WINDOW,
                                    d_sinks[i].reshape(SWA_KV_HEADS, r))
            y_d = _merge_heads(o_d.reshape(bsz, N_HEADS, slen, HEAD_DIM)) * jax.nn.silu(z_d)
            y = jnp.concatenate([y_c, y_d], axis=-1)
        x = x + jnp.einsum('bse,ed->bsd', y, w_out[layer]).astype(x.dtype)
    return x
```

```python
import contextlib
import os
import numpy as np
import ml_dtypes
import concourse.bass as bass
import concourse.mybir as mybir
from concourse.bass_utils import run_bass_kernel_spmd

F32 = mybir.dt.float32
BF16 = mybir.dt.bfloat16
ALU = mybir.AluOpType
AF = mybir.ActivationFunctionType
AX = mybir.AxisListType

S = 2048
D = 1024
NT = 16
NEGM = -30000.0
EPS = 1e-6


class _Op:
    __slots__ = ("eng", "fn", "deps", "chan", "signal", "val", "dmaval", "chanseq")

    def __init__(self, eng, fn, chan):
        self.eng = eng
        self.fn = fn
        self.deps = {}
        self.chan = chan
        self.signal = False
        self.val = 0
        self.dmaval = None


class Prog:
    ENGS = ("pe", "act", "dve", "pool", "sp")

    def __init__(self, nc):
        self.nc = nc
        self.ops = {e: [] for e in self.ENGS}
        self.res = {}
        self.chan_count = {}
        self.chan_last = {}
        self.all_ops = []
        self.bar = {}
        self.final_waits = {}
        self.pool_ctr = 0
        self.sp_ctr = 0
        self.pres = {}

    PERS = ("w", "wo")

    def _st(self, k):
        if isinstance(k, tuple) and k[0] in self.PERS or k in self.PERS:
            return self.pres
        return self.res

    def op(self, eng, fn, reads=(), writes=(), chan=None, nobar=False):
        if chan is not None and eng == "pool":
            chan = "pq%d" % (self.pool_ctr % 12)
            self.pool_ctr += 1
        elif chan == "c":
            chan = "pc%d" % (self.sp_ctr % 16)
            self.sp_ctr += 1
        o = _Op(eng, fn, chan)
        deps = o.deps
        if not nobar:
            deps.update(self.bar)
        if chan is not None and (chan.startswith("pq") or chan.startswith("pc")):
            prev = self.chan_last.get(chan)
            if prev is not None:
                deps[id(prev)] = prev
        for k in reads:
            st = self._st(k).get(k)
            if st is not None and st[0] is not None:
                deps[id(st[0])] = st[0]
        for k in writes:
            st = self._st(k).get(k)
            if st is not None:
                if st[0] is not None:
                    deps[id(st[0])] = st[0]
                for r in st[1]:
                    deps[id(r)] = r
        for k in reads:
            res = self._st(k)
            st = res.get(k)
            if st is None:
                st = [None, []]
                res[k] = st
            st[1].append(o)
        for k in writes:
            self._st(k)[k] = [o, []]
        o.dmaval = dict(self.chan_count)
        if chan is not None:
            self.chan_count[chan] = self.chan_count.get(chan, 0) + 1
            o.chanseq = self.chan_count[chan]
            self.chan_last[chan] = o
            o.signal = True
        self.ops[eng].append(o)
        self.all_ops.append(o)
        return o

    def barrier(self):
        bar = {}
        for e in self.ENGS:
            if self.ops[e]:
                o = self.ops[e][-1]
                bar[id(o)] = o
        for ch, o in self.chan_last.items():
            bar[id(o)] = o
        self.bar = bar
        self.res = {}

    def finish(self, eng, chans):
        self.final_waits = {eng: {ch: self.chan_count[ch] for ch in chans}}

    def emit(self):
        nc = self.nc
        for o in self.all_ops:
            for d in o.deps.values():
                if d.chan is None:
                    if d.eng == "pe" and o.eng == "pe":
                        continue
                    d.signal = True
        for e in self.ENGS:
            c = 0
            for o in self.ops[e]:
                if o.chan is None and o.signal:
                    c += 1
                    o.val = c
        import os
        if os.environ.get("KDBG"):
            print("sem counts", {e: max([o.val for o in self.ops[e]] + [0]) for e in self.ENGS}, {e: len(self.ops[e]) for e in self.ENGS},
                  {c: 16 * v for c, v in self.chan_count.items()})
        stack = contextlib.ExitStack()
        sems = {}
        for e in self.ENGS:
            sems[e] = stack.enter_context(nc.semaphore("s_" + e))
        for ch in self.chan_count:
            sems["c_" + ch] = stack.enter_context(nc.semaphore("c_" + ch))
        block = stack.enter_context(nc.Block())
        engobj = {"pe": "tensor", "act": "scalar", "dve": "vector", "pool": "gpsimd", "sp": "sync"}

        def make(e):
            def body(eng):
                waited = {}
                for o in self.ops[e]:
                    need = {}
                    for d in o.deps.values():
                        if d.chan is not None:
                            k = "c_" + d.chan
                            if d.chan.startswith("pq") or d.chan.startswith("pc"):
                                v = 16 * d.chanseq
                            else:
                                v = 16 * o.dmaval[d.chan]
                        else:
                            if d.eng == "pe" and e == "pe":
                                continue
                            k = d.eng
                            v = d.val
                        if v > need.get(k, 0):
                            need[k] = v
                    for k, v in need.items():
                        if waited.get(k, 0) >= v:
                            continue
                        eng.wait_ge(sems[k], v)
                        waited[k] = v
                    ins = o.fn(eng)
                    if o.chan is not None:
                        ins.then_inc(sems["c_" + o.chan], 16)
                    elif o.signal:
                        ins.then_inc(sems[e], 1)
                for ch, c in self.final_waits.get(e, {}).items():
                    eng.wait_ge(sems["c_" + ch], 16 * c)
            return body

        for e in self.ENGS:
            getattr(block, engobj[e])(make(e))
        stack.close()


def _consts():
    c = {}
    c["ident"] = np.eye(128, dtype=np.float32)
    k = np.arange(128)[:, None]
    q = np.arange(128)[None, :]
    c["tri"] = np.where(k <= q, 0.0, NEGM).astype(np.float32)
    c["up"] = np.where(k > q, 0.0, NEGM).astype(np.float32)
    t = np.arange(S)
    b = (t % 16).astype(np.float32)
    a = (t - t % 16).astype(np.float32)
    slopes = np.power(2.0, -8.0 * np.arange(1, 9) / 8).astype(np.float32)
    qal = np.zeros((S, 8, 4), np.float32)
    qal[:, :, 0] = -slopes[None, :] * a[:, None]
    qal[:, :, 1] = -slopes[None, :] * b[:, None]
    qal[:, :, 2] = slopes[None, :]
    qal[:, :, 3] = slopes[None, :]
    c["qal"] = qal.reshape(NT, 128, 32).transpose(1, 0, 2).copy()

    def kal(onehot_block):
        m = np.zeros((S, 36), np.float32)
        if onehot_block:
            m[t, t // onehot_block] = 1.0
        m[:, 32] = 1.0
        m[:, 33] = 1.0
        m[:, 34] = a
        m[:, 35] = b
        return m.reshape(NT, 128, 36).transpose(1, 0, 2).copy()

    c["kal_moba"] = kal(256)
    c["kal_slc"] = kal(64)
    c["kal_plain"] = kal(0)
    cc = np.arange(128)
    cend = 16 * cc + 31
    kc = np.zeros((128, 36), np.float32)
    kc[:, 32] = 1.0
    kc[:, 33] = 1.0
    kc[:, 34] = cend - cend % 16
    kc[:, 35] = cend % 16
    c["kal_cmp"] = kc
    c["cmaskneg"] = np.where(t[None, :] >= cend[:, None], 0.0, NEGM).astype(np.float32)
    start = np.arange(127)[:, None] * 16
    bs = np.arange(32)[None, :] * 64
    ov = np.zeros((128, 32), np.float32)
    ov[:127] = ((start < bs + 64) & (start + 32 > bs)).astype(np.float32)
    c["ovl"] = ov
    blk = np.arange(32)[None, :]
    cur = (t // 64)[:, None]
    forced = (blk == 0) | (blk == cur) | (blk == cur - 1)
    impc = 1e4 * forced.astype(np.float32) - 1e5 * (blk > cur).astype(np.float32)
    c["impc"] = impc.reshape(NT, 128, 32).transpose(1, 0, 2).copy()
    return c


def build_program(layers=(0, 1), stop=None):
    nc = bass.Bass("TRN2", target_bir_lowering=False)
    es = contextlib.ExitStack()
    dram = {}

    def din(name, shape, dt=F32):
        dram[name] = nc.dram_tensor(name, list(shape), dt, kind="ExternalInput").ap()
        return dram[name]

    x_d = din("x", [S, D])
    normg_d = din("norm_g", [2, D])
    wout_d = din("w_out", [2, D, D])
    ewin_d = din("e_w_in", [D, 3584])
    convw_d = din("a_conv_w", [31, 512])
    convb_d = din("a_conv_b", [512])
    lng_d = din("a_ln_g", [512])
    lnb_d = din("a_ln_b", [512])
    bq_d = din("b_qnorm_g", [1, 64])
    bk_d = din("b_knorm_g", [1, 64])
    ident_d = din("ident", [128, 128])
    tri_d = din("tri", [128, 128])
    up_d = din("up", [128, 128])
    qal_d = din("qal", [128, NT, 32])
    kalm_d = din("kal_moba", [128, NT, 36])
    kals_d = din("kal_slc", [128, NT, 36])
    kalp_d = din("kal_plain", [128, NT, 36])
    kalc_d = din("kal_cmp", [128, 36])
    cmn_d = din("cmaskneg", [128, S])
    ovl_d = din("ovl", [128, 32])
    impc_d = din("impc", [128, NT, 32])
    owin_d = din("o_w_in", [D, 3096])
    cq_d = din("c_qnorm_g", [1, 64])
    ckc_d = din("c_knorm_cmp_g", [1, 64])
    cks_d = din("c_knorm_slc_g", [1, 64])
    ckw_d = din("c_knorm_win_g", [1, 64])
    cposk_d = din("c_pos_k", [32, 64])
    cposv_d = din("c_pos_v", [32, 64])
    ckw1_d = din("c_k_w1", [2048, 256])
    ckb1_d = din("c_k_b1", [256])
    ckw2_d = din("c_k_w2", [256, 64])
    ckb2_d = din("c_k_b2", [1, 64])
    cvw1_d = din("c_v_w1", [2048, 256])
    cvb1_d = din("c_v_b1", [256])
    cvw2_d = din("c_v_w2", [256, 64])
    cvb2_d = din("c_v_b2", [1, 64])
    dq_d = din("d_qnorm_g", [1, 64])
    dk_d = din("d_knorm_g", [1, 64])
    dsink_d = din("d_sinks", [1, 8])
    y_d = nc.dram_tensor("y", [S, D], F32, kind="ExternalOutput").ap()

    def sb(name, shape, dt):
        return es.enter_context(nc.sbuf_tensor(name, list(shape), dt))

    def psum(name, shape, dt):
        return es.enter_context(nc.psum_tensor(name, list(shape), dt))

    P = Prog(nc)

    x_sb = sb("x_sb", [128, NT, D], F32)
    hT = sb("hT", [128, 8, S], BF16)
    wbuf = [sb("wbuf%d" % i, [128, 8, 512], BF16) for i in range(2)]
    wo = sb("wo", [128, 4, D], BF16)
    ident = sb("ident_sb", [128, 128], BF16)
    identf = sb("identf_sb", [128, 128], F32)
    onesf = sb("onesf", [128, 128], F32)
    tri = sb("tri_sb", [128, 128], BF16)
    upm = sb("up_sb", [128, 128], BF16)
    qal = sb("qal_sb", [128, NT, 32], BF16)
    kalm = sb("kalm_sb", [128, NT, 36], BF16)
    KTc = sb("KTc", [128, 2, 128], BF16)
    Vca = sb("Vca", [128, 2, 98], BF16)
    kals = sb("kals_sb", [128, NT, 36], BF16)
    kalp = sb("kalp_sb", [128, NT, 36], BF16)
    ss = sb("ss", [128, NT], F32)
    rstd = sb("rstd", [128, NT], F32)
    ARENA = 80 * 1024
    arena = sb("arena", [128, ARENA // 2], BF16)

    pf = [psum("pf%d" % i, [128, 512], F32) for i in range(6)]
    pb = [psum("pb%d" % i, [128, 1024], BF16) for i in range(2)]

    class Carver:
        def __init__(self, base=0):
            self.off = base

        def get(self, shape, dt):
            n = int(np.prod(shape[1:]))
            nb = n * (4 if dt == F32 else 2)
            nb = (nb + 63) // 64 * 64
            assert self.off + nb <= ARENA, ("arena overflow", self.off + nb, ARENA)
            a = arena[:, self.off // 2:(self.off + nb) // 2]
            self.off += nb
            if os.environ.get("KDBG"):
                print("carve", shape, dt, "->", self.off)
            if dt == F32:
                a = a.bitcast(F32)
            a = a[:, 0:n]
            if len(shape) == 3:
                a = a.rearrange("p (a b) -> p a b", b=shape[2])
            elif len(shape) == 4:
                a = a.rearrange("p (a b c) -> p a b c", b=shape[2], c=shape[3])
            if shape[0] < 128:
                a = a[0:shape[0]]
            return a

    def dma(eng, out, in_, chan, reads=(), writes=(), slow=False, nobar=False):
        if slow:
            P.op(eng, lambda e: e.dma_start(out=out, in_=in_, allow_slow_non_contiguous=True), reads=reads, writes=writes, chan=chan, nobar=nobar)
        else:
            P.op(eng, lambda e: e.dma_start(out=out, in_=in_), reads=reads, writes=writes, chan=chan, nobar=nobar)

    def mm(out, lhsT, rhs, start, stop, reads, writes):
        P.op("pe", lambda e: e.matmul(out, lhsT=lhsT, rhs=rhs, start=start, stop=stop), reads=reads, writes=writes)

    def tr(out, in_, reads, writes, idn=None):
        idn = ident[:] if idn is None else idn
        P.op("pe", lambda e: e.transpose(out=out, in_=in_, identity=idn), reads=list(reads) + ["const"], writes=writes)

    def act(out, in_, func, reads, writes, **kw):
        P.op("act", lambda e: e.activation(out=out, in_=in_, func=func, **kw), reads=reads, writes=writes)

    def V(fn, reads, writes, eng="dve"):
        P.op(eng, fn, reads=reads, writes=writes)

    def TT(out, in0, in1, op, reads, writes, eng="dve"):
        P.op(eng, lambda e: e.tensor_tensor(out=out, in0=in0, in1=in1, op=op), reads=reads, writes=writes)

    def TS(out, in0, s1, s2, op0, op1, reads, writes, eng="dve"):
        if op1 is None:
            P.op(eng, lambda e: e.tensor_scalar(out=out, in0=in0, scalar1=s1, scalar2=None, op0=op0), reads=reads, writes=writes)
        else:
            P.op(eng, lambda e: e.tensor_scalar(out=out, in0=in0, scalar1=s1, scalar2=s2, op0=op0, op1=op1), reads=reads, writes=writes)

    def STT(out, in0, scalar, in1, op0, op1, reads, writes, eng="dve"):
        P.op(eng, lambda e: e.scalar_tensor_tensor(out=out, in0=in0, scalar=scalar, in1=in1, op0=op0, op1=op1), reads=reads, writes=writes)

    def RED(out, in_, op, reads, writes):
        P.op("dve", lambda e: e.tensor_reduce(out=out, in_=in_, axis=AX.X, op=op), reads=reads, writes=writes)

    def RCP(out, in_, reads, writes):
        P.op("dve", lambda e: e.reciprocal(out=out, in_=in_), reads=reads, writes=writes)

    def CPY(out, in_, reads, writes, eng="dve"):
        P.op(eng, lambda e: e.tensor_copy(out=out, in_=in_), reads=reads, writes=writes)

    def MSET(ap, val, writes, eng="dve"):
        P.op(eng, lambda e: e.memset(ap, val), reads=[], writes=writes)

    import os
    KB = os.environ.get("KBIS", "abcdefg")
    if "a" in KB:
        dma("pool", ident[:], ident_d, "c", writes=["const"])
    if "b" in KB:
        dma("pool", tri[:], tri_d, "c", writes=["const"])
        dma("pool", upm[:], up_d, "c", writes=["const"])
    if "c" in KB:
        dma("pool", qal[:], qal_d, "c", writes=["const"])
    if "d" in KB:
        dma("pool", kalm[:], kalm_d, "c", writes=["const"])
        dma("pool", kals[:], kals_d, "c", writes=["const"])
        dma("pool", kalp[:], kalp_d, "c", writes=["const"])
    if "e" in KB:
        dma("sp", identf[:], ident_d, "c", writes=["const"])
    if "f" in KB:
        MSET(onesf[:], 1.0, ["const"])

    xv = x_d.rearrange("(i p) d -> p i d", p=128)
    yv = y_d.rearrange("(i p) d -> p i d", p=128)
    for i in range(NT):
        dma("sp", x_sb[:, i, :], xv[:, i, :], "x%d" % i, reads=([("x", i - 2)] if i >= 2 else []), writes=[("x", i)])

    wslot_ctr = [0]

    def load_w(wd, segs):
        s = wslot_ctr[0] % 2
        wslot_ctr[0] += 1
        wv = wd.rearrange("(k p) n -> p k n", p=128)
        o = 0
        for (c0, n) in segs:
            dma("pool", wbuf[s][:, :, o:o + n], wv[:, :, c0:c0 + n], "w%d" % s, writes=[("w", s)], nobar=True)
            o += n
        return s

    def norm_phase(layer, base=0, barrier=True):
        cvn = Carver(base=base)
        gbc = cvn.get([128, D], F32)
        junk = cvn.get([128, D], BF16)
        hb = [cvn.get([128, D], BF16) for _ in range(2)]
        dma("act", gbc[:], normg_d[layer:layer + 1, :].partition_broadcast(128), "c", writes=["gbc"])
        MSET(ss[:], 0.0, [("ss", gI) for gI in range(4)])

        def stats(gI):
            for i in range(4 * gI, 4 * gI + 4):
                act(junk[:], x_sb[:, i, :], AF.Square, [("x", i), ("ss", gI)], ["junk", ("ss", gI)], accum_out=ss[:, i:i + 1])
            sl = slice(4 * gI, 4 * gI + 4)
            TS(rstd[:, sl], ss[:, sl], 1.0 / D, EPS, ALU.mult, ALU.add, [("ss", gI)], [("rstd", gI)])
            act(rstd[:, sl], rstd[:, sl], AF.Sqrt, [("rstd", gI)], [("rstd", gI)])
            RCP(rstd[:, sl], rstd[:, sl], [("rstd", gI)], [("rstd", gI)])

        def na(i):
            h = hb[i % 2]
            STT(h[:], x_sb[:, i, :], rstd[:, i:i + 1], gbc[:], ALU.mult, ALU.mult, [("x", i), ("rstd", i // 4), "gbc"], [("hb", i % 2)])

        def nb(i):
            h = hb[i % 2]
            pt = pb[i % 2]
            for k in range(8):
                tr(pt[:, k * 128:(k + 1) * 128], h[:, k * 128:(k + 1) * 128], [("hb", i % 2)], [("pb", i % 2)])
            act(hT[:, :, i * 128:(i + 1) * 128], pt[:].rearrange("p (k t) -> p k t", k=8), AF.Copy, [("pb", i % 2)], [("hT", i)])

        pend = []
        for gI in range(4):
            stats(gI)
            if gI >= 1:
                for i in range(4 * (gI - 1), 4 * gI):
                    na(i)
                    pend.append(i)
                    if len(pend) > 1:
                        nb(pend.pop(0))
        for i in range(12, 16):
            na(i)
            pend.append(i)
            nb(pend.pop(0))
        nb(pend.pop(0))
        if barrier:
            P.barrier()

    def load_wo(layer, c0, n):
        wv = wout_d[layer].rearrange("(k p) n -> p k n", p=128)
        for hf in range(2):
            dma("pool", wo[:, 0:n, hf * 512:(hf + 1) * 512], wv[:, c0:c0 + n, hf * 512:(hf + 1) * 512], "wo", writes=["wo"], nobar=True)

    def outproj_tile(i, lhs_list, chunks, yT_reads, pbanks):
        for half in range(2):
            pp = pf[pbanks[half]]
            for n, (c, lhs) in enumerate(zip(chunks, lhs_list)):
                mm(pp[:], lhs, wo[:, c, half * 512:(half + 1) * 512], n == 0, n == len(chunks) - 1,
                   list(yT_reads) + ["wo"], [("pf", pbanks[half])])
            xs = x_sb[:, i, half * 512:(half + 1) * 512]
            TT(xs, xs, pp[:], ALU.add, [("pf", pbanks[half]), ("x", i)], [("x", i)])

    att_ctr = [0, 0]

    def lagged(n, stage_a, stage_b, lag=1):
        for i in range(n + lag):
            if i < n:
                stage_a(i)
            if i >= lag:
                stage_b(i - lag)

    def mm_acc(out, lhsT, rhs, start, stop, reads, writes):
        P.op("pe", lambda e: e.matmul(out, lhsT=lhsT, rhs=rhs, start=start, stop=stop, skip_group_check=True), reads=reads, writes=writes)

    def attention(QT, KT, nk, Vrhs, ncv, plan, finish, qkeys, kkeys, vkeys, ptb, tagk, addmask=None):
        NS = len(ptb)
        work = []
        for qc in range(4):
            items = plan(qc)
            if not items:
                continue
            ob = 3 + att_ctr[1] % 3
            att_ctr[1] += 1
            lastk = {}
            for (kt, jlo, jhi, masks) in items:
                for j in range(jlo, jhi + 1):
                    lastk[j] = kt
            for n, it in enumerate(items):
                work.append((qc, ob, it, n == 0, n == len(items) - 1, lastk))

        def front(w):
            qc, ob, (kt, jlo, jhi, masks), first, last, lastk = w
            sbk = att_ctr[0] % NS
            att_ctr[0] += 1
            Sp = pf[sbk]
            pt = ptb[sbk]
            c0, c1 = jlo * 128, (jhi + 1) * 128
            extra = list(masks.items())
            mm_acc(Sp[0:nk, c0:c1], KT(kt), QT[:, qc * 512 + c0:qc * 512 + c1], True, addmask is None and not extra,
                   list(qkeys(qc)) + list(kkeys(kt)), [("pf", sbk)])
            if addmask is not None:
                mm_acc(Sp[0:nk, c0:c1], ident[0:nk, 0:nk], addmask[0:nk, qc * 512 + c0:qc * 512 + c1], False, not extra,
                       ["const", "addmask"], [("pf", sbk)])
            for n, (j, mk) in enumerate(extra):
                mm_acc(Sp[0:nk, j * 128:(j + 1) * 128], ident[0:nk, 0:nk], mk[0:nk, :], False, n == len(extra) - 1,
                       ["const"], [("pf", sbk)])
            act(pt[0:nk, c0:c1], Sp[0:nk, c0:c1], AF.Exp, [("pf", sbk)], [("pt", tagk, sbk)])
            return sbk

        def back(w, sbk, started):
            qc, ob, (kt, jlo, jhi, masks), first, last, lastk = w
            pt = ptb[sbk]
            for j in range(jlo, jhi + 1):
                mm_acc(pf[ob][:, j * 128:j * 128 + ncv], pt[0:nk, j * 128:(j + 1) * 128], Vrhs(kt), len(started) == 0, lastk[j] == kt,
                       [("pt", tagk, sbk)] + list(vkeys(kt)), [("pf", ob)])
                started.add(j)
            if last:
                for j in sorted(started):
                    finish(qc * 4 + j, pf[ob][:, j * 128:j * 128 + ncv], ("pf", ob))
                started.clear()

        LAG = NS - 1
        started = set()
        pend = []
        for w in work:
            pend.append((w, front(w)))
            if len(pend) > LAG:
                pw, psb = pend.pop(0)
                back(pw, psb, started)
        for (pw, psb) in pend:
            back(pw, psb, started)

    def layer0():
        if stop == "load":
            return
        norm_phase(0, base=70 * 1024, barrier=False)
        if stop == "norm0":
            return
        load_wo(0, 0, 4)
        W = ewin_d
        if stop == "norm":
            return
        cv = Carver()
        yc = cv.get([128, 4, S], F32)
        hc = cv.get([128, S + 32], BF16)
        dg = cv.get([128, 31, 128], BF16)
        cw = cv.get([128, 4, 31], F32)
        cb = cv.get([128, 4], F32)
        lg = cv.get([128, 4], F32)
        lb = cv.get([128, 4], F32)
        Tsq = [cv.get([128, 512], F32) for _ in range(2)]
        mu2 = [cv.get([128, 512], F32) for _ in range(2)]
        msq = cv.get([128, 512], F32)
        rs2 = [cv.get([128, 512], F32) for _ in range(2)]
        sz4 = [cv.get([128, 512], BF16) for _ in range(4)]
        T4 = [cv.get([128, 512], F32) for _ in range(4)]
        yaT2 = [cv.get([128, 4, 512], BF16) for _ in range(2)]
        HO = 32
        stg = Tsq[0][0:32, :]
        stg2 = Tsq[1][0:12, 0:128]
        dma("sp", stg[0:31, :], convw_d, "c", writes=[("Tsq", 0)])
        dma("sp", stg2[0:4, :], convb_d.rearrange("(a c) -> a c", c=128), "c", writes=[("Tsq", 1)])
        dma("sp", stg2[4:8, :], lng_d.rearrange("(a c) -> a c", c=128), "c", writes=[("Tsq", 1)])
        dma("sp", stg2[8:12, :], lnb_d.rearrange("(a c) -> a c", c=128), "c", writes=[("Tsq", 1)])
        for cc in range(4):
            tr(pf[0][:, cc * 32:cc * 32 + 31], stg[0:31, cc * 128:(cc + 1) * 128], [("Tsq", 0)], [("pf", 0)], idn=identf[0:31, 0:31])
        tr(pf[0][:, 128:140], stg2[0:12, :], [("Tsq", 1)], [("pf", 0)], idn=identf[0:12, 0:12])
        CPY(cw[:], pf[0][:, 0:128].rearrange("p (a j) -> p a j", j=32)[:, :, 0:31], [("pf", 0)], ["cw"])
        CPY(cb[:], pf[0][:, 128:132], [("pf", 0)], ["cw"])
        CPY(lg[:], pf[0][:, 132:136], [("pf", 0)], ["cw"])
        CPY(lb[:], pf[0][:, 136:140], [("pf", 0)], ["cw"])
        MSET(hc[:, 0:HO], 0.0, ["hc0"])
        if stop == "convp":
            return
        for cc in range(4):
            if stop == "conv1" and cc == 1:
                return
            s = load_w(W, [(cc * 128, 128), (512 + cc * 128, 128)])
            for j in range(31):
                TS(dg[:, j, :], ident[:], cw[:, cc, j:j + 1], None, ALU.mult, None, ["const", "cw"], [("dg", j)])
            for tq in range(4):
                bv, bg = (tq % 2) * 2, (tq % 2) * 2 + 1
                for which, bk in ((0, bv), (1, bg)):
                    for k in range(8):
                        mm(pf[bk][:], wbuf[s][:, k, which * 128:(which + 1) * 128], hT[:, k, tq * 512:(tq + 1) * 512], k == 0, k == 7,
                           [("w", s)] + [("hT", 4 * tq + u) for u in range(4)], [("pf", bk)])
                act(T4[tq % 2][:], pf[bg][:], AF.Sigmoid, [("pf", bg)], [("T4", tq % 2)])
                TT(hc[:, HO + tq * 512:HO + (tq + 1) * 512], pf[bv][:], T4[tq % 2][:], ALU.mult, [("pf", bv), ("T4", tq % 2)], [("hc", tq)])
            for tq in range(4):
                pc = pf[4 + tq % 2]
                for j in range(31):
                    o = HO - 30 + j + tq * 512
                    mm(pc[:], dg[:, j, :], hc[:, o:o + 512], j == 0, j == 30,
                       [("dg", j), ("hc", tq)] + ([("hc", tq - 1)] if tq > 0 else ["hc0"]), [("pf", 4 + tq % 2)])
                act(yc[:, cc, tq * 512:(tq + 1) * 512], pc[:], AF.Identity, [("pf", 4 + tq % 2), "cw"], [("yc", cc, tq)],
                    bias=cb[:, cc:cc + 1], scale=1.0)
        if stop == "conv4":
            return
        P.barrier()
        sz_slot = load_w(W, [(1024, 512)])

        def st_a(tq):
            ts = slice(tq * 512, (tq + 1) * 512)
            p2 = tq % 2
            for cc in range(4):
                mm(pf[0][:], onesf[:], yc[:, cc, ts], cc == 0, cc == 3, [("yc", cc, tq), "const"], [("pf", 0)])
            for cc in range(4):
                act(Tsq[cc % 2][:], yc[:, cc, ts], AF.Square, [("yc", cc, tq)], [("Tsq", cc % 2)])
                mm(pf[1][:], onesf[:], Tsq[cc % 2][:], cc == 0, cc == 3, [("Tsq", cc % 2), "const"], [("pf", 1)])
            TS(mu2[p2][:], pf[0][:], 1.0 / 512, None, ALU.mult, None, [("pf", 0)], [("mu", p2)])
            TT(msq[:], mu2[p2][:], mu2[p2][:], ALU.mult, [("mu", p2)], ["msq"])
            STT(rs2[p2][:], pf[1][:], 1.0 / 512, msq[:], ALU.mult, ALU.subtract, [("pf", 1), "msq"], [("rs", p2)])
            TS(rs2[p2][:], rs2[p2][:], EPS, None, ALU.add, None, [("rs", p2)], [("rs", p2)])
            act(rs2[p2][:], rs2[p2][:], AF.Sqrt, [("rs", p2)], [("rs", p2)])
            RCP(rs2[p2][:], rs2[p2][:], [("rs", p2)], [("rs", p2)])

        def st_b(tq):
            ts = slice(tq * 512, (tq + 1) * 512)
            p2 = tq % 2
            for cc in range(4):
                pz = pf[2 + cc % 2]
                for k in range(8):
                    mm(pz[:], wbuf[sz_slot][:, k, cc * 128:(cc + 1) * 128], hT[:, k, ts], k == 0, k == 7,
                       [("w", sz_slot)] + [("hT", 4 * tq + u) for u in range(4)], [("pf", 2 + cc % 2)])
                act(sz4[cc][:], pz[:], AF.Silu, [("pf", 2 + cc % 2)], [("sz", cc)])
            for cc in range(4):
                tt = T4[cc]
                TT(tt[:], yc[:, cc, ts], mu2[p2][:], ALU.subtract, [("yc", cc, tq), ("mu", p2)], [("T4", cc)])
                TT(tt[:], tt[:], rs2[p2][:], ALU.mult, [("T4", cc), ("rs", p2)], [("T4", cc)])
            for cc in range(4):
                tt = T4[cc]
                act(tt[:], tt[:], AF.Silu, [("T4", cc), "cw"], [("T4", cc)], scale=lg[:, cc:cc + 1], bias=lb[:, cc:cc + 1])
            for cc in range(4):
                TT(yaT2[p2][:, cc, :], T4[cc][:], sz4[cc][:], ALU.mult, [("T4", cc), ("sz", cc)], [("yaT", p2, cc)])

        def st_c(tq):
            p2 = tq % 2
            for u in range(4):
                i = 4 * tq + u
                outproj_tile(i, [yaT2[p2][:, cc, u * 128:(u + 1) * 128] for cc in range(4)], [0, 1, 2, 3],
                             [("yaT", p2, cc) for cc in range(4)], (4, 5))

        for step in range(6):
            if step < 4:
                st_a(step)
            if 0 <= step - 1 < 4:
                st_b(step - 1)
            if step - 2 >= 0:
                st_c(step - 2)
        P.barrier()
        if stop == "conv":
            return
        for hh in range(2):
            moba_half(hh, W)
            P.barrier()
            if stop is not None:
                return

    def moba_half(hh, W):
        cv = Carver()
        QT = cv.get([128, 4, S], BF16)
        KT = cv.get([128, 4, S], BF16)
        Vt = cv.get([128, NT, 4, 66], BF16)
        szy = cv.get([128, NT, 256], BF16)
        ptb = [cv.get([128, 512], BF16) for _ in range(3)]
        gq = cv.get([128, 1], F32)
        gk = cv.get([128, 1], F32)
        kmf = cv.get([128, 4, 8], F32)
        kmb = cv.get([128, 4, 8], BF16)
        gs = cv.get([128, 4, 8], F32)
        cmp_ = cv.get([128, 4, 8, 8], F32)
        rank = cv.get([128, 4, 8], F32)
        nm = cv.get([128, 4, 32], BF16)
        rden = [cv.get([128, 1], F32) for _ in range(8)]
        yT = [cv.get([128, 2, 128], BF16) for _ in range(2)]
        tmpf = [cv.get([128, 512], F32) for _ in range(2)]
        s8 = [cv.get([128, 8], F32) for _ in range(3)]
        qa = [cv.get([128, 4, 128], BF16) for _ in range(3)]
        ka = [cv.get([128, 4, 128], BF16) for _ in range(3)]
        c_q = 1536 + hh * 256
        c_k = 2048 + hh * 256
        c_v = 2560 + hh * 256
        c_z = 3072 + hh * 256
        s = load_w(W, [(c_q, 256), (c_k, 256)])
        s2 = load_w(W, [(c_v, 256), (c_z, 256)])
        load_wo(0, 4 + 2 * hh, 2)
        load_gain_col(gq, bq_d, 1.0)
        load_gain_col(gk, bk_d, 8.0)
        for b in range(3):
            MSET(qa[b][:], 0.0, [("qa", b)])
            MSET(ka[b][:], 0.0, [("ka", b)])
        MSET(Vt[:], 1.0, ["Vt"])
        MSET(nm[:], 0.0, ["nm"])

        def m0(i):
            r = i % 3
            CPY(qa[r][:, :, 96:100], qal[:, i, hh * 16:hh * 16 + 16].rearrange("p (h c) -> p h c", c=4), ["const"], [("qa", r)])
            CPY(ka[r][:, :, 64:100], kalm[:, i, :].unsqueeze(1).to_broadcast([128, 4, 36]), ["const"], [("ka", r)])
            for k in range(8):
                mm(pf[r][:], hT[:, k, i * 128:(i + 1) * 128], wbuf[s][:, k, :], k == 0, k == 7, [("hT", i), ("w", s)], [("pf", r)])

        def m1(i):
            r = i % 3
            rstd_a(pf[r], ("pf", r), 512, tmpf[i % 2], ("tf", i % 2), s8[r], ("s8", r))

        def m2(i):
            r = i % 3
            rstd_b(512, s8[r], ("s8", r))
            TT(qa[r][:, :, 0:64], pf[r][:, 0:256].rearrange("p (h d) -> p h d", d=64), s8[r][:, 0:4].unsqueeze(2).to_broadcast([128, 4, 64]),
               ALU.mult, [("pf", r), ("s8", r)], [("qa", r)])
            TT(ka[r][:, :, 0:64], pf[r][:, 256:512].rearrange("p (h d) -> p h d", d=64), s8[r][:, 4:8].unsqueeze(2).to_broadcast([128, 4, 64]),
               ALU.mult, [("pf", r), ("s8", r)], [("ka", r)])

        def m3(i):
            r = i % 3
            b2 = i % 2
            pt = pb[b2]
            for h in range(4):
                tr(pt[:, h * 128:(h + 1) * 128], qa[r][:, h, :], [("qa", r)], [("pb", b2)])
            for h in range(4):
                tr(pt[:, (4 + h) * 128:(5 + h) * 128], ka[r][:, h, :], [("ka", r)], [("pb", b2)])
            act(QT[:, :, i * 128:(i + 1) * 128], pt[:, 0:512].rearrange("p (h t) -> p h t", h=4), AF.Copy, [("pb", b2), "gq"], [("QT", i)],
                scale=gq[:, 0:1])
            act(KT[:, :, i * 128:(i + 1) * 128], pt[:, 512:1024].rearrange("p (h t) -> p h t", h=4), AF.Copy, [("pb", b2), "gq"], [("KT", i)],
                scale=gk[:, 0:1])

        pipe4(NT, m0, m1, m2, m3)
        if hh == 1 and 1 in layers:
            prefetch_w1([("qa", r) for r in range(3)] + [("ka", r) for r in range(3)] + [("s8", r) for r in range(3)] + [("tf", 0), ("tf", 1)])
        if stop == "m1":
            return
        for i in range(NT):
            b2 = 2 + i % 2
            pp = pf[b2]
            for k in range(8):
                mm(pp[:], hT[:, k, i * 128:(i + 1) * 128], wbuf[s2][:, k, :], k == 0, k == 7, [("hT", i), ("w", s2)], [("pf", b2)])
            act(Vt[:, i, :, 0:64], pp[:, 0:256].rearrange("p (h d) -> p h d", d=64), AF.Copy, [("pf", b2), "Vt"], [("V", i)])
            act(szy[:, i, :], pp[:, 256:512], AF.Silu, [("pf", b2)], [("szy", i)])
        if stop == "m3":
            return
        for h in range(4):
            RED(kmf[:, h, :], KT[:, h, :].rearrange("p (n t) -> p n t", t=256), ALU.add, [("KT", i) for i in range(NT)], ["kmf"])
        TS(kmb[:], kmf[:], 1.0 / 256, None, ALU.mult, None, ["kmf"], ["kmb"])
        if stop == "mk":
            return
        for i in range(8, NT):
            npast = i // 2
            b2 = 4 + i % 2
            pg = pf[b2]
            for h in range(4):
                mm(pg[:, h * 8:h * 8 + 8], QT[0:64, h, i * 128:(i + 1) * 128], kmb[0:64, h, :], True, True, [("QT", i), "kmb"], [("pf", b2)])
            CPY(gs[:], pg[:, 0:32].rearrange("p (h n) -> p h n", n=8), [("pf", b2)], ["gs"])
            TT(cmp_[:, :, 0:npast, 0:npast], gs[:, :, 0:npast].unsqueeze(2).to_broadcast([128, 4, npast, npast]),
               gs[:, :, 0:npast].unsqueeze(3).to_broadcast([128, 4, npast, npast]), ALU.is_gt, ["gs"], ["cmp"])
            RED(rank[:, :, 0:npast], cmp_[:, :, 0:npast, 0:npast], ALU.add, ["cmp"], ["rank"])
            TS(nm[:, :, 0:npast], rank[:, :, 0:npast], 2.5, NEGM, ALU.is_ge, ALU.mult, ["rank"], ["nm"])
            pt = pb[i % 2]
            tr(pt[:, 0:128], nm[:].rearrange("p h n -> p (h n)"), ["nm"], [("pb", i % 2)])
            for h in range(4):
                act(QT[64:96, h, i * 128:(i + 1) * 128], pt[h * 32:(h + 1) * 32, 0:128], AF.Copy, [("pb", i % 2)], [("QT", i)])

        if stop == "m2":
            return

        def plan(qc):
            items = []
            for kt in range(4 * qc + 4):
                if kt < 4 * qc:
                    items.append((kt, 0, 3, {}))
                else:
                    m = kt - 4 * qc
                    items.append((kt, m, 3, {m: tri[:]}))
            return items

        for h in range(4):
            def finish(i, Ob, okey, h=h):
                r = (i + 4 * h) % 8
                rd = rden[r]
                RCP(rd[:], Ob[:, 64:65], [okey], [("rden", r)])
                dst = szy[:, i, h * 64:(h + 1) * 64]
                STT(dst, Ob[:, 0:64], rd[:], dst, ALU.mult, ALU.mult, [okey, ("rden", r), ("szy", i)], [("szy", i)])

            attention(QT[:, h, :], lambda kt, h=h: KT[:, h, kt * 128:(kt + 1) * 128], 128,
                      lambda kt, h=h: Vt[:, kt, h, 0:65], 65, plan, finish,
                      lambda qc: [("QT", 4 * qc + u) for u in range(4)], lambda kt: [("KT", kt)], lambda kt: [("V", kt)],
                      ptb, "m")
        if stop == "ma":
            return
        outproj_half(szy, yT)

    def rstd_a(pp, pkey, ncol, tf, tfkey, s8i, s8key):
        nh = ncol // 64
        act(tf[:, 0:ncol], pp[:, 0:ncol], AF.Square, [pkey], [tfkey])
        RED(s8i[:, 0:nh], tf[:, 0:ncol].rearrange("p (h d) -> p h d", d=64), ALU.add, [tfkey], [s8key])
        TS(s8i[:, 0:nh], s8i[:, 0:nh], 64.0 * EPS, None, ALU.add, None, [s8key], [s8key])
        act(s8i[:, 0:nh], s8i[:, 0:nh], AF.Sqrt, [s8key], [s8key])

    def rstd_b(ncol, s8i, s8key):
        nh = ncol // 64
        RCP(s8i[:, 0:nh], s8i[:, 0:nh], [s8key], [s8key])

    def head_rstd(pp, pkey, ncol, tf, tfkey, s8i, s8key):
        rstd_a(pp, pkey, ncol, tf, tfkey, s8i, s8key)
        rstd_b(ncol, s8i, s8key)

    def pipe4(n, s0, s1, s2, s3):
        for step in range(n + 2):
            if step < n:
                s0(step)
                s1(step)
            if 1 <= step <= n:
                s2(step - 1)
            if step >= 2:
                s3(step - 2)

    def load_gain_col(dst, gd, mult):
        MSET(dst[:], 1.0, ["gq"])
        dma("sp", dst[0:64, :], gd.rearrange("o d -> d o"), "c", writes=["gq"], slow=True)
        if mult != 1.0:
            TS(dst[0:64, :], dst[0:64, :], mult, None, ALU.mult, None, ["gq"], ["gq"])

    def outproj_half(szy, yT, store=False):
        def oa(i):
            pt = pb[i % 2]
            for c in range(2):
                tr(pt[:, c * 128:(c + 1) * 128], szy[:, i, c * 128:(c + 1) * 128], [("szy", i)], [("pb", i % 2)])
            y = yT[i % 2]
            act(y[:], pt[:, 0:256].rearrange("p (c t) -> p c t", c=2), AF.Copy, [("pb", i % 2)], [("yT", i % 2)])

        def ob(i):
            y = yT[i % 2]
            outproj_tile(i, [y[:, 0, :], y[:, 1, :]], [0, 1], [("yT", i % 2)], (4, 5))
            if store:
                dma("sp", yv[:, i, :], x_sb[:, i, :], "out", reads=[("x", i)])

        lagged(NT, oa, ob)

    def plan_causal(qc):
        items = []
        for kt in range(4 * qc + 4):
            if kt < 4 * qc:
                items.append((kt, 0, 3, {}))
            else:
                m = kt - 4 * qc
                items.append((kt, m, 3, {m: tri[:]}))
        return items

    def plan_band(wt):
        def plan(qc):
            items = []
            for kt in range(max(0, 4 * qc - wt), 4 * qc + 4):
                jlo = max(0, kt - 4 * qc)
                jhi = min(3, kt + wt - 4 * qc)
                if jlo > jhi:
                    continue
                masks = {}
                if 0 <= kt - 4 * qc <= 3:
                    masks[kt - 4 * qc] = tri[:]
                if 0 <= kt + wt - 4 * qc <= 3:
                    masks[kt + wt - 4 * qc] = upm[:]
                items.append((kt, jlo, jhi, masks))
            return items
        return plan

    W1_OFF = 64 * 1024

    def prefetch_w1(war_keys=()):
        cvp = Carver(base=W1_OFF)
        wA = cvp.get([128, 32, 256], BF16)
        for kv, wd in enumerate((ckw1_d, cvw1_d)):
            wv = wd.rearrange("(l d) j -> d l j", d=64)
            for lq in range(4):
                dma("pool", wA[kv * 64:(kv + 1) * 64, lq * 8:(lq + 1) * 8, :], wv[:, lq * 8:(lq + 1) * 8, :], "w1", writes=["w1A"] + list(war_keys))
        return wA

    def layer1():
        W = owin_d
        wA = Carver(base=W1_OFF).get([128, 32, 256], BF16)
        if 0 not in layers:
            prefetch_w1()
        cvc = Carver(base=16 * 1024)
        wB = cvc.get([128, 32, 256], BF16)
        dma("sp", wB[64:128, :, :], wA[0:64, :, :], "c", writes=["w1B"])
        dma("sp", wB[0:64, :, :], wA[64:128, :, :], "c", writes=["w1B"])
        w1 = [[wA, wB], [wB, wA]]
        B = compress_loads(W, cvc)
        norm_phase(1)
        compress_stage(W, B, w1)
        P.barrier()
        if stop == "cmp":
            return
        for g in range(2):
            nsa_half(g, W)
            P.barrier()
            if stop == "nsa0":
                return
        if stop == "nsa":
            return
        for g in range(2):
            swa_half(g, W)
            P.barrier()

    def compress_loads(W, cv):
        s = load_w(W, [(512, 128), (640, 128)])
        B = {}
        B["s"] = s
        B["KVD"] = [cv.get([128, 16, 128], BF16) for _ in range(2)]
        B["w2"] = [cv.get([128, 2, 64], BF16) for _ in range(2)]
        B["posn"] = cv.get([32, 2, 64], F32)
        B["posT"] = cv.get([64, 2, 32], BF16)
        B["stgb"] = cv.get([4, 128], F32)
        B["b1sb"] = cv.get([128, 4], F32)
        B["biasj"] = cv.get([128, 4], F32)
        B["b2bc"] = cv.get([128, 2, 64], F32)
        B["gcm"] = cv.get([128, 64], F32)
        B["kalc"] = cv.get([128, 36], BF16)
        B["hid"] = [cv.get([128, 2, 128], BF16) for _ in range(4)]
        B["kcf"] = cv.get([128, 64], F32)
        B["junk2"] = cv.get([128, 64], F32)
        B["ssc"] = cv.get([128, 1], F32)
        B["kaug"] = cv.get([128, 128], BF16)
        for kv, wd in enumerate((ckw2_d, cvw2_d)):
            dma("pool", B["w2"][kv][:], wd.rearrange("(c p) d -> p c d", p=128), "w2", writes=["w2"])
        dma("sp", B["posn"][:, 0, :], cposk_d, "c", writes=["posn"])
        dma("sp", B["posn"][:, 1, :], cposv_d, "c", writes=["posn"])
        dma("sp", B["stgb"][0:2, :], ckb1_d.rearrange("(a c) -> a c", c=128), "c", writes=["stgb"])
        dma("sp", B["stgb"][2:4, :], cvb1_d.rearrange("(a c) -> a c", c=128), "c", writes=["stgb"])
        dma("sp", B["b2bc"][:, 0, :], ckb2_d.partition_broadcast(128), "c", writes=["b2bc"])
        dma("sp", B["b2bc"][:, 1, :], cvb2_d.partition_broadcast(128), "c", writes=["b2bc"])
        dma("sp", B["gcm"][:], ckc_d.partition_broadcast(128), "c", writes=["gcm"])
        dma("pool", B["kalc"][:], kalc_d, "c", writes=["kalc"])
        for g in range(2):
            dma("pool", Vca[:, g, 65:97], ovl_d, "c", writes=[("Vca", g)])
            MSET(Vca[:, g, 64:65], 1.0, [("Vca", g)])
        MSET(B["kaug"][:], 0.0, ["kaug"])
        return B

    def compress_stage(W, B, w1):
        s = B["s"]
        KVD, w2, posn, posT, stgb, b1sb, biasj = B["KVD"], B["w2"], B["posn"], B["posT"], B["stgb"], B["b1sb"], B["biasj"]
        b2bc, gcm, kalc, hid, kcf, junk2, ssc, kaug = B["b2bc"], B["gcm"], B["kalc"], B["hid"], B["kcf"], B["junk2"], B["ssc"], B["kaug"]
        for kv in range(2):
            tr(pf[0][0:64, kv * 32:(kv + 1) * 32], posn[:, kv, :], ["posn"], [("pf", 0)], idn=identf[0:32, 0:32])
        tr(pf[0][:, 64:68], stgb[:], ["stgb"], [("pf", 0)], idn=identf[0:4, 0:4])
        CPY(posT[:], pf[0][0:64, 0:64].rearrange("p (k l) -> p k l", l=32), [("pf", 0)], ["posT"])
        CPY(b1sb[:], pf[0][:, 64:68], [("pf", 0)], ["b1sb"])
        for kv in range(2):
            for jc in range(2):
                col = kv * 2 + jc
                for l in range(32):
                    mm(pf[1][:, col:col + 1], w1[kv][0][0:64, l, jc * 128:(jc + 1) * 128], posT[0:64, kv, l:l + 1], l == 0, l == 31,
                       ["w1B", "posT"], [("pf", 1)])
        TT(biasj[:], pf[1][:, 0:4], b1sb[:], ALU.add, [("pf", 1), "b1sb"], ["biasj"])
        for tq in range(4):
            for which in range(2):
                pp = pf[2 + which]
                for k in range(8):
                    mm(pp[:], wbuf[s][:, k, which * 128:(which + 1) * 128], hT[:, k, tq * 512:(tq + 1) * 512], k == 0, k == 7,
                       [("w", s)] + [("hT", 4 * tq + u) for u in range(4)], [("pf", 2 + which)])
                act(KVD[which][:, :, tq * 32:(tq + 1) * 32].rearrange("p r m -> p m r"), pp[:].rearrange("p (m r) -> p m r", r=16),
                    AF.Copy, [("pf", 2 + which)], [("KVT", which)])
        n = 0
        for kv in range(2):
            for g in range(2):
                hb_ = hid[kv * 2 + g]
                for jc in range(2):
                    pp = pf[n % 2]
                    for l in range(32):
                        mm(pp[:, 0:127], w1[kv][g][g * 64:(g + 1) * 64, l, jc * 128:(jc + 1) * 128],
                           KVD[kv][g * 64:(g + 1) * 64, l % 16, (l // 16):(l // 16) + 127], l == 0, l == 31,
                           ["w1B", ("KVT", kv)], [("pf", n % 2)])
                    act(hb_[:, jc, 0:127], pp[:, 0:127], AF.Silu, [("pf", n % 2), "biasj"], [("hid", kv * 2 + g)],
                        bias=biasj[:, kv * 2 + jc:kv * 2 + jc + 1], scale=1.0)
                    n += 1
                po = pf[2 + g]
                for jc in range(2):
                    mm(po[0:127, 0:64], hb_[:, jc, 0:127], w2[kv][:, jc, :], jc == 0, jc == 1, [("hid", kv * 2 + g), "w2"], [("pf", 2 + g)])
                if kv == 0:
                    TT(kcf[0:127, :], po[0:127, 0:64], b2bc[0:127, 0, :], ALU.add, [("pf", 2 + g), "b2bc"], ["kcf"])
                    act(junk2[0:127, :], kcf[0:127, :], AF.Square, ["kcf"], ["junk2", "ssc"], accum_out=ssc[0:127, :])
                    TS(ssc[0:127, :], ssc[0:127, :], 1.0 / 64, EPS, ALU.mult, ALU.add, ["ssc"], ["ssc"])
                    act(ssc[0:127, :], ssc[0:127, :], AF.Sqrt, ["ssc"], ["ssc"])
                    RCP(ssc[0:127, :], ssc[0:127, :], ["ssc"], ["ssc"])
                    STT(kaug[0:127, 0:64], kcf[0:127, :], ssc[0:127, :], gcm[0:127, :], ALU.mult, ALU.mult, ["kcf", "ssc", "gcm"], ["kaug"])
                    CPY(kaug[:, 64:100], kalc[:], ["kalc"], ["kaug"])
                    tr(pb[g][:, 0:128], kaug[:], ["kaug"], [("pb", g)])
                    act(KTc[:, g, :], pb[g][:, 0:128], AF.Copy, [("pb", g)], [("KTc", g)])
                else:
                    TT(Vca[0:127, g, 0:64], po[0:127, 0:64], b2bc[0:127, 1, :], ALU.add, [("pf", 2 + g), "b2bc"], [("Vca", g)])

    def nsa_half(g, W):
        cv = Carver()
        QT = cv.get([128, 4, S], BF16)
        KTs = cv.get([128, S], BF16)
        KTw = cv.get([128, S], BF16)
        Vs = cv.get([128, NT, 66], BF16)
        Vw = cv.get([128, NT, 66], BF16)
        szy = cv.get([128, NT, 256], BF16)
        gates = cv.get([128, NT, 12], F32)
        oacc = cv.get([128, NT, 256], F32)
        imp = cv.get([128, 8, 32], F32)
        impc = cv.get([128, 8, 32], F32)
        cmn = cv.get([128, S], BF16)
        qa = [cv.get([128, 4, 128], BF16) for _ in range(3)]
        ksa = [cv.get([128, 128], BF16) for _ in range(3)]
        kwa = [cv.get([128, 128], BF16) for _ in range(3)]
        ptb = [cv.get([128, 512], BF16) for _ in range(3)]
        tmpf = [cv.get([128, 512], F32) for _ in range(2)]
        cmpb = cv.get([128, 32, 32], F32)
        rank = cv.get([128, 32], F32)
        nmt = cv.get([128, 128], BF16)
        s8 = [cv.get([128, 8], F32) for _ in range(3)]
        gq = cv.get([128, 1], F32)
        gks = cv.get([128, 1], F32)
        gkw = cv.get([128, 1], F32)
        rden = [cv.get([128, 1], F32) for _ in range(8)]
        yT = [cv.get([128, 2, 128], BF16) for _ in range(2)]
        s = load_w(W, [(g * 256, 256), (768 + 64 * g, 64), (1024 + 64 * g, 64), (896 + 64 * g, 64), (1152 + 64 * g, 64)])
        s2 = load_w(W, [(1304 + g * 256, 256), (1280 + 4 * g, 4), (1288 + 4 * g, 4), (1296 + 4 * g, 4)])
        load_wo(1, 2 * g, 2)
        load_gain_col(gq, cq_d, 1.0)
        load_gain_col(gks, cks_d, 8.0)
        load_gain_col(gkw, ckw_d, 8.0)
        dma("sp", impc[:], impc_d[:, 8:16, :], "c", writes=["impc"])
        dma("pool", cmn[:], cmn_d, "c", writes=["addmask"])
        for b in range(3):
            MSET(qa[b][:], 0.0, [("qa", b)])
            MSET(ksa[b][:], 0.0, [("ksa", b)])
            MSET(kwa[b][:], 0.0, [("kwa", b)])
        MSET(Vs[:], 1.0, ["Vs"])
        MSET(Vw[:], 1.0, ["Vw"])
        MSET(nmt[:], 0.0, ["nmt"])
        def pa(i):
            b2 = i % 2
            pp = pf[b2]
            qai, ksi, kwi = qa[b2], ksa[b2], kwa[b2]
            CPY(qai[:, :, 96:100], qal[:, i, g * 16:g * 16 + 16].rearrange("p (h c) -> p h c", c=4), ["const"], [("qa", b2)])
            CPY(ksi[:, 64:100], kals[:, i, :], ["const"], [("ksa", b2)])
            CPY(kwi[:, 64:100], kalp[:, i, :], ["const"], [("kwa", b2)])
            for k in range(8):
                mm(pp[:], hT[:, k, i * 128:(i + 1) * 128], wbuf[s][:, k, :], k == 0, k == 7, [("hT", i), ("w", s)], [("pf", b2)])
            tf = tmpf[b2]
            head_rstd(pp, ("pf", b2), 384, tf, ("tf", b2), s8[b2], ("s8", b2))
            TT(qai[:, :, 0:64], pp[:, 0:256].rearrange("p (h d) -> p h d", d=64), s8[b2][:, 0:4].unsqueeze(2).to_broadcast([128, 4, 64]),
               ALU.mult, [("pf", b2), ("s8", b2)], [("qa", b2)])
            TS(ksi[:, 0:64], pp[:, 256:320], s8[b2][:, 4:5], None, ALU.mult, None, [("pf", b2), ("s8", b2)], [("ksa", b2)])
            TS(kwi[:, 0:64], pp[:, 320:384], s8[b2][:, 5:6], None, ALU.mult, None, [("pf", b2), ("s8", b2)], [("kwa", b2)])
            act(Vs[:, i, 0:64], pp[:, 384:448], AF.Copy, [("pf", b2), "Vs"], [("Vs", i)])
            act(Vw[:, i, 0:64], pp[:, 448:512], AF.Copy, [("pf", b2), "Vw"], [("Vw", i)])

        def pbk(i):
            b2 = i % 2
            qai, ksi, kwi = qa[b2], ksa[b2], kwa[b2]
            pt = pb[b2]
            for h in range(4):
                tr(pt[:, h * 128:(h + 1) * 128], qai[:, h, :], [("qa", b2)], [("pb", b2)])
            tr(pt[:, 512:640], ksi[:], [("ksa", b2)], [("pb", b2)])
            tr(pt[:, 640:768], kwi[:], [("kwa", b2)], [("pb", b2)])
            act(QT[:, :, i * 128:(i + 1) * 128], pt[:, 0:512].rearrange("p (h t) -> p h t", h=4), AF.Copy, [("pb", b2), "gq"], [("QT", i)],
                scale=gq[:, 0:1])
            act(KTs[:, i * 128:(i + 1) * 128], pt[:, 512:640], AF.Copy, [("pb", b2), "gq"], [("KTs", i)], scale=gks[:, 0:1])
            act(KTw[:, i * 128:(i + 1) * 128], pt[:, 640:768], AF.Copy, [("pb", b2), "gq"], [("KTw", i)], scale=gkw[:, 0:1])

        lagged(NT, pa, pbk)
        for i in range(NT):
            b2 = 2 + i % 2
            pp = pf[b2]
            for k in range(8):
                mm(pp[:, 0:256], hT[:, k, i * 128:(i + 1) * 128], wbuf[s2][:, k, 0:256], k == 0, k == 7, [("hT", i), ("w", s2)], [("pf", b2)])
            act(szy[:, i, :], pp[:, 0:256], AF.Silu, [("pf", b2)], [("szy", i)])
        for i in range(NT):
            b2 = 2 + i % 2
            pp = pf[b2]
            for k in range(8):
                mm(pp[:, 0:12], hT[:, k, i * 128:(i + 1) * 128], wbuf[s2][:, k, 256:268], k == 0, k == 7, [("hT", i), ("w", s2)], [("pf", b2)])
            act(gates[:, i, :], pp[:, 0:12], AF.Sigmoid, [("pf", b2)], [("gates", i)])
        if stop == "nsaproj":
            return
        qk = lambda qc: [("QT", 4 * qc + u) for u in range(4)]
        for r in range(4):
            def fin_cmp(i, Ob, okey, r=r):
                ri = (i + 4 * r) % 8
                rd = rden[ri]
                TS(rd[:], Ob[:, 64:65], 1e-30, None, ALU.max, None, [okey], [("rden", ri)])
                RCP(rd[:], rd[:], [("rden", ri)], [("rden", ri)])
                TS(oacc[:, i, r * 64:(r + 1) * 64], Ob[:, 0:64], rd[:], gates[:, i, r:r + 1], ALU.mult, ALU.mult,
                   [okey, ("rden", ri), ("gates", i)], [("oacc", i)])
                if i >= 8:
                    if r == 0:
                        TS(imp[:, i - 8, :], Ob[:, 65:97], rd[:], None, ALU.mult, None, [okey, ("rden", ri)], [("imp", i)])
                    else:
                        STT(imp[:, i - 8, :], Ob[:, 65:97], rd[:], imp[:, i - 8, :], ALU.mult, ALU.add, [okey, ("rden", ri), ("imp", i)], [("imp", i)])

            attention(QT[:, r, :], lambda kt: KTc[:, g, 0:127], 127, lambda kt: Vca[0:127, g, 0:97], 97,
                      lambda qc: [(0, 0, 3, {})], fin_cmp, qk, lambda kt: [("KTc", g)], lambda kt: [("Vca", g)], ptb, "n", addmask=cmn)
        if stop == "nsacmp":
            return
        for i in range(8, NT):
            im = imp[:, i - 8, :]
            TT(im, im, impc[:, i - 8, :], ALU.add, [("imp", i), "impc"], [("imp", i)])
            TT(cmpb[:], im.unsqueeze(1).to_broadcast([128, 32, 32]), im.unsqueeze(2).to_broadcast([128, 32, 32]), ALU.is_gt,
               [("imp", i)], ["cmpb"])
            RED(rank[:], cmpb[:], ALU.add, ["cmpb"], ["rank"])
            TS(nmt[:, 0:32], rank[:], 15.5, NEGM, ALU.is_ge, ALU.mult, ["rank"], ["nmt"])
            pt = pb[i % 2]
            tr(pt[:, 0:128], nmt[:], ["nmt"], [("pb", i % 2)])
            for r in range(4):
                act(QT[64:96, r, i * 128:(i + 1) * 128], pt[0:32, 0:128], AF.Copy, [("pb", i % 2)], [("QT", i)])
        for (KTx, Vx, kname, vname, plan, gcol) in ((KTs, Vs, "KTs", "Vs", plan_causal, 4), (KTw, Vw, "KTw", "Vw", plan_band(4), 8)):
            for r in range(4):
                def fin_add(i, Ob, okey, r=r, gcol=gcol):
                    ri = (i + 4 * r) % 8
                    rd = rden[ri]
                    RCP(rd[:], Ob[:, 64:65], [okey], [("rden", ri)])
                    TT(rd[:], rd[:], gates[:, i, gcol + r:gcol + r + 1], ALU.mult, [("rden", ri), ("gates", i)], [("rden", ri)])
                    dst = oacc[:, i, r * 64:(r + 1) * 64]
                    STT(dst, Ob[:, 0:64], rd[:], dst, ALU.mult, ALU.add, [okey, ("rden", ri), ("oacc", i)], [("oacc", i)])

                attention(QT[:, r, :], lambda kt, KTx=KTx: KTx[:, kt * 128:(kt + 1) * 128], 128,
                          lambda kt, Vx=Vx: Vx[:, kt, 0:65], 65, plan, fin_add, qk,
                          lambda kt, kname=kname: [(kname, kt)], lambda kt, vname=vname: [(vname, kt)], ptb, "n")
        for i in range(NT):
            TT(szy[:, i, :], oacc[:, i, :], szy[:, i, :], ALU.mult, [("oacc", i), ("szy", i)], [("szy", i)])
        outproj_half(szy, yT)

    def swa_half(g, W):
        cv = Carver()
        QT = cv.get([128, 4, S], BF16)
        KT = cv.get([128, S], BF16)
        Vt = cv.get([128, NT, 66], BF16)
        szy = cv.get([128, NT, 256], BF16)
        qa = [cv.get([128, 4, 128], BF16) for _ in range(3)]
        ka = [cv.get([128, 128], BF16) for _ in range(3)]
        ptb = [cv.get([128, 512], BF16) for _ in range(3)]
        tmpf = [cv.get([128, 512], F32) for _ in range(2)]
        s8 = [cv.get([128, 8], F32) for _ in range(3)]
        gq = cv.get([128, 1], F32)
        gk = cv.get([128, 1], F32)
        esink = cv.get([128, 8], F32)
        rden = [cv.get([128, 1], F32) for _ in range(8)]
        yT = [cv.get([128, 2, 128], BF16) for _ in range(2)]
        load_gain_col(gq, dq_d, 1.0)
        load_gain_col(gk, dk_d, 8.0)
        dma("sp", esink[:], dsink_d.partition_broadcast(128), "c", writes=["esink"])
        act(esink[:], esink[:], AF.Exp, ["esink"], ["esink"])
        for b in range(3):
            MSET(qa[b][:], 0.0, [("qa", b)])
            MSET(ka[b][:], 0.0, [("ka", b)])
        MSET(Vt[:], 1.0, ["Vt"])
        s = load_w(W, [(1816 + g * 256, 256), (2328 + 64 * g, 64), (2456 + 64 * g, 64)])
        s2 = load_w(W, [(2584 + g * 256, 256)])
        load_wo(1, 4 + 2 * g, 2)
        def pa(i):
            b2 = i % 2
            pp = pf[b2]
            qai, kai = qa[b2], ka[b2]
            CPY(qai[:, :, 96:100], qal[:, i, g * 16:g * 16 + 16].rearrange("p (h c) -> p h c", c=4), ["const"], [("qa", b2)])
            CPY(kai[:, 64:100], kalp[:, i, :], ["const"], [("ka", b2)])
            for k in range(8):
                mm(pp[:, 0:384], hT[:, k, i * 128:(i + 1) * 128], wbuf[s][:, k, 0:384], k == 0, k == 7, [("hT", i), ("w", s)], [("pf", b2)])
            tf = tmpf[b2]
            head_rstd(pp, ("pf", b2), 320, tf, ("tf", b2), s8[b2], ("s8", b2))
            TT(qai[:, :, 0:64], pp[:, 0:256].rearrange("p (h d) -> p h d", d=64), s8[b2][:, 0:4].unsqueeze(2).to_broadcast([128, 4, 64]),
               ALU.mult, [("pf", b2), ("s8", b2)], [("qa", b2)])
            TS(kai[:, 0:64], pp[:, 256:320], s8[b2][:, 4:5], None, ALU.mult, None, [("pf", b2), ("s8", b2)], [("ka", b2)])
            act(Vt[:, i, 0:64], pp[:, 320:384], AF.Copy, [("pf", b2), "Vt"], [("V", i)])

        def pbk(i):
            b2 = i % 2
            qai, kai = qa[b2], ka[b2]
            pt = pb[b2]
            for h in range(4):
                tr(pt[:, h * 128:(h + 1) * 128], qai[:, h, :], [("qa", b2)], [("pb", b2)])
            tr(pt[:, 512:640], kai[:], [("ka", b2)], [("pb", b2)])
            act(QT[:, :, i * 128:(i + 1) * 128], pt[:, 0:512].rearrange("p (h t) -> p h t", h=4), AF.Copy, [("pb", b2), "gq"], [("QT", i)],
                scale=gq[:, 0:1])
            act(KT[:, i * 128:(i + 1) * 128], pt[:, 512:640], AF.Copy, [("pb", b2), "gq"], [("KT", i)], scale=gk[:, 0:1])

        lagged(NT, pa, pbk)
        for i in range(NT):
            b2 = 2 + i % 2
            pp = pf[b2]
            for k in range(8):
                mm(pp[:, 0:256], hT[:, k, i * 128:(i + 1) * 128], wbuf[s2][:, k, 0:256], k == 0, k == 7, [("hT", i), ("w", s2)], [("pf", b2)])
            act(szy[:, i, :], pp[:, 0:256], AF.Silu, [("pf", b2)], [("szy", i)])
        for r in range(4):
            def fin(i, Ob, okey, r=r):
                ri = (i + 4 * r) % 8
                rd = rden[ri]
                TS(rd[:], Ob[:, 64:65], esink[:, 4 * g + r:4 * g + r + 1], None, ALU.add, None, [okey, "esink"], [("rden", ri)])
                RCP(rd[:], rd[:], [("rden", ri)], [("rden", ri)])
                dst = szy[:, i, r * 64:(r + 1) * 64]
                STT(dst, Ob[:, 0:64], rd[:], dst, ALU.mult, ALU.mult, [okey, ("rden", ri), ("szy", i)], [("szy", i)])

            attention(QT[:, r, :], lambda kt: KT[:, kt * 128:(kt + 1) * 128], 128, lambda kt: Vt[:, kt, 0:65], 65,
                      plan_band(1), fin, lambda qc: [("QT", 4 * qc + u) for u in range(4)], lambda kt: [("KT", kt)],
                      lambda kt: [("V", kt)], ptb, "d")
        outproj_half(szy, yT, store=(g == 1 and stop is None))

    if 0 in layers:
        layer0()
    if 1 in layers:
        layer1()

    if not (1 in layers and stop is None):
        for i in range(NT):
            dma("sp", yv[:, i, :], x_sb[:, i, :], "out", reads=[("x", i)])
    P.finish("sp", list(P.chan_count.keys()))
    P.emit()
    es.close()
    return nc


_CACHE = {}


def kernel(**inputs):
    consts = _consts()
    shared = {}
    shared["norm_g"] = np.ascontiguousarray(inputs["norm_g"], dtype=np.float32)
    shared["w_out"] = np.ascontiguousarray(inputs["w_out"], dtype=np.float32)
    shared["e_w_in"] = np.ascontiguousarray(inputs["e_w_in"][0], dtype=np.float32)
    shared["a_conv_w"] = np.ascontiguousarray(inputs["a_conv_w"][0], dtype=np.float32)
    shared["a_conv_b"] = np.ascontiguousarray(inputs["a_conv_b"][0], dtype=np.float32)
    shared["a_ln_g"] = np.ascontiguousarray(inputs["a_ln_g"][0], dtype=np.float32)
    shared["a_ln_b"] = np.ascontiguousarray(inputs["a_ln_b"][0], dtype=np.float32)
    shared["b_qnorm_g"] = np.ascontiguousarray(inputs["b_qnorm_g"], dtype=np.float32)
    shared["b_knorm_g"] = np.ascontiguousarray(inputs["b_knorm_g"], dtype=np.float32)
    for k in ("ident", "tri", "up", "qal", "kal_moba", "kal_slc", "kal_plain", "kal_cmp", "cmaskneg", "ovl", "impc"):
        shared[k] = consts[k]
    shared["o_w_in"] = np.ascontiguousarray(inputs["o_w_in"][0], dtype=np.float32)
    for k in ("c_qnorm_g", "c_knorm_cmp_g", "c_knorm_slc_g", "c_knorm_win_g", "c_k_b2", "c_v_b2", "d_qnorm_g", "d_knorm_g", "d_sinks"):
        shared[k] = np.ascontiguousarray(inputs[k], dtype=np.float32).reshape(1, -1)
    for k in ("c_pos_k", "c_pos_v", "c_k_w1", "c_k_b1", "c_k_w2", "c_v_w1", "c_v_b1", "c_v_w2"):
        shared[k] = np.ascontiguousarray(inputs[k][0], dtype=np.float32)
    x = np.ascontiguousarray(inputs["x"], dtype=np.float32)
    nb = x.shape[0]
    layers = inputs.get("_layers", (0, 1))
    nc = build_program(layers, inputs.get("_stop"))
    in_maps = [dict(shared, x=x[b]) for b in range(nb)]
    res = run_bass_kernel_spmd(nc, in_maps, core_ids=list(range(nb)))
    return np.stack([np.asarray(r["y"], dtype=np.float32) for r in res.results], axis=0)
```

```python
import contextlib
import os
import numpy as np
import ml_dtypes
import concourse.bass as bass
import concourse.mybir as mybir
from concourse.bass_utils import run_bass_kernel_spmd

F32 = mybir.dt.float32
BF16 = mybir.dt.bfloat16
ALU = mybir.AluOpType
AF = mybir.ActivationFunctionType
AX = mybir.AxisListType

S = 2048
D = 1024
NT = 16
NEGM = -30000.0
EPS = 1e-6


class _Op:
    __slots__ = ("eng", "fn", "deps", "chan", "signal", "val", "dmaval", "chanseq")

    def __init__(self, eng, fn, chan):
        self.eng = eng
        self.fn = fn
        self.deps = {}
        self.chan = chan
        self.signal = False
        self.val = 0
        self.dmaval = None


class Prog:
    ENGS = ("pe", "act", "dve", "pool", "sp")

    def __init__(self, nc):
        self.nc = nc
        self.ops = {e: [] for e in self.ENGS}
        self.res = {}
        self.chan_count = {}
        self.chan_last = {}
        self.all_ops = []
        self.bar = {}
        self.final_waits = {}
        self.pool_ctr = 0
        self.sp_ctr = 0
        self.pres = {}

    PERS = ("w", "wo")

    def _st(self, k):
        if isinstance(k, tuple) and k[0] in self.PERS or k in self.PERS:
            return self.pres
        return self.res

    def op(self, eng, fn, reads=(), writes=(), chan=None, nobar=False):
        if chan is not None and eng == "pool":
            chan = "pq%d" % (self.pool_ctr % 12)
            self.pool_ctr += 1
        elif chan == "c":
            chan = "pc%d" % (self.sp_ctr % 16)
            self.sp_ctr += 1
        o = _Op(eng, fn, chan)
        deps = o.deps
        if not nobar:
            deps.update(self.bar)
        if chan is not None and (chan.startswith("pq") or chan.startswith("pc")):
            prev = self.chan_last.get(chan)
            if prev is not None:
                deps[id(prev)] = prev
        for k in reads:
            st = self._st(k).get(k)
            if st is not None and st[0] is not None:
                deps[id(st[0])] = st[0]
        for k in writes:
            st = self._st(k).get(k)
            if st is not None:
                if st[0] is not None:
                    deps[id(st[0])] = st[0]
                for r in st[1]:
                    deps[id(r)] = r
        for k in reads:
            res = self._st(k)
            st = res.get(k)
            if st is None:
                st = [None, []]
                res[k] = st
            st[1].append(o)
        for k in writes:
            self._st(k)[k] = [o, []]
        o.dmaval = dict(self.chan_count)
        if chan is not None:
            self.chan_count[chan] = self.chan_count.get(chan, 0) + 1
            o.chanseq = self.chan_count[chan]
            self.chan_last[chan] = o
            o.signal = True
        self.ops[eng].append(o)
        self.all_ops.append(o)
        return o

    def barrier(self):
        bar = {}
        for e in self.ENGS:
            if self.ops[e]:
                o = self.ops[e][-1]
                bar[id(o)] = o
        for ch, o in self.chan_last.items():
            bar[id(o)] = o
        self.bar = bar
        self.res = {}

    def finish(self, eng, chans):
        self.final_waits = {eng: {ch: self.chan_count[ch] for ch in chans}}

    def emit(self):
        nc = self.nc
        for o in self.all_ops:
            for d in o.deps.values():
                if d.chan is None:
                    if d.eng == "pe" and o.eng == "pe":
                        continue
                    d.signal = True
        for e in self.ENGS:
            c = 0
            for o in self.ops[e]:
                if o.chan is None and o.signal:
                    c += 1
                    o.val = c
        import os
        if os.environ.get("KDBG"):
            print("sem counts", {e: max([o.val for o in self.ops[e]] + [0]) for e in self.ENGS}, {e: len(self.ops[e]) for e in self.ENGS},
                  {c: 16 * v for c, v in self.chan_count.items()})
        stack = contextlib.ExitStack()
        sems = {}
        for e in self.ENGS:
            sems[e] = stack.enter_context(nc.semaphore("s_" + e))
        for ch in self.chan_count:
            sems["c_" + ch] = stack.enter_context(nc.semaphore("c_" + ch))
        block = stack.enter_context(nc.Block())
        engobj = {"pe": "tensor", "act": "scalar", "dve": "vector", "pool": "gpsimd", "sp": "sync"}

        def make(e):
            def body(eng):
                waited = {}
                for o in self.ops[e]:
                    need = {}
                    for d in o.deps.values():
                        if d.chan is not None:
                            k = "c_" + d.chan
                            if d.chan.startswith("pq") or d.chan.startswith("pc"):
                                v = 16 * d.chanseq
                            else:
                                v = 16 * o.dmaval[d.chan]
                        else:
                            if d.eng == "pe" and e == "pe":
                                continue
                            k = d.eng
                            v = d.val
                        if v > need.get(k, 0):
                            need[k] = v
                    for k, v in need.items():
                        if waited.get(k, 0) >= v:
                            continue
                        eng.wait_ge(sems[k], v)
                        waited[k] = v
                    ins = o.fn(eng)
                    if o.chan is not None:
                        ins.then_inc(sems["c_" + o.chan], 16)
                    elif o.signal:
                        ins.then_inc(sems[e], 1)
                for ch, c in self.final_waits.get(e, {}).items():
                    eng.wait_ge(sems["c_" + ch], 16 * c)
            return body

        for e in self.ENGS:
            getattr(block, engobj[e])(make(e))
        stack.close()


def _consts():
    c = {}
    c["ident"] = np.eye(128, dtype=np.float32)
    k = np.arange(128)[:, None]
    q = np.arange(128)[None, :]
    c["tri"] = np.where(k <= q, 0.0, NEGM).astype(np.float32)
    c["up"] = np.where(k > q, 0.0, NEGM).astype(np.float32)
    t = np.arange(S)
    b = (t % 16).astype(np.float32)
    a = (t - t % 16).astype(np.float32)
    slopes = np.power(2.0, -8.0 * np.arange(1, 9) / 8).astype(np.float32)
    qal = np.zeros((S, 8, 4), np.float32)
    qal[:, :, 0] = -slopes[None, :] * a[:, None]
    qal[:, :, 1] = -slopes[None, :] * b[:, None]
    qal[:, :, 2] = slopes[None, :]
    qal[:, :, 3] = slopes[None, :]
    c["qal"] = qal.reshape(NT, 128, 32).transpose(1, 0, 2).copy()

    def kal(onehot_block):
        m = np.zeros((S, 36), np.float32)
        if onehot_block:
            m[t, t // onehot_block] = 1.0
        m[:, 32] = 1.0
        m[:, 33] = 1.0
        m[:, 34] = a
        m[:, 35] = b
        return m.reshape(NT, 128, 36).transpose(1, 0, 2).copy()

    c["kal_moba"] = kal(256)
    c["kal_slc"] = kal(64)
    c["kal_plain"] = kal(0)
    cc = np.arange(128)
    cend = 16 * cc + 31
    kc = np.zeros((128, 36), np.float32)
    kc[:, 32] = 1.0
    kc[:, 33] = 1.0
    kc[:, 34] = cend - cend % 16
    kc[:, 35] = cend % 16
    c["kal_cmp"] = kc
    c["cmaskneg"] = np.where(t[None, :] >= cend[:, None], 0.0, NEGM).astype(np.float32)
    start = np.arange(127)[:, None] * 16
    bs = np.arange(32)[None, :] * 64
    ov = np.zeros((128, 32), np.float32)
    ov[:127] = ((start < bs + 64) & (start + 32 > bs)).astype(np.float32)
    c["ovl"] = ov
    blk = np.arange(32)[None, :]
    cur = (t // 64)[:, None]
    forced = (blk == 0) | (blk == cur) | (blk == cur - 1)
    impc = 1e4 * forced.astype(np.float32) - 1e5 * (blk > cur).astype(np.float32)
    c["impc"] = impc.reshape(NT, 128, 32).transpose(1, 0, 2).copy()
    return c


def build_program(layers=(0, 1), stop=None):
    nc = bass.Bass("TRN2", target_bir_lowering=False)
    es = contextlib.ExitStack()
    dram = {}

    def din(name, shape, dt=F32):
        dram[name] = nc.dram_tensor(name, list(shape), dt, kind="ExternalInput").ap()
        return dram[name]

    x_d = din("x", [S, D])
    normg_d = din("norm_g", [2, D])
    wout_d = din("w_out", [2, D, D])
    ewin_d = din("e_w_in", [D, 3584])
    convw_d = din("a_conv_w", [31, 512])
    convb_d = din("a_conv_b", [512])
    lng_d = din("a_ln_g", [512])
    lnb_d = din("a_ln_b", [512])
    bq_d = din("b_qnorm_g", [1, 64])
    bk_d = din("b_knorm_g", [1, 64])
    ident_d = din("ident", [128, 128])
    tri_d = din("tri", [128, 128])
    up_d = din("up", [128, 128])
    qal_d = din("qal", [128, NT, 32])
    kalm_d = din("kal_moba", [128, NT, 36])
    kals_d = din("kal_slc", [128, NT, 36])
    kalp_d = din("kal_plain", [128, NT, 36])
    kalc_d = din("kal_cmp", [128, 36])
    cmn_d = din("cmaskneg", [128, S])
    ovl_d = din("ovl", [128, 32])
    impc_d = din("impc", [128, NT, 32])
    owin_d = din("o_w_in", [D, 3096])
    cq_d = din("c_qnorm_g", [1, 64])
    ckc_d = din("c_knorm_cmp_g", [1, 64])
    cks_d = din("c_knorm_slc_g", [1, 64])
    ckw_d = din("c_knorm_win_g", [1, 64])
    cposk_d = din("c_pos_k", [32, 64])
    cposv_d = din("c_pos_v", [32, 64])
    ckw1_d = din("c_k_w1", [2048, 256])
    ckb1_d = din("c_k_b1", [256])
    ckw2_d = din("c_k_w2", [256, 64])
    ckb2_d = din("c_k_b2", [1, 64])
    cvw1_d = din("c_v_w1", [2048, 256])
    cvb1_d = din("c_v_b1", [256])
    cvw2_d = din("c_v_w2", [256, 64])
    cvb2_d = din("c_v_b2", [1, 64])
    dq_d = din("d_qnorm_g", [1, 64])
    dk_d = din("d_knorm_g", [1, 64])
    dsink_d = din("d_sinks", [1, 8])
    y_d = nc.dram_tensor("y", [S, D], F32, kind="ExternalOutput").ap()

    def sb(name, shape, dt):
        return es.enter_context(nc.sbuf_tensor(name, list(shape), dt))

    def psum(name, shape, dt):
        return es.enter_context(nc.psum_tensor(name, list(shape), dt))

    P = Prog(nc)

    x_sb = sb("x_sb", [128, NT, D], F32)
    hT = sb("hT", [128, 8, S], BF16)
    wbuf = [sb("wbuf%d" % i, [128, 8, 512], BF16) for i in range(2)]
    wo = sb("wo", [128, 4, D], BF16)
    ident = sb("ident_sb", [128, 128], BF16)
    identf = sb("identf_sb", [128, 128], F32)
    onesf = sb("onesf", [128, 128], F32)
    tri = sb("tri_sb", [128, 128], BF16)
    upm = sb("up_sb", [128, 128], BF16)
    qal = sb("qal_sb", [128, NT, 32], BF16)
    kalm = sb("kalm_sb", [128, NT, 36], BF16)
    KTc = sb("KTc", [128, 2, 128], BF16)
    Vca = sb("Vca", [128, 2, 98], BF16)
    kals = sb("kals_sb", [128, NT, 36], BF16)
    kalp = sb("kalp_sb", [128, NT, 36], BF16)
    ss = sb("ss", [128, NT], F32)
    rstd = sb("rstd", [128, NT], F32)
    ARENA = 80 * 1024
    arena = sb("arena", [128, ARENA // 2], BF16)

    pf = [psum("pf%d" % i, [128, 512], F32) for i in range(6)]
    pb = [psum("pb%d" % i, [128, 1024], BF16) for i in range(2)]

    class Carver:
        def __init__(self, base=0):
            self.off = base

        def get(self, shape, dt):
            n = int(np.prod(shape[1:]))
            nb = n * (4 if dt == F32 else 2)
            nb = (nb + 63) // 64 * 64
            assert self.off + nb <= ARENA, ("arena overflow", self.off + nb, ARENA)
            a = arena[:, self.off // 2:(self.off + nb) // 2]
            self.off += nb
            if os.environ.get("KDBG"):
                print("carve", shape, dt, "->", self.off)
            if dt == F32:
                a = a.bitcast(F32)
            a = a[:, 0:n]
            if len(shape) == 3:
                a = a.rearrange("p (a b) -> p a b", b=shape[2])
            elif len(shape) == 4:
                a = a.rearrange("p (a b c) -> p a b c", b=shape[2], c=shape[3])
            if shape[0] < 128:
                a = a[0:shape[0]]
            return a

    def dma(eng, out, in_, chan, reads=(), writes=(), slow=False, nobar=False):
        if slow:
            P.op(eng, lambda e: e.dma_start(out=out, in_=in_, allow_slow_non_contiguous=True), reads=reads, writes=writes, chan=chan, nobar=nobar)
        else:
            P.op(eng, lambda e: e.dma_start(out=out, in_=in_), reads=reads, writes=writes, chan=chan, nobar=nobar)

    def mm(out, lhsT, rhs, start, stop, reads, writes):
        P.op("pe", lambda e: e.matmul(out, lhsT=lhsT, rhs=rhs, start=start, stop=stop), reads=reads, writes=writes)

    def tr(out, in_, reads, writes, idn=None):
        idn = ident[:] if idn is None else idn
        P.op("pe", lambda e: e.transpose(out=out, in_=in_, identity=idn), reads=list(reads) + ["const"], writes=writes)

    def act(out, in_, func, reads, writes, **kw):
        P.op("act", lambda e: e.activation(out=out, in_=in_, func=func, **kw), reads=reads, writes=writes)

    def V(fn, reads, writes, eng="dve"):
        P.op(eng, fn, reads=reads, writes=writes)

    def TT(out, in0, in1, op, reads, writes, eng="dve"):
        P.op(eng, lambda e: e.tensor_tensor(out=out, in0=in0, in1=in1, op=op), reads=reads, writes=writes)

    def TS(out, in0, s1, s2, op0, op1, reads, writes, eng="dve"):
        if op1 is None:
            P.op(eng, lambda e: e.tensor_scalar(out=out, in0=in0, scalar1=s1, scalar2=None, op0=op0), reads=reads, writes=writes)
        else:
            P.op(eng, lambda e: e.tensor_scalar(out=out, in0=in0, scalar1=s1, scalar2=s2, op0=op0, op1=op1), reads=reads, writes=writes)

    def STT(out, in0, scalar, in1, op0, op1, reads, writes, eng="dve"):
        P.op(eng, lambda e: e.scalar_tensor_tensor(out=out, in0=in0, scalar=scalar, in1=in1, op0=op0, op1=op1), reads=reads, writes=writes)

    def RED(out, in_, op, reads, writes):
        P.op("dve", lambda e: e.tensor_reduce(out=out, in_=in_, axis=AX.X, op=op), reads=reads, writes=writes)

    def RCP(out, in_, reads, writes):
        P.op("dve", lambda e: e.reciprocal(out=out, in_=in_), reads=reads, writes=writes)

    def CPY(out, in_, reads, writes, eng="dve"):
        P.op(eng, lambda e: e.tensor_copy(out=out, in_=in_), reads=reads, writes=writes)

    def MSET(ap, val, writes, eng="dve"):
        P.op(eng, lambda e: e.memset(ap, val), reads=[], writes=writes)

    import os
    KB = os.environ.get("KBIS", "abcdefg")
    if "a" in KB:
        dma("pool", ident[:], ident_d, "c", writes=["const"])
    if "b" in KB:
        dma("pool", tri[:], tri_d, "c", writes=["const"])
        dma("pool", upm[:], up_d, "c", writes=["const"])
    if "c" in KB:
        dma("pool", qal[:], qal_d, "c", writes=["const"])
    if "d" in KB:
        dma("pool", kalm[:], kalm_d, "c", writes=["const"])
        dma("pool", kals[:], kals_d, "c", writes=["const"])
        dma("pool", kalp[:], kalp_d, "c", writes=["const"])
    if "e" in KB:
        dma("sp", identf[:], ident_d, "c", writes=["const"])
    if "f" in KB:
        MSET(onesf[:], 1.0, ["const"])

    xv = x_d.rearrange("(i p) d -> p i d", p=128)
    yv = y_d.rearrange("(i p) d -> p i d", p=128)
    for i in range(NT):
        dma("sp", x_sb[:, i, :], xv[:, i, :], "x%d" % i, writes=[("x", i)])

    wslot_ctr = [0]

    def load_w(wd, segs):
        s = wslot_ctr[0] % 2
        wslot_ctr[0] += 1
        wv = wd.rearrange("(k p) n -> p k n", p=128)
        o = 0
        for (c0, n) in segs:
            dma("pool", wbuf[s][:, :, o:o + n], wv[:, :, c0:c0 + n], "w%d" % s, writes=[("w", s)], nobar=True)
            o += n
        return s

    def norm_phase(layer, base=0, barrier=True):
        cvn = Carver(base=base)
        gbc = cvn.get([128, D], F32)
        junk = cvn.get([128, D], BF16)
        hb = [cvn.get([128, D], BF16) for _ in range(2)]
        dma("act", gbc[:], normg_d[layer:layer + 1, :].partition_broadcast(128), "c", writes=["gbc"])
        MSET(ss[:], 0.0, [("ss", gI) for gI in range(4)])

        def stats(gI):
            for i in range(4 * gI, 4 * gI + 4):
                act(junk[:], x_sb[:, i, :], AF.Square, [("x", i), ("ss", gI)], ["junk", ("ss", gI)], accum_out=ss[:, i:i + 1])
            sl = slice(4 * gI, 4 * gI + 4)
            TS(rstd[:, sl], ss[:, sl], 1.0 / D, EPS, ALU.mult, ALU.add, [("ss", gI)], [("rstd", gI)])
            act(rstd[:, sl], rstd[:, sl], AF.Sqrt, [("rstd", gI)], [("rstd", gI)])
            RCP(rstd[:, sl], rstd[:, sl], [("rstd", gI)], [("rstd", gI)])

        def na(i):
            h = hb[i % 2]
            STT(h[:], x_sb[:, i, :], rstd[:, i:i + 1], gbc[:], ALU.mult, ALU.mult, [("x", i), ("rstd", i // 4), "gbc"], [("hb", i % 2)])

        def nb(i):
            h = hb[i % 2]
            pt = pb[i % 2]
            for k in range(8):
                tr(pt[:, k * 128:(k + 1) * 128], h[:, k * 128:(k + 1) * 128], [("hb", i % 2)], [("pb", i % 2)])
            act(hT[:, :, i * 128:(i + 1) * 128], pt[:].rearrange("p (k t) -> p k t", k=8), AF.Copy, [("pb", i % 2)], [("hT", i)])

        pend = []
        for gI in range(4):
            stats(gI)
            for i in range(4 * gI, 4 * gI + 4):
                na(i)
                pend.append(i)
                if len(pend) > 1:
                    nb(pend.pop(0))
        nb(pend.pop(0))
        if barrier:
            P.barrier()

    def load_wo(layer, c0, n):
        wv = wout_d[layer].rearrange("(k p) n -> p k n", p=128)
        for hf in range(2):
            dma("pool", wo[:, 0:n, hf * 512:(hf + 1) * 512], wv[:, c0:c0 + n, hf * 512:(hf + 1) * 512], "wo", writes=["wo"], nobar=True)

    def outproj_tile(i, lhs_list, chunks, yT_reads, pbanks):
        for half in range(2):
            pp = pf[pbanks[half]]
            for n, (c, lhs) in enumerate(zip(chunks, lhs_list)):
                mm(pp[:], lhs, wo[:, c, half * 512:(half + 1) * 512], n == 0, n == len(chunks) - 1,
                   list(yT_reads) + ["wo"], [("pf", pbanks[half])])
            xs = x_sb[:, i, half * 512:(half + 1) * 512]
            TT(xs, xs, pp[:], ALU.add, [("pf", pbanks[half]), ("x", i)], [("x", i)])

    att_ctr = [0, 0]

    def lagged(n, stage_a, stage_b, lag=1):
        for i in range(n + lag):
            if i < n:
                stage_a(i)
            if i >= lag:
                stage_b(i - lag)

    def mm_acc(out, lhsT, rhs, start, stop, reads, writes):
        P.op("pe", lambda e: e.matmul(out, lhsT=lhsT, rhs=rhs, start=start, stop=stop, skip_group_check=True), reads=reads, writes=writes)

    def attention(QT, KT, nk, Vrhs, ncv, plan, finish, qkeys, kkeys, vkeys, ptb, tagk, addmask=None):
        NS = len(ptb)
        work = []
        for qc in range(4):
            items = plan(qc)
            if not items:
                continue
            ob = 3 + att_ctr[1] % 3
            att_ctr[1] += 1
            lastk = {}
            for (kt, jlo, jhi, masks) in items:
                for j in range(jlo, jhi + 1):
                    lastk[j] = kt
            for n, it in enumerate(items):
                work.append((qc, ob, it, n == 0, n == len(items) - 1, lastk))

        def front(w):
            qc, ob, (kt, jlo, jhi, masks), first, last, lastk = w
            sbk = att_ctr[0] % NS
            att_ctr[0] += 1
            Sp = pf[sbk]
            pt = ptb[sbk]
            c0, c1 = jlo * 128, (jhi + 1) * 128
            extra = list(masks.items())
            mm_acc(Sp[0:nk, c0:c1], KT(kt), QT[:, qc * 512 + c0:qc * 512 + c1], True, addmask is None and not extra,
                   list(qkeys(qc)) + list(kkeys(kt)), [("pf", sbk)])
            if addmask is not None:
                mm_acc(Sp[0:nk, c0:c1], ident[0:nk, 0:nk], addmask[0:nk, qc * 512 + c0:qc * 512 + c1], False, not extra,
                       ["const", "addmask"], [("pf", sbk)])
            for n, (j, mk) in enumerate(extra):
                mm_acc(Sp[0:nk, j * 128:(j + 1) * 128], ident[0:nk, 0:nk], mk[0:nk, :], False, n == len(extra) - 1,
                       ["const"], [("pf", sbk)])
            act(pt[0:nk, c0:c1], Sp[0:nk, c0:c1], AF.Exp, [("pf", sbk)], [("pt", tagk, sbk)])
            return sbk

        def back(w, sbk, started):
            qc, ob, (kt, jlo, jhi, masks), first, last, lastk = w
            pt = ptb[sbk]
            for j in range(jlo, jhi + 1):
                mm_acc(pf[ob][:, j * 128:j * 128 + ncv], pt[0:nk, j * 128:(j + 1) * 128], Vrhs(kt), len(started) == 0, lastk[j] == kt,
                       [("pt", tagk, sbk)] + list(vkeys(kt)), [("pf", ob)])
                started.add(j)
            if last:
                for j in sorted(started):
                    finish(qc * 4 + j, pf[ob][:, j * 128:j * 128 + ncv], ("pf", ob))
                started.clear()

        LAG = NS - 1
        started = set()
        pend = []
        for w in work:
            pend.append((w, front(w)))
            if len(pend) > LAG:
                pw, psb = pend.pop(0)
                back(pw, psb, started)
        for (pw, psb) in pend:
            back(pw, psb, started)

    def layer0():
        if stop == "load":
            return
        norm_phase(0, base=70 * 1024, barrier=False)
        if stop == "norm0":
            return
        load_wo(0, 0, 4)
        W = ewin_d
        if stop == "norm":
            return
        cv = Carver()
        yc = cv.get([128, 4, S], F32)
        hc = cv.get([128, S + 32], BF16)
        dg = cv.get([128, 31, 128], BF16)
        cw = cv.get([128, 4, 31], F32)
        cb = cv.get([128, 4], F32)
        lg = cv.get([128, 4], F32)
        lb = cv.get([128, 4], F32)
        Tsq = [cv.get([128, 512], F32) for _ in range(2)]
        mu2 = [cv.get([128, 512], F32) for _ in range(2)]
        msq = cv.get([128, 512], F32)
        rs2 = [cv.get([128, 512], F32) for _ in range(2)]
        sz4 = [cv.get([128, 512], BF16) for _ in range(4)]
        T4 = [cv.get([128, 512], F32) for _ in range(4)]
        yaT2 = [cv.get([128, 4, 512], BF16) for _ in range(2)]
        HO = 32
        stg = Tsq[0][0:32, :]
        stg2 = Tsq[1][0:12, 0:128]
        dma("sp", stg[0:31, :], convw_d, "c", writes=[("Tsq", 0)])
        dma("sp", stg2[0:4, :], convb_d.rearrange("(a c) -> a c", c=128), "c", writes=[("Tsq", 1)])
        dma("sp", stg2[4:8, :], lng_d.rearrange("(a c) -> a c", c=128), "c", writes=[("Tsq", 1)])
        dma("sp", stg2[8:12, :], lnb_d.rearrange("(a c) -> a c", c=128), "c", writes=[("Tsq", 1)])
        for cc in range(4):
            tr(pf[0][:, cc * 32:cc * 32 + 31], stg[0:31, cc * 128:(cc + 1) * 128], [("Tsq", 0)], [("pf", 0)], idn=identf[0:31, 0:31])
        tr(pf[0][:, 128:140], stg2[0:12, :], [("Tsq", 1)], [("pf", 0)], idn=identf[0:12, 0:12])
        CPY(cw[:], pf[0][:, 0:128].rearrange("p (a j) -> p a j", j=32)[:, :, 0:31], [("pf", 0)], ["cw"])
        CPY(cb[:], pf[0][:, 128:132], [("pf", 0)], ["cw"])
        CPY(lg[:], pf[0][:, 132:136], [("pf", 0)], ["cw"])
        CPY(lb[:], pf[0][:, 136:140], [("pf", 0)], ["cw"])
        MSET(hc[:, 0:HO], 0.0, ["hc0"])
        if stop == "convp":
            return
        for cc in range(4):
            if stop == "conv1" and cc == 1:
                return
            s = load_w(W, [(cc * 128, 128), (512 + cc * 128, 128)])
            for j in range(31):
                TS(dg[:, j, :], ident[:], cw[:, cc, j:j + 1], None, ALU.mult, None, ["const", "cw"], [("dg", j)])
            for tq in range(4):
                bv, bg = (tq % 2) * 2, (tq % 2) * 2 + 1
                for which, bk in ((0, bv), (1, bg)):
                    for k in range(8):
                        mm(pf[bk][:], wbuf[s][:, k, which * 128:(which + 1) * 128], hT[:, k, tq * 512:(tq + 1) * 512], k == 0, k == 7,
                           [("w", s)] + [("hT", 4 * tq + u) for u in range(4)], [("pf", bk)])
                act(T4[tq % 2][:], pf[bg][:], AF.Sigmoid, [("pf", bg)], [("T4", tq % 2)])
                TT(hc[:, HO + tq * 512:HO + (tq + 1) * 512], pf[bv][:], T4[tq % 2][:], ALU.mult, [("pf", bv), ("T4", tq % 2)], [("hc", tq)])
            for tq in range(4):
                pc = pf[4 + tq % 2]
                for j in range(31):
                    o = HO - 30 + j + tq * 512
                    mm(pc[:], dg[:, j, :], hc[:, o:o + 512], j == 0, j == 30,
                       [("dg", j), ("hc", tq)] + ([("hc", tq - 1)] if tq > 0 else ["hc0"]), [("pf", 4 + tq % 2)])
                act(yc[:, cc, tq * 512:(tq + 1) * 512], pc[:], AF.Identity, [("pf", 4 + tq % 2), "cw"], [("yc", cc, tq)],
                    bias=cb[:, cc:cc + 1], scale=1.0)
        if stop == "conv4":
            return
        P.barrier()
        sz_slot = load_w(W, [(1024, 512)])

        def st_a(tq):
            ts = slice(tq * 512, (tq + 1) * 512)
            p2 = tq % 2
            for cc in range(4):
                mm(pf[0][:], onesf[:], yc[:, cc, ts], cc == 0, cc == 3, [("yc", cc, tq), "const"], [("pf", 0)])
            for cc in range(4):
                act(Tsq[cc % 2][:], yc[:, cc, ts], AF.Square, [("yc", cc, tq)], [("Tsq", cc % 2)])
                mm(pf[1][:], onesf[:], Tsq[cc % 2][:], cc == 0, cc == 3, [("Tsq", cc % 2), "const"], [("pf", 1)])
            TS(mu2[p2][:], pf[0][:], 1.0 / 512, None, ALU.mult, None, [("pf", 0)], [("mu", p2)])
            TT(msq[:], mu2[p2][:], mu2[p2][:], ALU.mult, [("mu", p2)], ["msq"])
            STT(rs2[p2][:], pf[1][:], 1.0 / 512, msq[:], ALU.mult, ALU.subtract, [("pf", 1), "msq"], [("rs", p2)])
            TS(rs2[p2][:], rs2[p2][:], EPS, None, ALU.add, None, [("rs", p2)], [("rs", p2)])
            act(rs2[p2][:], rs2[p2][:], AF.Sqrt, [("rs", p2)], [("rs", p2)])
            RCP(rs2[p2][:], rs2[p2][:], [("rs", p2)], [("rs", p2)])

        def st_b(tq):
            ts = slice(tq * 512, (tq + 1) * 512)
            p2 = tq % 2
            for cc in range(4):
                pz = pf[2 + cc % 2]
                for k in range(8):
                    mm(pz[:], wbuf[sz_slot][:, k, cc * 128:(cc + 1) * 128], hT[:, k, ts], k == 0, k == 7,
                       [("w", sz_slot)] + [("hT", 4 * tq + u) for u in range(4)], [("pf", 2 + cc % 2)])
                act(sz4[cc][:], pz[:], AF.Silu, [("pf", 2 + cc % 2)], [("sz", cc)])
            for cc in range(4):
                tt = T4[cc]
                TT(tt[:], yc[:, cc, ts], mu2[p2][:], ALU.subtract, [("yc", cc, tq), ("mu", p2)], [("T4", cc)])
                TT(tt[:], tt[:], rs2[p2][:], ALU.mult, [("T4", cc), ("rs", p2)], [("T4", cc)])
            for cc in range(4):
                tt = T4[cc]
                act(tt[:], tt[:], AF.Silu, [("T4", cc), "cw"], [("T4", cc)], scale=lg[:, cc:cc + 1], bias=lb[:, cc:cc + 1])
            for cc in range(4):
                TT(yaT2[p2][:, cc, :], T4[cc][:], sz4[cc][:], ALU.mult, [("T4", cc), ("sz", cc)], [("yaT", p2, cc)])

        def st_c(tq):
            p2 = tq % 2
            for u in range(4):
                i = 4 * tq + u
                outproj_tile(i, [yaT2[p2][:, cc, u * 128:(u + 1) * 128] for cc in range(4)], [0, 1, 2, 3],
                             [("yaT", p2, cc) for cc in range(4)], (4, 5))

        for step in range(6):
            if step < 4:
                st_a(step)
            if 0 <= step - 1 < 4:
                st_b(step - 1)
            if step - 2 >= 0:
                st_c(step - 2)
        P.barrier()
        if stop == "conv":
            return
        for hh in range(2):
            moba_half(hh, W)
            P.barrier()
            if stop is not None:
                return

    def moba_half(hh, W):
        cv = Carver()
        QT = cv.get([128, 4, S], BF16)
        KT = cv.get([128, 4, S], BF16)
        Vt = cv.get([128, NT, 4, 66], BF16)
        szy = cv.get([128, NT, 256], BF16)
        ptb = [cv.get([128, 512], BF16) for _ in range(3)]
        gq = cv.get([128, 1], F32)
        gk = cv.get([128, 1], F32)
        kmf = cv.get([128, 4, 8], F32)
        kmb = cv.get([128, 4, 8], BF16)
        gs = cv.get([128, 4, 8], F32)
        cmp_ = cv.get([128, 4, 8, 8], F32)
        rank = cv.get([128, 4, 8], F32)
        nm = cv.get([128, 4, 32], BF16)
        rden = [cv.get([128, 1], F32) for _ in range(8)]
        yT = [cv.get([128, 2, 128], BF16) for _ in range(2)]
        tmpf = [cv.get([128, 512], F32) for _ in range(2)]
        s8 = [cv.get([128, 8], F32) for _ in range(3)]
        qa = [cv.get([128, 4, 128], BF16) for _ in range(3)]
        ka = [cv.get([128, 4, 128], BF16) for _ in range(3)]
        c_q = 1536 + hh * 256
        c_k = 2048 + hh * 256
        c_v = 2560 + hh * 256
        c_z = 3072 + hh * 256
        s = load_w(W, [(c_q, 256), (c_k, 256)])
        s2 = load_w(W, [(c_v, 256), (c_z, 256)])
        load_wo(0, 4 + 2 * hh, 2)
        load_gain_col(gq, bq_d, 1.0)
        load_gain_col(gk, bk_d, 8.0)
        for b in range(3):
            MSET(qa[b][:], 0.0, [("qa", b)])
            MSET(ka[b][:], 0.0, [("ka", b)])
        MSET(Vt[:], 1.0, ["Vt"])
        MSET(nm[:], 0.0, ["nm"])

        def m0(i):
            r = i % 3
            CPY(qa[r][:, :, 96:100], qal[:, i, hh * 16:hh * 16 + 16].rearrange("p (h c) -> p h c", c=4), ["const"], [("qa", r)])
            CPY(ka[r][:, :, 64:100], kalm[:, i, :].unsqueeze(1).to_broadcast([128, 4, 36]), ["const"], [("ka", r)])
            for k in range(8):
                mm(pf[r][:], hT[:, k, i * 128:(i + 1) * 128], wbuf[s][:, k, :], k == 0, k == 7, [("hT", i), ("w", s)], [("pf", r)])

        def m1(i):
            r = i % 3
            rstd_a(pf[r], ("pf", r), 512, tmpf[i % 2], ("tf", i % 2), s8[r], ("s8", r))

        def m2(i):
            r = i % 3
            rstd_b(512, s8[r], ("s8", r))
            TT(qa[r][:, :, 0:64], pf[r][:, 0:256].rearrange("p (h d) -> p h d", d=64), s8[r][:, 0:4].unsqueeze(2).to_broadcast([128, 4, 64]),
               ALU.mult, [("pf", r), ("s8", r)], [("qa", r)])
            TT(ka[r][:, :, 0:64], pf[r][:, 256:512].rearrange("p (h d) -> p h d", d=64), s8[r][:, 4:8].unsqueeze(2).to_broadcast([128, 4, 64]),
               ALU.mult, [("pf", r), ("s8", r)], [("ka", r)])

        def m3(i):
            r = i % 3
            b2 = i % 2
            pt = pb[b2]
            for h in range(4):
                tr(pt[:, h * 128:(h + 1) * 128], qa[r][:, h, :], [("qa", r)], [("pb", b2)])
            for h in range(4):
                tr(pt[:, (4 + h) * 128:(5 + h) * 128], ka[r][:, h, :], [("ka", r)], [("pb", b2)])
            act(QT[:, :, i * 128:(i + 1) * 128], pt[:, 0:512].rearrange("p (h t) -> p h t", h=4), AF.Copy, [("pb", b2), "gq"], [("QT", i)],
                scale=gq[:, 0:1])
            act(KT[:, :, i * 128:(i + 1) * 128], pt[:, 512:1024].rearrange("p (h t) -> p h t", h=4), AF.Copy, [("pb", b2), "gq"], [("KT", i)],
                scale=gk[:, 0:1])

        pipe4(NT, m0, m1, m2, m3)
        if hh == 1 and 1 in layers:
            prefetch_w1([("qa", r) for r in range(3)] + [("ka", r) for r in range(3)] + [("s8", r) for r in range(3)] + [("tf", 0), ("tf", 1)])
        if stop == "m1":
            return
        for i in range(NT):
            b2 = 2 + i % 2
            pp = pf[b2]
            for k in range(8):
                mm(pp[:], hT[:, k, i * 128:(i + 1) * 128], wbuf[s2][:, k, :], k == 0, k == 7, [("hT", i), ("w", s2)], [("pf", b2)])
            act(Vt[:, i, :, 0:64], pp[:, 0:256].rearrange("p (h d) -> p h d", d=64), AF.Copy, [("pf", b2), "Vt"], [("V", i)])
            act(szy[:, i, :], pp[:, 256:512], AF.Silu, [("pf", b2)], [("szy", i)])
        if stop == "m3":
            return
        for h in range(4):
            RED(kmf[:, h, :], KT[:, h, :].rearrange("p (n t) -> p n t", t=256), ALU.add, [("KT", i) for i in range(NT)], ["kmf"])
        TS(kmb[:], kmf[:], 1.0 / 256, None, ALU.mult, None, ["kmf"], ["kmb"])
        if stop == "mk":
            return
        for i in range(8, NT):
            npast = i // 2
            b2 = 4 + i % 2
            pg = pf[b2]
            for h in range(4):
                mm(pg[:, h * 8:h * 8 + 8], QT[0:64, h, i * 128:(i + 1) * 128], kmb[0:64, h, :], True, True, [("QT", i), "kmb"], [("pf", b2)])
            CPY(gs[:], pg[:, 0:32].rearrange("p (h n) -> p h n", n=8), [("pf", b2)], ["gs"])
            TT(cmp_[:, :, 0:npast, 0:npast], gs[:, :, 0:npast].unsqueeze(2).to_broadcast([128, 4, npast, npast]),
               gs[:, :, 0:npast].unsqueeze(3).to_broadcast([128, 4, npast, npast]), ALU.is_gt, ["gs"], ["cmp"])
            RED(rank[:, :, 0:npast], cmp_[:, :, 0:npast, 0:npast], ALU.add, ["cmp"], ["rank"])
            TS(nm[:, :, 0:npast], rank[:, :, 0:npast], 2.5, NEGM, ALU.is_ge, ALU.mult, ["rank"], ["nm"])
            pt = pb[i % 2]
            tr(pt[:, 0:128], nm[:].rearrange("p h n -> p (h n)"), ["nm"], [("pb", i % 2)])
            for h in range(4):
                act(QT[64:96, h, i * 128:(i + 1) * 128], pt[h * 32:(h + 1) * 32, 0:128], AF.Copy, [("pb", i % 2)], [("QT", i)])

        if stop == "m2":
            return

        def plan(qc):
            items = []
            for kt in range(4 * qc + 4):
                if kt < 4 * qc:
                    items.append((kt, 0, 3, {}))
                else:
                    m = kt - 4 * qc
                    items.append((kt, m, 3, {m: tri[:]}))
            return items

        for h in range(4):
            def finish(i, Ob, okey, h=h):
                r = (i + 4 * h) % 8
                rd = rden[r]
                RCP(rd[:], Ob[:, 64:65], [okey], [("rden", r)])
                dst = szy[:, i, h * 64:(h + 1) * 64]
                STT(dst, Ob[:, 0:64], rd[:], dst, ALU.mult, ALU.mult, [okey, ("rden", r), ("szy", i)], [("szy", i)])

            attention(QT[:, h, :], lambda kt, h=h: KT[:, h, kt * 128:(kt + 1) * 128], 128,
                      lambda kt, h=h: Vt[:, kt, h, 0:65], 65, plan, finish,
                      lambda qc: [("QT", 4 * qc + u) for u in range(4)], lambda kt: [("KT", kt)], lambda kt: [("V", kt)],
                      ptb, "m")
        if stop == "ma":
            return
        outproj_half(szy, yT)

    def rstd_a(pp, pkey, ncol, tf, tfkey, s8i, s8key):
        nh = ncol // 64
        act(tf[:, 0:ncol], pp[:, 0:ncol], AF.Square, [pkey], [tfkey])
        RED(s8i[:, 0:nh], tf[:, 0:ncol].rearrange("p (h d) -> p h d", d=64), ALU.add, [tfkey], [s8key])
        TS(s8i[:, 0:nh], s8i[:, 0:nh], 64.0 * EPS, None, ALU.add, None, [s8key], [s8key])
        act(s8i[:, 0:nh], s8i[:, 0:nh], AF.Sqrt, [s8key], [s8key])

    def rstd_b(ncol, s8i, s8key):
        nh = ncol // 64
        RCP(s8i[:, 0:nh], s8i[:, 0:nh], [s8key], [s8key])

    def head_rstd(pp, pkey, ncol, tf, tfkey, s8i, s8key):
        rstd_a(pp, pkey, ncol, tf, tfkey, s8i, s8key)
        rstd_b(ncol, s8i, s8key)

    def pipe4(n, s0, s1, s2, s3):
        for step in range(n + 2):
            if step < n:
                s0(step)
                s1(step)
            if 1 <= step <= n:
                s2(step - 1)
            if step >= 2:
                s3(step - 2)

    def load_gain_col(dst, gd, mult):
        MSET(dst[:], 1.0, ["gq"])
        dma("sp", dst[0:64, :], gd.rearrange("o d -> d o"), "c", writes=["gq"], slow=True)
        if mult != 1.0:
            TS(dst[0:64, :], dst[0:64, :], mult, None, ALU.mult, None, ["gq"], ["gq"])

    def outproj_half(szy, yT, store=False):
        def oa(i):
            pt = pb[i % 2]
            for c in range(2):
                tr(pt[:, c * 128:(c + 1) * 128], szy[:, i, c * 128:(c + 1) * 128], [("szy", i)], [("pb", i % 2)])
            y = yT[i % 2]
            act(y[:], pt[:, 0:256].rearrange("p (c t) -> p c t", c=2), AF.Copy, [("pb", i % 2)], [("yT", i % 2)])

        def ob(i):
            y = yT[i % 2]
            outproj_tile(i, [y[:, 0, :], y[:, 1, :]], [0, 1], [("yT", i % 2)], (4, 5))
            if store:
                dma("sp", yv[:, i, :], x_sb[:, i, :], "out", reads=[("x", i)])

        lagged(NT, oa, ob)

    def plan_causal(qc):
        items = []
        for kt in range(4 * qc + 4):
            if kt < 4 * qc:
                items.append((kt, 0, 3, {}))
            else:
                m = kt - 4 * qc
                items.append((kt, m, 3, {m: tri[:]}))
        return items

    def plan_band(wt):
        def plan(qc):
            items = []
            for kt in range(max(0, 4 * qc - wt), 4 * qc + 4):
                jlo = max(0, kt - 4 * qc)
                jhi = min(3, kt + wt - 4 * qc)
                if jlo > jhi:
                    continue
                masks = {}
                if 0 <= kt - 4 * qc <= 3:
                    masks[kt - 4 * qc] = tri[:]
                if 0 <= kt + wt - 4 * qc <= 3:
                    masks[kt + wt - 4 * qc] = upm[:]
                items.append((kt, jlo, jhi, masks))
            return items
        return plan

    W1_OFF = 64 * 1024

    def prefetch_w1(war_keys=()):
        cvp = Carver(base=W1_OFF)
        wA = cvp.get([128, 32, 256], BF16)
        for kv, wd in enumerate((ckw1_d, cvw1_d)):
            wv = wd.rearrange("(l d) j -> d l j", d=64)
            for lq in range(4):
                dma("pool", wA[kv * 64:(kv + 1) * 64, lq * 8:(lq + 1) * 8, :], wv[:, lq * 8:(lq + 1) * 8, :], "w1", writes=["w1A"] + list(war_keys))
        return wA

    def layer1():
        W = owin_d
        wA = Carver(base=W1_OFF).get([128, 32, 256], BF16)
        if 0 not in layers:
            prefetch_w1()
        cvc = Carver(base=16 * 1024)
        wB = cvc.get([128, 32, 256], BF16)
        dma("sp", wB[64:128, :, :], wA[0:64, :, :], "c", writes=["w1B"])
        dma("sp", wB[0:64, :, :], wA[64:128, :, :], "c", writes=["w1B"])
        w1 = [[wA, wB], [wB, wA]]
        B = compress_loads(W, cvc)
        norm_phase(1)
        compress_stage(W, B, w1)
        P.barrier()
        if stop == "cmp":
            return
        for g in range(2):
            nsa_half(g, W)
            P.barrier()
            if stop == "nsa0":
                return
        if stop == "nsa":
            return
        for g in range(2):
            swa_half(g, W)
            P.barrier()

    def compress_loads(W, cv):
        s = load_w(W, [(512, 128), (640, 128)])
        B = {}
        B["s"] = s
        B["KVD"] = [cv.get([128, 16, 128], BF16) for _ in range(2)]
        B["w2"] = [cv.get([128, 2, 64], BF16) for _ in range(2)]
        B["posn"] = cv.get([32, 2, 64], F32)
        B["posT"] = cv.get([64, 2, 32], BF16)
        B["stgb"] = cv.get([4, 128], F32)
        B["b1sb"] = cv.get([128, 4], F32)
        B["biasj"] = cv.get([128, 4], F32)
        B["b2bc"] = cv.get([128, 2, 64], F32)
        B["gcm"] = cv.get([128, 64], F32)
        B["kalc"] = cv.get([128, 36], BF16)
        B["hid"] = [cv.get([128, 2, 128], BF16) for _ in range(4)]
        B["kcf"] = cv.get([128, 64], F32)
        B["junk2"] = cv.get([128, 64], F32)
        B["ssc"] = cv.get([128, 1], F32)
        B["kaug"] = cv.get([128, 128], BF16)
        for kv, wd in enumerate((ckw2_d, cvw2_d)):
            dma("pool", B["w2"][kv][:], wd.rearrange("(c p) d -> p c d", p=128), "w2", writes=["w2"])
        dma("sp", B["posn"][:, 0, :], cposk_d, "c", writes=["posn"])
        dma("sp", B["posn"][:, 1, :], cposv_d, "c", writes=["posn"])
        dma("sp", B["stgb"][0:2, :], ckb1_d.rearrange("(a c) -> a c", c=128), "c", writes=["stgb"])
        dma("sp", B["stgb"][2:4, :], cvb1_d.rearrange("(a c) -> a c", c=128), "c", writes=["stgb"])
        dma("sp", B["b2bc"][:, 0, :], ckb2_d.partition_broadcast(128), "c", writes=["b2bc"])
        dma("sp", B["b2bc"][:, 1, :], cvb2_d.partition_broadcast(128), "c", writes=["b2bc"])
        dma("sp", B["gcm"][:], ckc_d.partition_broadcast(128), "c", writes=["gcm"])
        dma("pool", B["kalc"][:], kalc_d, "c", writes=["kalc"])
        for g in range(2):
            dma("pool", Vca[:, g, 65:97], ovl_d, "c", writes=[("Vca", g)])
            MSET(Vca[:, g, 64:65], 1.0, [("Vca", g)])
        MSET(B["kaug"][:], 0.0, ["kaug"])
        return B

    def compress_stage(W, B, w1):
        s = B["s"]
        KVD, w2, posn, posT, stgb, b1sb, biasj = B["KVD"], B["w2"], B["posn"], B["posT"], B["stgb"], B["b1sb"], B["biasj"]
        b2bc, gcm, kalc, hid, kcf, junk2, ssc, kaug = B["b2bc"], B["gcm"], B["kalc"], B["hid"], B["kcf"], B["junk2"], B["ssc"], B["kaug"]
        for kv in range(2):
            tr(pf[0][0:64, kv * 32:(kv + 1) * 32], posn[:, kv, :], ["posn"], [("pf", 0)], idn=identf[0:32, 0:32])
        tr(pf[0][:, 64:68], stgb[:], ["stgb"], [("pf", 0)], idn=identf[0:4, 0:4])
        CPY(posT[:], pf[0][0:64, 0:64].rearrange("p (k l) -> p k l", l=32), [("pf", 0)], ["posT"])
        CPY(b1sb[:], pf[0][:, 64:68], [("pf", 0)], ["b1sb"])
        for kv in range(2):
            for jc in range(2):
                col = kv * 2 + jc
                for l in range(32):
                    mm(pf[1][:, col:col + 1], w1[kv][0][0:64, l, jc * 128:(jc + 1) * 128], posT[0:64, kv, l:l + 1], l == 0, l == 31,
                       ["w1B", "posT"], [("pf", 1)])
        TT(biasj[:], pf[1][:, 0:4], b1sb[:], ALU.add, [("pf", 1), "b1sb"], ["biasj"])
        for tq in range(4):
            for which in range(2):
                pp = pf[2 + which]
                for k in range(8):
                    mm(pp[:], wbuf[s][:, k, which * 128:(which + 1) * 128], hT[:, k, tq * 512:(tq + 1) * 512], k == 0, k == 7,
                       [("w", s)] + [("hT", 4 * tq + u) for u in range(4)], [("pf", 2 + which)])
                act(KVD[which][:, :, tq * 32:(tq + 1) * 32].rearrange("p r m -> p m r"), pp[:].rearrange("p (m r) -> p m r", r=16),
                    AF.Copy, [("pf", 2 + which)], [("KVT", which)])
        n = 0
        for kv in range(2):
            for g in range(2):
                hb_ = hid[kv * 2 + g]
                for jc in range(2):
                    pp = pf[n % 2]
                    for l in range(32):
                        mm(pp[:, 0:127], w1[kv][g][g * 64:(g + 1) * 64, l, jc * 128:(jc + 1) * 128],
                           KVD[kv][g * 64:(g + 1) * 64, l % 16, (l // 16):(l // 16) + 127], l == 0, l == 31,
                           ["w1B", ("KVT", kv)], [("pf", n % 2)])
                    act(hb_[:, jc, 0:127], pp[:, 0:127], AF.Silu, [("pf", n % 2), "biasj"], [("hid", kv * 2 + g)],
                        bias=biasj[:, kv * 2 + jc:kv * 2 + jc + 1], scale=1.0)
                    n += 1
                po = pf[2 + g]
                for jc in range(2):
                    mm(po[0:127, 0:64], hb_[:, jc, 0:127], w2[kv][:, jc, :], jc == 0, jc == 1, [("hid", kv * 2 + g), "w2"], [("pf", 2 + g)])
                if kv == 0:
                    TT(kcf[0:127, :], po[0:127, 0:64], b2bc[0:127, 0, :], ALU.add, [("pf", 2 + g), "b2bc"], ["kcf"])
                    act(junk2[0:127, :], kcf[0:127, :], AF.Square, ["kcf"], ["junk2", "ssc"], accum_out=ssc[0:127, :])
                    TS(ssc[0:127, :], ssc[0:127, :], 1.0 / 64, EPS, ALU.mult, ALU.add, ["ssc"], ["ssc"])
                    act(ssc[0:127, :], ssc[0:127, :], AF.Sqrt, ["ssc"], ["ssc"])
                    RCP(ssc[0:127, :], ssc[0:127, :], ["ssc"], ["ssc"])
                    STT(kaug[0:127, 0:64], kcf[0:127, :], ssc[0:127, :], gcm[0:127, :], ALU.mult, ALU.mult, ["kcf", "ssc", "gcm"], ["kaug"])
                    CPY(kaug[:, 64:100], kalc[:], ["kalc"], ["kaug"])
                    tr(pb[g][:, 0:128], kaug[:], ["kaug"], [("pb", g)])
                    act(KTc[:, g, :], pb[g][:, 0:128], AF.Copy, [("pb", g)], [("KTc", g)])
                else:
                    TT(Vca[0:127, g, 0:64], po[0:127, 0:64], b2bc[0:127, 1, :], ALU.add, [("pf", 2 + g), "b2bc"], [("Vca", g)])

    def nsa_half(g, W):
        cv = Carver()
        QT = cv.get([128, 4, S], BF16)
        KTs = cv.get([128, S], BF16)
        KTw = cv.get([128, S], BF16)
        Vs = cv.get([128, NT, 66], BF16)
        Vw = cv.get([128, NT, 66], BF16)
        szy = cv.get([128, NT, 256], BF16)
        gates = cv.get([128, NT, 12], F32)
        oacc = cv.get([128, NT, 256], F32)
        imp = cv.get([128, 8, 32], F32)
        impc = cv.get([128, 8, 32], F32)
        cmn = cv.get([128, S], BF16)
        qa = [cv.get([128, 4, 128], BF16) for _ in range(3)]
        ksa = [cv.get([128, 128], BF16) for _ in range(3)]
        kwa = [cv.get([128, 128], BF16) for _ in range(3)]
        ptb = [cv.get([128, 512], BF16) for _ in range(3)]
        tmpf = [cv.get([128, 512], F32) for _ in range(2)]
        cmpb = cv.get([128, 32, 32], F32)
        rank = cv.get([128, 32], F32)
        nmt = cv.get([128, 128], BF16)
        s8 = [cv.get([128, 8], F32) for _ in range(3)]
        gq = cv.get([128, 1], F32)
        gks = cv.get([128, 1], F32)
        gkw = cv.get([128, 1], F32)
        rden = [cv.get([128, 1], F32) for _ in range(8)]
        yT = [cv.get([128, 2, 128], BF16) for _ in range(2)]
        s = load_w(W, [(g * 256, 256), (768 + 64 * g, 64), (1024 + 64 * g, 64), (896 + 64 * g, 64), (1152 + 64 * g, 64)])
        s2 = load_w(W, [(1304 + g * 256, 256), (1280 + 4 * g, 4), (1288 + 4 * g, 4), (1296 + 4 * g, 4)])
        load_wo(1, 2 * g, 2)
        load_gain_col(gq, cq_d, 1.0)
        load_gain_col(gks, cks_d, 8.0)
        load_gain_col(gkw, ckw_d, 8.0)
        dma("sp", impc[:], impc_d[:, 8:16, :], "c", writes=["impc"])
        dma("pool", cmn[:], cmn_d, "c", writes=["addmask"])
        for b in range(3):
            MSET(qa[b][:], 0.0, [("qa", b)])
            MSET(ksa[b][:], 0.0, [("ksa", b)])
            MSET(kwa[b][:], 0.0, [("kwa", b)])
        MSET(Vs[:], 1.0, ["Vs"])
        MSET(Vw[:], 1.0, ["Vw"])
        MSET(nmt[:], 0.0, ["nmt"])
        def pa(i):
            b2 = i % 2
            pp = pf[b2]
            qai, ksi, kwi = qa[b2], ksa[b2], kwa[b2]
            CPY(qai[:, :, 96:100], qal[:, i, g * 16:g * 16 + 16].rearrange("p (h c) -> p h c", c=4), ["const"], [("qa", b2)])
            CPY(ksi[:, 64:100], kals[:, i, :], ["const"], [("ksa", b2)])
            CPY(kwi[:, 64:100], kalp[:, i, :], ["const"], [("kwa", b2)])
            for k in range(8):
                mm(pp[:], hT[:, k, i * 128:(i + 1) * 128], wbuf[s][:, k, :], k == 0, k == 7, [("hT", i), ("w", s)], [("pf", b2)])
            tf = tmpf[b2]
            head_rstd(pp, ("pf", b2), 384, tf, ("tf", b2), s8[b2], ("s8", b2))
            TT(qai[:, :, 0:64], pp[:, 0:256].rearrange("p (h d) -> p h d", d=64), s8[b2][:, 0:4].unsqueeze(2).to_broadcast([128, 4, 64]),
               ALU.mult, [("pf", b2), ("s8", b2)], [("qa", b2)])
            TS(ksi[:, 0:64], pp[:, 256:320], s8[b2][:, 4:5], None, ALU.mult, None, [("pf", b2), ("s8", b2)], [("ksa", b2)])
            TS(kwi[:, 0:64], pp[:, 320:384], s8[b2][:, 5:6], None, ALU.mult, None, [("pf", b2), ("s8", b2)], [("kwa", b2)])
            act(Vs[:, i, 0:64], pp[:, 384:448], AF.Copy, [("pf", b2), "Vs"], [("Vs", i)])
            act(Vw[:, i, 0:64], pp[:, 448:512], AF.Copy, [("pf", b2), "Vw"], [("Vw", i)])

        def pbk(i):
            b2 = i % 2
            qai, ksi, kwi = qa[b2], ksa[b2], kwa[b2]
            pt = pb[b2]
            for h in range(4):
                tr(pt[:, h * 128:(h + 1) * 128], qai[:, h, :], [("qa", b2)], [("pb", b2)])
            tr(pt[:, 512:640], ksi[:], [("ksa", b2)], [("pb", b2)])
            tr(pt[:, 640:768], kwi[:], [("kwa", b2)], [("pb", b2)])
            act(QT[:, :, i * 128:(i + 1) * 128], pt[:, 0:512].rearrange("p (h t) -> p h t", h=4), AF.Copy, [("pb", b2), "gq"], [("QT", i)],
                scale=gq[:, 0:1])
            act(KTs[:, i * 128:(i + 1) * 128], pt[:, 512:640], AF.Copy, [("pb", b2), "gq"], [("KTs", i)], scale=gks[:, 0:1])
            act(KTw[:, i * 128:(i + 1) * 128], pt[:, 640:768], AF.Copy, [("pb", b2), "gq"], [("KTw", i)], scale=gkw[:, 0:1])

        lagged(NT, pa, pbk)
        for i in range(NT):
            b2 = 2 + i % 2
            pp = pf[b2]
            for k in range(8):
                mm(pp[:, 0:256], hT[:, k, i * 128:(i + 1) * 128], wbuf[s2][:, k, 0:256], k == 0, k == 7, [("hT", i), ("w", s2)], [("pf", b2)])
            act(szy[:, i, :], pp[:, 0:256], AF.Silu, [("pf", b2)], [("szy", i)])
        for i in range(NT):
            b2 = 2 + i % 2
            pp = pf[b2]
            for k in range(8):
                mm(pp[:, 0:12], hT[:, k, i * 128:(i + 1) * 128], wbuf[s2][:, k, 256:268], k == 0, k == 7, [("hT", i), ("w", s2)], [("pf", b2)])
            act(gates[:, i, :], pp[:, 0:12], AF.Sigmoid, [("pf", b2)], [("gates", i)])
        if stop == "nsaproj":
            return
        qk = lambda qc: [("QT", 4 * qc + u) for u in range(4)]
        for r in range(4):
            def fin_cmp(i, Ob, okey, r=r):
                ri = (i + 4 * r) % 8
                rd = rden[ri]
                TS(rd[:], Ob[:, 64:65], 1e-30, None, ALU.max, None, [okey], [("rden", ri)])
                RCP(rd[:], rd[:], [("rden", ri)], [("rden", ri)])
                TS(oacc[:, i, r * 64:(r + 1) * 64], Ob[:, 0:64], rd[:], gates[:, i, r:r + 1], ALU.mult, ALU.mult,
                   [okey, ("rden", ri), ("gates", i)], [("oacc", i)])
                if i >= 8:
                    if r == 0:
                        TS(imp[:, i - 8, :], Ob[:, 65:97], rd[:], None, ALU.mult, None, [okey, ("rden", ri)], [("imp", i)])
                    else:
                        STT(imp[:, i - 8, :], Ob[:, 65:97], rd[:], imp[:, i - 8, :], ALU.mult, ALU.add, [okey, ("rden", ri), ("imp", i)], [("imp", i)])

            attention(QT[:, r, :], lambda kt: KTc[:, g, 0:127], 127, lambda kt: Vca[0:127, g, 0:97], 97,
                      lambda qc: [(0, 0, 3, {})], fin_cmp, qk, lambda kt: [("KTc", g)], lambda kt: [("Vca", g)], ptb, "n", addmask=cmn)
        if stop == "nsacmp":
            return
        for i in range(8, NT):
            im = imp[:, i - 8, :]
            TT(im, im, impc[:, i - 8, :], ALU.add, [("imp", i), "impc"], [("imp", i)])
            TT(cmpb[:], im.unsqueeze(1).to_broadcast([128, 32, 32]), im.unsqueeze(2).to_broadcast([128, 32, 32]), ALU.is_gt,
               [("imp", i)], ["cmpb"])
            RED(rank[:], cmpb[:], ALU.add, ["cmpb"], ["rank"])
            TS(nmt[:, 0:32], rank[:], 15.5, NEGM, ALU.is_ge, ALU.mult, ["rank"], ["nmt"])
            pt = pb[i % 2]
            tr(pt[:, 0:128], nmt[:], ["nmt"], [("pb", i % 2)])
            for r in range(4):
                act(QT[64:96, r, i * 128:(i + 1) * 128], pt[0:32, 0:128], AF.Copy, [("pb", i % 2)], [("QT", i)])
        for (KTx, Vx, kname, vname, plan, gcol) in ((KTs, Vs, "KTs", "Vs", plan_causal, 4), (KTw, Vw, "KTw", "Vw", plan_band(4), 8)):
            for r in range(4):
                def fin_add(i, Ob, okey, r=r, gcol=gcol):
                    ri = (i + 4 * r) % 8
                    rd = rden[ri]
                    RCP(rd[:], Ob[:, 64:65], [okey], [("rden", ri)])
                    TT(rd[:], rd[:], gates[:, i, gcol + r:gcol + r + 1], ALU.mult, [("rden", ri), ("gates", i)], [("rden", ri)])
                    dst = oacc[:, i, r * 64:(r + 1) * 64]
                    STT(dst, Ob[:, 0:64], rd[:], dst, ALU.mult, ALU.add, [okey, ("rden", ri), ("oacc", i)], [("oacc", i)])

                attention(QT[:, r, :], lambda kt, KTx=KTx: KTx[:, kt * 128:(kt + 1) * 128], 128,
                          lambda kt, Vx=Vx: Vx[:, kt, 0:65], 65, plan, fin_add, qk,
                          lambda kt, kname=kname: [(kname, kt)], lambda kt, vname=vname: [(vname, kt)], ptb, "n")
        for i in range(NT):
            TT(szy[:, i, :], oacc[:, i, :], szy[:, i, :], ALU.mult, [("oacc", i), ("szy", i)], [("szy", i)])
        outproj_half(szy, yT)

    def swa_half(g, W):
        cv = Carver()
        QT = cv.get([128, 4, S], BF16)
        KT = cv.get([128, S], BF16)
        Vt = cv.get([128, NT, 66], BF16)
        szy = cv.get([128, NT, 256], BF16)
        qa = [cv.get([128, 4, 128], BF16) for _ in range(3)]
        ka = [cv.get([128, 128], BF16) for _ in range(3)]
        ptb = [cv.get([128, 512], BF16) for _ in range(3)]
        tmpf = [cv.get([128, 512], F32) for _ in range(2)]
        s8 = [cv.get([128, 8], F32) for _ in range(3)]
        gq = cv.get([128, 1], F32)
        gk = cv.get([128, 1], F32)
        esink = cv.get([128, 8], F32)
        rden = [cv.get([128, 1], F32) for _ in range(8)]
        yT = [cv.get([128, 2, 128], BF16) for _ in range(2)]
        load_gain_col(gq, dq_d, 1.0)
        load_gain_col(gk, dk_d, 8.0)
        dma("sp", esink[:], dsink_d.partition_broadcast(128), "c", writes=["esink"])
        act(esink[:], esink[:], AF.Exp, ["esink"], ["esink"])
        for b in range(3):
            MSET(qa[b][:], 0.0, [("qa", b)])
            MSET(ka[b][:], 0.0, [("ka", b)])
        MSET(Vt[:], 1.0, ["Vt"])
        s = load_w(W, [(1816 + g * 256, 256), (2328 + 64 * g, 64), (2456 + 64 * g, 64)])
        s2 = load_w(W, [(2584 + g * 256, 256)])
        load_wo(1, 4 + 2 * g, 2)
        def pa(i):
            b2 = i % 2
            pp = pf[b2]
            qai, kai = qa[b2], ka[b2]
            CPY(qai[:, :, 96:100], qal[:, i, g * 16:g * 16 + 16].rearrange("p (h c) -> p h c", c=4), ["const"], [("qa", b2)])
            CPY(kai[:, 64:100], kalp[:, i, :], ["const"], [("ka", b2)])
            for k in range(8):
                mm(pp[:, 0:384], hT[:, k, i * 128:(i + 1) * 128], wbuf[s][:, k, 0:384], k == 0, k == 7, [("hT", i), ("w", s)], [("pf", b2)])
            tf = tmpf[b2]
            head_rstd(pp, ("pf", b2), 320, tf, ("tf", b2), s8[b2], ("s8", b2))
            TT(qai[:, :, 0:64], pp[:, 0:256].rearrange("p (h d) -> p h d", d=64), s8[b2][:, 0:4].unsqueeze(2).to_broadcast([128, 4, 64]),
               ALU.mult, [("pf", b2), ("s8", b2)], [("qa", b2)])
            TS(kai[:, 0:64], pp[:, 256:320], s8[b2][:, 4:5], None, ALU.mult, None, [("pf", b2), ("s8", b2)], [("ka", b2)])
            act(Vt[:, i, 0:64], pp[:, 320:384], AF.Copy, [("pf", b2), "Vt"], [("V", i)])

        def pbk(i):
            b2 = i % 2
            qai, kai = qa[b2], ka[b2]
            pt = pb[b2]
            for h in range(4):
                tr(pt[:, h * 128:(h + 1) * 128], qai[:, h, :], [("qa", b2)], [("pb", b2)])
            tr(pt[:, 512:640], kai[:], [("ka", b2)], [("pb", b2)])
            act(QT[:, :, i * 128:(i + 1) * 128], pt[:, 0:512].rearrange("p (h t) -> p h t", h=4), AF.Copy, [("pb", b2), "gq"], [("QT", i)],
                scale=gq[:, 0:1])
            act(KT[:, i * 128:(i + 1) * 128], pt[:, 512:640], AF.Copy, [("pb", b2), "gq"], [("KT", i)], scale=gk[:, 0:1])

        lagged(NT, pa, pbk)
        for i in range(NT):
            b2 = 2 + i % 2
            pp = pf[b2]
            for k in range(8):
                mm(pp[:, 0:256], hT[:, k, i * 128:(i + 1) * 128], wbuf[s2][:, k, 0:256], k == 0, k == 7, [("hT", i), ("w", s2)], [("pf", b2)])
            act(szy[:, i, :], pp[:, 0:256], AF.Silu, [("pf", b2)], [("szy", i)])
        for r in range(4):
            def fin(i, Ob, okey, r=r):
                ri = (i + 4 * r) % 8
                rd = rden[ri]
                TS(rd[:], Ob[:, 64:65], esink[:, 4 * g + r:4 * g + r + 1], None, ALU.add, None, [okey, "esink"], [("rden", ri)])
                RCP(rd[:], rd[:], [("rden", ri)], [("rden", ri)])
                dst = szy[:, i, r * 64:(r + 1) * 64]
                STT(dst, Ob[:, 0:64], rd[:], dst, ALU.mult, ALU.mult, [okey, ("rden", ri), ("szy", i)], [("szy", i)])

            attention(QT[:, r, :], lambda kt: KT[:, kt * 128:(kt + 1) * 128], 128, lambda kt: Vt[:, kt, 0:65], 65,
                      plan_band(1), fin, lambda qc: [("QT", 4 * qc + u) for u in range(4)], lambda kt: [("KT", kt)],
                      lambda kt: [("V", kt)], ptb, "d")
        outproj_half(szy, yT, store=(g == 1 and stop is None))

    if 0 in layers:
        layer0()
    if 1 in layers:
        layer1()

    if not (1 in layers and stop is None):
        for i in range(NT):
            dma("sp", yv[:, i, :], x_sb[:, i, :], "out", reads=[("x", i)])
    P.finish("sp", list(P.chan_count.keys()))
    P.emit()
    es.close()
    return nc


_CACHE = {}


def kernel(**inputs):
    consts = _consts()
    shared = {}
    shared["norm_g"] = np.ascontiguousarray(inputs["norm_g"], dtype=np.float32)
    shared["w_out"] = np.ascontiguousarray(inputs["w_out"], dtype=np.float32)
    shared["e_w_in"] = np.ascontiguousarray(inputs["e_w_in"][0], dtype=np.float32)
    shared["a_conv_w"] = np.ascontiguousarray(inputs["a_conv_w"][0], dtype=np.float32)
    shared["a_conv_b"] = np.ascontiguousarray(inputs["a_conv_b"][0], dtype=np.float32)
    shared["a_ln_g"] = np.ascontiguousarray(inputs["a_ln_g"][0], dtype=np.float32)
    shared["a_ln_b"] = np.ascontiguousarray(inputs["a_ln_b"][0], dtype=np.float32)
    shared["b_qnorm_g"] = np.ascontiguousarray(inputs["b_qnorm_g"], dtype=np.float32)
    shared["b_knorm_g"] = np.ascontiguousarray(inputs["b_knorm_g"], dtype=np.float32)
    for k in ("ident", "tri", "up", "qal", "kal_moba", "kal_slc", "kal_plain", "kal_cmp", "cmaskneg", "ovl", "impc"):
        shared[k] = consts[k]
    shared["o_w_in"] = np.ascontiguousarray(inputs["o_w_in"][0], dtype=np.float32)
    for k in ("c_qnorm_g", "c_knorm_cmp_g", "c_knorm_slc_g", "c_knorm_win_g", "c_k_b2", "c_v_b2", "d_qnorm_g", "d_knorm_g", "d_sinks"):
        shared[k] = np.ascontiguousarray(inputs[k], dtype=np.float32).reshape(1, -1)
    for k in ("c_pos_k", "c_pos_v", "c_k_w1", "c_k_b1", "c_k_w2", "c_v_w1", "c_v_b1", "c_v_w2"):
        shared[k] = np.ascontiguousarray(inputs[k][0], dtype=np.float32)
    x = np.ascontiguousarray(inputs["x"], dtype=np.float32)
    nb = x.shape[0]
    layers = inputs.get("_layers", (0, 1))
    nc = build_program(layers, inputs.get("_stop"))
    in_maps = [dict(shared, x=x[b]) for b in range(nb)]
    res = run_bass_kernel_spmd(nc, in_maps, core_ids=list(range(nb)))
    return np.stack([np.asarray(r["y"], dtype=np.float32) for r in res.results], axis=0)
```

```python
import contextlib
import os
import numpy as np
import ml_dtypes
import concourse.bass as bass
import concourse.mybir as mybir
from concourse.bass_utils import run_bass_kernel_spmd

F32 = mybir.dt.float32
BF16 = mybir.dt.bfloat16
ALU = mybir.AluOpType
AF = mybir.ActivationFunctionType
AX = mybir.AxisListType

S = 2048
D = 1024
NT = 16
NEGM = -30000.0
EPS = 1e-6


class _Op:
    __slots__ = ("eng", "fn", "deps", "chan", "signal", "val", "dmaval", "chanseq")

    def __init__(self, eng, fn, chan):
        self.eng = eng
        self.fn = fn
        self.deps = {}
        self.chan = chan
        self.signal = False
        self.val = 0
        self.dmaval = None


class Prog:
    ENGS = ("pe", "act", "dve", "pool", "sp")

    def __init__(self, nc):
        self.nc = nc
        self.ops = {e: [] for e in self.ENGS}
        self.res = {}
        self.chan_count = {}
        self.chan_last = {}
        self.all_ops = []
        self.bar = {}
        self.final_waits = {}
        self.pool_ctr = 0
        self.sp_ctr = 0
        self.const_keys = []
        self.pres = {}

    PERS = ("w", "wo")

    def _st(self, k):
        if isinstance(k, tuple) and k[0] in self.PERS or k in self.PERS:
            return self.pres
        return self.res

    def op(self, eng, fn, reads=(), writes=(), chan=None, nobar=False):
        if "const" in writes:
            k = ("ck", len(self.const_keys))
            self.const_keys.append(k)
            writes = [k if w == "const" else w for w in writes]
        if "const" in reads:
            reads = [k for r in reads for k in (self.const_keys if r == "const" else (r,))]
        if chan is not None and eng == "pool":
            chan = "pq%d" % (self.pool_ctr % 12)
            self.pool_ctr += 1
        elif chan == "c":
            chan = "pc%d" % (self.sp_ctr % 16)
            self.sp_ctr += 1
        o = _Op(eng, fn, chan)
        deps = o.deps
        if not nobar:
            deps.update(self.bar)
        if chan is not None and (chan.startswith("pq") or chan.startswith("pc")):
            prev = self.chan_last.get(chan)
            if prev is not None:
                deps[id(prev)] = prev
        for k in reads:
            st = self._st(k).get(k)
            if st is not None and st[0] is not None:
                deps[id(st[0])] = st[0]
        for k in writes:
            st = self._st(k).get(k)
            if st is not None:
                if st[0] is not None:
                    deps[id(st[0])] = st[0]
                for r in st[1]:
                    deps[id(r)] = r
        for k in reads:
            res = self._st(k)
            st = res.get(k)
            if st is None:
                st = [None, []]
                res[k] = st
            st[1].append(o)
        for k in writes:
            self._st(k)[k] = [o, []]
        o.dmaval = dict(self.chan_count)
        if chan is not None:
            self.chan_count[chan] = self.chan_count.get(chan, 0) + 1
            o.chanseq = self.chan_count[chan]
            self.chan_last[chan] = o
            o.signal = True
        self.ops[eng].append(o)
        self.all_ops.append(o)
        return o

    def barrier(self):
        bar = {}
        for e in self.ENGS:
            if self.ops[e]:
                o = self.ops[e][-1]
                bar[id(o)] = o
        for ch, o in self.chan_last.items():
            bar[id(o)] = o
        self.bar = bar
        self.res = {}

    def finish(self, eng, chans):
        self.final_waits = {eng: {ch: self.chan_count[ch] for ch in chans}}

    def emit(self):
        nc = self.nc
        for o in self.all_ops:
            for d in o.deps.values():
                if d.chan is None:
                    if d.eng == "pe" and o.eng == "pe":
                        continue
                    d.signal = True
        for e in self.ENGS:
            c = 0
            for o in self.ops[e]:
                if o.chan is None and o.signal:
                    c += 1
                    o.val = c
        import os
        if os.environ.get("KDBG"):
            print("sem counts", {e: max([o.val for o in self.ops[e]] + [0]) for e in self.ENGS}, {e: len(self.ops[e]) for e in self.ENGS},
                  {c: 16 * v for c, v in self.chan_count.items()})
        stack = contextlib.ExitStack()
        sems = {}
        for e in self.ENGS:
            sems[e] = stack.enter_context(nc.semaphore("s_" + e))
        for ch in self.chan_count:
            sems["c_" + ch] = stack.enter_context(nc.semaphore("c_" + ch))
        block = stack.enter_context(nc.Block())
        engobj = {"pe": "tensor", "act": "scalar", "dve": "vector", "pool": "gpsimd", "sp": "sync"}

        def make(e):
            def body(eng):
                waited = {}
                for o in self.ops[e]:
                    need = {}
                    for d in o.deps.values():
                        if d.chan is not None:
                            k = "c_" + d.chan
                            if d.chan.startswith("pq") or d.chan.startswith("pc"):
                                v = 16 * d.chanseq
                            else:
                                v = 16 * o.dmaval[d.chan]
                        else:
                            if d.eng == "pe" and e == "pe":
                                continue
                            k = d.eng
                            v = d.val
                        if v > need.get(k, 0):
                            need[k] = v
                    for k, v in need.items():
                        if waited.get(k, 0) >= v:
                            continue
                        eng.wait_ge(sems[k], v)
                        waited[k] = v
                    ins = o.fn(eng)
                    if o.chan is not None:
                        ins.then_inc(sems["c_" + o.chan], 16)
                    elif o.signal:
                        ins.then_inc(sems[e], 1)
                for ch, c in self.final_waits.get(e, {}).items():
                    eng.wait_ge(sems["c_" + ch], 16 * c)
            return body

        for e in self.ENGS:
            getattr(block, engobj[e])(make(e))
        stack.close()


def _consts():
    c = {}
    c["ident"] = np.eye(128, dtype=np.float32)
    k = np.arange(128)[:, None]
    q = np.arange(128)[None, :]
    c["tri"] = np.where(k <= q, 0.0, NEGM).astype(np.float32)
    c["up"] = np.where(k > q, 0.0, NEGM).astype(np.float32)
    t = np.arange(S)
    b = (t % 16).astype(np.float32)
    a = (t - t % 16).astype(np.float32)
    slopes = np.power(2.0, -8.0 * np.arange(1, 9) / 8).astype(np.float32)
    qal = np.zeros((S, 8, 4), np.float32)
    qal[:, :, 0] = -slopes[None, :] * a[:, None]
    qal[:, :, 1] = -slopes[None, :] * b[:, None]
    qal[:, :, 2] = slopes[None, :]
    qal[:, :, 3] = slopes[None, :]
    c["qal"] = qal.reshape(NT, 128, 32).transpose(1, 0, 2).copy()

    def kal(onehot_block):
        m = np.zeros((S, 36), np.float32)
        if onehot_block:
            m[t, t // onehot_block] = 1.0
        m[:, 32] = 1.0
        m[:, 33] = 1.0
        m[:, 34] = a
        m[:, 35] = b
        return m.reshape(NT, 128, 36).transpose(1, 0, 2).copy()

    c["kal_moba"] = kal(256)
    c["kal_slc"] = kal(64)
    c["kal_plain"] = kal(0)
    cc = np.arange(128)
    cend = 16 * cc + 31
    kc = np.zeros((128, 36), np.float32)
    kc[:, 32] = 1.0
    kc[:, 33] = 1.0
    kc[:, 34] = cend - cend % 16
    kc[:, 35] = cend % 16
    c["kal_cmp"] = kc
    c["cmaskneg"] = np.where(t[None, :] >= cend[:, None], 0.0, NEGM).astype(np.float32)
    start = np.arange(127)[:, None] * 16
    bs = np.arange(32)[None, :] * 64
    ov = np.zeros((128, 32), np.float32)
    ov[:127] = ((start < bs + 64) & (start + 32 > bs)).astype(np.float32)
    c["ovl"] = ov
    blk = np.arange(32)[None, :]
    cur = (t // 64)[:, None]
    forced = (blk == 0) | (blk == cur) | (blk == cur - 1)
    impc = 1e4 * forced.astype(np.float32) - 1e5 * (blk > cur).astype(np.float32)
    c["impc"] = impc.reshape(NT, 128, 32).transpose(1, 0, 2).copy()
    return c


def build_program(layers=(0, 1), stop=None):
    nc = bass.Bass("TRN2", target_bir_lowering=False)
    es = contextlib.ExitStack()
    dram = {}

    def din(name, shape, dt=F32):
        dram[name] = nc.dram_tensor(name, list(shape), dt, kind="ExternalInput").ap()
        return dram[name]

    x_d = din("x", [S, D])
    normg_d = din("norm_g", [2, D])
    wout_d = din("w_out", [2, D, D])
    ewin_d = din("e_w_in", [D, 3584])
    convw_d = din("a_conv_w", [31, 512])
    convb_d = din("a_conv_b", [512])
    lng_d = din("a_ln_g", [512])
    lnb_d = din("a_ln_b", [512])
    bq_d = din("b_qnorm_g", [1, 64])
    bk_d = din("b_knorm_g", [1, 64])
    ident_d = din("ident", [128, 128])
    tri_d = din("tri", [128, 128])
    up_d = din("up", [128, 128])
    qal_d = din("qal", [128, NT, 32])
    kalm_d = din("kal_moba", [128, NT, 36])
    kals_d = din("kal_slc", [128, NT, 36])
    kalp_d = din("kal_plain", [128, NT, 36])
    kalc_d = din("kal_cmp", [128, 36])
    cmn_d = din("cmaskneg", [128, S])
    ovl_d = din("ovl", [128, 32])
    impc_d = din("impc", [128, NT, 32])
    owin_d = din("o_w_in", [D, 3096])
    cq_d = din("c_qnorm_g", [1, 64])
    ckc_d = din("c_knorm_cmp_g", [1, 64])
    cks_d = din("c_knorm_slc_g", [1, 64])
    ckw_d = din("c_knorm_win_g", [1, 64])
    cposk_d = din("c_pos_k", [32, 64])
    cposv_d = din("c_pos_v", [32, 64])
    ckw1_d = din("c_k_w1", [2048, 256])
    ckb1_d = din("c_k_b1", [256])
    ckw2_d = din("c_k_w2", [256, 64])
    ckb2_d = din("c_k_b2", [1, 64])
    cvw1_d = din("c_v_w1", [2048, 256])
    cvb1_d = din("c_v_b1", [256])
    cvw2_d = din("c_v_w2", [256, 64])
    cvb2_d = din("c_v_b2", [1, 64])
    dq_d = din("d_qnorm_g", [1, 64])
    dk_d = din("d_knorm_g", [1, 64])
    dsink_d = din("d_sinks", [1, 8])
    y_d = nc.dram_tensor("y", [S, D], F32, kind="ExternalOutput").ap()

    def sb(name, shape, dt):
        return es.enter_context(nc.sbuf_tensor(name, list(shape), dt))

    def psum(name, shape, dt):
        return es.enter_context(nc.psum_tensor(name, list(shape), dt))

    P = Prog(nc)

    x_sb = sb("x_sb", [128, NT, D], F32)
    hT = sb("hT", [128, 8, S], BF16)
    wbuf = [sb("wbuf%d" % i, [128, 8, 512], BF16) for i in range(2)]
    wo = sb("wo", [128, 4, D], BF16)
    ident = sb("ident_sb", [128, 128], BF16)
    identf = sb("identf_sb", [128, 128], F32)
    onesf = sb("onesf", [128, 128], F32)
    tri = sb("tri_sb", [128, 128], BF16)
    upm = sb("up_sb", [128, 128], BF16)
    qal = sb("qal_sb", [128, NT, 32], BF16)
    kalm = sb("kalm_sb", [128, NT, 36], BF16)
    KTc = sb("KTc", [128, 2, 128], BF16)
    Vca = sb("Vca", [128, 2, 98], BF16)
    kals = sb("kals_sb", [128, NT, 36], BF16)
    kalp = sb("kalp_sb", [128, NT, 36], BF16)
    ss = sb("ss", [128, NT], F32)
    rstd = sb("rstd", [128, NT], F32)
    ARENA = 80 * 1024
    arena = sb("arena", [128, ARENA // 2], BF16)

    pf = [psum("pf%d" % i, [128, 512], F32) for i in range(6)]
    pb = [psum("pb%d" % i, [128, 1024], BF16) for i in range(2)]

    class Carver:
        def __init__(self, base=0):
            self.off = base

        def get(self, shape, dt):
            n = int(np.prod(shape[1:]))
            nb = n * (4 if dt == F32 else 2)
            nb = (nb + 63) // 64 * 64
            assert self.off + nb <= ARENA, ("arena overflow", self.off + nb, ARENA)
            a = arena[:, self.off // 2:(self.off + nb) // 2]
            self.off += nb
            if os.environ.get("KDBG"):
                print("carve", shape, dt, "->", self.off)
            if dt == F32:
                a = a.bitcast(F32)
            a = a[:, 0:n]
            if len(shape) == 3:
                a = a.rearrange("p (a b) -> p a b", b=shape[2])
            elif len(shape) == 4:
                a = a.rearrange("p (a b c) -> p a b c", b=shape[2], c=shape[3])
            if shape[0] < 128:
                a = a[0:shape[0]]
            return a

    def dma(eng, out, in_, chan, reads=(), writes=(), slow=False, nobar=False):
        if slow:
            P.op(eng, lambda e: e.dma_start(out=out, in_=in_, allow_slow_non_contiguous=True), reads=reads, writes=writes, chan=chan, nobar=nobar)
        else:
            P.op(eng, lambda e: e.dma_start(out=out, in_=in_), reads=reads, writes=writes, chan=chan, nobar=nobar)

    def mm(out, lhsT, rhs, start, stop, reads, writes):
        P.op("pe", lambda e: e.matmul(out, lhsT=lhsT, rhs=rhs, start=start, stop=stop), reads=reads, writes=writes)

    def tr(out, in_, reads, writes, idn=None):
        idn = ident[:] if idn is None else idn
        P.op("pe", lambda e: e.transpose(out=out, in_=in_, identity=idn), reads=list(reads) + ["const"], writes=writes)

    def act(out, in_, func, reads, writes, **kw):
        P.op("act", lambda e: e.activation(out=out, in_=in_, func=func, **kw), reads=reads, writes=writes)

    def V(fn, reads, writes, eng="dve"):
        P.op(eng, fn, reads=reads, writes=writes)

    def TT(out, in0, in1, op, reads, writes, eng="dve"):
        P.op(eng, lambda e: e.tensor_tensor(out=out, in0=in0, in1=in1, op=op), reads=reads, writes=writes)

    def TS(out, in0, s1, s2, op0, op1, reads, writes, eng="dve"):
        if op1 is None:
            P.op(eng, lambda e: e.tensor_scalar(out=out, in0=in0, scalar1=s1, scalar2=None, op0=op0), reads=reads, writes=writes)
        else:
            P.op(eng, lambda e: e.tensor_scalar(out=out, in0=in0, scalar1=s1, scalar2=s2, op0=op0, op1=op1), reads=reads, writes=writes)

    def STT(out, in0, scalar, in1, op0, op1, reads, writes, eng="dve"):
        P.op(eng, lambda e: e.scalar_tensor_tensor(out=out, in0=in0, scalar=scalar, in1=in1, op0=op0, op1=op1), reads=reads, writes=writes)

    def RED(out, in_, op, reads, writes):
        P.op("dve", lambda e: e.tensor_reduce(out=out, in_=in_, axis=AX.X, op=op), reads=reads, writes=writes)

    def RCP(out, in_, reads, writes):
        P.op("dve", lambda e: e.reciprocal(out=out, in_=in_), reads=reads, writes=writes)

    def CPY(out, in_, reads, writes, eng="dve"):
        P.op(eng, lambda e: e.tensor_copy(out=out, in_=in_), reads=reads, writes=writes)

    def MSET(ap, val, writes, eng="dve"):
        P.op(eng, lambda e: e.memset(ap, val), reads=[], writes=writes)

    import os
    KB = os.environ.get("KBIS", "abcdefg")
    if "a" in KB:
        dma("pool", ident[:], ident_d, "c", writes=["const"])
    if "b" in KB:
        dma("pool", tri[:], tri_d, "c", writes=["const"])
        dma("pool", upm[:], up_d, "c", writes=["const"])
    if "c" in KB:
        dma("pool", qal[:], qal_d, "c", writes=["const"])
    if "d" in KB:
        dma("pool", kalm[:], kalm_d, "c", writes=["const"])
        dma("pool", kals[:], kals_d, "c", writes=["const"])
        dma("pool", kalp[:], kalp_d, "c", writes=["const"])
    if "e" in KB:
        dma("sp", identf[:], ident_d, "c", writes=["const"])
    if "f" in KB:
        MSET(onesf[:], 1.0, ["const"])

    xv = x_d.rearrange("(i p) d -> p i d", p=128)
    yv = y_d.rearrange("(i p) d -> p i d", p=128)
    for i in range(NT):
        dma("sp", x_sb[:, i, :], xv[:, i, :], "x%d" % i, writes=[("x", i)])

    wslot_ctr = [0]

    def load_w(wd, segs):
        s = wslot_ctr[0] % 2
        wslot_ctr[0] += 1
        wv = wd.rearrange("(k p) n -> p k n", p=128)
        o = 0
        for (c0, n) in segs:
            dma("pool", wbuf[s][:, :, o:o + n], wv[:, :, c0:c0 + n], "w%d" % s, writes=[("w", s)], nobar=True)
            o += n
        return s

    def norm_phase(layer, base=0, barrier=True):
        cvn = Carver(base=base)
        gbc = cvn.get([128, D], F32)
        junk = cvn.get([128, D], BF16)
        hb = [cvn.get([128, D], BF16) for _ in range(2)]
        dma("act", gbc[:], normg_d[layer:layer + 1, :].partition_broadcast(128), "c", writes=["gbc"])
        MSET(ss[:], 0.0, [("ss", gI) for gI in range(4)])

        def stats(gI):
            for i in range(4 * gI, 4 * gI + 4):
                act(junk[:], x_sb[:, i, :], AF.Square, [("x", i), ("ss", gI)], ["junk", ("ss", gI)], accum_out=ss[:, i:i + 1])
            sl = slice(4 * gI, 4 * gI + 4)
            TS(rstd[:, sl], ss[:, sl], 1.0 / D, EPS, ALU.mult, ALU.add, [("ss", gI)], [("rstd", gI)])
            act(rstd[:, sl], rstd[:, sl], AF.Sqrt, [("rstd", gI)], [("rstd", gI)])
            RCP(rstd[:, sl], rstd[:, sl], [("rstd", gI)], [("rstd", gI)])

        def na(i):
            h = hb[i % 2]
            STT(h[:], x_sb[:, i, :], rstd[:, i:i + 1], gbc[:], ALU.mult, ALU.mult, [("x", i), ("rstd", i // 4), "gbc"], [("hb", i % 2)])

        def nb(i):
            h = hb[i % 2]
            pt = pb[i % 2]
            for k in range(8):
                tr(pt[:, k * 128:(k + 1) * 128], h[:, k * 128:(k + 1) * 128], [("hb", i % 2)], [("pb", i % 2)])
            act(hT[:, :, i * 128:(i + 1) * 128], pt[:].rearrange("p (k t) -> p k t", k=8), AF.Copy, [("pb", i % 2)], [("hT", i)])

        pend = []
        for gI in range(4):
            stats(gI)
            for i in range(4 * gI, 4 * gI + 4):
                na(i)
                pend.append(i)
                if len(pend) > 1:
                    nb(pend.pop(0))
        nb(pend.pop(0))
        if barrier:
            P.barrier()

    def load_wo(layer, c0, n):
        wv = wout_d[layer].rearrange("(k p) n -> p k n", p=128)
        for hf in range(2):
            dma("pool", wo[:, 0:n, hf * 512:(hf + 1) * 512], wv[:, c0:c0 + n, hf * 512:(hf + 1) * 512], "wo", writes=["wo"], nobar=True)

    def outproj_tile(i, lhs_list, chunks, yT_reads, pbanks):
        for half in range(2):
            pp = pf[pbanks[half]]
            for n, (c, lhs) in enumerate(zip(chunks, lhs_list)):
                mm(pp[:], lhs, wo[:, c, half * 512:(half + 1) * 512], n == 0, n == len(chunks) - 1,
                   list(yT_reads) + ["wo"], [("pf", pbanks[half])])
            xs = x_sb[:, i, half * 512:(half + 1) * 512]
            TT(xs, xs, pp[:], ALU.add, [("pf", pbanks[half]), ("x", i)], [("x", i)])

    att_ctr = [0, 0]

    def lagged(n, stage_a, stage_b, lag=1):
        for i in range(n + lag):
            if i < n:
                stage_a(i)
            if i >= lag:
                stage_b(i - lag)

    def mm_acc(out, lhsT, rhs, start, stop, reads, writes):
        P.op("pe", lambda e: e.matmul(out, lhsT=lhsT, rhs=rhs, start=start, stop=stop, skip_group_check=True), reads=reads, writes=writes)

    def attention(QT, KT, nk, Vrhs, ncv, plan, finish, qkeys, kkeys, vkeys, ptb, tagk, addmask=None):
        NS = len(ptb)
        work = []
        for qc in range(4):
            items = plan(qc)
            if not items:
                continue
            ob = 3 + att_ctr[1] % 3
            att_ctr[1] += 1
            lastk = {}
            for (kt, jlo, jhi, masks) in items:
                for j in range(jlo, jhi + 1):
                    lastk[j] = kt
            for n, it in enumerate(items):
                work.append((qc, ob, it, n == 0, n == len(items) - 1, lastk))

        def front(w):
            qc, ob, (kt, jlo, jhi, masks), first, last, lastk = w
            sbk = att_ctr[0] % NS
            att_ctr[0] += 1
            Sp = pf[sbk]
            pt = ptb[sbk]
            c0, c1 = jlo * 128, (jhi + 1) * 128
            extra = list(masks.items())
            mm_acc(Sp[0:nk, c0:c1], KT(kt), QT[:, qc * 512 + c0:qc * 512 + c1], True, addmask is None and not extra,
                   list(qkeys(qc)) + list(kkeys(kt)), [("pf", sbk)])
            if addmask is not None:
                mm_acc(Sp[0:nk, c0:c1], ident[0:nk, 0:nk], addmask[0:nk, qc * 512 + c0:qc * 512 + c1], False, not extra,
                       ["const", "addmask"], [("pf", sbk)])
            for n, (j, mk) in enumerate(extra):
                mm_acc(Sp[0:nk, j * 128:(j + 1) * 128], ident[0:nk, 0:nk], mk[0:nk, :], False, n == len(extra) - 1,
                       ["const"], [("pf", sbk)])
            act(pt[0:nk, c0:c1], Sp[0:nk, c0:c1], AF.Exp, [("pf", sbk)], [("pt", tagk, sbk)])
            return sbk

        def back(w, sbk, started):
            qc, ob, (kt, jlo, jhi, masks), first, last, lastk = w
            pt = ptb[sbk]
            for j in range(jlo, jhi + 1):
                mm_acc(pf[ob][:, j * 128:j * 128 + ncv], pt[0:nk, j * 128:(j + 1) * 128], Vrhs(kt), len(started) == 0, lastk[j] == kt,
                       [("pt", tagk, sbk)] + list(vkeys(kt)), [("pf", ob)])
                started.add(j)
            if last:
                for j in sorted(started):
                    finish(qc * 4 + j, pf[ob][:, j * 128:j * 128 + ncv], ("pf", ob))
                started.clear()

        LAG = NS - 1
        started = set()
        pend = []
        for w in work:
            pend.append((w, front(w)))
            if len(pend) > LAG:
                pw, psb = pend.pop(0)
                back(pw, psb, started)
        for (pw, psb) in pend:
            back(pw, psb, started)

    def layer0():
        if stop == "load":
            return
        norm_phase(0, base=70 * 1024, barrier=False)
        if stop == "norm0":
            return
        load_wo(0, 0, 4)
        W = ewin_d
        if stop == "norm":
            return
        cv = Carver()
        yc = cv.get([128, 4, S], F32)
        hc = cv.get([128, S + 32], BF16)
        dg = cv.get([128, 31, 128], BF16)
        cw = cv.get([128, 4, 31], F32)
        cb = cv.get([128, 4], F32)
        lg = cv.get([128, 4], F32)
        lb = cv.get([128, 4], F32)
        Tsq = [cv.get([128, 512], F32) for _ in range(2)]
        mu2 = [cv.get([128, 512], F32) for _ in range(2)]
        msq = cv.get([128, 512], F32)
        rs2 = [cv.get([128, 512], F32) for _ in range(2)]
        sz4 = [cv.get([128, 512], BF16) for _ in range(4)]
        T4 = [cv.get([128, 512], F32) for _ in range(4)]
        yaT2 = [cv.get([128, 4, 512], BF16) for _ in range(2)]
        HO = 32
        stg = Tsq[0][0:32, :]
        stg2 = Tsq[1][0:12, 0:128]
        dma("sp", stg[0:31, :], convw_d, "c", writes=[("Tsq", 0)])
        dma("sp", stg2[0:4, :], convb_d.rearrange("(a c) -> a c", c=128), "c", writes=[("Tsq", 1)])
        dma("sp", stg2[4:8, :], lng_d.rearrange("(a c) -> a c", c=128), "c", writes=[("Tsq", 1)])
        dma("sp", stg2[8:12, :], lnb_d.rearrange("(a c) -> a c", c=128), "c", writes=[("Tsq", 1)])
        for cc in range(4):
            tr(pf[0][:, cc * 32:cc * 32 + 31], stg[0:31, cc * 128:(cc + 1) * 128], [("Tsq", 0)], [("pf", 0)], idn=identf[0:31, 0:31])
        tr(pf[0][:, 128:140], stg2[0:12, :], [("Tsq", 1)], [("pf", 0)], idn=identf[0:12, 0:12])
        CPY(cw[:], pf[0][:, 0:128].rearrange("p (a j) -> p a j", j=32)[:, :, 0:31], [("pf", 0)], ["cw"])
        CPY(cb[:], pf[0][:, 128:132], [("pf", 0)], ["cw"])
        CPY(lg[:], pf[0][:, 132:136], [("pf", 0)], ["cw"])
        CPY(lb[:], pf[0][:, 136:140], [("pf", 0)], ["cw"])
        MSET(hc[:, 0:HO], 0.0, ["hc0"])
        if stop == "convp":
            return
        for cc in range(4):
            if stop == "conv1" and cc == 1:
                return
            s = load_w(W, [(cc * 128, 128), (512 + cc * 128, 128)])
            for j in range(31):
                TS(dg[:, j, :], ident[:], cw[:, cc, j:j + 1], None, ALU.mult, None, ["const", "cw"], [("dg", j)])
            for tq in range(4):
                bv, bg = (tq % 2) * 2, (tq % 2) * 2 + 1
                for which, bk in ((0, bv), (1, bg)):
                    for k in range(8):
                        mm(pf[bk][:], wbuf[s][:, k, which * 128:(which + 1) * 128], hT[:, k, tq * 512:(tq + 1) * 512], k == 0, k == 7,
                           [("w", s)] + [("hT", 4 * tq + u) for u in range(4)], [("pf", bk)])
                act(T4[tq % 2][:], pf[bg][:], AF.Sigmoid, [("pf", bg)], [("T4", tq % 2)])
                TT(hc[:, HO + tq * 512:HO + (tq + 1) * 512], pf[bv][:], T4[tq % 2][:], ALU.mult, [("pf", bv), ("T4", tq % 2)], [("hc", tq)])
            for tq in range(4):
                pc = pf[4 + tq % 2]
                for j in range(31):
                    o = HO - 30 + j + tq * 512
                    mm(pc[:], dg[:, j, :], hc[:, o:o + 512], j == 0, j == 30,
                       [("dg", j), ("hc", tq)] + ([("hc", tq - 1)] if tq > 0 else ["hc0"]), [("pf", 4 + tq % 2)])
                act(yc[:, cc, tq * 512:(tq + 1) * 512], pc[:], AF.Identity, [("pf", 4 + tq % 2), "cw"], [("yc", cc, tq)],
                    bias=cb[:, cc:cc + 1], scale=1.0)
        if stop == "conv4":
            return
        P.barrier()
        sz_slot = load_w(W, [(1024, 512)])

        def st_a(tq):
            ts = slice(tq * 512, (tq + 1) * 512)
            p2 = tq % 2
            for cc in range(4):
                mm(pf[0][:], onesf[:], yc[:, cc, ts], cc == 0, cc == 3, [("yc", cc, tq), "const"], [("pf", 0)])
            for cc in range(4):
                act(Tsq[cc % 2][:], yc[:, cc, ts], AF.Square, [("yc", cc, tq)], [("Tsq", cc % 2)])
                mm(pf[1][:], onesf[:], Tsq[cc % 2][:], cc == 0, cc == 3, [("Tsq", cc % 2), "const"], [("pf", 1)])
            TS(mu2[p2][:], pf[0][:], 1.0 / 512, None, ALU.mult, None, [("pf", 0)], [("mu", p2)])
            TT(msq[:], mu2[p2][:], mu2[p2][:], ALU.mult, [("mu", p2)], ["msq"])
            STT(rs2[p2][:], pf[1][:], 1.0 / 512, msq[:], ALU.mult, ALU.subtract, [("pf", 1), "msq"], [("rs", p2)])
            TS(rs2[p2][:], rs2[p2][:], EPS, None, ALU.add, None, [("rs", p2)], [("rs", p2)])
            act(rs2[p2][:], rs2[p2][:], AF.Sqrt, [("rs", p2)], [("rs", p2)])
            RCP(rs2[p2][:], rs2[p2][:], [("rs", p2)], [("rs", p2)])

        def st_b(tq):
            ts = slice(tq * 512, (tq + 1) * 512)
            p2 = tq % 2
            for cc in range(4):
                pz = pf[2 + cc % 2]
                for k in range(8):
                    mm(pz[:], wbuf[sz_slot][:, k, cc * 128:(cc + 1) * 128], hT[:, k, ts], k == 0, k == 7,
                       [("w", sz_slot)] + [("hT", 4 * tq + u) for u in range(4)], [("pf", 2 + cc % 2)])
                act(sz4[cc][:], pz[:], AF.Silu, [("pf", 2 + cc % 2)], [("sz", cc)])
            for cc in range(4):
                tt = T4[cc]
                TT(tt[:], yc[:, cc, ts], mu2[p2][:], ALU.subtract, [("yc", cc, tq), ("mu", p2)], [("T4", cc)])
                TT(tt[:], tt[:], rs2[p2][:], ALU.mult, [("T4", cc), ("rs", p2)], [("T4", cc)])
            for cc in range(4):
                tt = T4[cc]
                act(tt[:], tt[:], AF.Silu, [("T4", cc), "cw"], [("T4", cc)], scale=lg[:, cc:cc + 1], bias=lb[:, cc:cc + 1])
            for cc in range(4):
                TT(yaT2[p2][:, cc, :], T4[cc][:], sz4[cc][:], ALU.mult, [("T4", cc), ("sz", cc)], [("yaT", p2, cc)])

        def st_c(tq):
            p2 = tq % 2
            for u in range(4):
                i = 4 * tq + u
                outproj_tile(i, [yaT2[p2][:, cc, u * 128:(u + 1) * 128] for cc in range(4)], [0, 1, 2, 3],
                             [("yaT", p2, cc) for cc in range(4)], (4, 5))

        for step in range(6):
            if step < 4:
                st_a(step)
            if 0 <= step - 1 < 4:
                st_b(step - 1)
            if step - 2 >= 0:
                st_c(step - 2)
        P.barrier()
        if stop == "conv":
            return
        for hh in range(2):
            moba_half(hh, W)
            P.barrier()
            if stop is not None:
                return

    def moba_half(hh, W):
        cv = Carver()
        QT = cv.get([128, 4, S], BF16)
        KT = cv.get([128, 4, S], BF16)
        Vt = cv.get([128, NT, 4, 66], BF16)
        szy = cv.get([128, NT, 256], BF16)
        ptb = [cv.get([128, 512], BF16) for _ in range(3)]
        gq = cv.get([128, 1], F32)
        gk = cv.get([128, 1], F32)
        kmf = cv.get([128, 4, 8], F32)
        kmb = cv.get([128, 4, 8], BF16)
        gs = cv.get([128, 4, 8], F32)
        cmp_ = cv.get([128, 4, 8, 8], F32)
        rank = cv.get([128, 4, 8], F32)
        nm = cv.get([128, 4, 32], BF16)
        rden = [cv.get([128, 1], F32) for _ in range(8)]
        yT = [cv.get([128, 2, 128], BF16) for _ in range(2)]
        tmpf = [cv.get([128, 512], F32) for _ in range(2)]
        s8 = [cv.get([128, 8], F32) for _ in range(3)]
        qa = [cv.get([128, 4, 128], BF16) for _ in range(3)]
        ka = [cv.get([128, 4, 128], BF16) for _ in range(3)]
        c_q = 1536 + hh * 256
        c_k = 2048 + hh * 256
        c_v = 2560 + hh * 256
        c_z = 3072 + hh * 256
        s = load_w(W, [(c_q, 256), (c_k, 256)])
        s2 = load_w(W, [(c_v, 256), (c_z, 256)])
        load_wo(0, 4 + 2 * hh, 2)
        load_gain_col(gq, bq_d, 1.0)
        load_gain_col(gk, bk_d, 8.0)
        for b in range(3):
            MSET(qa[b][:], 0.0, [("qa", b)])
            MSET(ka[b][:], 0.0, [("ka", b)])
        MSET(Vt[:], 1.0, ["Vt"])
        MSET(nm[:], 0.0, ["nm"])

        def m0(i):
            r = i % 3
            CPY(qa[r][:, :, 96:100], qal[:, i, hh * 16:hh * 16 + 16].rearrange("p (h c) -> p h c", c=4), ["const"], [("qa", r)])
            CPY(ka[r][:, :, 64:100], kalm[:, i, :].unsqueeze(1).to_broadcast([128, 4, 36]), ["const"], [("ka", r)])
            for k in range(8):
                mm(pf[r][:], hT[:, k, i * 128:(i + 1) * 128], wbuf[s][:, k, :], k == 0, k == 7, [("hT", i), ("w", s)], [("pf", r)])

        def m1(i):
            r = i % 3
            rstd_a(pf[r], ("pf", r), 512, tmpf[i % 2], ("tf", i % 2), s8[r], ("s8", r))

        def m2(i):
            r = i % 3
            rstd_b(512, s8[r], ("s8", r))
            TT(qa[r][:, :, 0:64], pf[r][:, 0:256].rearrange("p (h d) -> p h d", d=64), s8[r][:, 0:4].unsqueeze(2).to_broadcast([128, 4, 64]),
               ALU.mult, [("pf", r), ("s8", r)], [("qa", r)])
            TT(ka[r][:, :, 0:64], pf[r][:, 256:512].rearrange("p (h d) -> p h d", d=64), s8[r][:, 4:8].unsqueeze(2).to_broadcast([128, 4, 64]),
               ALU.mult, [("pf", r), ("s8", r)], [("ka", r)])

        def m3(i):
            r = i % 3
            b2 = i % 2
            pt = pb[b2]
            for h in range(4):
                tr(pt[:, h * 128:(h + 1) * 128], qa[r][:, h, :], [("qa", r)], [("pb", b2)])
            for h in range(4):
                tr(pt[:, (4 + h) * 128:(5 + h) * 128], ka[r][:, h, :], [("ka", r)], [("pb", b2)])
            act(QT[:, :, i * 128:(i + 1) * 128], pt[:, 0:512].rearrange("p (h t) -> p h t", h=4), AF.Copy, [("pb", b2), "gq"], [("QT", i)],
                scale=gq[:, 0:1])
            act(KT[:, :, i * 128:(i + 1) * 128], pt[:, 512:1024].rearrange("p (h t) -> p h t", h=4), AF.Copy, [("pb", b2), "gq"], [("KT", i)],
                scale=gk[:, 0:1])

        pipe4(NT, m0, m1, m2, m3)
        if hh == 1 and 1 in layers:
            prefetch_w1([("qa", r) for r in range(3)] + [("ka", r) for r in range(3)] + [("s8", r) for r in range(3)] + [("tf", 0), ("tf", 1)])
        if stop == "m1":
            return
        for i in range(NT):
            b2 = 2 + i % 2
            pp = pf[b2]
            for k in range(8):
                mm(pp[:], hT[:, k, i * 128:(i + 1) * 128], wbuf[s2][:, k, :], k == 0, k == 7, [("hT", i), ("w", s2)], [("pf", b2)])
            act(Vt[:, i, :, 0:64], pp[:, 0:256].rearrange("p (h d) -> p h d", d=64), AF.Copy, [("pf", b2), "Vt"], [("V", i)])
            act(szy[:, i, :], pp[:, 256:512], AF.Silu, [("pf", b2)], [("szy", i)])
        if stop == "m3":
            return
        for h in range(4):
            RED(kmf[:, h, :], KT[:, h, :].rearrange("p (n t) -> p n t", t=256), ALU.add, [("KT", i) for i in range(NT)], ["kmf"])
        TS(kmb[:], kmf[:], 1.0 / 256, None, ALU.mult, None, ["kmf"], ["kmb"])
        if stop == "mk":
            return
        for i in range(8, NT):
            npast = i // 2
            b2 = 4 + i % 2
            pg = pf[b2]
            for h in range(4):
                mm(pg[:, h * 8:h * 8 + 8], QT[0:64, h, i * 128:(i + 1) * 128], kmb[0:64, h, :], True, True, [("QT", i), "kmb"], [("pf", b2)])
            CPY(gs[:], pg[:, 0:32].rearrange("p (h n) -> p h n", n=8), [("pf", b2)], ["gs"])
            TT(cmp_[:, :, 0:npast, 0:npast], gs[:, :, 0:npast].unsqueeze(2).to_broadcast([128, 4, npast, npast]),
               gs[:, :, 0:npast].unsqueeze(3).to_broadcast([128, 4, npast, npast]), ALU.is_gt, ["gs"], ["cmp"])
            RED(rank[:, :, 0:npast], cmp_[:, :, 0:npast, 0:npast], ALU.add, ["cmp"], ["rank"])
            TS(nm[:, :, 0:npast], rank[:, :, 0:npast], 2.5, NEGM, ALU.is_ge, ALU.mult, ["rank"], ["nm"])
            pt = pb[i % 2]
            tr(pt[:, 0:128], nm[:].rearrange("p h n -> p (h n)"), ["nm"], [("pb", i % 2)])
            for h in range(4):
                act(QT[64:96, h, i * 128:(i + 1) * 128], pt[h * 32:(h + 1) * 32, 0:128], AF.Copy, [("pb", i % 2)], [("QT", i)])

        if stop == "m2":
            return

        def plan(qc):
            items = []
            for kt in range(4 * qc + 4):
                if kt < 4 * qc:
                    items.append((kt, 0, 3, {}))
                else:
                    m = kt - 4 * qc
                    items.append((kt, m, 3, {m: tri[:]}))
            return items

        for h in range(4):
            def finish(i, Ob, okey, h=h):
                r = (i + 4 * h) % 8
                rd = rden[r]
                RCP(rd[:], Ob[:, 64:65], [okey], [("rden", r)])
                dst = szy[:, i, h * 64:(h + 1) * 64]
                STT(dst, Ob[:, 0:64], rd[:], dst, ALU.mult, ALU.mult, [okey, ("rden", r), ("szy", i)], [("szy", i)])

            attention(QT[:, h, :], lambda kt, h=h: KT[:, h, kt * 128:(kt + 1) * 128], 128,
                      lambda kt, h=h: Vt[:, kt, h, 0:65], 65, plan, finish,
                      lambda qc: [("QT", 4 * qc + u) for u in range(4)], lambda kt: [("KT", kt)], lambda kt: [("V", kt)],
                      ptb, "m")
        if stop == "ma":
            return
        outproj_half(szy, yT)

    def rstd_a(pp, pkey, ncol, tf, tfkey, s8i, s8key):
        nh = ncol // 64
        act(tf[:, 0:ncol], pp[:, 0:ncol], AF.Square, [pkey], [tfkey])
        RED(s8i[:, 0:nh], tf[:, 0:ncol].rearrange("p (h d) -> p h d", d=64), ALU.add, [tfkey], [s8key])
        TS(s8i[:, 0:nh], s8i[:, 0:nh], 64.0 * EPS, None, ALU.add, None, [s8key], [s8key])
        act(s8i[:, 0:nh], s8i[:, 0:nh], AF.Sqrt, [s8key], [s8key])

    def rstd_b(ncol, s8i, s8key):
        nh = ncol // 64
        RCP(s8i[:, 0:nh], s8i[:, 0:nh], [s8key], [s8key])

    def head_rstd(pp, pkey, ncol, tf, tfkey, s8i, s8key):
        rstd_a(pp, pkey, ncol, tf, tfkey, s8i, s8key)
        rstd_b(ncol, s8i, s8key)

    def pipe4(n, s0, s1, s2, s3):
        for step in range(n + 2):
            if step < n:
                s0(step)
                s1(step)
            if 1 <= step <= n:
                s2(step - 1)
            if step >= 2:
                s3(step - 2)

    def load_gain_col(dst, gd, mult):
        MSET(dst[:], 1.0, ["gq"])
        dma("sp", dst[0:64, :], gd.rearrange("o d -> d o"), "c", writes=["gq"], slow=True)
        if mult != 1.0:
            TS(dst[0:64, :], dst[0:64, :], mult, None, ALU.mult, None, ["gq"], ["gq"])

    def outproj_half(szy, yT, store=False):
        def oa(i):
            pt = pb[i % 2]
            for c in range(2):
                tr(pt[:, c * 128:(c + 1) * 128], szy[:, i, c * 128:(c + 1) * 128], [("szy", i)], [("pb", i % 2)])
            y = yT[i % 2]
            act(y[:], pt[:, 0:256].rearrange("p (c t) -> p c t", c=2), AF.Copy, [("pb", i % 2)], [("yT", i % 2)])

        def ob(i):
            y = yT[i % 2]
            outproj_tile(i, [y[:, 0, :], y[:, 1, :]], [0, 1], [("yT", i % 2)], (4, 5))
            if store:
                dma("sp", yv[:, i, :], x_sb[:, i, :], "out", reads=[("x", i)])

        lagged(NT, oa, ob)

    def plan_causal(qc):
        items = []
        for kt in range(4 * qc + 4):
            if kt < 4 * qc:
                items.append((kt, 0, 3, {}))
            else:
                m = kt - 4 * qc
                items.append((kt, m, 3, {m: tri[:]}))
        return items

    def plan_band(wt):
        def plan(qc):
            items = []
            for kt in range(max(0, 4 * qc - wt), 4 * qc + 4):
                jlo = max(0, kt - 4 * qc)
                jhi = min(3, kt + wt - 4 * qc)
                if jlo > jhi:
                    continue
                masks = {}
                if 0 <= kt - 4 * qc <= 3:
                    masks[kt - 4 * qc] = tri[:]
                if 0 <= kt + wt - 4 * qc <= 3:
                    masks[kt + wt - 4 * qc] = upm[:]
                items.append((kt, jlo, jhi, masks))
            return items
        return plan

    W1_OFF = 64 * 1024

    def prefetch_w1(war_keys=()):
        cvp = Carver(base=W1_OFF)
        wA = cvp.get([128, 32, 256], BF16)
        for kv, wd in enumerate((ckw1_d, cvw1_d)):
            wv = wd.rearrange("(l d) j -> d l j", d=64)
            for lq in range(4):
                dma("pool", wA[kv * 64:(kv + 1) * 64, lq * 8:(lq + 1) * 8, :], wv[:, lq * 8:(lq + 1) * 8, :], "w1", writes=["w1A"] + list(war_keys))
        return wA

    def layer1():
        W = owin_d
        wA = Carver(base=W1_OFF).get([128, 32, 256], BF16)
        if 0 not in layers:
            prefetch_w1()
        cvc = Carver(base=16 * 1024)
        wB = cvc.get([128, 32, 256], BF16)
        dma("sp", wB[64:128, :, :], wA[0:64, :, :], "c", writes=["w1B"])
        dma("sp", wB[0:64, :, :], wA[64:128, :, :], "c", writes=["w1B"])
        w1 = [[wA, wB], [wB, wA]]
        B = compress_loads(W, cvc)
        norm_phase(1)
        compress_stage(W, B, w1)
        P.barrier()
        if stop == "cmp":
            return
        for g in range(2):
            nsa_half(g, W)
            P.barrier()
            if stop == "nsa0":
                return
        if stop == "nsa":
            return
        for g in range(2):
            swa_half(g, W)
            P.barrier()

    def compress_loads(W, cv):
        s = load_w(W, [(512, 128), (640, 128)])
        B = {}
        B["s"] = s
        B["KVD"] = [cv.get([128, 16, 128], BF16) for _ in range(2)]
        B["w2"] = [cv.get([128, 2, 64], BF16) for _ in range(2)]
        B["posn"] = cv.get([32, 2, 64], F32)
        B["posT"] = cv.get([64, 2, 32], BF16)
        B["stgb"] = cv.get([4, 128], F32)
        B["b1sb"] = cv.get([128, 4], F32)
        B["biasj"] = cv.get([128, 4], F32)
        B["b2bc"] = cv.get([128, 2, 64], F32)
        B["gcm"] = cv.get([128, 64], F32)
        B["kalc"] = cv.get([128, 36], BF16)
        B["hid"] = [cv.get([128, 2, 128], BF16) for _ in range(4)]
        B["kcf"] = cv.get([128, 64], F32)
        B["junk2"] = cv.get([128, 64], F32)
        B["ssc"] = cv.get([128, 1], F32)
        B["kaug"] = cv.get([128, 128], BF16)
        for kv, wd in enumerate((ckw2_d, cvw2_d)):
            dma("pool", B["w2"][kv][:], wd.rearrange("(c p) d -> p c d", p=128), "w2", writes=["w2"])
        dma("sp", B["posn"][:, 0, :], cposk_d, "c", writes=["posn"])
        dma("sp", B["posn"][:, 1, :], cposv_d, "c", writes=["posn"])
        dma("sp", B["stgb"][0:2, :], ckb1_d.rearrange("(a c) -> a c", c=128), "c", writes=["stgb"])
        dma("sp", B["stgb"][2:4, :], cvb1_d.rearrange("(a c) -> a c", c=128), "c", writes=["stgb"])
        dma("sp", B["b2bc"][:, 0, :], ckb2_d.partition_broadcast(128), "c", writes=["b2bc"])
        dma("sp", B["b2bc"][:, 1, :], cvb2_d.partition_broadcast(128), "c", writes=["b2bc"])
        dma("sp", B["gcm"][:], ckc_d.partition_broadcast(128), "c", writes=["gcm"])
        dma("pool", B["kalc"][:], kalc_d, "c", writes=["kalc"])
        for g in range(2):
            dma("pool", Vca[:, g, 65:97], ovl_d, "c", writes=[("Vca", g)])
            MSET(Vca[:, g, 64:65], 1.0, [("Vca", g)])
        MSET(B["kaug"][:], 0.0, ["kaug"])
        return B

    def compress_stage(W, B, w1):
        s = B["s"]
        KVD, w2, posn, posT, stgb, b1sb, biasj = B["KVD"], B["w2"], B["posn"], B["posT"], B["stgb"], B["b1sb"], B["biasj"]
        b2bc, gcm, kalc, hid, kcf, junk2, ssc, kaug = B["b2bc"], B["gcm"], B["kalc"], B["hid"], B["kcf"], B["junk2"], B["ssc"], B["kaug"]
        for kv in range(2):
            tr(pf[0][0:64, kv * 32:(kv + 1) * 32], posn[:, kv, :], ["posn"], [("pf", 0)], idn=identf[0:32, 0:32])
        tr(pf[0][:, 64:68], stgb[:], ["stgb"], [("pf", 0)], idn=identf[0:4, 0:4])
        CPY(posT[:], pf[0][0:64, 0:64].rearrange("p (k l) -> p k l", l=32), [("pf", 0)], ["posT"])
        CPY(b1sb[:], pf[0][:, 64:68], [("pf", 0)], ["b1sb"])
        for kv in range(2):
            for jc in range(2):
                col = kv * 2 + jc
                for l in range(32):
                    mm(pf[1][:, col:col + 1], w1[kv][0][0:64, l, jc * 128:(jc + 1) * 128], posT[0:64, kv, l:l + 1], l == 0, l == 31,
                       ["w1B", "posT"], [("pf", 1)])
        TT(biasj[:], pf[1][:, 0:4], b1sb[:], ALU.add, [("pf", 1), "b1sb"], ["biasj"])
        for tq in range(4):
            for which in range(2):
                pp = pf[2 + which]
                for k in range(8):
                    mm(pp[:], wbuf[s][:, k, which * 128:(which + 1) * 128], hT[:, k, tq * 512:(tq + 1) * 512], k == 0, k == 7,
                       [("w", s)] + [("hT", 4 * tq + u) for u in range(4)], [("pf", 2 + which)])
                act(KVD[which][:, :, tq * 32:(tq + 1) * 32].rearrange("p r m -> p m r"), pp[:].rearrange("p (m r) -> p m r", r=16),
                    AF.Copy, [("pf", 2 + which)], [("KVT", which)])
        n = 0
        for kv in range(2):
            for g in range(2):
                hb_ = hid[kv * 2 + g]
                for jc in range(2):
                    pp = pf[n % 2]
                    for l in range(32):
                        mm(pp[:, 0:127], w1[kv][g][g * 64:(g + 1) * 64, l, jc * 128:(jc + 1) * 128],
                           KVD[kv][g * 64:(g + 1) * 64, l % 16, (l // 16):(l // 16) + 127], l == 0, l == 31,
                           ["w1B", ("KVT", kv)], [("pf", n % 2)])
                    act(hb_[:, jc, 0:127], pp[:, 0:127], AF.Silu, [("pf", n % 2), "biasj"], [("hid", kv * 2 + g)],
                        bias=biasj[:, kv * 2 + jc:kv * 2 + jc + 1], scale=1.0)
                    n += 1
                po = pf[2 + g]
                for jc in range(2):
                    mm(po[0:127, 0:64], hb_[:, jc, 0:127], w2[kv][:, jc, :], jc == 0, jc == 1, [("hid", kv * 2 + g), "w2"], [("pf", 2 + g)])
                if kv == 0:
                    TT(kcf[0:127, :], po[0:127, 0:64], b2bc[0:127, 0, :], ALU.add, [("pf", 2 + g), "b2bc"], ["kcf"])
                    act(junk2[0:127, :], kcf[0:127, :], AF.Square, ["kcf"], ["junk2", "ssc"], accum_out=ssc[0:127, :])
                    TS(ssc[0:127, :], ssc[0:127, :], 1.0 / 64, EPS, ALU.mult, ALU.add, ["ssc"], ["ssc"])
                    act(ssc[0:127, :], ssc[0:127, :], AF.Sqrt, ["ssc"], ["ssc"])
                    RCP(ssc[0:127, :], ssc[0:127, :], ["ssc"], ["ssc"])
                    STT(kaug[0:127, 0:64], kcf[0:127, :], ssc[0:127, :], gcm[0:127, :], ALU.mult, ALU.mult, ["kcf", "ssc", "gcm"], ["kaug"])
                    CPY(kaug[:, 64:100], kalc[:], ["kalc"], ["kaug"])
                    tr(pb[g][:, 0:128], kaug[:], ["kaug"], [("pb", g)])
                    act(KTc[:, g, :], pb[g][:, 0:128], AF.Copy, [("pb", g)], [("KTc", g)])
                else:
                    TT(Vca[0:127, g, 0:64], po[0:127, 0:64], b2bc[0:127, 1, :], ALU.add, [("pf", 2 + g), "b2bc"], [("Vca", g)])

    def nsa_half(g, W):
        cv = Carver()
        QT = cv.get([128, 4, S], BF16)
        KTs = cv.get([128, S], BF16)
        KTw = cv.get([128, S], BF16)
        Vs = cv.get([128, NT, 66], BF16)
        Vw = cv.get([128, NT, 66], BF16)
        szy = cv.get([128, NT, 256], BF16)
        gates = cv.get([128, NT, 12], F32)
        oacc = cv.get([128, NT, 256], F32)
        imp = cv.get([128, 8, 32], F32)
        impc = cv.get([128, 8, 32], F32)
        cmn = cv.get([128, S], BF16)
        qa = [cv.get([128, 4, 128], BF16) for _ in range(3)]
        ksa = [cv.get([128, 128], BF16) for _ in range(3)]
        kwa = [cv.get([128, 128], BF16) for _ in range(3)]
        ptb = [cv.get([128, 512], BF16) for _ in range(3)]
        tmpf = [cv.get([128, 512], F32) for _ in range(2)]
        cmpb = cv.get([128, 32, 32], F32)
        rank = cv.get([128, 32], F32)
        nmt = cv.get([128, 128], BF16)
        s8 = [cv.get([128, 8], F32) for _ in range(3)]
        gq = cv.get([128, 1], F32)
        gks = cv.get([128, 1], F32)
        gkw = cv.get([128, 1], F32)
        rden = [cv.get([128, 1], F32) for _ in range(8)]
        yT = [cv.get([128, 2, 128], BF16) for _ in range(2)]
        s = load_w(W, [(g * 256, 256), (768 + 64 * g, 64), (1024 + 64 * g, 64), (896 + 64 * g, 64), (1152 + 64 * g, 64)])
        s2 = load_w(W, [(1304 + g * 256, 256), (1280 + 4 * g, 4), (1288 + 4 * g, 4), (1296 + 4 * g, 4)])
        load_wo(1, 2 * g, 2)
        load_gain_col(gq, cq_d, 1.0)
        load_gain_col(gks, cks_d, 8.0)
        load_gain_col(gkw, ckw_d, 8.0)
        dma("sp", impc[:], impc_d[:, 8:16, :], "c", writes=["impc"])
        dma("pool", cmn[:], cmn_d, "c", writes=["addmask"])
        for b in range(3):
            MSET(qa[b][:], 0.0, [("qa", b)])
            MSET(ksa[b][:], 0.0, [("ksa", b)])
            MSET(kwa[b][:], 0.0, [("kwa", b)])
        MSET(Vs[:], 1.0, ["Vs"])
        MSET(Vw[:], 1.0, ["Vw"])
        MSET(nmt[:], 0.0, ["nmt"])
        def pa(i):
            b2 = i % 2
            pp = pf[b2]
            qai, ksi, kwi = qa[b2], ksa[b2], kwa[b2]
            CPY(qai[:, :, 96:100], qal[:, i, g * 16:g * 16 + 16].rearrange("p (h c) -> p h c", c=4), ["const"], [("qa", b2)])
            CPY(ksi[:, 64:100], kals[:, i, :], ["const"], [("ksa", b2)])
            CPY(kwi[:, 64:100], kalp[:, i, :], ["const"], [("kwa", b2)])
            for k in range(8):
                mm(pp[:], hT[:, k, i * 128:(i + 1) * 128], wbuf[s][:, k, :], k == 0, k == 7, [("hT", i), ("w", s)], [("pf", b2)])
            tf = tmpf[b2]
            head_rstd(pp, ("pf", b2), 384, tf, ("tf", b2), s8[b2], ("s8", b2))
            TT(qai[:, :, 0:64], pp[:, 0:256].rearrange("p (h d) -> p h d", d=64), s8[b2][:, 0:4].unsqueeze(2).to_broadcast([128, 4, 64]),
               ALU.mult, [("pf", b2), ("s8", b2)], [("qa", b2)])
            TS(ksi[:, 0:64], pp[:, 256:320], s8[b2][:, 4:5], None, ALU.mult, None, [("pf", b2), ("s8", b2)], [("ksa", b2)])
            TS(kwi[:, 0:64], pp[:, 320:384], s8[b2][:, 5:6], None, ALU.mult, None, [("pf", b2), ("s8", b2)], [("kwa", b2)])
            act(Vs[:, i, 0:64], pp[:, 384:448], AF.Copy, [("pf", b2), "Vs"], [("Vs", i)])
            act(Vw[:, i, 0:64], pp[:, 448:512], AF.Copy, [("pf", b2), "Vw"], [("Vw", i)])

        def pbk(i):
            b2 = i % 2
            qai, ksi, kwi = qa[b2], ksa[b2], kwa[b2]
            pt = pb[b2]
            for h in range(4):
                tr(pt[:, h * 128:(h + 1) * 128], qai[:, h, :], [("qa", b2)], [("pb", b2)])
            tr(pt[:, 512:640], ksi[:], [("ksa", b2)], [("pb", b2)])
            tr(pt[:, 640:768], kwi[:], [("kwa", b2)], [("pb", b2)])
            act(QT[:, :, i * 128:(i + 1) * 128], pt[:, 0:512].rearrange("p (h t) -> p h t", h=4), AF.Copy, [("pb", b2), "gq"], [("QT", i)],
                scale=gq[:, 0:1])
            act(KTs[:, i * 128:(i + 1) * 128], pt[:, 512:640], AF.Copy, [("pb", b2), "gq"], [("KTs", i)], scale=gks[:, 0:1])
            act(KTw[:, i * 128:(i + 1) * 128], pt[:, 640:768], AF.Copy, [("pb", b2), "gq"], [("KTw", i)], scale=gkw[:, 0:1])

        lagged(NT, pa, pbk)
        for i in range(NT):
            b2 = 2 + i % 2
            pp = pf[b2]
            for k in range(8):
                mm(pp[:, 0:256], hT[:, k, i * 128:(i + 1) * 128], wbuf[s2][:, k, 0:256], k == 0, k == 7, [("hT", i), ("w", s2)], [("pf", b2)])
            act(szy[:, i, :], pp[:, 0:256], AF.Silu, [("pf", b2)], [("szy", i)])
        for i in range(NT):
            b2 = 2 + i % 2
            pp = pf[b2]
            for k in range(8):
                mm(pp[:, 0:12], hT[:, k, i * 128:(i + 1) * 128], wbuf[s2][:, k, 256:268], k == 0, k == 7, [("hT", i), ("w", s2)], [("pf", b2)])
            act(gates[:, i, :], pp[:, 0:12], AF.Sigmoid, [("pf", b2)], [("gates", i)])
        if stop == "nsaproj":
            return
        qk = lambda qc: [("QT", 4 * qc + u) for u in range(4)]
        for r in range(4):
            def fin_cmp(i, Ob, okey, r=r):
                ri = (i + 4 * r) % 8
                rd = rden[ri]
                TS(rd[:], Ob[:, 64:65], 1e-30, None, ALU.max, None, [okey], [("rden", ri)])
                RCP(rd[:], rd[:], [("rden", ri)], [("rden", ri)])
                TS(oacc[:, i, r * 64:(r + 1) * 64], Ob[:, 0:64], rd[:], gates[:, i, r:r + 1], ALU.mult, ALU.mult,
                   [okey, ("rden", ri), ("gates", i)], [("oacc", i)])
                if i >= 8:
                    if r == 0:
                        TS(imp[:, i - 8, :], Ob[:, 65:97], rd[:], None, ALU.mult, None, [okey, ("rden", ri)], [("imp", i)])
                    else:
                        STT(imp[:, i - 8, :], Ob[:, 65:97], rd[:], imp[:, i - 8, :], ALU.mult, ALU.add, [okey, ("rden", ri), ("imp", i)], [("imp", i)])

            attention(QT[:, r, :], lambda kt: KTc[:, g, 0:127], 127, lambda kt: Vca[0:127, g, 0:97], 97,
                      lambda qc: [(0, 0, 3, {})], fin_cmp, qk, lambda kt: [("KTc", g)], lambda kt: [("Vca", g)], ptb, "n", addmask=cmn)
        if stop == "nsacmp":
            return
        for i in range(8, NT):
            im = imp[:, i - 8, :]
            TT(im, im, impc[:, i - 8, :], ALU.add, [("imp", i), "impc"], [("imp", i)])
            TT(cmpb[:], im.unsqueeze(1).to_broadcast([128, 32, 32]), im.unsqueeze(2).to_broadcast([128, 32, 32]), ALU.is_gt,
               [("imp", i)], ["cmpb"])
            RED(rank[:], cmpb[:], ALU.add, ["cmpb"], ["rank"])
            TS(nmt[:, 0:32], rank[:], 15.5, NEGM, ALU.is_ge, ALU.mult, ["rank"], ["nmt"])
            pt = pb[i % 2]
            tr(pt[:, 0:128], nmt[:], ["nmt"], [("pb", i % 2)])
            for r in range(4):
                act(QT[64:96, r, i * 128:(i + 1) * 128], pt[0:32, 0:128], AF.Copy, [("pb", i % 2)], [("QT", i)])
        for (KTx, Vx, kname, vname, plan, gcol) in ((KTs, Vs, "KTs", "Vs", plan_causal, 4), (KTw, Vw, "KTw", "Vw", plan_band(4), 8)):
            for r in range(4):
                def fin_add(i, Ob, okey, r=r, gcol=gcol):
                    ri = (i + 4 * r) % 8
                    rd = rden[ri]
                    RCP(rd[:], Ob[:, 64:65], [okey], [("rden", ri)])
                    TT(rd[:], rd[:], gates[:, i, gcol + r:gcol + r + 1], ALU.mult, [("rden", ri), ("gates", i)], [("rden", ri)])
                    dst = oacc[:, i, r * 64:(r + 1) * 64]
                    STT(dst, Ob[:, 0:64], rd[:], dst, ALU.mult, ALU.add, [okey, ("rden", ri), ("oacc", i)], [("oacc", i)])

                attention(QT[:, r, :], lambda kt, KTx=KTx: KTx[:, kt * 128:(kt + 1) * 128], 128,
                          lambda kt, Vx=Vx: Vx[:, kt, 0:65], 65, plan, fin_add, qk,
                          lambda kt, kname=kname: [(kname, kt)], lambda kt, vname=vname: [(vname, kt)], ptb, "n")
        for i in range(NT):
            TT(szy[:, i, :], oacc[:, i, :], szy[:, i, :], ALU.mult, [("oacc", i), ("szy", i)], [("szy", i)])
        outproj_half(szy, yT)

    def swa_half(g, W):
        cv = Carver()
        QT = cv.get([128, 4, S], BF16)
        KT = cv.get([128, S], BF16)
        Vt = cv.get([128, NT, 66], BF16)
        szy = cv.get([128, NT, 256], BF16)
        qa = [cv.get([128, 4, 128], BF16) for _ in range(3)]
        ka = [cv.get([128, 128], BF16) for _ in range(3)]
        ptb = [cv.get([128, 512], BF16) for _ in range(3)]
        tmpf = [cv.get([128, 512], F32) for _ in range(2)]
        s8 = [cv.get([128, 8], F32) for _ in range(3)]
        gq = cv.get([128, 1], F32)
        gk = cv.get([128, 1], F32)
        esink = cv.get([128, 8], F32)
        rden = [cv.get([128, 1], F32) for _ in range(8)]
        yT = [cv.get([128, 2, 128], BF16) for _ in range(2)]
        load_gain_col(gq, dq_d, 1.0)
        load_gain_col(gk, dk_d, 8.0)
        dma("sp", esink[:], dsink_d.partition_broadcast(128), "c", writes=["esink"])
        act(esink[:], esink[:], AF.Exp, ["esink"], ["esink"])
        for b in range(3):
            MSET(qa[b][:], 0.0, [("qa", b)])
            MSET(ka[b][:], 0.0, [("ka", b)])
        MSET(Vt[:], 1.0, ["Vt"])
        s = load_w(W, [(1816 + g * 256, 256), (2328 + 64 * g, 64), (2456 + 64 * g, 64)])
        s2 = load_w(W, [(2584 + g * 256, 256)])
        load_wo(1, 4 + 2 * g, 2)
        def pa(i):
            b2 = i % 2
            pp = pf[b2]
            qai, kai = qa[b2], ka[b2]
            CPY(qai[:, :, 96:100], qal[:, i, g * 16:g * 16 + 16].rearrange("p (h c) -> p h c", c=4), ["const"], [("qa", b2)])
            CPY(kai[:, 64:100], kalp[:, i, :], ["const"], [("ka", b2)])
            for k in range(8):
                mm(pp[:, 0:384], hT[:, k, i * 128:(i + 1) * 128], wbuf[s][:, k, 0:384], k == 0, k == 7, [("hT", i), ("w", s)], [("pf", b2)])
            tf = tmpf[b2]
            head_rstd(pp, ("pf", b2), 320, tf, ("tf", b2), s8[b2], ("s8", b2))
            TT(qai[:, :, 0:64], pp[:, 0:256].rearrange("p (h d) -> p h d", d=64), s8[b2][:, 0:4].unsqueeze(2).to_broadcast([128, 4, 64]),
               ALU.mult, [("pf", b2), ("s8", b2)], [("qa", b2)])
            TS(kai[:, 0:64], pp[:, 256:320], s8[b2][:, 4:5], None, ALU.mult, None, [("pf", b2), ("s8", b2)], [("ka", b2)])
            act(Vt[:, i, 0:64], pp[:, 320:384], AF.Copy, [("pf", b2), "Vt"], [("V", i)])

        def pbk(i):
            b2 = i % 2
            qai, kai = qa[b2], ka[b2]
            pt = pb[b2]
            for h in range(4):
                tr(pt[:, h * 128:(h + 1) * 128], qai[:, h, :], [("qa", b2)], [("pb", b2)])
            tr(pt[:, 512:640], kai[:], [("ka", b2)], [("pb", b2)])
            act(QT[:, :, i * 128:(i + 1) * 128], pt[:, 0:512].rearrange("p (h t) -> p h t", h=4), AF.Copy, [("pb", b2), "gq"], [("QT", i)],
                scale=gq[:, 0:1])
            act(KT[:, i * 128:(i + 1) * 128], pt[:, 512:640], AF.Copy, [("pb", b2), "gq"], [("KT", i)], scale=gk[:, 0:1])

        lagged(NT, pa, pbk)
        for i in range(NT):
            b2 = 2 + i % 2
            pp = pf[b2]
            for k in range(8):
                mm(pp[:, 0:256], hT[:, k, i * 128:(i + 1) * 128], wbuf[s2][:, k, 0:256], k == 0, k == 7, [("hT", i), ("w", s2)], [("pf", b2)])
            act(szy[:, i, :], pp[:, 0:256], AF.Silu, [("pf", b2)], [("szy", i)])
        for r in range(4):
            def fin(i, Ob, okey, r=r):
                ri = (i + 4 * r) % 8
                rd = rden[ri]
                TS(rd[:], Ob[:, 64:65], esink[:, 4 * g + r:4 * g + r + 1], None, ALU.add, None, [okey, "esink"], [("rden", ri)])
                RCP(rd[:], rd[:], [("rden", ri)], [("rden", ri)])
                dst = szy[:, i, r * 64:(r + 1) * 64]
                STT(dst, Ob[:, 0:64], rd[:], dst, ALU.mult, ALU.mult, [okey, ("rden", ri), ("szy", i)], [("szy", i)])

            attention(QT[:, r, :], lambda kt: KT[:, kt * 128:(kt + 1) * 128], 128, lambda kt: Vt[:, kt, 0:65], 65,
                      plan_band(1), fin, lambda qc: [("QT", 4 * qc + u) for u in range(4)], lambda kt: [("KT", kt)],
                      lambda kt: [("V", kt)], ptb, "d")
        outproj_half(szy, yT, store=(g == 1 and stop is None))

    if 0 in layers:
        layer0()
    if 1 in layers:
        layer1()

    if not (1 in layers and stop is None):
        for i in range(NT):
            dma("sp", yv[:, i, :], x_sb[:, i, :], "out", reads=[("x", i)])
    P.finish("sp", list(P.chan_count.keys()))
    P.emit()
    es.close()
    return nc


_CACHE = {}


def kernel(**inputs):
    consts = _consts()
    shared = {}
    shared["norm_g"] = np.ascontiguousarray(inputs["norm_g"], dtype=np.float32)
    shared["w_out"] = np.ascontiguousarray(inputs["w_out"], dtype=np.float32)
    shared["e_w_in"] = np.ascontiguousarray(inputs["e_w_in"][0], dtype=np.float32)
    shared["a_conv_w"] = np.ascontiguousarray(inputs["a_conv_w"][0], dtype=np.float32)
    shared["a_conv_b"] = np.ascontiguousarray(inputs["a_conv_b"][0], dtype=np.float32)
    shared["a_ln_g"] = np.ascontiguousarray(inputs["a_ln_g"][0], dtype=np.float32)
    shared["a_ln_b"] = np.ascontiguousarray(inputs["a_ln_b"][0], dtype=np.float32)
    shared["b_qnorm_g"] = np.ascontiguousarray(inputs["b_qnorm_g"], dtype=np.float32)
    shared["b_knorm_g"] = np.ascontiguousarray(inputs["b_knorm_g"], dtype=np.float32)
    for k in ("ident", "tri", "up", "qal", "kal_moba", "kal_slc", "kal_plain", "kal_cmp", "cmaskneg", "ovl", "impc"):
        shared[k] = consts[k]
    shared["o_w_in"] = np.ascontiguousarray(inputs["o_w_in"][0], dtype=np.float32)
    for k in ("c_qnorm_g", "c_knorm_cmp_g", "c_knorm_slc_g", "c_knorm_win_g", "c_k_b2", "c_v_b2", "d_qnorm_g", "d_knorm_g", "d_sinks"):
        shared[k] = np.ascontiguousarray(inputs[k], dtype=np.float32).reshape(1, -1)
    for k in ("c_pos_k", "c_pos_v", "c_k_w1", "c_k_b1", "c_k_w2", "c_v_w1", "c_v_b1", "c_v_w2"):
        shared[k] = np.ascontiguousarray(inputs[k][0], dtype=np.float32)
    x = np.ascontiguousarray(inputs["x"], dtype=np.float32)
    nb = x.shape[0]
    layers = inputs.get("_layers", (0, 1))
    nc = build_program(layers, inputs.get("_stop"))
    in_maps = [dict(shared, x=x[b]) for b in range(nb)]
    res = run_bass_kernel_spmd(nc, in_maps, core_ids=list(range(nb)))
    return np.stack([np.asarray(r["y"], dtype=np.float32) for r in res.results], axis=0)
```

```python
import contextlib
import os
import numpy as np
import ml_dtypes
import concourse.bass as bass
import concourse.mybir as mybir
from concourse.bass_utils import run_bass_kernel_spmd

F32 = mybir.dt.float32
BF16 = mybir.dt.bfloat16
ALU = mybir.AluOpType
AF = mybir.ActivationFunctionType
AX = mybir.AxisListType

S = 2048
D = 1024
NT = 16
NEGM = -30000.0
EPS = 1e-6


class _Op:
    __slots__ = ("eng", "fn", "deps", "chan", "signal", "val", "dmaval", "chanseq")

    def __init__(self, eng, fn, chan):
        self.eng = eng
        self.fn = fn
        self.deps = {}
        self.chan = chan
        self.signal = False
        self.val = 0
        self.dmaval = None


class Prog:
    ENGS = ("pe", "act", "dve", "pool", "sp")

    def __init__(self, nc):
        self.nc = nc
        self.ops = {e: [] for e in self.ENGS}
        self.res = {}
        self.chan_count = {}
        self.chan_last = {}
        self.all_ops = []
        self.bar = {}
        self.final_waits = {}
        self.pool_ctr = 0
        self.sp_ctr = 0
        self.const_keys = []
        self.pres = {}

    PERS = ("w", "wo")

    def _st(self, k):
        if isinstance(k, tuple) and k[0] in self.PERS or k in self.PERS:
            return self.pres
        return self.res

    def op(self, eng, fn, reads=(), writes=(), chan=None, nobar=False):
        for w in writes:
            if isinstance(w, tuple) and w[0] == "ck" and w not in self.const_keys:
                self.const_keys.append(w)
        if "const" in reads:
            reads = [k for r in reads for k in (self.const_keys if r == "const" else (r,))]
        if chan is not None and eng == "pool":
            chan = "pq%d" % (self.pool_ctr % 12)
            self.pool_ctr += 1
        elif chan == "c":
            chan = "pc%d" % (self.sp_ctr % 16)
            self.sp_ctr += 1
        o = _Op(eng, fn, chan)
        deps = o.deps
        if not nobar:
            deps.update(self.bar)
        if chan is not None and (chan.startswith("pq") or chan.startswith("pc")):
            prev = self.chan_last.get(chan)
            if prev is not None:
                deps[id(prev)] = prev
        for k in reads:
            st = self._st(k).get(k)
            if st is not None and st[0] is not None:
                deps[id(st[0])] = st[0]
        for k in writes:
            st = self._st(k).get(k)
            if st is not None:
                if st[0] is not None:
                    deps[id(st[0])] = st[0]
                for r in st[1]:
                    deps[id(r)] = r
        for k in reads:
            res = self._st(k)
            st = res.get(k)
            if st is None:
                st = [None, []]
                res[k] = st
            st[1].append(o)
        for k in writes:
            self._st(k)[k] = [o, []]
        o.dmaval = dict(self.chan_count)
        if chan is not None:
            self.chan_count[chan] = self.chan_count.get(chan, 0) + 1
            o.chanseq = self.chan_count[chan]
            self.chan_last[chan] = o
            o.signal = True
        self.ops[eng].append(o)
        self.all_ops.append(o)
        return o

    def barrier(self):
        bar = {}
        for e in self.ENGS:
            if self.ops[e]:
                o = self.ops[e][-1]
                bar[id(o)] = o
        for ch, o in self.chan_last.items():
            bar[id(o)] = o
        self.bar = bar
        self.res = {}

    def finish(self, eng, chans):
        self.final_waits = {eng: {ch: self.chan_count[ch] for ch in chans}}

    def emit(self):
        nc = self.nc
        for o in self.all_ops:
            for d in o.deps.values():
                if d.chan is None:
                    if d.eng == "pe" and o.eng == "pe":
                        continue
                    d.signal = True
        for e in self.ENGS:
            c = 0
            for o in self.ops[e]:
                if o.chan is None and o.signal:
                    c += 1
                    o.val = c
        import os
        if os.environ.get("KDBG"):
            print("sem counts", {e: max([o.val for o in self.ops[e]] + [0]) for e in self.ENGS}, {e: len(self.ops[e]) for e in self.ENGS},
                  {c: 16 * v for c, v in self.chan_count.items()})
        stack = contextlib.ExitStack()
        sems = {}
        for e in self.ENGS:
            sems[e] = stack.enter_context(nc.semaphore("s_" + e))
        for ch in self.chan_count:
            sems["c_" + ch] = stack.enter_context(nc.semaphore("c_" + ch))
        block = stack.enter_context(nc.Block())
        engobj = {"pe": "tensor", "act": "scalar", "dve": "vector", "pool": "gpsimd", "sp": "sync"}

        def make(e):
            def body(eng):
                waited = {}
                for o in self.ops[e]:
                    need = {}
                    for d in o.deps.values():
                        if d.chan is not None:
                            k = "c_" + d.chan
                            if d.chan.startswith("pq") or d.chan.startswith("pc"):
                                v = 16 * d.chanseq
                            else:
                                v = 16 * o.dmaval[d.chan]
                        else:
                            if d.eng == "pe" and e == "pe":
                                continue
                            k = d.eng
                            v = d.val
                        if v > need.get(k, 0):
                            need[k] = v
                    for k, v in need.items():
                        if waited.get(k, 0) >= v:
                            continue
                        eng.wait_ge(sems[k], v)
                        waited[k] = v
                    ins = o.fn(eng)
                    if o.chan is not None:
                        ins.then_inc(sems["c_" + o.chan], 16)
                    elif o.signal:
                        ins.then_inc(sems[e], 1)
                for ch, c in self.final_waits.get(e, {}).items():
                    eng.wait_ge(sems["c_" + ch], 16 * c)
            return body

        for e in self.ENGS:
            getattr(block, engobj[e])(make(e))
        stack.close()


def _consts():
    c = {}
    c["ident"] = np.eye(128, dtype=np.float32)
    k = np.arange(128)[:, None]
    q = np.arange(128)[None, :]
    c["tri"] = np.where(k <= q, 0.0, NEGM).astype(np.float32)
    c["up"] = np.where(k > q, 0.0, NEGM).astype(np.float32)
    t = np.arange(S)
    b = (t % 16).astype(np.float32)
    a = (t - t % 16).astype(np.float32)
    slopes = np.power(2.0, -8.0 * np.arange(1, 9) / 8).astype(np.float32)
    qal = np.zeros((S, 8, 4), np.float32)
    qal[:, :, 0] = -slopes[None, :] * a[:, None]
    qal[:, :, 1] = -slopes[None, :] * b[:, None]
    qal[:, :, 2] = slopes[None, :]
    qal[:, :, 3] = slopes[None, :]
    c["qal"] = qal.reshape(NT, 128, 32).transpose(1, 0, 2).copy()

    def kal(onehot_block):
        m = np.zeros((S, 36), np.float32)
        if onehot_block:
            m[t, t // onehot_block] = 1.0
        m[:, 32] = 1.0
        m[:, 33] = 1.0
        m[:, 34] = a
        m[:, 35] = b
        return m.reshape(NT, 128, 36).transpose(1, 0, 2).copy()

    c["kal_moba"] = kal(256)
    c["kal_slc"] = kal(64)
    c["kal_plain"] = kal(0)
    cc = np.arange(128)
    cend = 16 * cc + 31
    kc = np.zeros((128, 36), np.float32)
    kc[:, 32] = 1.0
    kc[:, 33] = 1.0
    kc[:, 34] = cend - cend % 16
    kc[:, 35] = cend % 16
    c["kal_cmp"] = kc
    c["cmaskneg"] = np.where(t[None, :] >= cend[:, None], 0.0, NEGM).astype(np.float32)
    start = np.arange(127)[:, None] * 16
    bs = np.arange(32)[None, :] * 64
    ov = np.zeros((128, 32), np.float32)
    ov[:127] = ((start < bs + 64) & (start + 32 > bs)).astype(np.float32)
    c["ovl"] = ov
    blk = np.arange(32)[None, :]
    cur = (t // 64)[:, None]
    forced = (blk == 0) | (blk == cur) | (blk == cur - 1)
    impc = 1e4 * forced.astype(np.float32) - 1e5 * (blk > cur).astype(np.float32)
    c["impc"] = impc.reshape(NT, 128, 32).transpose(1, 0, 2).copy()
    return c


def build_program(layers=(0, 1), stop=None):
    nc = bass.Bass("TRN2", target_bir_lowering=False)
    es = contextlib.ExitStack()
    dram = {}

    def din(name, shape, dt=F32):
        dram[name] = nc.dram_tensor(name, list(shape), dt, kind="ExternalInput").ap()
        return dram[name]

    x_d = din("x", [S, D])
    normg_d = din("norm_g", [2, D])
    wout_d = din("w_out", [2, D, D])
    ewin_d = din("e_w_in", [D, 3584])
    convw_d = din("a_conv_w", [31, 512])
    convb_d = din("a_conv_b", [512])
    lng_d = din("a_ln_g", [512])
    lnb_d = din("a_ln_b", [512])
    bq_d = din("b_qnorm_g", [1, 64])
    bk_d = din("b_knorm_g", [1, 64])
    ident_d = din("ident", [128, 128])
    tri_d = din("tri", [128, 128])
    up_d = din("up", [128, 128])
    qal_d = din("qal", [128, NT, 32])
    kalm_d = din("kal_moba", [128, NT, 36])
    kals_d = din("kal_slc", [128, NT, 36])
    kalp_d = din("kal_plain", [128, NT, 36])
    kalc_d = din("kal_cmp", [128, 36])
    cmn_d = din("cmaskneg", [128, S])
    ovl_d = din("ovl", [128, 32])
    impc_d = din("impc", [128, NT, 32])
    owin_d = din("o_w_in", [D, 3096])
    cq_d = din("c_qnorm_g", [1, 64])
    ckc_d = din("c_knorm_cmp_g", [1, 64])
    cks_d = din("c_knorm_slc_g", [1, 64])
    ckw_d = din("c_knorm_win_g", [1, 64])
    cposk_d = din("c_pos_k", [32, 64])
    cposv_d = din("c_pos_v", [32, 64])
    ckw1_d = din("c_k_w1", [2048, 256])
    ckb1_d = din("c_k_b1", [256])
    ckw2_d = din("c_k_w2", [256, 64])
    ckb2_d = din("c_k_b2", [1, 64])
    cvw1_d = din("c_v_w1", [2048, 256])
    cvb1_d = din("c_v_b1", [256])
    cvw2_d = din("c_v_w2", [256, 64])
    cvb2_d = din("c_v_b2", [1, 64])
    dq_d = din("d_qnorm_g", [1, 64])
    dk_d = din("d_knorm_g", [1, 64])
    dsink_d = din("d_sinks", [1, 8])
    y_d = nc.dram_tensor("y", [S, D], F32, kind="ExternalOutput").ap()

    def sb(name, shape, dt):
        return es.enter_context(nc.sbuf_tensor(name, list(shape), dt))

    def psum(name, shape, dt):
        return es.enter_context(nc.psum_tensor(name, list(shape), dt))

    P = Prog(nc)

    x_sb = sb("x_sb", [128, NT, D], F32)
    hT = sb("hT", [128, 8, S], BF16)
    wbuf = [sb("wbuf%d" % i, [128, 8, 512], BF16) for i in range(2)]
    wo = sb("wo", [128, 4, D], BF16)
    ident = sb("ident_sb", [128, 128], BF16)
    identf = sb("identf_sb", [128, 128], F32)
    onesf = sb("onesf", [128, 128], F32)
    tri = sb("tri_sb", [128, 128], BF16)
    upm = sb("up_sb", [128, 128], BF16)
    qal = sb("qal_sb", [128, NT, 32], BF16)
    kalm = sb("kalm_sb", [128, NT, 36], BF16)
    KTc = sb("KTc", [128, 2, 128], BF16)
    Vca = sb("Vca", [128, 2, 98], BF16)
    kals = sb("kals_sb", [128, NT, 36], BF16)
    kalp = sb("kalp_sb", [128, NT, 36], BF16)
    ss = sb("ss", [128, NT], F32)
    rstd = sb("rstd", [128, NT], F32)
    ARENA = 80 * 1024
    arena = sb("arena", [128, ARENA // 2], BF16)

    pf = [psum("pf%d" % i, [128, 512], F32) for i in range(6)]
    pb = [psum("pb%d" % i, [128, 1024], BF16) for i in range(2)]

    class Carver:
        def __init__(self, base=0):
            self.off = base

        def get(self, shape, dt):
            n = int(np.prod(shape[1:]))
            nb = n * (4 if dt == F32 else 2)
            nb = (nb + 63) // 64 * 64
            assert self.off + nb <= ARENA, ("arena overflow", self.off + nb, ARENA)
            a = arena[:, self.off // 2:(self.off + nb) // 2]
            self.off += nb
            if os.environ.get("KDBG"):
                print("carve", shape, dt, "->", self.off)
            if dt == F32:
                a = a.bitcast(F32)
            a = a[:, 0:n]
            if len(shape) == 3:
                a = a.rearrange("p (a b) -> p a b", b=shape[2])
            elif len(shape) == 4:
                a = a.rearrange("p (a b c) -> p a b c", b=shape[2], c=shape[3])
            if shape[0] < 128:
                a = a[0:shape[0]]
            return a

    def dma(eng, out, in_, chan, reads=(), writes=(), slow=False, nobar=False):
        if slow:
            P.op(eng, lambda e: e.dma_start(out=out, in_=in_, allow_slow_non_contiguous=True), reads=reads, writes=writes, chan=chan, nobar=nobar)
        else:
            P.op(eng, lambda e: e.dma_start(out=out, in_=in_), reads=reads, writes=writes, chan=chan, nobar=nobar)

    def mm(out, lhsT, rhs, start, stop, reads, writes):
        P.op("pe", lambda e: e.matmul(out, lhsT=lhsT, rhs=rhs, start=start, stop=stop), reads=reads, writes=writes)

    def tr(out, in_, reads, writes, idn=None):
        ck = ("ck", "ident") if idn is None else ("ck", "identf")
        idn = ident[:] if idn is None else idn
        P.op("pe", lambda e: e.transpose(out=out, in_=in_, identity=idn), reads=list(reads) + [ck], writes=writes)

    def act(out, in_, func, reads, writes, **kw):
        P.op("act", lambda e: e.activation(out=out, in_=in_, func=func, **kw), reads=reads, writes=writes)

    def V(fn, reads, writes, eng="dve"):
        P.op(eng, fn, reads=reads, writes=writes)

    def TT(out, in0, in1, op, reads, writes, eng="dve"):
        P.op(eng, lambda e: e.tensor_tensor(out=out, in0=in0, in1=in1, op=op), reads=reads, writes=writes)

    def TS(out, in0, s1, s2, op0, op1, reads, writes, eng="dve"):
        if op1 is None:
            P.op(eng, lambda e: e.tensor_scalar(out=out, in0=in0, scalar1=s1, scalar2=None, op0=op0), reads=reads, writes=writes)
        else:
            P.op(eng, lambda e: e.tensor_scalar(out=out, in0=in0, scalar1=s1, scalar2=s2, op0=op0, op1=op1), reads=reads, writes=writes)

    def STT(out, in0, scalar, in1, op0, op1, reads, writes, eng="dve"):
        P.op(eng, lambda e: e.scalar_tensor_tensor(out=out, in0=in0, scalar=scalar, in1=in1, op0=op0, op1=op1), reads=reads, writes=writes)

    def RED(out, in_, op, reads, writes):
        P.op("dve", lambda e: e.tensor_reduce(out=out, in_=in_, axis=AX.X, op=op), reads=reads, writes=writes)

    def RCP(out, in_, reads, writes):
        P.op("dve", lambda e: e.reciprocal(out=out, in_=in_), reads=reads, writes=writes)

    def CPY(out, in_, reads, writes, eng="dve"):
        P.op(eng, lambda e: e.tensor_copy(out=out, in_=in_), reads=reads, writes=writes)

    def MSET(ap, val, writes, eng="dve"):
        P.op(eng, lambda e: e.memset(ap, val), reads=[], writes=writes)

    import os
    KB = os.environ.get("KBIS", "abcdefg")
    if "a" in KB:
        dma("pool", ident[:], ident_d, "c", writes=[("ck", "ident")])
    if "b" in KB:
        dma("pool", tri[:], tri_d, "c", writes=[("ck", "tri")])
        dma("pool", upm[:], up_d, "c", writes=[("ck", "up")])
    if "c" in KB:
        dma("pool", qal[:], qal_d, "c", writes=[("ck", "qal")])
    if "d" in KB:
        dma("pool", kalm[:], kalm_d, "c", writes=[("ck", "kalm")])
        dma("pool", kals[:], kals_d, "c", writes=[("ck", "kals")])
        dma("pool", kalp[:], kalp_d, "c", writes=[("ck", "kalp")])
    if "e" in KB:
        dma("sp", identf[:], ident_d, "c", writes=[("ck", "identf")])
    if "f" in KB:
        MSET(onesf[:], 1.0, [("ck", "onesf")])

    xv = x_d.rearrange("(i p) d -> p i d", p=128)
    yv = y_d.rearrange("(i p) d -> p i d", p=128)
    for i in range(NT):
        dma("sp", x_sb[:, i, :], xv[:, i, :], "x%d" % i, writes=[("x", i)])

    wslot_ctr = [0]

    def load_w(wd, segs):
        s = wslot_ctr[0] % 2
        wslot_ctr[0] += 1
        wv = wd.rearrange("(k p) n -> p k n", p=128)
        o = 0
        for (c0, n) in segs:
            dma("pool", wbuf[s][:, :, o:o + n], wv[:, :, c0:c0 + n], "w%d" % s, writes=[("w", s)], nobar=True)
            o += n
        return s

    def norm_phase(layer, base=0, barrier=True):
        cvn = Carver(base=base)
        gbc = cvn.get([128, D], F32)
        junk = cvn.get([128, D], BF16)
        hb = [cvn.get([128, D], BF16) for _ in range(2)]
        dma("act", gbc[:], normg_d[layer:layer + 1, :].partition_broadcast(128), "c", writes=["gbc"])
        MSET(ss[:], 0.0, [("ss", gI) for gI in range(4)])

        def stats(gI):
            for i in range(4 * gI, 4 * gI + 4):
                act(junk[:], x_sb[:, i, :], AF.Square, [("x", i), ("ss", gI)], ["junk", ("ss", gI)], accum_out=ss[:, i:i + 1])
            sl = slice(4 * gI, 4 * gI + 4)
            TS(rstd[:, sl], ss[:, sl], 1.0 / D, EPS, ALU.mult, ALU.add, [("ss", gI)], [("rstd", gI)])
            act(rstd[:, sl], rstd[:, sl], AF.Sqrt, [("rstd", gI)], [("rstd", gI)])
            RCP(rstd[:, sl], rstd[:, sl], [("rstd", gI)], [("rstd", gI)])

        def na(i):
            h = hb[i % 2]
            STT(h[:], x_sb[:, i, :], rstd[:, i:i + 1], gbc[:], ALU.mult, ALU.mult, [("x", i), ("rstd", i // 4), "gbc"], [("hb", i % 2)])

        def nb(i):
            h = hb[i % 2]
            pt = pb[i % 2]
            for k in range(8):
                tr(pt[:, k * 128:(k + 1) * 128], h[:, k * 128:(k + 1) * 128], [("hb", i % 2)], [("pb", i % 2)])
            act(hT[:, :, i * 128:(i + 1) * 128], pt[:].rearrange("p (k t) -> p k t", k=8), AF.Copy, [("pb", i % 2)], [("hT", i)])

        pend = []
        for gI in range(4):
            stats(gI)
            for i in range(4 * gI, 4 * gI + 4):
                na(i)
                pend.append(i)
                if len(pend) > 1:
                    nb(pend.pop(0))
        nb(pend.pop(0))
        if barrier:
            P.barrier()

    def load_wo(layer, c0, n):
        wv = wout_d[layer].rearrange("(k p) n -> p k n", p=128)
        for hf in range(2):
            dma("pool", wo[:, 0:n, hf * 512:(hf + 1) * 512], wv[:, c0:c0 + n, hf * 512:(hf + 1) * 512], "wo", writes=["wo"], nobar=True)

    def outproj_tile(i, lhs_list, chunks, yT_reads, pbanks):
        for half in range(2):
            pp = pf[pbanks[half]]
            for n, (c, lhs) in enumerate(zip(chunks, lhs_list)):
                mm(pp[:], lhs, wo[:, c, half * 512:(half + 1) * 512], n == 0, n == len(chunks) - 1,
                   list(yT_reads) + ["wo"], [("pf", pbanks[half])])
            xs = x_sb[:, i, half * 512:(half + 1) * 512]
            TT(xs, xs, pp[:], ALU.add, [("pf", pbanks[half]), ("x", i)], [("x", i)])

    att_ctr = [0, 0]

    def lagged(n, stage_a, stage_b, lag=1):
        for i in range(n + lag):
            if i < n:
                stage_a(i)
            if i >= lag:
                stage_b(i - lag)

    def mm_acc(out, lhsT, rhs, start, stop, reads, writes):
        P.op("pe", lambda e: e.matmul(out, lhsT=lhsT, rhs=rhs, start=start, stop=stop, skip_group_check=True), reads=reads, writes=writes)

    def attention(QT, KT, nk, Vrhs, ncv, plan, finish, qkeys, kkeys, vkeys, ptb, tagk, addmask=None):
        NS = len(ptb)
        work = []
        for qc in range(4):
            items = plan(qc)
            if not items:
                continue
            ob = 3 + att_ctr[1] % 3
            att_ctr[1] += 1
            lastk = {}
            for (kt, jlo, jhi, masks) in items:
                for j in range(jlo, jhi + 1):
                    lastk[j] = kt
            for n, it in enumerate(items):
                work.append((qc, ob, it, n == 0, n == len(items) - 1, lastk))

        def front(w):
            qc, ob, (kt, jlo, jhi, masks), first, last, lastk = w
            sbk = att_ctr[0] % NS
            att_ctr[0] += 1
            Sp = pf[sbk]
            pt = ptb[sbk]
            c0, c1 = jlo * 128, (jhi + 1) * 128
            extra = list(masks.items())
            mm_acc(Sp[0:nk, c0:c1], KT(kt), QT[:, qc * 512 + c0:qc * 512 + c1], True, addmask is None and not extra,
                   list(qkeys(qc)) + list(kkeys(kt)), [("pf", sbk)])
            if addmask is not None:
                mm_acc(Sp[0:nk, c0:c1], ident[0:nk, 0:nk], addmask[0:nk, qc * 512 + c0:qc * 512 + c1], False, not extra,
                       ["const", "addmask"], [("pf", sbk)])
            for n, (j, mk) in enumerate(extra):
                mm_acc(Sp[0:nk, j * 128:(j + 1) * 128], ident[0:nk, 0:nk], mk[0:nk, :], False, n == len(extra) - 1,
                       ["const"], [("pf", sbk)])
            act(pt[0:nk, c0:c1], Sp[0:nk, c0:c1], AF.Exp, [("pf", sbk)], [("pt", tagk, sbk)])
            return sbk

        def back(w, sbk, started):
            qc, ob, (kt, jlo, jhi, masks), first, last, lastk = w
            pt = ptb[sbk]
            for j in range(jlo, jhi + 1):
                mm_acc(pf[ob][:, j * 128:j * 128 + ncv], pt[0:nk, j * 128:(j + 1) * 128], Vrhs(kt), len(started) == 0, lastk[j] == kt,
                       [("pt", tagk, sbk)] + list(vkeys(kt)), [("pf", ob)])
                started.add(j)
            if last:
                for j in sorted(started):
                    finish(qc * 4 + j, pf[ob][:, j * 128:j * 128 + ncv], ("pf", ob))
                started.clear()

        LAG = NS - 1
        started = set()
        pend = []
        for w in work:
            pend.append((w, front(w)))
            if len(pend) > LAG:
                pw, psb = pend.pop(0)
                back(pw, psb, started)
        for (pw, psb) in pend:
            back(pw, psb, started)

    def layer0():
        if stop == "load":
            return
        norm_phase(0, base=70 * 1024, barrier=False)
        if stop == "norm0":
            return
        load_wo(0, 0, 4)
        W = ewin_d
        if stop == "norm":
            return
        cv = Carver()
        yc = cv.get([128, 4, S], F32)
        hc = cv.get([128, S + 32], BF16)
        dg = cv.get([128, 31, 128], BF16)
        cw = cv.get([128, 4, 31], F32)
        cb = cv.get([128, 4], F32)
        lg = cv.get([128, 4], F32)
        lb = cv.get([128, 4], F32)
        Tsq = [cv.get([128, 512], F32) for _ in range(2)]
        mu2 = [cv.get([128, 512], F32) for _ in range(2)]
        msq = cv.get([128, 512], F32)
        rs2 = [cv.get([128, 512], F32) for _ in range(2)]
        sz4 = [cv.get([128, 512], BF16) for _ in range(4)]
        T4 = [cv.get([128, 512], F32) for _ in range(4)]
        yaT2 = [cv.get([128, 4, 512], BF16) for _ in range(2)]
        HO = 32
        stg = Tsq[0][0:32, :]
        stg2 = Tsq[1][0:12, 0:128]
        dma("sp", stg[0:31, :], convw_d, "c", writes=[("Tsq", 0)])
        dma("sp", stg2[0:4, :], convb_d.rearrange("(a c) -> a c", c=128), "c", writes=[("Tsq", 1)])
        dma("sp", stg2[4:8, :], lng_d.rearrange("(a c) -> a c", c=128), "c", writes=[("Tsq", 1)])
        dma("sp", stg2[8:12, :], lnb_d.rearrange("(a c) -> a c", c=128), "c", writes=[("Tsq", 1)])
        for cc in range(4):
            tr(pf[0][:, cc * 32:cc * 32 + 31], stg[0:31, cc * 128:(cc + 1) * 128], [("Tsq", 0)], [("pf", 0)], idn=identf[0:31, 0:31])
        tr(pf[0][:, 128:140], stg2[0:12, :], [("Tsq", 1)], [("pf", 0)], idn=identf[0:12, 0:12])
        CPY(cw[:], pf[0][:, 0:128].rearrange("p (a j) -> p a j", j=32)[:, :, 0:31], [("pf", 0)], ["cw"])
        CPY(cb[:], pf[0][:, 128:132], [("pf", 0)], ["cw"])
        CPY(lg[:], pf[0][:, 132:136], [("pf", 0)], ["cw"])
        CPY(lb[:], pf[0][:, 136:140], [("pf", 0)], ["cw"])
        MSET(hc[:, 0:HO], 0.0, ["hc0"])
        if stop == "convp":
            return
        for cc in range(4):
            if stop == "conv1" and cc == 1:
                return
            s = load_w(W, [(cc * 128, 128), (512 + cc * 128, 128)])
            for j in range(31):
                TS(dg[:, j, :], ident[:], cw[:, cc, j:j + 1], None, ALU.mult, None, ["const", "cw"], [("dg", j)])
            for tq in range(4):
                bv, bg = (tq % 2) * 2, (tq % 2) * 2 + 1
                for which, bk in ((0, bv), (1, bg)):
                    for k in range(8):
                        mm(pf[bk][:], wbuf[s][:, k, which * 128:(which + 1) * 128], hT[:, k, tq * 512:(tq + 1) * 512], k == 0, k == 7,
                           [("w", s)] + [("hT", 4 * tq + u) for u in range(4)], [("pf", bk)])
                act(T4[tq % 2][:], pf[bg][:], AF.Sigmoid, [("pf", bg)], [("T4", tq % 2)])
                TT(hc[:, HO + tq * 512:HO + (tq + 1) * 512], pf[bv][:], T4[tq % 2][:], ALU.mult, [("pf", bv), ("T4", tq % 2)], [("hc", tq)])
            for tq in range(4):
                pc = pf[4 + tq % 2]
                for j in range(31):
                    o = HO - 30 + j + tq * 512
                    mm(pc[:], dg[:, j, :], hc[:, o:o + 512], j == 0, j == 30,
                       [("dg", j), ("hc", tq)] + ([("hc", tq - 1)] if tq > 0 else ["hc0"]), [("pf", 4 + tq % 2)])
                act(yc[:, cc, tq * 512:(tq + 1) * 512], pc[:], AF.Identity, [("pf", 4 + tq % 2), "cw"], [("yc", cc, tq)],
                    bias=cb[:, cc:cc + 1], scale=1.0)
        if stop == "conv4":
            return
        P.barrier()
        sz_slot = load_w(W, [(1024, 512)])

        def st_a(tq):
            ts = slice(tq * 512, (tq + 1) * 512)
            p2 = tq % 2
            for cc in range(4):
                mm(pf[0][:], onesf[:], yc[:, cc, ts], cc == 0, cc == 3, [("yc", cc, tq), "const"], [("pf", 0)])
            for cc in range(4):
                act(Tsq[cc % 2][:], yc[:, cc, ts], AF.Square, [("yc", cc, tq)], [("Tsq", cc % 2)])
                mm(pf[1][:], onesf[:], Tsq[cc % 2][:], cc == 0, cc == 3, [("Tsq", cc % 2), "const"], [("pf", 1)])
            TS(mu2[p2][:], pf[0][:], 1.0 / 512, None, ALU.mult, None, [("pf", 0)], [("mu", p2)])
            TT(msq[:], mu2[p2][:], mu2[p2][:], ALU.mult, [("mu", p2)], ["msq"])
            STT(rs2[p2][:], pf[1][:], 1.0 / 512, msq[:], ALU.mult, ALU.subtract, [("pf", 1), "msq"], [("rs", p2)])
            TS(rs2[p2][:], rs2[p2][:], EPS, None, ALU.add, None, [("rs", p2)], [("rs", p2)])
            act(rs2[p2][:], rs2[p2][:], AF.Sqrt, [("rs", p2)], [("rs", p2)])
            RCP(rs2[p2][:], rs2[p2][:], [("rs", p2)], [("rs", p2)])

        def st_b(tq):
            ts = slice(tq * 512, (tq + 1) * 512)
            p2 = tq % 2
            for cc in range(4):
                pz = pf[2 + cc % 2]
                for k in range(8):
                    mm(pz[:], wbuf[sz_slot][:, k, cc * 128:(cc + 1) * 128], hT[:, k, ts], k == 0, k == 7,
                       [("w", sz_slot)] + [("hT", 4 * tq + u) for u in range(4)], [("pf", 2 + cc % 2)])
                act(sz4[cc][:], pz[:], AF.Silu, [("pf", 2 + cc % 2)], [("sz", cc)])
            for cc in range(4):
                tt = T4[cc]
                TT(tt[:], yc[:, cc, ts], mu2[p2][:], ALU.subtract, [("yc", cc, tq), ("mu", p2)], [("T4", cc)])
                TT(tt[:], tt[:], rs2[p2][:], ALU.mult, [("T4", cc), ("rs", p2)], [("T4", cc)])
            for cc in range(4):
                tt = T4[cc]
                act(tt[:], tt[:], AF.Silu, [("T4", cc), "cw"], [("T4", cc)], scale=lg[:, cc:cc + 1], bias=lb[:, cc:cc + 1])
            for cc in range(4):
                TT(yaT2[p2][:, cc, :], T4[cc][:], sz4[cc][:], ALU.mult, [("T4", cc), ("sz", cc)], [("yaT", p2, cc)])

        def st_c(tq):
            p2 = tq % 2
            for u in range(4):
                i = 4 * tq + u
                outproj_tile(i, [yaT2[p2][:, cc, u * 128:(u + 1) * 128] for cc in range(4)], [0, 1, 2, 3],
                             [("yaT", p2, cc) for cc in range(4)], (4, 5))

        for step in range(6):
            if step < 4:
                st_a(step)
            if 0 <= step - 1 < 4:
                st_b(step - 1)
            if step - 2 >= 0:
                st_c(step - 2)
        P.barrier()
        if stop == "conv":
            return
        for hh in range(2):
            moba_half(hh, W)
            P.barrier()
            if stop is not None:
                return

    def moba_half(hh, W):
        cv = Carver()
        QT = cv.get([128, 4, S], BF16)
        KT = cv.get([128, 4, S], BF16)
        Vt = cv.get([128, NT, 4, 66], BF16)
        szy = cv.get([128, NT, 256], BF16)
        ptb = [cv.get([128, 512], BF16) for _ in range(3)]
        gq = cv.get([128, 1], F32)
        gk = cv.get([128, 1], F32)
        kmf = cv.get([128, 4, 8], F32)
        kmb = cv.get([128, 4, 8], BF16)
        gs = cv.get([128, 4, 8], F32)
        cmp_ = cv.get([128, 4, 8, 8], F32)
        rank = cv.get([128, 4, 8], F32)
        nm = cv.get([128, 4, 32], BF16)
        rden = [cv.get([128, 1], F32) for _ in range(8)]
        yT = [cv.get([128, 2, 128], BF16) for _ in range(2)]
        tmpf = [cv.get([128, 512], F32) for _ in range(2)]
        s8 = [cv.get([128, 8], F32) for _ in range(3)]
        qa = [cv.get([128, 4, 128], BF16) for _ in range(3)]
        ka = [cv.get([128, 4, 128], BF16) for _ in range(3)]
        c_q = 1536 + hh * 256
        c_k = 2048 + hh * 256
        c_v = 2560 + hh * 256
        c_z = 3072 + hh * 256
        s = load_w(W, [(c_q, 256), (c_k, 256)])
        s2 = load_w(W, [(c_v, 256), (c_z, 256)])
        load_wo(0, 4 + 2 * hh, 2)
        load_gain_col(gq, bq_d, 1.0)
        load_gain_col(gk, bk_d, 8.0)
        for b in range(3):
            MSET(qa[b][:], 0.0, [("qa", b)])
            MSET(ka[b][:], 0.0, [("ka", b)])
        MSET(Vt[:], 1.0, ["Vt"])
        MSET(nm[:], 0.0, ["nm"])

        def m0(i):
            r = i % 3
            CPY(qa[r][:, :, 96:100], qal[:, i, hh * 16:hh * 16 + 16].rearrange("p (h c) -> p h c", c=4), ["const"], [("qa", r)])
            CPY(ka[r][:, :, 64:100], kalm[:, i, :].unsqueeze(1).to_broadcast([128, 4, 36]), ["const"], [("ka", r)])
            for k in range(8):
                mm(pf[r][:], hT[:, k, i * 128:(i + 1) * 128], wbuf[s][:, k, :], k == 0, k == 7, [("hT", i), ("w", s)], [("pf", r)])

        def m1(i):
            r = i % 3
            rstd_a(pf[r], ("pf", r), 512, tmpf[i % 2], ("tf", i % 2), s8[r], ("s8", r))

        def m2(i):
            r = i % 3
            rstd_b(512, s8[r], ("s8", r))
            TT(qa[r][:, :, 0:64], pf[r][:, 0:256].rearrange("p (h d) -> p h d", d=64), s8[r][:, 0:4].unsqueeze(2).to_broadcast([128, 4, 64]),
               ALU.mult, [("pf", r), ("s8", r)], [("qa", r)])
            TT(ka[r][:, :, 0:64], pf[r][:, 256:512].rearrange("p (h d) -> p h d", d=64), s8[r][:, 4:8].unsqueeze(2).to_broadcast([128, 4, 64]),
               ALU.mult, [("pf", r), ("s8", r)], [("ka", r)])

        def m3(i):
            r = i % 3
            b2 = i % 2
            pt = pb[b2]
            for h in range(4):
                tr(pt[:, h * 128:(h + 1) * 128], qa[r][:, h, :], [("qa", r)], [("pb", b2)])
            for h in range(4):
                tr(pt[:, (4 + h) * 128:(5 + h) * 128], ka[r][:, h, :], [("ka", r)], [("pb", b2)])
            act(QT[:, :, i * 128:(i + 1) * 128], pt[:, 0:512].rearrange("p (h t) -> p h t", h=4), AF.Copy, [("pb", b2), "gq"], [("QT", i)],
                scale=gq[:, 0:1])
            act(KT[:, :, i * 128:(i + 1) * 128], pt[:, 512:1024].rearrange("p (h t) -> p h t", h=4), AF.Copy, [("pb", b2), "gq"], [("KT", i)],
                scale=gk[:, 0:1])

        pipe4(NT, m0, m1, m2, m3)
        if hh == 1 and 1 in layers:
            prefetch_w1([("qa", r) for r in range(3)] + [("ka", r) for r in range(3)] + [("s8", r) for r in range(3)] + [("tf", 0), ("tf", 1)])
        if stop == "m1":
            return
        for i in range(NT):
            b2 = 2 + i % 2
            pp = pf[b2]
            for k in range(8):
                mm(pp[:], hT[:, k, i * 128:(i + 1) * 128], wbuf[s2][:, k, :], k == 0, k == 7, [("hT", i), ("w", s2)], [("pf", b2)])
            act(Vt[:, i, :, 0:64], pp[:, 0:256].rearrange("p (h d) -> p h d", d=64), AF.Copy, [("pf", b2), "Vt"], [("V", i)])
            act(szy[:, i, :], pp[:, 256:512], AF.Silu, [("pf", b2)], [("szy", i)])
        if stop == "m3":
            return
        for h in range(4):
            RED(kmf[:, h, :], KT[:, h, :].rearrange("p (n t) -> p n t", t=256), ALU.add, [("KT", i) for i in range(NT)], ["kmf"])
        TS(kmb[:], kmf[:], 1.0 / 256, None, ALU.mult, None, ["kmf"], ["kmb"])
        if stop == "mk":
            return
        for i in range(8, NT):
            npast = i // 2
            b2 = 4 + i % 2
            pg = pf[b2]
            for h in range(4):
                mm(pg[:, h * 8:h * 8 + 8], QT[0:64, h, i * 128:(i + 1) * 128], kmb[0:64, h, :], True, True, [("QT", i), "kmb"], [("pf", b2)])
            CPY(gs[:], pg[:, 0:32].rearrange("p (h n) -> p h n", n=8), [("pf", b2)], ["gs"])
            TT(cmp_[:, :, 0:npast, 0:npast], gs[:, :, 0:npast].unsqueeze(2).to_broadcast([128, 4, npast, npast]),
               gs[:, :, 0:npast].unsqueeze(3).to_broadcast([128, 4, npast, npast]), ALU.is_gt, ["gs"], ["cmp"])
            RED(rank[:, :, 0:npast], cmp_[:, :, 0:npast, 0:npast], ALU.add, ["cmp"], ["rank"])
            TS(nm[:, :, 0:npast], rank[:, :, 0:npast], 2.5, NEGM, ALU.is_ge, ALU.mult, ["rank"], ["nm"])
            pt = pb[i % 2]
            tr(pt[:, 0:128], nm[:].rearrange("p h n -> p (h n)"), ["nm"], [("pb", i % 2)])
            for h in range(4):
                act(QT[64:96, h, i * 128:(i + 1) * 128], pt[h * 32:(h + 1) * 32, 0:128], AF.Copy, [("pb", i % 2)], [("QT", i)])

        if stop == "m2":
            return

        def plan(qc):
            items = []
            for kt in range(4 * qc + 4):
                if kt < 4 * qc:
                    items.append((kt, 0, 3, {}))
                else:
                    m = kt - 4 * qc
                    items.append((kt, m, 3, {m: tri[:]}))
            return items

        for h in range(4):
            def finish(i, Ob, okey, h=h):
                r = (i + 4 * h) % 8
                rd = rden[r]
                RCP(rd[:], Ob[:, 64:65], [okey], [("rden", r)])
                dst = szy[:, i, h * 64:(h + 1) * 64]
                STT(dst, Ob[:, 0:64], rd[:], dst, ALU.mult, ALU.mult, [okey, ("rden", r), ("szy", i)], [("szy", i)])

            attention(QT[:, h, :], lambda kt, h=h: KT[:, h, kt * 128:(kt + 1) * 128], 128,
                      lambda kt, h=h: Vt[:, kt, h, 0:65], 65, plan, finish,
                      lambda qc: [("QT", 4 * qc + u) for u in range(4)], lambda kt: [("KT", kt)], lambda kt: [("V", kt)],
                      ptb, "m")
        if stop == "ma":
            return
        outproj_half(szy, yT)

    def rstd_a(pp, pkey, ncol, tf, tfkey, s8i, s8key):
        nh = ncol // 64
        act(tf[:, 0:ncol], pp[:, 0:ncol], AF.Square, [pkey], [tfkey])
        RED(s8i[:, 0:nh], tf[:, 0:ncol].rearrange("p (h d) -> p h d", d=64), ALU.add, [tfkey], [s8key])
        TS(s8i[:, 0:nh], s8i[:, 0:nh], 64.0 * EPS, None, ALU.add, None, [s8key], [s8key])
        act(s8i[:, 0:nh], s8i[:, 0:nh], AF.Sqrt, [s8key], [s8key])

    def rstd_b(ncol, s8i, s8key):
        nh = ncol // 64
        RCP(s8i[:, 0:nh], s8i[:, 0:nh], [s8key], [s8key])

    def head_rstd(pp, pkey, ncol, tf, tfkey, s8i, s8key):
        rstd_a(pp, pkey, ncol, tf, tfkey, s8i, s8key)
        rstd_b(ncol, s8i, s8key)

    def pipe4(n, s0, s1, s2, s3):
        for step in range(n + 2):
            if step < n:
                s0(step)
                s1(step)
            if 1 <= step <= n:
                s2(step - 1)
            if step >= 2:
                s3(step - 2)

    def load_gain_col(dst, gd, mult):
        MSET(dst[:], 1.0, ["gq"])
        dma("sp", dst[0:64, :], gd.rearrange("o d -> d o"), "c", writes=["gq"], slow=True)
        if mult != 1.0:
            TS(dst[0:64, :], dst[0:64, :], mult, None, ALU.mult, None, ["gq"], ["gq"])

    def outproj_half(szy, yT, store=False):
        def oa(i):
            pt = pb[i % 2]
            for c in range(2):
                tr(pt[:, c * 128:(c + 1) * 128], szy[:, i, c * 128:(c + 1) * 128], [("szy", i)], [("pb", i % 2)])
            y = yT[i % 2]
            act(y[:], pt[:, 0:256].rearrange("p (c t) -> p c t", c=2), AF.Copy, [("pb", i % 2)], [("yT", i % 2)])

        def ob(i):
            y = yT[i % 2]
            outproj_tile(i, [y[:, 0, :], y[:, 1, :]], [0, 1], [("yT", i % 2)], (4, 5))
            if store:
                dma("sp", yv[:, i, :], x_sb[:, i, :], "out", reads=[("x", i)])

        lagged(NT, oa, ob)

    def plan_causal(qc):
        items = []
        for kt in range(4 * qc + 4):
            if kt < 4 * qc:
                items.append((kt, 0, 3, {}))
            else:
                m = kt - 4 * qc
                items.append((kt, m, 3, {m: tri[:]}))
        return items

    def plan_band(wt):
        def plan(qc):
            items = []
            for kt in range(max(0, 4 * qc - wt), 4 * qc + 4):
                jlo = max(0, kt - 4 * qc)
                jhi = min(3, kt + wt - 4 * qc)
                if jlo > jhi:
                    continue
                masks = {}
                if 0 <= kt - 4 * qc <= 3:
                    masks[kt - 4 * qc] = tri[:]
                if 0 <= kt + wt - 4 * qc <= 3:
                    masks[kt + wt - 4 * qc] = upm[:]
                items.append((kt, jlo, jhi, masks))
            return items
        return plan

    W1_OFF = 64 * 1024

    def prefetch_w1(war_keys=()):
        cvp = Carver(base=W1_OFF)
        wA = cvp.get([128, 32, 256], BF16)
        for kv, wd in enumerate((ckw1_d, cvw1_d)):
            wv = wd.rearrange("(l d) j -> d l j", d=64)
            for lq in range(4):
                dma("pool", wA[kv * 64:(kv + 1) * 64, lq * 8:(lq + 1) * 8, :], wv[:, lq * 8:(lq + 1) * 8, :], "w1", writes=["w1A"] + list(war_keys))
        return wA

    def layer1():
        W = owin_d
        wA = Carver(base=W1_OFF).get([128, 32, 256], BF16)
        if 0 not in layers:
            prefetch_w1()
        cvc = Carver(base=16 * 1024)
        wB = cvc.get([128, 32, 256], BF16)
        dma("sp", wB[64:128, :, :], wA[0:64, :, :], "c", writes=["w1B"])
        dma("sp", wB[0:64, :, :], wA[64:128, :, :], "c", writes=["w1B"])
        w1 = [[wA, wB], [wB, wA]]
        B = compress_loads(W, cvc)
        norm_phase(1)
        compress_stage(W, B, w1)
        P.barrier()
        if stop == "cmp":
            return
        for g in range(2):
            nsa_half(g, W)
            P.barrier()
            if stop == "nsa0":
                return
        if stop == "nsa":
            return
        for g in range(2):
            swa_half(g, W)
            P.barrier()

    def compress_loads(W, cv):
        s = load_w(W, [(512, 128), (640, 128)])
        B = {}
        B["s"] = s
        B["KVD"] = [cv.get([128, 16, 128], BF16) for _ in range(2)]
        B["w2"] = [cv.get([128, 2, 64], BF16) for _ in range(2)]
        B["posn"] = cv.get([32, 2, 64], F32)
        B["posT"] = cv.get([64, 2, 32], BF16)
        B["stgb"] = cv.get([4, 128], F32)
        B["b1sb"] = cv.get([128, 4], F32)
        B["biasj"] = cv.get([128, 4], F32)
        B["b2bc"] = cv.get([128, 2, 64], F32)
        B["gcm"] = cv.get([128, 64], F32)
        B["kalc"] = cv.get([128, 36], BF16)
        B["hid"] = [cv.get([128, 2, 128], BF16) for _ in range(4)]
        B["kcf"] = cv.get([128, 64], F32)
        B["junk2"] = cv.get([128, 64], F32)
        B["ssc"] = cv.get([128, 1], F32)
        B["kaug"] = cv.get([128, 128], BF16)
        for kv, wd in enumerate((ckw2_d, cvw2_d)):
            dma("pool", B["w2"][kv][:], wd.rearrange("(c p) d -> p c d", p=128), "w2", writes=["w2"])
        dma("sp", B["posn"][:, 0, :], cposk_d, "c", writes=["posn"])
        dma("sp", B["posn"][:, 1, :], cposv_d, "c", writes=["posn"])
        dma("sp", B["stgb"][0:2, :], ckb1_d.rearrange("(a c) -> a c", c=128), "c", writes=["stgb"])
        dma("sp", B["stgb"][2:4, :], cvb1_d.rearrange("(a c) -> a c", c=128), "c", writes=["stgb"])
        dma("sp", B["b2bc"][:, 0, :], ckb2_d.partition_broadcast(128), "c", writes=["b2bc"])
        dma("sp", B["b2bc"][:, 1, :], cvb2_d.partition_broadcast(128), "c", writes=["b2bc"])
        dma("sp", B["gcm"][:], ckc_d.partition_broadcast(128), "c", writes=["gcm"])
        dma("pool", B["kalc"][:], kalc_d, "c", writes=["kalc"])
        for g in range(2):
            dma("pool", Vca[:, g, 65:97], ovl_d, "c", writes=[("Vca", g)])
            MSET(Vca[:, g, 64:65], 1.0, [("Vca", g)])
        MSET(B["kaug"][:], 0.0, ["kaug"])
        return B

    def compress_stage(W, B, w1):
        s = B["s"]
        KVD, w2, posn, posT, stgb, b1sb, biasj = B["KVD"], B["w2"], B["posn"], B["posT"], B["stgb"], B["b1sb"], B["biasj"]
        b2bc, gcm, kalc, hid, kcf, junk2, ssc, kaug = B["b2bc"], B["gcm"], B["kalc"], B["hid"], B["kcf"], B["junk2"], B["ssc"], B["kaug"]
        for kv in range(2):
            tr(pf[0][0:64, kv * 32:(kv + 1) * 32], posn[:, kv, :], ["posn"], [("pf", 0)], idn=identf[0:32, 0:32])
        tr(pf[0][:, 64:68], stgb[:], ["stgb"], [("pf", 0)], idn=identf[0:4, 0:4])
        CPY(posT[:], pf[0][0:64, 0:64].rearrange("p (k l) -> p k l", l=32), [("pf", 0)], ["posT"])
        CPY(b1sb[:], pf[0][:, 64:68], [("pf", 0)], ["b1sb"])
        for kv in range(2):
            for jc in range(2):
                col = kv * 2 + jc
                for l in range(32):
                    mm(pf[1][:, col:col + 1], w1[kv][0][0:64, l, jc * 128:(jc + 1) * 128], posT[0:64, kv, l:l + 1], l == 0, l == 31,
                       ["w1B", "posT"], [("pf", 1)])
        TT(biasj[:], pf[1][:, 0:4], b1sb[:], ALU.add, [("pf", 1), "b1sb"], ["biasj"])
        for tq in range(4):
            for which in range(2):
                pp = pf[2 + which]
                for k in range(8):
                    mm(pp[:], wbuf[s][:, k, which * 128:(which + 1) * 128], hT[:, k, tq * 512:(tq + 1) * 512], k == 0, k == 7,
                       [("w", s)] + [("hT", 4 * tq + u) for u in range(4)], [("pf", 2 + which)])
                act(KVD[which][:, :, tq * 32:(tq + 1) * 32].rearrange("p r m -> p m r"), pp[:].rearrange("p (m r) -> p m r", r=16),
                    AF.Copy, [("pf", 2 + which)], [("KVT", which)])
        n = 0
        for kv in range(2):
            for g in range(2):
                hb_ = hid[kv * 2 + g]
                for jc in range(2):
                    pp = pf[n % 2]
                    for l in range(32):
                        mm(pp[:, 0:127], w1[kv][g][g * 64:(g + 1) * 64, l, jc * 128:(jc + 1) * 128],
                           KVD[kv][g * 64:(g + 1) * 64, l % 16, (l // 16):(l // 16) + 127], l == 0, l == 31,
                           ["w1B", ("KVT", kv)], [("pf", n % 2)])
                    act(hb_[:, jc, 0:127], pp[:, 0:127], AF.Silu, [("pf", n % 2), "biasj"], [("hid", kv * 2 + g)],
                        bias=biasj[:, kv * 2 + jc:kv * 2 + jc + 1], scale=1.0)
                    n += 1
                po = pf[2 + g]
                for jc in range(2):
                    mm(po[0:127, 0:64], hb_[:, jc, 0:127], w2[kv][:, jc, :], jc == 0, jc == 1, [("hid", kv * 2 + g), "w2"], [("pf", 2 + g)])
                if kv == 0:
                    TT(kcf[0:127, :], po[0:127, 0:64], b2bc[0:127, 0, :], ALU.add, [("pf", 2 + g), "b2bc"], ["kcf"])
                    act(junk2[0:127, :], kcf[0:127, :], AF.Square, ["kcf"], ["junk2", "ssc"], accum_out=ssc[0:127, :])
                    TS(ssc[0:127, :], ssc[0:127, :], 1.0 / 64, EPS, ALU.mult, ALU.add, ["ssc"], ["ssc"])
                    act(ssc[0:127, :], ssc[0:127, :], AF.Sqrt, ["ssc"], ["ssc"])
                    RCP(ssc[0:127, :], ssc[0:127, :], ["ssc"], ["ssc"])
                    STT(kaug[0:127, 0:64], kcf[0:127, :], ssc[0:127, :], gcm[0:127, :], ALU.mult, ALU.mult, ["kcf", "ssc", "gcm"], ["kaug"])
                    CPY(kaug[:, 64:100], kalc[:], ["kalc"], ["kaug"])
                    tr(pb[g][:, 0:128], kaug[:], ["kaug"], [("pb", g)])
                    act(KTc[:, g, :], pb[g][:, 0:128], AF.Copy, [("pb", g)], [("KTc", g)])
                else:
                    TT(Vca[0:127, g, 0:64], po[0:127, 0:64], b2bc[0:127, 1, :], ALU.add, [("pf", 2 + g), "b2bc"], [("Vca", g)])

    def nsa_half(g, W):
        cv = Carver()
        QT = cv.get([128, 4, S], BF16)
        KTs = cv.get([128, S], BF16)
        KTw = cv.get([128, S], BF16)
        Vs = cv.get([128, NT, 66], BF16)
        Vw = cv.get([128, NT, 66], BF16)
        szy = cv.get([128, NT, 256], BF16)
        gates = cv.get([128, NT, 12], F32)
        oacc = cv.get([128, NT, 256], F32)
        imp = cv.get([128, 8, 32], F32)
        impc = cv.get([128, 8, 32], F32)
        cmn = cv.get([128, S], BF16)
        qa = [cv.get([128, 4, 128], BF16) for _ in range(3)]
        ksa = [cv.get([128, 128], BF16) for _ in range(3)]
        kwa = [cv.get([128, 128], BF16) for _ in range(3)]
        ptb = [cv.get([128, 512], BF16) for _ in range(3)]
        tmpf = [cv.get([128, 512], F32) for _ in range(2)]
        cmpb = cv.get([128, 32, 32], F32)
        rank = cv.get([128, 32], F32)
        nmt = cv.get([128, 128], BF16)
        s8 = [cv.get([128, 8], F32) for _ in range(3)]
        gq = cv.get([128, 1], F32)
        gks = cv.get([128, 1], F32)
        gkw = cv.get([128, 1], F32)
        rden = [cv.get([128, 1], F32) for _ in range(8)]
        yT = [cv.get([128, 2, 128], BF16) for _ in range(2)]
        s = load_w(W, [(g * 256, 256), (768 + 64 * g, 64), (1024 + 64 * g, 64), (896 + 64 * g, 64), (1152 + 64 * g, 64)])
        s2 = load_w(W, [(1304 + g * 256, 256), (1280 + 4 * g, 4), (1288 + 4 * g, 4), (1296 + 4 * g, 4)])
        load_wo(1, 2 * g, 2)
        load_gain_col(gq, cq_d, 1.0)
        load_gain_col(gks, cks_d, 8.0)
        load_gain_col(gkw, ckw_d, 8.0)
        dma("sp", impc[:], impc_d[:, 8:16, :], "c", writes=["impc"])
        dma("pool", cmn[:], cmn_d, "c", writes=["addmask"])
        for b in range(3):
            MSET(qa[b][:], 0.0, [("qa", b)])
            MSET(ksa[b][:], 0.0, [("ksa", b)])
            MSET(kwa[b][:], 0.0, [("kwa", b)])
        MSET(Vs[:], 1.0, ["Vs"])
        MSET(Vw[:], 1.0, ["Vw"])
        MSET(nmt[:], 0.0, ["nmt"])
        def pa(i):
            b2 = i % 2
            pp = pf[b2]
            qai, ksi, kwi = qa[b2], ksa[b2], kwa[b2]
            CPY(qai[:, :, 96:100], qal[:, i, g * 16:g * 16 + 16].rearrange("p (h c) -> p h c", c=4), ["const"], [("qa", b2)])
            CPY(ksi[:, 64:100], kals[:, i, :], ["const"], [("ksa", b2)])
            CPY(kwi[:, 64:100], kalp[:, i, :], ["const"], [("kwa", b2)])
            for k in range(8):
                mm(pp[:], hT[:, k, i * 128:(i + 1) * 128], wbuf[s][:, k, :], k == 0, k == 7, [("hT", i), ("w", s)], [("pf", b2)])
            tf = tmpf[b2]
            head_rstd(pp, ("pf", b2), 384, tf, ("tf", b2), s8[b2], ("s8", b2))
            TT(qai[:, :, 0:64], pp[:, 0:256].rearrange("p (h d) -> p h d", d=64), s8[b2][:, 0:4].unsqueeze(2).to_broadcast([128, 4, 64]),
               ALU.mult, [("pf", b2), ("s8", b2)], [("qa", b2)])
            TS(ksi[:, 0:64], pp[:, 256:320], s8[b2][:, 4:5], None, ALU.mult, None, [("pf", b2), ("s8", b2)], [("ksa", b2)])
            TS(kwi[:, 0:64], pp[:, 320:384], s8[b2][:, 5:6], None, ALU.mult, None, [("pf", b2), ("s8", b2)], [("kwa", b2)])
            act(Vs[:, i, 0:64], pp[:, 384:448], AF.Copy, [("pf", b2), "Vs"], [("Vs", i)])
            act(Vw[:, i, 0:64], pp[:, 448:512], AF.Copy, [("pf", b2), "Vw"], [("Vw", i)])

        def pbk(i):
            b2 = i % 2
            qai, ksi, kwi = qa[b2], ksa[b2], kwa[b2]
            pt = pb[b2]
            for h in range(4):
                tr(pt[:, h * 128:(h + 1) * 128], qai[:, h, :], [("qa", b2)], [("pb", b2)])
            tr(pt[:, 512:640], ksi[:], [("ksa", b2)], [("pb", b2)])
            tr(pt[:, 640:768], kwi[:], [("kwa", b2)], [("pb", b2)])
            act(QT[:, :, i * 128:(i + 1) * 128], pt[:, 0:512].rearrange("p (h t) -> p h t", h=4), AF.Copy, [("pb", b2), "gq"], [("QT", i)],
                scale=gq[:, 0:1])
            act(KTs[:, i * 128:(i + 1) * 128], pt[:, 512:640], AF.Copy, [("pb", b2), "gq"], [("KTs", i)], scale=gks[:, 0:1])
            act(KTw[:, i * 128:(i + 1) * 128], pt[:, 640:768], AF.Copy, [("pb", b2), "gq"], [("KTw", i)], scale=gkw[:, 0:1])

        lagged(NT, pa, pbk)
        for i in range(NT):
            b2 = 2 + i % 2
            pp = pf[b2]
            for k in range(8):
                mm(pp[:, 0:256], hT[:, k, i * 128:(i + 1) * 128], wbuf[s2][:, k, 0:256], k == 0, k == 7, [("hT", i), ("w", s2)], [("pf", b2)])
            act(szy[:, i, :], pp[:, 0:256], AF.Silu, [("pf", b2)], [("szy", i)])
        for i in range(NT):
            b2 = 2 + i % 2
            pp = pf[b2]
            for k in range(8):
                mm(pp[:, 0:12], hT[:, k, i * 128:(i + 1) * 128], wbuf[s2][:, k, 256:268], k == 0, k == 7, [("hT", i), ("w", s2)], [("pf", b2)])
            act(gates[:, i, :], pp[:, 0:12], AF.Sigmoid, [("pf", b2)], [("gates", i)])
        if stop == "nsaproj":
            return
        qk = lambda qc: [("QT", 4 * qc + u) for u in range(4)]
        for r in range(4):
            def fin_cmp(i, Ob, okey, r=r):
                ri = (i + 4 * r) % 8
                rd = rden[ri]
                TS(rd[:], Ob[:, 64:65], 1e-30, None, ALU.max, None, [okey], [("rden", ri)])
                RCP(rd[:], rd[:], [("rden", ri)], [("rden", ri)])
                TS(oacc[:, i, r * 64:(r + 1) * 64], Ob[:, 0:64], rd[:], gates[:, i, r:r + 1], ALU.mult, ALU.mult,
                   [okey, ("rden", ri), ("gates", i)], [("oacc", i)])
                if i >= 8:
                    if r == 0:
                        TS(imp[:, i - 8, :], Ob[:, 65:97], rd[:], None, ALU.mult, None, [okey, ("rden", ri)], [("imp", i)])
                    else:
                        STT(imp[:, i - 8, :], Ob[:, 65:97], rd[:], imp[:, i - 8, :], ALU.mult, ALU.add, [okey, ("rden", ri), ("imp", i)], [("imp", i)])

            attention(QT[:, r, :], lambda kt: KTc[:, g, 0:127], 127, lambda kt: Vca[0:127, g, 0:97], 97,
                      lambda qc: [(0, 0, 3, {})], fin_cmp, qk, lambda kt: [("KTc", g)], lambda kt: [("Vca", g)], ptb, "n", addmask=cmn)
        if stop == "nsacmp":
            return
        for i in range(8, NT):
            im = imp[:, i - 8, :]
            TT(im, im, impc[:, i - 8, :], ALU.add, [("imp", i), "impc"], [("imp", i)])
            TT(cmpb[:], im.unsqueeze(1).to_broadcast([128, 32, 32]), im.unsqueeze(2).to_broadcast([128, 32, 32]), ALU.is_gt,
               [("imp", i)], ["cmpb"])
            RED(rank[:], cmpb[:], ALU.add, ["cmpb"], ["rank"])
            TS(nmt[:, 0:32], rank[:], 15.5, NEGM, ALU.is_ge, ALU.mult, ["rank"], ["nmt"])
            pt = pb[i % 2]
            tr(pt[:, 0:128], nmt[:], ["nmt"], [("pb", i % 2)])
            for r in range(4):
                act(QT[64:96, r, i * 128:(i + 1) * 128], pt[0:32, 0:128], AF.Copy, [("pb", i % 2)], [("QT", i)])
        for (KTx, Vx, kname, vname, plan, gcol) in ((KTs, Vs, "KTs", "Vs", plan_causal, 4), (KTw, Vw, "KTw", "Vw", plan_band(4), 8)):
            for r in range(4):
                def fin_add(i, Ob, okey, r=r, gcol=gcol):
                    ri = (i + 4 * r) % 8
                    rd = rden[ri]
                    RCP(rd[:], Ob[:, 64:65], [okey], [("rden", ri)])
                    TT(rd[:], rd[:], gates[:, i, gcol + r:gcol + r + 1], ALU.mult, [("rden", ri), ("gates", i)], [("rden", ri)])
                    dst = oacc[:, i, r * 64:(r + 1) * 64]
                    STT(dst, Ob[:, 0:64], rd[:], dst, ALU.mult, ALU.add, [okey, ("rden", ri), ("oacc", i)], [("oacc", i)])

                attention(QT[:, r, :], lambda kt, KTx=KTx: KTx[:, kt * 128:(kt + 1) * 128], 128,
                          lambda kt, Vx=Vx: Vx[:, kt, 0:65], 65, plan, fin_add, qk,
                          lambda kt, kname=kname: [(kname, kt)], lambda kt, vname=vname: [(vname, kt)], ptb, "n")
        for i in range(NT):
            TT(szy[:, i, :], oacc[:, i, :], szy[:, i, :], ALU.mult, [("oacc", i), ("szy", i)], [("szy", i)])
        outproj_half(szy, yT)

    def swa_half(g, W):
        cv = Carver()
        QT = cv.get([128, 4, S], BF16)
        KT = cv.get([128, S], BF16)
        Vt = cv.get([128, NT, 66], BF16)
        szy = cv.get([128, NT, 256], BF16)
        qa = [cv.get([128, 4, 128], BF16) for _ in range(3)]
        ka = [cv.get([128, 128], BF16) for _ in range(3)]
        ptb = [cv.get([128, 512], BF16) for _ in range(3)]
        tmpf = [cv.get([128, 512], F32) for _ in range(2)]
        s8 = [cv.get([128, 8], F32) for _ in range(3)]
        gq = cv.get([128, 1], F32)
        gk = cv.get([128, 1], F32)
        esink = cv.get([128, 8], F32)
        rden = [cv.get([128, 1], F32) for _ in range(8)]
        yT = [cv.get([128, 2, 128], BF16) for _ in range(2)]
        load_gain_col(gq, dq_d, 1.0)
        load_gain_col(gk, dk_d, 8.0)
        dma("sp", esink[:], dsink_d.partition_broadcast(128), "c", writes=["esink"])
        act(esink[:], esink[:], AF.Exp, ["esink"], ["esink"])
        for b in range(3):
            MSET(qa[b][:], 0.0, [("qa", b)])
            MSET(ka[b][:], 0.0, [("ka", b)])
        MSET(Vt[:], 1.0, ["Vt"])
        s = load_w(W, [(1816 + g * 256, 256), (2328 + 64 * g, 64), (2456 + 64 * g, 64)])
        s2 = load_w(W, [(2584 + g * 256, 256)])
        load_wo(1, 4 + 2 * g, 2)
        def pa(i):
            b2 = i % 2
            pp = pf[b2]
            qai, kai = qa[b2], ka[b2]
            CPY(qai[:, :, 96:100], qal[:, i, g * 16:g * 16 + 16].rearrange("p (h c) -> p h c", c=4), ["const"], [("qa", b2)])
            CPY(kai[:, 64:100], kalp[:, i, :], ["const"], [("ka", b2)])
            for k in range(8):
                mm(pp[:, 0:384], hT[:, k, i * 128:(i + 1) * 128], wbuf[s][:, k, 0:384], k == 0, k == 7, [("hT", i), ("w", s)], [("pf", b2)])
            tf = tmpf[b2]
            head_rstd(pp, ("pf", b2), 320, tf, ("tf", b2), s8[b2], ("s8", b2))
            TT(qai[:, :, 0:64], pp[:, 0:256].rearrange("p (h d) -> p h d", d=64), s8[b2][:, 0:4].unsqueeze(2).to_broadcast([128, 4, 64]),
               ALU.mult, [("pf", b2), ("s8", b2)], [("qa", b2)])
            TS(kai[:, 0:64], pp[:, 256:320], s8[b2][:, 4:5], None, ALU.mult, None, [("pf", b2), ("s8", b2)], [("ka", b2)])
            act(Vt[:, i, 0:64], pp[:, 320:384], AF.Copy, [("pf", b2), "Vt"], [("V", i)])

        def pbk(i):
            b2 = i % 2
            qai, kai = qa[b2], ka[b2]
            pt = pb[b2]
            for h in range(4):
                tr(pt[:, h * 128:(h + 1) * 128], qai[:, h, :], [("qa", b2)], [("pb", b2)])
            tr(pt[:, 512:640], kai[:], [("ka", b2)], [("pb", b2)])
            act(QT[:, :, i * 128:(i + 1) * 128], pt[:, 0:512].rearrange("p (h t) -> p h t", h=4), AF.Copy, [("pb", b2), "gq"], [("QT", i)],
                scale=gq[:, 0:1])
            act(KT[:, i * 128:(i + 1) * 128], pt[:, 512:640], AF.Copy, [("pb", b2), "gq"], [("KT", i)], scale=gk[:, 0:1])

        lagged(NT, pa, pbk)
        for i in range(NT):
            b2 = 2 + i % 2
            pp = pf[b2]
            for k in range(8):
                mm(pp[:, 0:256], hT[:, k, i * 128:(i + 1) * 128], wbuf[s2][:, k, 0:256], k == 0, k == 7, [("hT", i), ("w", s2)], [("pf", b2)])
            act(szy[:, i, :], pp[:, 0:256], AF.Silu, [("pf", b2)], [("szy", i)])
        for r in range(4):
            def fin(i, Ob, okey, r=r):
                ri = (i + 4 * r) % 8
                rd = rden[ri]
                TS(rd[:], Ob[:, 64:65], esink[:, 4 * g + r:4 * g + r + 1], None, ALU.add, None, [okey, "esink"], [("rden", ri)])
                RCP(rd[:], rd[:], [("rden", ri)], [("rden", ri)])
                dst = szy[:, i, r * 64:(r + 1) * 64]
                STT(dst, Ob[:, 0:64], rd[:], dst, ALU.mult, ALU.mult, [okey, ("rden", ri), ("szy", i)], [("szy", i)])

            attention(QT[:, r, :], lambda kt: KT[:, kt * 128:(kt + 1) * 128], 128, lambda kt: Vt[:, kt, 0:65], 65,
                      plan_band(1), fin, lambda qc: [("QT", 4 * qc + u) for u in range(4)], lambda kt: [("KT", kt)],
                      lambda kt: [("V", kt)], ptb, "d")
        outproj_half(szy, yT, store=(g == 1 and stop is None))

    if 0 in layers:
        layer0()
    if 1 in layers:
        layer1()

    if not (1 in layers and stop is None):
        for i in range(NT):
            dma("sp", yv[:, i, :], x_sb[:, i, :], "out", reads=[("x", i)])
    P.finish("sp", list(P.chan_count.keys()))
    P.emit()
    es.close()
    return nc


_CACHE = {}


def kernel(**inputs):
    consts = _consts()
    shared = {}
    shared["norm_g"] = np.ascontiguousarray(inputs["norm_g"], dtype=np.float32)
    shared["w_out"] = np.ascontiguousarray(inputs["w_out"], dtype=np.float32)
    shared["e_w_in"] = np.ascontiguousarray(inputs["e_w_in"][0], dtype=np.float32)
    shared["a_conv_w"] = np.ascontiguousarray(inputs["a_conv_w"][0], dtype=np.float32)
    shared["a_conv_b"] = np.ascontiguousarray(inputs["a_conv_b"][0], dtype=np.float32)
    shared["a_ln_g"] = np.ascontiguousarray(inputs["a_ln_g"][0], dtype=np.float32)
    shared["a_ln_b"] = np.ascontiguousarray(inputs["a_ln_b"][0], dtype=np.float32)
    shared["b_qnorm_g"] = np.ascontiguousarray(inputs["b_qnorm_g"], dtype=np.float32)
    shared["b_knorm_g"] = np.ascontiguousarray(inputs["b_knorm_g"], dtype=np.float32)
    for k in ("ident", "tri", "up", "qal", "kal_moba", "kal_slc", "kal_plain", "kal_cmp", "cmaskneg", "ovl", "impc"):
        shared[k] = consts[k]
    shared["o_w_in"] = np.ascontiguousarray(inputs["o_w_in"][0], dtype=np.float32)
    for k in ("c_qnorm_g", "c_knorm_cmp_g", "c_knorm_slc_g", "c_knorm_win_g", "c_k_b2", "c_v_b2", "d_qnorm_g", "d_knorm_g", "d_sinks"):
        shared[k] = np.ascontiguousarray(inputs[k], dtype=np.float32).reshape(1, -1)
    for k in ("c_pos_k", "c_pos_v", "c_k_w1", "c_k_b1", "c_k_w2", "c_v_w1", "c_v_b1", "c_v_w2"):
        shared[k] = np.ascontiguousarray(inputs[k][0], dtype=np.float32)
    x = np.ascontiguousarray(inputs["x"], dtype=np.float32)
    nb = x.shape[0]
    layers = inputs.get("_layers", (0, 1))
    nc = build_program(layers, inputs.get("_stop"))
    in_maps = [dict(shared, x=x[b]) for b in range(nb)]
    res = run_bass_kernel_spmd(nc, in_maps, core_ids=list(range(nb)))
    return np.stack([np.asarray(r["y"], dtype=np.float32) for r in res.results], axis=0)
```

```python
import contextlib
import os
import numpy as np
import ml_dtypes
import concourse.bass as bass
import concourse.mybir as mybir
from concourse.bass_utils import run_bass_kernel_spmd

F32 = mybir.dt.float32
BF16 = mybir.dt.bfloat16
ALU = mybir.AluOpType
AF = mybir.ActivationFunctionType
AX = mybir.AxisListType

S = 2048
D = 1024
NT = 16
NEGM = -30000.0
EPS = 1e-6


class _Op:
    __slots__ = ("eng", "fn", "deps", "chan", "signal", "val", "dmaval", "chanseq")

    def __init__(self, eng, fn, chan):
        self.eng = eng
        self.fn = fn
        self.deps = {}
        self.chan = chan
        self.signal = False
        self.val = 0
        self.dmaval = None


class Prog:
    ENGS = ("pe", "act", "dve", "pool", "sp")

    def __init__(self, nc):
        self.nc = nc
        self.ops = {e: [] for e in self.ENGS}
        self.res = {}
        self.chan_count = {}
        self.chan_last = {}
        self.all_ops = []
        self.bar = {}
        self.final_waits = {}
        self.pool_ctr = 0
        self.sp_ctr = 0
        self.const_keys = []
        self.pres = {}

    PERS = ("w", "wo")

    def _st(self, k):
        if isinstance(k, tuple) and k[0] in self.PERS or k in self.PERS:
            return self.pres
        return self.res

    def op(self, eng, fn, reads=(), writes=(), chan=None, nobar=False):
        for w in writes:
            if isinstance(w, tuple) and w[0] == "ck" and w not in self.const_keys:
                self.const_keys.append(w)
        if "const" in reads:
            reads = [k for r in reads for k in (self.const_keys if r == "const" else (r,))]
        if chan is not None and eng == "pool":
            chan = "pq%d" % (self.pool_ctr % 12)
            self.pool_ctr += 1
        elif chan == "c":
            chan = "pc%d" % (self.sp_ctr % 16)
            self.sp_ctr += 1
        o = _Op(eng, fn, chan)
        deps = o.deps
        if not nobar:
            deps.update(self.bar)
        if chan is not None and (chan.startswith("pq") or chan.startswith("pc")):
            prev = self.chan_last.get(chan)
            if prev is not None:
                deps[id(prev)] = prev
        for k in reads:
            st = self._st(k).get(k)
            if st is not None and st[0] is not None:
                deps[id(st[0])] = st[0]
        for k in writes:
            st = self._st(k).get(k)
            if st is not None:
                if st[0] is not None:
                    deps[id(st[0])] = st[0]
                for r in st[1]:
                    deps[id(r)] = r
        for k in reads:
            res = self._st(k)
            st = res.get(k)
            if st is None:
                st = [None, []]
                res[k] = st
            st[1].append(o)
        for k in writes:
            self._st(k)[k] = [o, []]
        o.dmaval = dict(self.chan_count)
        if chan is not None:
            self.chan_count[chan] = self.chan_count.get(chan, 0) + 1
            o.chanseq = self.chan_count[chan]
            self.chan_last[chan] = o
            o.signal = True
        self.ops[eng].append(o)
        self.all_ops.append(o)
        return o

    def barrier(self):
        bar = {}
        for e in self.ENGS:
            if self.ops[e]:
                o = self.ops[e][-1]
                bar[id(o)] = o
        for ch, o in self.chan_last.items():
            bar[id(o)] = o
        self.bar = bar
        self.res = {}

    def finish(self, eng, chans):
        self.final_waits = {eng: {ch: self.chan_count[ch] for ch in chans}}

    def emit(self):
        nc = self.nc
        for o in self.all_ops:
            for d in o.deps.values():
                if d.chan is None:
                    if d.eng == "pe" and o.eng == "pe":
                        continue
                    d.signal = True
        for e in self.ENGS:
            c = 0
            for o in self.ops[e]:
                if o.chan is None and o.signal:
                    c += 1
                    o.val = c
        import os
        if os.environ.get("KDBG"):
            print("sem counts", {e: max([o.val for o in self.ops[e]] + [0]) for e in self.ENGS}, {e: len(self.ops[e]) for e in self.ENGS},
                  {c: 16 * v for c, v in self.chan_count.items()})
        stack = contextlib.ExitStack()
        sems = {}
        for e in self.ENGS:
            sems[e] = stack.enter_context(nc.semaphore("s_" + e))
        for ch in self.chan_count:
            sems["c_" + ch] = stack.enter_context(nc.semaphore("c_" + ch))
        block = stack.enter_context(nc.Block())
        engobj = {"pe": "tensor", "act": "scalar", "dve": "vector", "pool": "gpsimd", "sp": "sync"}

        def make(e):
            def body(eng):
                waited = {}
                for o in self.ops[e]:
                    need = {}
                    for d in o.deps.values():
                        if d.chan is not None:
                            k = "c_" + d.chan
                            if d.chan.startswith("pq") or d.chan.startswith("pc"):
                                v = 16 * d.chanseq
                            else:
                                v = 16 * o.dmaval[d.chan]
                        else:
                            if d.eng == "pe" and e == "pe":
                                continue
                            k = d.eng
                            v = d.val
                        if v > need.get(k, 0):
                            need[k] = v
                    for k, v in need.items():
                        if waited.get(k, 0) >= v:
                            continue
                        eng.wait_ge(sems[k], v)
                        waited[k] = v
                    ins = o.fn(eng)
                    if o.chan is not None:
                        ins.then_inc(sems["c_" + o.chan], 16)
                    elif o.signal:
                        ins.then_inc(sems[e], 1)
                for ch, c in self.final_waits.get(e, {}).items():
                    eng.wait_ge(sems["c_" + ch], 16 * c)
            return body

        for e in self.ENGS:
            getattr(block, engobj[e])(make(e))
        stack.close()


def _consts():
    c = {}
    c["ident"] = np.eye(128, dtype=np.float32)
    k = np.arange(128)[:, None]
    q = np.arange(128)[None, :]
    c["tri"] = np.where(k <= q, 0.0, NEGM).astype(np.float32)
    c["up"] = np.where(k > q, 0.0, NEGM).astype(np.float32)
    t = np.arange(S)
    b = (t % 16).astype(np.float32)
    a = (t - t % 16).astype(np.float32)
    slopes = np.power(2.0, -8.0 * np.arange(1, 9) / 8).astype(np.float32)
    qal = np.zeros((S, 8, 4), np.float32)
    qal[:, :, 0] = -slopes[None, :] * a[:, None]
    qal[:, :, 1] = -slopes[None, :] * b[:, None]
    qal[:, :, 2] = slopes[None, :]
    qal[:, :, 3] = slopes[None, :]
    c["qal"] = qal.reshape(NT, 128, 32).transpose(1, 0, 2).copy()

    def kal(onehot_block):
        m = np.zeros((S, 36), np.float32)
        if onehot_block:
            m[t, t // onehot_block] = 1.0
        m[:, 32] = 1.0
        m[:, 33] = 1.0
        m[:, 34] = a
        m[:, 35] = b
        return m.reshape(NT, 128, 36).transpose(1, 0, 2).copy()

    c["kal_moba"] = kal(256)
    c["kal_slc"] = kal(64)
    c["kal_plain"] = kal(0)
    cc = np.arange(128)
    cend = 16 * cc + 31
    kc = np.zeros((128, 36), np.float32)
    kc[:, 32] = 1.0
    kc[:, 33] = 1.0
    kc[:, 34] = cend - cend % 16
    kc[:, 35] = cend % 16
    c["kal_cmp"] = kc
    c["cmaskneg"] = np.where(t[None, :] >= cend[:, None], 0.0, NEGM).astype(np.float32)
    start = np.arange(127)[:, None] * 16
    bs = np.arange(32)[None, :] * 64
    ov = np.zeros((128, 32), np.float32)
    ov[:127] = ((start < bs + 64) & (start + 32 > bs)).astype(np.float32)
    c["ovl"] = ov
    blk = np.arange(32)[None, :]
    cur = (t // 64)[:, None]
    forced = (blk == 0) | (blk == cur) | (blk == cur - 1)
    impc = 1e4 * forced.astype(np.float32) - 1e5 * (blk > cur).astype(np.float32)
    c["impc"] = impc.reshape(NT, 128, 32).transpose(1, 0, 2).copy()
    return c


def build_program(layers=(0, 1), stop=None):
    nc = bass.Bass("TRN2", target_bir_lowering=False)
    es = contextlib.ExitStack()
    dram = {}

    def din(name, shape, dt=F32):
        dram[name] = nc.dram_tensor(name, list(shape), dt, kind="ExternalInput").ap()
        return dram[name]

    x_d = din("x", [S, D])
    normg_d = din("norm_g", [2, D])
    wout_d = din("w_out", [2, D, D])
    ewin_d = din("e_w_in", [D, 3584])
    convw_d = din("a_conv_w", [31, 512])
    convb_d = din("a_conv_b", [512])
    lng_d = din("a_ln_g", [512])
    lnb_d = din("a_ln_b", [512])
    bq_d = din("b_qnorm_g", [1, 64])
    bk_d = din("b_knorm_g", [1, 64])
    ident_d = din("ident", [128, 128])
    tri_d = din("tri", [128, 128])
    up_d = din("up", [128, 128])
    qal_d = din("qal", [128, NT, 32])
    kalm_d = din("kal_moba", [128, NT, 36])
    kals_d = din("kal_slc", [128, NT, 36])
    kalp_d = din("kal_plain", [128, NT, 36])
    kalc_d = din("kal_cmp", [128, 36])
    cmn_d = din("cmaskneg", [128, S])
    ovl_d = din("ovl", [128, 32])
    impc_d = din("impc", [128, NT, 32])
    owin_d = din("o_w_in", [D, 3096])
    cq_d = din("c_qnorm_g", [1, 64])
    ckc_d = din("c_knorm_cmp_g", [1, 64])
    cks_d = din("c_knorm_slc_g", [1, 64])
    ckw_d = din("c_knorm_win_g", [1, 64])
    cposk_d = din("c_pos_k", [32, 64])
    cposv_d = din("c_pos_v", [32, 64])
    ckw1_d = din("c_k_w1", [2048, 256])
    ckb1_d = din("c_k_b1", [256])
    ckw2_d = din("c_k_w2", [256, 64])
    ckb2_d = din("c_k_b2", [1, 64])
    cvw1_d = din("c_v_w1", [2048, 256])
    cvb1_d = din("c_v_b1", [256])
    cvw2_d = din("c_v_w2", [256, 64])
    cvb2_d = din("c_v_b2", [1, 64])
    dq_d = din("d_qnorm_g", [1, 64])
    dk_d = din("d_knorm_g", [1, 64])
    dsink_d = din("d_sinks", [1, 8])
    y_d = nc.dram_tensor("y", [S, D], F32, kind="ExternalOutput").ap()

    def sb(name, shape, dt):
        return es.enter_context(nc.sbuf_tensor(name, list(shape), dt))

    def psum(name, shape, dt):
        return es.enter_context(nc.psum_tensor(name, list(shape), dt))

    P = Prog(nc)

    x_sb = sb("x_sb", [128, NT, D], F32)
    hT = sb("hT", [128, 8, S], BF16)
    wbuf = [sb("wbuf%d" % i, [128, 8, 512], BF16) for i in range(2)]
    wo = sb("wo", [128, 4, D], BF16)
    ident = sb("ident_sb", [128, 128], BF16)
    identf = sb("identf_sb", [128, 128], F32)
    onesf = sb("onesf", [128, 128], F32)
    tri = sb("tri_sb", [128, 128], BF16)
    upm = sb("up_sb", [128, 128], BF16)
    qal = sb("qal_sb", [128, NT, 32], BF16)
    kalm = sb("kalm_sb", [128, NT, 36], BF16)
    KTc = sb("KTc", [128, 2, 128], BF16)
    Vca = sb("Vca", [128, 2, 98], BF16)
    kals = sb("kals_sb", [128, NT, 36], BF16)
    kalp = sb("kalp_sb", [128, NT, 36], BF16)
    ss = sb("ss", [128, NT], F32)
    rstd = sb("rstd", [128, NT], F32)
    ARENA = 80 * 1024
    arena = sb("arena", [128, ARENA // 2], BF16)

    pf = [psum("pf%d" % i, [128, 512], F32) for i in range(6)]
    pb = [psum("pb%d" % i, [128, 1024], BF16) for i in range(2)]

    class Carver:
        def __init__(self, base=0):
            self.off = base

        def get(self, shape, dt):
            n = int(np.prod(shape[1:]))
            nb = n * (4 if dt == F32 else 2)
            nb = (nb + 63) // 64 * 64
            assert self.off + nb <= ARENA, ("arena overflow", self.off + nb, ARENA)
            a = arena[:, self.off // 2:(self.off + nb) // 2]
            self.off += nb
            if os.environ.get("KDBG"):
                print("carve", shape, dt, "->", self.off)
            if dt == F32:
                a = a.bitcast(F32)
            a = a[:, 0:n]
            if len(shape) == 3:
                a = a.rearrange("p (a b) -> p a b", b=shape[2])
            elif len(shape) == 4:
                a = a.rearrange("p (a b c) -> p a b c", b=shape[2], c=shape[3])
            if shape[0] < 128:
                a = a[0:shape[0]]
            return a

    def dma(eng, out, in_, chan, reads=(), writes=(), slow=False, nobar=False):
        if slow:
            P.op(eng, lambda e: e.dma_start(out=out, in_=in_, allow_slow_non_contiguous=True), reads=reads, writes=writes, chan=chan, nobar=nobar)
        else:
            P.op(eng, lambda e: e.dma_start(out=out, in_=in_), reads=reads, writes=writes, chan=chan, nobar=nobar)

    def mm(out, lhsT, rhs, start, stop, reads, writes):
        P.op("pe", lambda e: e.matmul(out, lhsT=lhsT, rhs=rhs, start=start, stop=stop), reads=reads, writes=writes)

    def tr(out, in_, reads, writes, idn=None):
        ck = ("ck", "ident") if idn is None else ("ck", "identf")
        idn = ident[:] if idn is None else idn
        P.op("pe", lambda e: e.transpose(out=out, in_=in_, identity=idn), reads=list(reads) + [ck], writes=writes)

    def act(out, in_, func, reads, writes, **kw):
        P.op("act", lambda e: e.activation(out=out, in_=in_, func=func, **kw), reads=reads, writes=writes)

    def V(fn, reads, writes, eng="dve"):
        P.op(eng, fn, reads=reads, writes=writes)

    def TT(out, in0, in1, op, reads, writes, eng="dve"):
        P.op(eng, lambda e: e.tensor_tensor(out=out, in0=in0, in1=in1, op=op), reads=reads, writes=writes)

    def TS(out, in0, s1, s2, op0, op1, reads, writes, eng="dve"):
        if op1 is None:
            P.op(eng, lambda e: e.tensor_scalar(out=out, in0=in0, scalar1=s1, scalar2=None, op0=op0), reads=reads, writes=writes)
        else:
            P.op(eng, lambda e: e.tensor_scalar(out=out, in0=in0, scalar1=s1, scalar2=s2, op0=op0, op1=op1), reads=reads, writes=writes)

    def STT(out, in0, scalar, in1, op0, op1, reads, writes, eng="dve"):
        P.op(eng, lambda e: e.scalar_tensor_tensor(out=out, in0=in0, scalar=scalar, in1=in1, op0=op0, op1=op1), reads=reads, writes=writes)

    def RED(out, in_, op, reads, writes):
        P.op("dve", lambda e: e.tensor_reduce(out=out, in_=in_, axis=AX.X, op=op), reads=reads, writes=writes)

    def RCP(out, in_, reads, writes):
        P.op("dve", lambda e: e.reciprocal(out=out, in_=in_), reads=reads, writes=writes)

    def CPY(out, in_, reads, writes, eng="dve"):
        P.op(eng, lambda e: e.tensor_copy(out=out, in_=in_), reads=reads, writes=writes)

    def MSET(ap, val, writes, eng="dve"):
        P.op(eng, lambda e: e.memset(ap, val), reads=[], writes=writes)

    import os
    KB = os.environ.get("KBIS", "abcdefg")
    if "a" in KB:
        dma("pool", ident[:], ident_d, "c", writes=[("ck", "ident")])
    if "b" in KB:
        dma("pool", tri[:], tri_d, "c", writes=[("ck", "tri")])
        dma("pool", upm[:], up_d, "c", writes=[("ck", "up")])
    if "c" in KB:
        dma("pool", qal[:], qal_d, "c", writes=[("ck", "qal")])
    if "d" in KB:
        dma("pool", kalm[:], kalm_d, "c", writes=[("ck", "kalm")])
        dma("pool", kals[:], kals_d, "c", writes=[("ck", "kals")])
        dma("pool", kalp[:], kalp_d, "c", writes=[("ck", "kalp")])
    if "e" in KB:
        dma("sp", identf[:], ident_d, "c", writes=[("ck", "identf")])
    if "f" in KB:
        MSET(onesf[:], 1.0, [("ck", "onesf")])

    xv = x_d.rearrange("(i p) d -> p i d", p=128)
    yv = y_d.rearrange("(i p) d -> p i d", p=128)
    for i in range(NT):
        dma("sp", x_sb[:, i, :], xv[:, i, :], "x%d" % i, writes=[("x", i)])

    wslot_ctr = [0]

    def load_w(wd, segs):
        s = wslot_ctr[0] % 2
        wslot_ctr[0] += 1
        wv = wd.rearrange("(k p) n -> p k n", p=128)
        o = 0
        for (c0, n) in segs:
            dma("pool", wbuf[s][:, :, o:o + n], wv[:, :, c0:c0 + n], "w%d" % s, writes=[("w", s)], nobar=True)
            o += n
        return s

    def norm_phase(layer, base=0, barrier=True):
        cvn = Carver(base=base)
        gbc = cvn.get([128, D], F32)
        junk = cvn.get([128, D], BF16)
        hb = [cvn.get([128, D], BF16) for _ in range(2)]
        dma("act", gbc[:], normg_d[layer:layer + 1, :].partition_broadcast(128), "c", writes=["gbc"])
        MSET(ss[:], 0.0, [("ss", gI) for gI in range(4)])

        def stats(gI):
            for i in range(4 * gI, 4 * gI + 4):
                act(junk[:], x_sb[:, i, :], AF.Square, [("x", i), ("ss", gI)], ["junk", ("ss", gI)], accum_out=ss[:, i:i + 1])
            sl = slice(4 * gI, 4 * gI + 4)
            TS(rstd[:, sl], ss[:, sl], 1.0 / D, EPS, ALU.mult, ALU.add, [("ss", gI)], [("rstd", gI)])
            act(rstd[:, sl], rstd[:, sl], AF.Sqrt, [("rstd", gI)], [("rstd", gI)])
            RCP(rstd[:, sl], rstd[:, sl], [("rstd", gI)], [("rstd", gI)])

        def na(i):
            h = hb[i % 2]
            STT(h[:], x_sb[:, i, :], rstd[:, i:i + 1], gbc[:], ALU.mult, ALU.mult, [("x", i), ("rstd", i // 4), "gbc"], [("hb", i % 2)])

        def nb(i):
            h = hb[i % 2]
            pt = pb[i % 2]
            for k in range(8):
                tr(pt[:, k * 128:(k + 1) * 128], h[:, k * 128:(k + 1) * 128], [("hb", i % 2)], [("pb", i % 2)])
            act(hT[:, :, i * 128:(i + 1) * 128], pt[:].rearrange("p (k t) -> p k t", k=8), AF.Copy, [("pb", i % 2)], [("hT", i)])

        pend = []
        for gI in range(4):
            stats(gI)
            for i in range(4 * gI, 4 * gI + 4):
                na(i)
                pend.append(i)
                if len(pend) > 1:
                    nb(pend.pop(0))
        nb(pend.pop(0))
        if barrier:
            P.barrier()

    def load_wo(layer, c0, n):
        wv = wout_d[layer].rearrange("(k p) n -> p k n", p=128)
        for hf in range(2):
            dma("pool", wo[:, 0:n, hf * 512:(hf + 1) * 512], wv[:, c0:c0 + n, hf * 512:(hf + 1) * 512], "wo", writes=["wo"], nobar=True)

    def outproj_tile(i, lhs_list, chunks, yT_reads, pbanks):
        for half in range(2):
            pp = pf[pbanks[half]]
            for n, (c, lhs) in enumerate(zip(chunks, lhs_list)):
                mm(pp[:], lhs, wo[:, c, half * 512:(half + 1) * 512], n == 0, n == len(chunks) - 1,
                   list(yT_reads) + ["wo"], [("pf", pbanks[half])])
            xs = x_sb[:, i, half * 512:(half + 1) * 512]
            TT(xs, xs, pp[:], ALU.add, [("pf", pbanks[half]), ("x", i)], [("x", i)])

    att_ctr = [0, 0]

    def lagged(n, stage_a, stage_b, lag=1):
        for i in range(n + lag):
            if i < n:
                stage_a(i)
            if i >= lag:
                stage_b(i - lag)

    def mm_acc(out, lhsT, rhs, start, stop, reads, writes):
        P.op("pe", lambda e: e.matmul(out, lhsT=lhsT, rhs=rhs, start=start, stop=stop, skip_group_check=True), reads=reads, writes=writes)

    def attention(QT, KT, nk, Vrhs, ncv, plan, finish, qkeys, kkeys, vkeys, ptb, tagk, addmask=None):
        NS = len(ptb)
        work = []
        for qc in range(4):
            items = plan(qc)
            if not items:
                continue
            ob = 3 + att_ctr[1] % 3
            att_ctr[1] += 1
            lastk = {}
            for (kt, jlo, jhi, masks) in items:
                for j in range(jlo, jhi + 1):
                    lastk[j] = kt
            for n, it in enumerate(items):
                work.append((qc, ob, it, n == 0, n == len(items) - 1, lastk))

        def front(w):
            qc, ob, (kt, jlo, jhi, masks), first, last, lastk = w
            sbk = att_ctr[0] % NS
            att_ctr[0] += 1
            Sp = pf[sbk]
            pt = ptb[sbk]
            c0, c1 = jlo * 128, (jhi + 1) * 128
            extra = list(masks.items())
            mm_acc(Sp[0:nk, c0:c1], KT(kt), QT[:, qc * 512 + c0:qc * 512 + c1], True, addmask is None and not extra,
                   list(qkeys(qc)) + list(kkeys(kt)), [("pf", sbk)])
            if addmask is not None:
                mm_acc(Sp[0:nk, c0:c1], ident[0:nk, 0:nk], addmask[0:nk, qc * 512 + c0:qc * 512 + c1], False, not extra,
                       ["const", "addmask"], [("pf", sbk)])
            for n, (j, mk) in enumerate(extra):
                mm_acc(Sp[0:nk, j * 128:(j + 1) * 128], ident[0:nk, 0:nk], mk[0:nk, :], False, n == len(extra) - 1,
                       ["const"], [("pf", sbk)])
            act(pt[0:nk, c0:c1], Sp[0:nk, c0:c1], AF.Exp, [("pf", sbk)], [("pt", tagk, sbk)])
            return sbk

        def back(w, sbk, started):
            qc, ob, (kt, jlo, jhi, masks), first, last, lastk = w
            pt = ptb[sbk]
            for j in range(jlo, jhi + 1):
                mm_acc(pf[ob][:, j * 128:j * 128 + ncv], pt[0:nk, j * 128:(j + 1) * 128], Vrhs(kt), len(started) == 0, lastk[j] == kt,
                       [("pt", tagk, sbk)] + list(vkeys(kt)), [("pf", ob)])
                started.add(j)
            if last:
                for j in sorted(started):
                    finish(qc * 4 + j, pf[ob][:, j * 128:j * 128 + ncv], ("pf", ob))
                started.clear()

        LAG = NS - 1
        started = set()
        pend = []
        for w in work:
            pend.append((w, front(w)))
            if len(pend) > LAG:
                pw, psb = pend.pop(0)
                back(pw, psb, started)
        for (pw, psb) in pend:
            back(pw, psb, started)

    def layer0():
        if stop == "load":
            return
        norm_phase(0, base=70 * 1024, barrier=False)
        if stop == "norm0":
            return
        load_wo(0, 0, 4)
        W = ewin_d
        if stop == "norm":
            return
        cv = Carver()
        yc = cv.get([128, 4, S], F32)
        hc = cv.get([128, S + 32], BF16)
        dg = cv.get([128, 31, 128], BF16)
        cw = cv.get([128, 4, 31], F32)
        cb = cv.get([128, 4], F32)
        lg = cv.get([128, 4], F32)
        lb = cv.get([128, 4], F32)
        Tsq = [cv.get([128, 512], F32) for _ in range(2)]
        mu2 = [cv.get([128, 512], F32) for _ in range(2)]
        msq = cv.get([128, 512], F32)
        rs2 = [cv.get([128, 512], F32) for _ in range(2)]
        sz4 = [cv.get([128, 512], BF16) for _ in range(4)]
        T4 = [cv.get([128, 512], F32) for _ in range(4)]
        yaT2 = [cv.get([128, 4, 512], BF16) for _ in range(2)]
        HO = 32
        stg = Tsq[0][0:32, :]
        stg2 = Tsq[1][0:12, 0:128]
        dma("sp", stg[0:31, :], convw_d, "c", writes=[("Tsq", 0)])
        dma("sp", stg2[0:4, :], convb_d.rearrange("(a c) -> a c", c=128), "c", writes=[("Tsq", 1)])
        dma("sp", stg2[4:8, :], lng_d.rearrange("(a c) -> a c", c=128), "c", writes=[("Tsq", 1)])
        dma("sp", stg2[8:12, :], lnb_d.rearrange("(a c) -> a c", c=128), "c", writes=[("Tsq", 1)])
        for cc in range(4):
            tr(pf[0][:, cc * 32:cc * 32 + 31], stg[0:31, cc * 128:(cc + 1) * 128], [("Tsq", 0)], [("pf", 0)], idn=identf[0:31, 0:31])
        tr(pf[0][:, 128:140], stg2[0:12, :], [("Tsq", 1)], [("pf", 0)], idn=identf[0:12, 0:12])
        CPY(cw[:], pf[0][:, 0:128].rearrange("p (a j) -> p a j", j=32)[:, :, 0:31], [("pf", 0)], ["cw"])
        CPY(cb[:], pf[0][:, 128:132], [("pf", 0)], ["cw"])
        CPY(lg[:], pf[0][:, 132:136], [("pf", 0)], ["cw"])
        CPY(lb[:], pf[0][:, 136:140], [("pf", 0)], ["cw"])
        MSET(hc[:, 0:HO], 0.0, ["hc0"])
        if stop == "convp":
            return
        for cc in range(4):
            if stop == "conv1" and cc == 1:
                return
            s = load_w(W, [(cc * 128, 128), (512 + cc * 128, 128)])
            for j in range(31):
                TS(dg[:, j, :], ident[:], cw[:, cc, j:j + 1], None, ALU.mult, None, ["const", "cw"], [("dg", j)])
            for tq in range(4):
                bv, bg = (tq % 2) * 2, (tq % 2) * 2 + 1
                for which, bk in ((0, bv), (1, bg)):
                    for k in range(8):
                        mm(pf[bk][:], wbuf[s][:, k, which * 128:(which + 1) * 128], hT[:, k, tq * 512:(tq + 1) * 512], k == 0, k == 7,
                           [("w", s)] + [("hT", 4 * tq + u) for u in range(4)], [("pf", bk)])
                act(T4[tq % 2][:], pf[bg][:], AF.Sigmoid, [("pf", bg)], [("T4", tq % 2)])
                TT(hc[:, HO + tq * 512:HO + (tq + 1) * 512], pf[bv][:], T4[tq % 2][:], ALU.mult, [("pf", bv), ("T4", tq % 2)], [("hc", tq)])
            for tq in range(4):
                pc = pf[4 + tq % 2]
                for j in range(31):
                    o = HO - 30 + j + tq * 512
                    mm(pc[:], dg[:, j, :], hc[:, o:o + 512], j == 0, j == 30,
                       [("dg", j), ("hc", tq)] + ([("hc", tq - 1)] if tq > 0 else ["hc0"]), [("pf", 4 + tq % 2)])
                act(yc[:, cc, tq * 512:(tq + 1) * 512], pc[:], AF.Identity, [("pf", 4 + tq % 2), "cw"], [("yc", cc, tq)],
                    bias=cb[:, cc:cc + 1], scale=1.0)
        if stop == "conv4":
            return
        P.barrier()
        sz_slot = load_w(W, [(1024, 512)])

        def st_a(tq):
            ts = slice(tq * 512, (tq + 1) * 512)
            p2 = tq % 2
            for cc in range(4):
                mm(pf[0][:], onesf[:], yc[:, cc, ts], cc == 0, cc == 3, [("yc", cc, tq), "const"], [("pf", 0)])
            for cc in range(4):
                act(Tsq[cc % 2][:], yc[:, cc, ts], AF.Square, [("yc", cc, tq)], [("Tsq", cc % 2)])
                mm(pf[1][:], onesf[:], Tsq[cc % 2][:], cc == 0, cc == 3, [("Tsq", cc % 2), "const"], [("pf", 1)])
            TS(mu2[p2][:], pf[0][:], 1.0 / 512, None, ALU.mult, None, [("pf", 0)], [("mu", p2)])
            TT(msq[:], mu2[p2][:], mu2[p2][:], ALU.mult, [("mu", p2)], ["msq"])
            STT(rs2[p2][:], pf[1][:], 1.0 / 512, msq[:], ALU.mult, ALU.subtract, [("pf", 1), "msq"], [("rs", p2)])
            TS(rs2[p2][:], rs2[p2][:], EPS, None, ALU.add, None, [("rs", p2)], [("rs", p2)])
            act(rs2[p2][:], rs2[p2][:], AF.Sqrt, [("rs", p2)], [("rs", p2)])
            RCP(rs2[p2][:], rs2[p2][:], [("rs", p2)], [("rs", p2)])

        def st_b(tq):
            ts = slice(tq * 512, (tq + 1) * 512)
            p2 = tq % 2
            for cc in range(4):
                pz = pf[2 + cc % 2]
                for k in range(8):
                    mm(pz[:], wbuf[sz_slot][:, k, cc * 128:(cc + 1) * 128], hT[:, k, ts], k == 0, k == 7,
                       [("w", sz_slot)] + [("hT", 4 * tq + u) for u in range(4)], [("pf", 2 + cc % 2)])
                act(sz4[cc][:], pz[:], AF.Silu, [("pf", 2 + cc % 2)], [("sz", cc)])
            for cc in range(4):
                tt = T4[cc]
                TT(tt[:], yc[:, cc, ts], mu2[p2][:], ALU.subtract, [("yc", cc, tq), ("mu", p2)], [("T4", cc)])
                TT(tt[:], tt[:], rs2[p2][:], ALU.mult, [("T4", cc), ("rs", p2)], [("T4", cc)])
            for cc in range(4):
                tt = T4[cc]
                act(tt[:], tt[:], AF.Silu, [("T4", cc), "cw"], [("T4", cc)], scale=lg[:, cc:cc + 1], bias=lb[:, cc:cc + 1])
            for cc in range(4):
                TT(yaT2[p2][:, cc, :], T4[cc][:], sz4[cc][:], ALU.mult, [("T4", cc), ("sz", cc)], [("yaT", p2, cc)])

        def st_c(tq):
            p2 = tq % 2
            for u in range(4):
                i = 4 * tq + u
                outproj_tile(i, [yaT2[p2][:, cc, u * 128:(u + 1) * 128] for cc in range(4)], [0, 1, 2, 3],
                             [("yaT", p2, cc) for cc in range(4)], (4, 5))

        for step in range(6):
            if step < 4:
                st_a(step)
            if 0 <= step - 1 < 4:
                st_b(step - 1)
            if step - 2 >= 0:
                st_c(step - 2)
        P.barrier()
        if stop == "conv":
            return
        for hh in range(2):
            moba_half(hh, W)
            P.barrier()
            if stop is not None:
                return

    def moba_half(hh, W):
        cv = Carver()
        QT = cv.get([128, 4, S], BF16)
        KT = cv.get([128, 4, S], BF16)
        Vt = cv.get([128, NT, 4, 66], BF16)
        szy = cv.get([128, NT, 256], BF16)
        ptb = [cv.get([128, 512], BF16) for _ in range(3)]
        gq = cv.get([128, 1], F32)
        gk = cv.get([128, 1], F32)
        kmf = cv.get([128, 4, 8], F32)
        kmb = cv.get([128, 4, 8], BF16)
        gs = cv.get([128, 4, 8], F32)
        cmp_ = cv.get([128, 4, 8, 8], F32)
        rank = cv.get([128, 4, 8], F32)
        nm = cv.get([128, 4, 32], BF16)
        rden = [cv.get([128, 1], F32) for _ in range(8)]
        yT = [cv.get([128, 2, 128], BF16) for _ in range(2)]
        tmpf = [cv.get([128, 512], F32) for _ in range(2)]
        s8 = [cv.get([128, 8], F32) for _ in range(3)]
        qa = [cv.get([128, 4, 128], BF16) for _ in range(3)]
        ka = [cv.get([128, 4, 128], BF16) for _ in range(3)]
        c_q = 1536 + hh * 256
        c_k = 2048 + hh * 256
        c_v = 2560 + hh * 256
        c_z = 3072 + hh * 256
        s = load_w(W, [(c_q, 256), (c_k, 256)])
        s2 = load_w(W, [(c_v, 256), (c_z, 256)])
        load_wo(0, 4 + 2 * hh, 2)
        load_gain_col(gq, bq_d, 1.0)
        load_gain_col(gk, bk_d, 8.0)
        for b in range(3):
            MSET(qa[b][:, :, 64:128], 0.0, [("qa", b)])
            MSET(ka[b][:, :, 64:128], 0.0, [("ka", b)])
        MSET(Vt[:, :, :, 64:65], 1.0, ["Vt"])
        MSET(nm[:], 0.0, ["nm"])

        def m0(i):
            r = i % 3
            CPY(qa[r][:, :, 96:100], qal[:, i, hh * 16:hh * 16 + 16].rearrange("p (h c) -> p h c", c=4), ["const"], [("qa", r)])
            CPY(ka[r][:, :, 64:100], kalm[:, i, :].unsqueeze(1).to_broadcast([128, 4, 36]), ["const"], [("ka", r)])
            for k in range(8):
                mm(pf[r][:], hT[:, k, i * 128:(i + 1) * 128], wbuf[s][:, k, :], k == 0, k == 7, [("hT", i), ("w", s)], [("pf", r)])

        def m1(i):
            r = i % 3
            rstd_a(pf[r], ("pf", r), 512, tmpf[i % 2], ("tf", i % 2), s8[r], ("s8", r))

        def m2(i):
            r = i % 3
            rstd_b(512, s8[r], ("s8", r))
            TT(qa[r][:, :, 0:64], pf[r][:, 0:256].rearrange("p (h d) -> p h d", d=64), s8[r][:, 0:4].unsqueeze(2).to_broadcast([128, 4, 64]),
               ALU.mult, [("pf", r), ("s8", r)], [("qa", r)])
            TT(ka[r][:, :, 0:64], pf[r][:, 256:512].rearrange("p (h d) -> p h d", d=64), s8[r][:, 4:8].unsqueeze(2).to_broadcast([128, 4, 64]),
               ALU.mult, [("pf", r), ("s8", r)], [("ka", r)])

        def m3(i):
            r = i % 3
            b2 = i % 2
            pt = pb[b2]
            for h in range(4):
                tr(pt[:, h * 128:(h + 1) * 128], qa[r][:, h, :], [("qa", r)], [("pb", b2)])
            for h in range(4):
                tr(pt[:, (4 + h) * 128:(5 + h) * 128], ka[r][:, h, :], [("ka", r)], [("pb", b2)])
            act(QT[:, :, i * 128:(i + 1) * 128], pt[:, 0:512].rearrange("p (h t) -> p h t", h=4), AF.Copy, [("pb", b2), "gq"], [("QT", i)],
                scale=gq[:, 0:1])
            act(KT[:, :, i * 128:(i + 1) * 128], pt[:, 512:1024].rearrange("p (h t) -> p h t", h=4), AF.Copy, [("pb", b2), "gq"], [("KT", i)],
                scale=gk[:, 0:1])

        pipe4(NT, m0, m1, m2, m3)
        if hh == 1 and 1 in layers:
            prefetch_w1([("qa", r) for r in range(3)] + [("ka", r) for r in range(3)] + [("s8", r) for r in range(3)] + [("tf", 0), ("tf", 1)])
        if stop == "m1":
            return
        for i in range(NT):
            b2 = 2 + i % 2
            pp = pf[b2]
            for k in range(8):
                mm(pp[:], hT[:, k, i * 128:(i + 1) * 128], wbuf[s2][:, k, :], k == 0, k == 7, [("hT", i), ("w", s2)], [("pf", b2)])
            act(Vt[:, i, :, 0:64], pp[:, 0:256].rearrange("p (h d) -> p h d", d=64), AF.Copy, [("pf", b2), "Vt"], [("V", i)])
            act(szy[:, i, :], pp[:, 256:512], AF.Silu, [("pf", b2)], [("szy", i)])
        if stop == "m3":
            return
        for h in range(4):
            RED(kmf[:, h, :], KT[:, h, :].rearrange("p (n t) -> p n t", t=256), ALU.add, [("KT", i) for i in range(NT)], ["kmf"])
        TS(kmb[:], kmf[:], 1.0 / 256, None, ALU.mult, None, ["kmf"], ["kmb"])
        if stop == "mk":
            return
        for i in range(8, NT):
            npast = i // 2
            b2 = 4 + i % 2
            pg = pf[b2]
            for h in range(4):
                mm(pg[:, h * 8:h * 8 + 8], QT[0:64, h, i * 128:(i + 1) * 128], kmb[0:64, h, :], True, True, [("QT", i), "kmb"], [("pf", b2)])
            CPY(gs[:], pg[:, 0:32].rearrange("p (h n) -> p h n", n=8), [("pf", b2)], ["gs"])
            TT(cmp_[:, :, 0:npast, 0:npast], gs[:, :, 0:npast].unsqueeze(2).to_broadcast([128, 4, npast, npast]),
               gs[:, :, 0:npast].unsqueeze(3).to_broadcast([128, 4, npast, npast]), ALU.is_gt, ["gs"], ["cmp"])
            RED(rank[:, :, 0:npast], cmp_[:, :, 0:npast, 0:npast], ALU.add, ["cmp"], ["rank"])
            TS(nm[:, :, 0:npast], rank[:, :, 0:npast], 2.5, NEGM, ALU.is_ge, ALU.mult, ["rank"], ["nm"])
            pt = pb[i % 2]
            tr(pt[:, 0:128], nm[:].rearrange("p h n -> p (h n)"), ["nm"], [("pb", i % 2)])
            for h in range(4):
                act(QT[64:96, h, i * 128:(i + 1) * 128], pt[h * 32:(h + 1) * 32, 0:128], AF.Copy, [("pb", i % 2)], [("QT", i)])

        if stop == "m2":
            return

        def plan(qc):
            items = []
            for kt in range(4 * qc + 4):
                if kt < 4 * qc:
                    items.append((kt, 0, 3, {}))
                else:
                    m = kt - 4 * qc
                    items.append((kt, m, 3, {m: tri[:]}))
            return items

        for h in range(4):
            def finish(i, Ob, okey, h=h):
                r = (i + 4 * h) % 8
                rd = rden[r]
                RCP(rd[:], Ob[:, 64:65], [okey], [("rden", r)])
                dst = szy[:, i, h * 64:(h + 1) * 64]
                STT(dst, Ob[:, 0:64], rd[:], dst, ALU.mult, ALU.mult, [okey, ("rden", r), ("szy", i)], [("szy", i)])

            attention(QT[:, h, :], lambda kt, h=h: KT[:, h, kt * 128:(kt + 1) * 128], 128,
                      lambda kt, h=h: Vt[:, kt, h, 0:65], 65, plan, finish,
                      lambda qc: [("QT", 4 * qc + u) for u in range(4)], lambda kt: [("KT", kt)], lambda kt: [("V", kt)],
                      ptb, "m")
        if stop == "ma":
            return
        outproj_half(szy, yT)

    def rstd_a(pp, pkey, ncol, tf, tfkey, s8i, s8key):
        nh = ncol // 64
        act(tf[:, 0:ncol], pp[:, 0:ncol], AF.Square, [pkey], [tfkey])
        RED(s8i[:, 0:nh], tf[:, 0:ncol].rearrange("p (h d) -> p h d", d=64), ALU.add, [tfkey], [s8key])
        TS(s8i[:, 0:nh], s8i[:, 0:nh], 64.0 * EPS, None, ALU.add, None, [s8key], [s8key])
        act(s8i[:, 0:nh], s8i[:, 0:nh], AF.Sqrt, [s8key], [s8key])

    def rstd_b(ncol, s8i, s8key):
        nh = ncol // 64
        RCP(s8i[:, 0:nh], s8i[:, 0:nh], [s8key], [s8key])

    def head_rstd(pp, pkey, ncol, tf, tfkey, s8i, s8key):
        rstd_a(pp, pkey, ncol, tf, tfkey, s8i, s8key)
        rstd_b(ncol, s8i, s8key)

    def pipe4(n, s0, s1, s2, s3):
        for step in range(n + 2):
            if step < n:
                s0(step)
                s1(step)
            if 1 <= step <= n:
                s2(step - 1)
            if step >= 2:
                s3(step - 2)

    def load_gain_col(dst, gd, mult):
        MSET(dst[:], 1.0, ["gq"])
        dma("sp", dst[0:64, :], gd.rearrange("o d -> d o"), "c", writes=["gq"], slow=True)
        if mult != 1.0:
            TS(dst[0:64, :], dst[0:64, :], mult, None, ALU.mult, None, ["gq"], ["gq"])

    def outproj_half(szy, yT, store=False):
        def oa(i):
            pt = pb[i % 2]
            for c in range(2):
                tr(pt[:, c * 128:(c + 1) * 128], szy[:, i, c * 128:(c + 1) * 128], [("szy", i)], [("pb", i % 2)])
            y = yT[i % 2]
            act(y[:], pt[:, 0:256].rearrange("p (c t) -> p c t", c=2), AF.Copy, [("pb", i % 2)], [("yT", i % 2)])

        def ob(i):
            y = yT[i % 2]
            outproj_tile(i, [y[:, 0, :], y[:, 1, :]], [0, 1], [("yT", i % 2)], (4, 5))
            if store:
                dma("sp", yv[:, i, :], x_sb[:, i, :], "out", reads=[("x", i)])

        lagged(NT, oa, ob)

    def plan_causal(qc):
        items = []
        for kt in range(4 * qc + 4):
            if kt < 4 * qc:
                items.append((kt, 0, 3, {}))
            else:
                m = kt - 4 * qc
                items.append((kt, m, 3, {m: tri[:]}))
        return items

    def plan_band(wt):
        def plan(qc):
            items = []
            for kt in range(max(0, 4 * qc - wt), 4 * qc + 4):
                jlo = max(0, kt - 4 * qc)
                jhi = min(3, kt + wt - 4 * qc)
                if jlo > jhi:
                    continue
                masks = {}
                if 0 <= kt - 4 * qc <= 3:
                    masks[kt - 4 * qc] = tri[:]
                if 0 <= kt + wt - 4 * qc <= 3:
                    masks[kt + wt - 4 * qc] = upm[:]
                items.append((kt, jlo, jhi, masks))
            return items
        return plan

    W1_OFF = 64 * 1024

    def prefetch_w1(war_keys=()):
        cvp = Carver(base=W1_OFF)
        wA = cvp.get([128, 32, 256], BF16)
        for kv, wd in enumerate((ckw1_d, cvw1_d)):
            wv = wd.rearrange("(l d) j -> d l j", d=64)
            for lq in range(4):
                dma("pool", wA[kv * 64:(kv + 1) * 64, lq * 8:(lq + 1) * 8, :], wv[:, lq * 8:(lq + 1) * 8, :], "w1", writes=["w1A"] + list(war_keys))
        return wA

    def layer1():
        W = owin_d
        wA = Carver(base=W1_OFF).get([128, 32, 256], BF16)
        if 0 not in layers:
            prefetch_w1()
        cvc = Carver(base=16 * 1024)
        wB = cvc.get([128, 32, 256], BF16)
        dma("sp", wB[64:128, :, :], wA[0:64, :, :], "c", writes=["w1B"])
        dma("sp", wB[0:64, :, :], wA[64:128, :, :], "c", writes=["w1B"])
        w1 = [[wA, wB], [wB, wA]]
        B = compress_loads(W, cvc)
        norm_phase(1)
        compress_stage(W, B, w1)
        P.barrier()
        if stop == "cmp":
            return
        for g in range(2):
            nsa_half(g, W)
            P.barrier()
            if stop == "nsa0":
                return
        if stop == "nsa":
            return
        for g in range(2):
            swa_half(g, W)
            P.barrier()

    def compress_loads(W, cv):
        s = load_w(W, [(512, 128), (640, 128)])
        B = {}
        B["s"] = s
        B["KVD"] = [cv.get([128, 16, 128], BF16) for _ in range(2)]
        B["w2"] = [cv.get([128, 2, 64], BF16) for _ in range(2)]
        B["posn"] = cv.get([32, 2, 64], F32)
        B["posT"] = cv.get([64, 2, 32], BF16)
        B["stgb"] = cv.get([4, 128], F32)
        B["b1sb"] = cv.get([128, 4], F32)
        B["biasj"] = cv.get([128, 4], F32)
        B["b2bc"] = cv.get([128, 2, 64], F32)
        B["gcm"] = cv.get([128, 64], F32)
        B["kalc"] = cv.get([128, 36], BF16)
        B["hid"] = [cv.get([128, 2, 128], BF16) for _ in range(4)]
        B["kcf"] = cv.get([128, 64], F32)
        B["junk2"] = cv.get([128, 64], F32)
        B["ssc"] = cv.get([128, 1], F32)
        B["kaug"] = cv.get([128, 128], BF16)
        for kv, wd in enumerate((ckw2_d, cvw2_d)):
            dma("pool", B["w2"][kv][:], wd.rearrange("(c p) d -> p c d", p=128), "w2", writes=["w2"])
        dma("sp", B["posn"][:, 0, :], cposk_d, "c", writes=["posn"])
        dma("sp", B["posn"][:, 1, :], cposv_d, "c", writes=["posn"])
        dma("sp", B["stgb"][0:2, :], ckb1_d.rearrange("(a c) -> a c", c=128), "c", writes=["stgb"])
        dma("sp", B["stgb"][2:4, :], cvb1_d.rearrange("(a c) -> a c", c=128), "c", writes=["stgb"])
        dma("sp", B["b2bc"][:, 0, :], ckb2_d.partition_broadcast(128), "c", writes=["b2bc"])
        dma("sp", B["b2bc"][:, 1, :], cvb2_d.partition_broadcast(128), "c", writes=["b2bc"])
        dma("sp", B["gcm"][:], ckc_d.partition_broadcast(128), "c", writes=["gcm"])
        dma("pool", B["kalc"][:], kalc_d, "c", writes=["kalc"])
        for g in range(2):
            dma("pool", Vca[:, g, 65:97], ovl_d, "c", writes=[("Vca", g)])
            MSET(Vca[:, g, 64:65], 1.0, [("Vca", g)])
        MSET(B["kaug"][:], 0.0, ["kaug"])
        return B

    def compress_stage(W, B, w1):
        s = B["s"]
        KVD, w2, posn, posT, stgb, b1sb, biasj = B["KVD"], B["w2"], B["posn"], B["posT"], B["stgb"], B["b1sb"], B["biasj"]
        b2bc, gcm, kalc, hid, kcf, junk2, ssc, kaug = B["b2bc"], B["gcm"], B["kalc"], B["hid"], B["kcf"], B["junk2"], B["ssc"], B["kaug"]
        for kv in range(2):
            tr(pf[0][0:64, kv * 32:(kv + 1) * 32], posn[:, kv, :], ["posn"], [("pf", 0)], idn=identf[0:32, 0:32])
        tr(pf[0][:, 64:68], stgb[:], ["stgb"], [("pf", 0)], idn=identf[0:4, 0:4])
        CPY(posT[:], pf[0][0:64, 0:64].rearrange("p (k l) -> p k l", l=32), [("pf", 0)], ["posT"])
        CPY(b1sb[:], pf[0][:, 64:68], [("pf", 0)], ["b1sb"])
        for kv in range(2):
            for jc in range(2):
                col = kv * 2 + jc
                for l in range(32):
                    mm(pf[1][:, col:col + 1], w1[kv][0][0:64, l, jc * 128:(jc + 1) * 128], posT[0:64, kv, l:l + 1], l == 0, l == 31,
                       ["w1B", "posT"], [("pf", 1)])
        TT(biasj[:], pf[1][:, 0:4], b1sb[:], ALU.add, [("pf", 1), "b1sb"], ["biasj"])
        for tq in range(4):
            for which in range(2):
                pp = pf[2 + which]
                for k in range(8):
                    mm(pp[:], wbuf[s][:, k, which * 128:(which + 1) * 128], hT[:, k, tq * 512:(tq + 1) * 512], k == 0, k == 7,
                       [("w", s)] + [("hT", 4 * tq + u) for u in range(4)], [("pf", 2 + which)])
                act(KVD[which][:, :, tq * 32:(tq + 1) * 32].rearrange("p r m -> p m r"), pp[:].rearrange("p (m r) -> p m r", r=16),
                    AF.Copy, [("pf", 2 + which)], [("KVT", which)])
        n = 0
        for kv in range(2):
            for g in range(2):
                hb_ = hid[kv * 2 + g]
                for jc in range(2):
                    pp = pf[n % 2]
                    for l in range(32):
                        mm(pp[:, 0:127], w1[kv][g][g * 64:(g + 1) * 64, l, jc * 128:(jc + 1) * 128],
                           KVD[kv][g * 64:(g + 1) * 64, l % 16, (l // 16):(l // 16) + 127], l == 0, l == 31,
                           ["w1B", ("KVT", kv)], [("pf", n % 2)])
                    act(hb_[:, jc, 0:127], pp[:, 0:127], AF.Silu, [("pf", n % 2), "biasj"], [("hid", kv * 2 + g)],
                        bias=biasj[:, kv * 2 + jc:kv * 2 + jc + 1], scale=1.0)
                    n += 1
                po = pf[2 + g]
                for jc in range(2):
                    mm(po[0:127, 0:64], hb_[:, jc, 0:127], w2[kv][:, jc, :], jc == 0, jc == 1, [("hid", kv * 2 + g), "w2"], [("pf", 2 + g)])
                if kv == 0:
                    TT(kcf[0:127, :], po[0:127, 0:64], b2bc[0:127, 0, :], ALU.add, [("pf", 2 + g), "b2bc"], ["kcf"])
                    act(junk2[0:127, :], kcf[0:127, :], AF.Square, ["kcf"], ["junk2", "ssc"], accum_out=ssc[0:127, :])
                    TS(ssc[0:127, :], ssc[0:127, :], 1.0 / 64, EPS, ALU.mult, ALU.add, ["ssc"], ["ssc"])
                    act(ssc[0:127, :], ssc[0:127, :], AF.Sqrt, ["ssc"], ["ssc"])
                    RCP(ssc[0:127, :], ssc[0:127, :], ["ssc"], ["ssc"])
                    STT(kaug[0:127, 0:64], kcf[0:127, :], ssc[0:127, :], gcm[0:127, :], ALU.mult, ALU.mult, ["kcf", "ssc", "gcm"], ["kaug"])
                    CPY(kaug[:, 64:100], kalc[:], ["kalc"], ["kaug"])
                    tr(pb[g][:, 0:128], kaug[:], ["kaug"], [("pb", g)])
                    act(KTc[:, g, :], pb[g][:, 0:128], AF.Copy, [("pb", g)], [("KTc", g)])
                else:
                    TT(Vca[0:127, g, 0:64], po[0:127, 0:64], b2bc[0:127, 1, :], ALU.add, [("pf", 2 + g), "b2bc"], [("Vca", g)])

    def nsa_half(g, W):
        cv = Carver()
        QT = cv.get([128, 4, S], BF16)
        KTs = cv.get([128, S], BF16)
        KTw = cv.get([128, S], BF16)
        Vs = cv.get([128, NT, 66], BF16)
        Vw = cv.get([128, NT, 66], BF16)
        szy = cv.get([128, NT, 256], BF16)
        gates = cv.get([128, NT, 12], F32)
        oacc = cv.get([128, NT, 256], F32)
        imp = cv.get([128, 8, 32], F32)
        impc = cv.get([128, 8, 32], F32)
        cmn = cv.get([128, S], BF16)
        qa = [cv.get([128, 4, 128], BF16) for _ in range(3)]
        ksa = [cv.get([128, 128], BF16) for _ in range(3)]
        kwa = [cv.get([128, 128], BF16) for _ in range(3)]
        ptb = [cv.get([128, 512], BF16) for _ in range(3)]
        tmpf = [cv.get([128, 512], F32) for _ in range(2)]
        cmpb = cv.get([128, 32, 32], F32)
        rank = cv.get([128, 32], F32)
        nmt = cv.get([128, 128], BF16)
        s8 = [cv.get([128, 8], F32) for _ in range(3)]
        gq = cv.get([128, 1], F32)
        gks = cv.get([128, 1], F32)
        gkw = cv.get([128, 1], F32)
        rden = [cv.get([128, 1], F32) for _ in range(8)]
        yT = [cv.get([128, 2, 128], BF16) for _ in range(2)]
        s = load_w(W, [(g * 256, 256), (768 + 64 * g, 64), (1024 + 64 * g, 64), (896 + 64 * g, 64), (1152 + 64 * g, 64)])
        s2 = load_w(W, [(1304 + g * 256, 256), (1280 + 4 * g, 4), (1288 + 4 * g, 4), (1296 + 4 * g, 4)])
        load_wo(1, 2 * g, 2)
        load_gain_col(gq, cq_d, 1.0)
        load_gain_col(gks, cks_d, 8.0)
        load_gain_col(gkw, ckw_d, 8.0)
        dma("sp", impc[:], impc_d[:, 8:16, :], "c", writes=["impc"])
        dma("pool", cmn[:], cmn_d, "c", writes=["addmask"])
        for b in range(3):
            MSET(qa[b][:, :, 64:128], 0.0, [("qa", b)])
            MSET(ksa[b][:, 64:128], 0.0, [("ksa", b)])
            MSET(kwa[b][:, 64:128], 0.0, [("kwa", b)])
        MSET(Vs[:, :, 64:65], 1.0, ["Vs"])
        MSET(Vw[:, :, 64:65], 1.0, ["Vw"])
        MSET(nmt[:], 0.0, ["nmt"])
        def pa(i):
            b2 = i % 2
            pp = pf[b2]
            qai, ksi, kwi = qa[b2], ksa[b2], kwa[b2]
            CPY(qai[:, :, 96:100], qal[:, i, g * 16:g * 16 + 16].rearrange("p (h c) -> p h c", c=4), ["const"], [("qa", b2)])
            CPY(ksi[:, 64:100], kals[:, i, :], ["const"], [("ksa", b2)])
            CPY(kwi[:, 64:100], kalp[:, i, :], ["const"], [("kwa", b2)])
            for k in range(8):
                mm(pp[:], hT[:, k, i * 128:(i + 1) * 128], wbuf[s][:, k, :], k == 0, k == 7, [("hT", i), ("w", s)], [("pf", b2)])
            tf = tmpf[b2]
            head_rstd(pp, ("pf", b2), 384, tf, ("tf", b2), s8[b2], ("s8", b2))
            TT(qai[:, :, 0:64], pp[:, 0:256].rearrange("p (h d) -> p h d", d=64), s8[b2][:, 0:4].unsqueeze(2).to_broadcast([128, 4, 64]),
               ALU.mult, [("pf", b2), ("s8", b2)], [("qa", b2)])
            TS(ksi[:, 0:64], pp[:, 256:320], s8[b2][:, 4:5], None, ALU.mult, None, [("pf", b2), ("s8", b2)], [("ksa", b2)])
            TS(kwi[:, 0:64], pp[:, 320:384], s8[b2][:, 5:6], None, ALU.mult, None, [("pf", b2), ("s8", b2)], [("kwa", b2)])
            act(Vs[:, i, 0:64], pp[:, 384:448], AF.Copy, [("pf", b2), "Vs"], [("Vs", i)])
            act(Vw[:, i, 0:64], pp[:, 448:512], AF.Copy, [("pf", b2), "Vw"], [("Vw", i)])

        def pbk(i):
            b2 = i % 2
            qai, ksi, kwi = qa[b2], ksa[b2], kwa[b2]
            pt = pb[b2]
            for h in range(4):
                tr(pt[:, h * 128:(h + 1) * 128], qai[:, h, :], [("qa", b2)], [("pb", b2)])
            tr(pt[:, 512:640], ksi[:], [("ksa", b2)], [("pb", b2)])
            tr(pt[:, 640:768], kwi[:], [("kwa", b2)], [("pb", b2)])
            act(QT[:, :, i * 128:(i + 1) * 128], pt[:, 0:512].rearrange("p (h t) -> p h t", h=4), AF.Copy, [("pb", b2), "gq"], [("QT", i)],
                scale=gq[:, 0:1])
            act(KTs[:, i * 128:(i + 1) * 128], pt[:, 512:640], AF.Copy, [("pb", b2), "gq"], [("KTs", i)], scale=gks[:, 0:1])
            act(KTw[:, i * 128:(i + 1) * 128], pt[:, 640:768], AF.Copy, [("pb", b2), "gq"], [("KTw", i)], scale=gkw[:, 0:1])

        lagged(NT, pa, pbk)
        for i in range(NT):
            b2 = 2 + i % 2
            pp = pf[b2]
            for k in range(8):
                mm(pp[:, 0:256], hT[:, k, i * 128:(i + 1) * 128], wbuf[s2][:, k, 0:256], k == 0, k == 7, [("hT", i), ("w", s2)], [("pf", b2)])
            act(szy[:, i, :], pp[:, 0:256], AF.Silu, [("pf", b2)], [("szy", i)])
        for i in range(NT):
            b2 = 2 + i % 2
            pp = pf[b2]
            for k in range(8):
                mm(pp[:, 0:12], hT[:, k, i * 128:(i + 1) * 128], wbuf[s2][:, k, 256:268], k == 0, k == 7, [("hT", i), ("w", s2)], [("pf", b2)])
            act(gates[:, i, :], pp[:, 0:12], AF.Sigmoid, [("pf", b2)], [("gates", i)])
        if stop == "nsaproj":
            return
        qk = lambda qc: [("QT", 4 * qc + u) for u in range(4)]
        for r in range(4):
            def fin_cmp(i, Ob, okey, r=r):
                ri = (i + 4 * r) % 8
                rd = rden[ri]
                TS(rd[:], Ob[:, 64:65], 1e-30, None, ALU.max, None, [okey], [("rden", ri)])
                RCP(rd[:], rd[:], [("rden", ri)], [("rden", ri)])
                TS(oacc[:, i, r * 64:(r + 1) * 64], Ob[:, 0:64], rd[:], gates[:, i, r:r + 1], ALU.mult, ALU.mult,
                   [okey, ("rden", ri), ("gates", i)], [("oacc", i)])
                if i >= 8:
                    if r == 0:
                        TS(imp[:, i - 8, :], Ob[:, 65:97], rd[:], None, ALU.mult, None, [okey, ("rden", ri)], [("imp", i)])
                    else:
                        STT(imp[:, i - 8, :], Ob[:, 65:97], rd[:], imp[:, i - 8, :], ALU.mult, ALU.add, [okey, ("rden", ri), ("imp", i)], [("imp", i)])

            attention(QT[:, r, :], lambda kt: KTc[:, g, 0:127], 127, lambda kt: Vca[0:127, g, 0:97], 97,
                      lambda qc: [(0, 0, 3, {})], fin_cmp, qk, lambda kt: [("KTc", g)], lambda kt: [("Vca", g)], ptb, "n", addmask=cmn)
        if stop == "nsacmp":
            return
        for i in range(8, NT):
            im = imp[:, i - 8, :]
            TT(im, im, impc[:, i - 8, :], ALU.add, [("imp", i), "impc"], [("imp", i)])
            TT(cmpb[:], im.unsqueeze(1).to_broadcast([128, 32, 32]), im.unsqueeze(2).to_broadcast([128, 32, 32]), ALU.is_gt,
               [("imp", i)], ["cmpb"])
            RED(rank[:], cmpb[:], ALU.add, ["cmpb"], ["rank"])
            TS(nmt[:, 0:32], rank[:], 15.5, NEGM, ALU.is_ge, ALU.mult, ["rank"], ["nmt"])
            pt = pb[i % 2]
            tr(pt[:, 0:128], nmt[:], ["nmt"], [("pb", i % 2)])
            for r in range(4):
                act(QT[64:96, r, i * 128:(i + 1) * 128], pt[0:32, 0:128], AF.Copy, [("pb", i % 2)], [("QT", i)])
        for (KTx, Vx, kname, vname, plan, gcol) in ((KTs, Vs, "KTs", "Vs", plan_causal, 4), (KTw, Vw, "KTw", "Vw", plan_band(4), 8)):
            for r in range(4):
                def fin_add(i, Ob, okey, r=r, gcol=gcol):
                    ri = (i + 4 * r) % 8
                    rd = rden[ri]
                    RCP(rd[:], Ob[:, 64:65], [okey], [("rden", ri)])
                    TT(rd[:], rd[:], gates[:, i, gcol + r:gcol + r + 1], ALU.mult, [("rden", ri), ("gates", i)], [("rden", ri)])
                    dst = oacc[:, i, r * 64:(r + 1) * 64]
                    STT(dst, Ob[:, 0:64], rd[:], dst, ALU.mult, ALU.add, [okey, ("rden", ri), ("oacc", i)], [("oacc", i)])

                attention(QT[:, r, :], lambda kt, KTx=KTx: KTx[:, kt * 128:(kt + 1) * 128], 128,
                          lambda kt, Vx=Vx: Vx[:, kt, 0:65], 65, plan, fin_add, qk,
                          lambda kt, kname=kname: [(kname, kt)], lambda kt, vname=vname: [(vname, kt)], ptb, "n")
        for i in range(NT):
            TT(szy[:, i, :], oacc[:, i, :], szy[:, i, :], ALU.mult, [("oacc", i), ("szy", i)], [("szy", i)])
        outproj_half(szy, yT)

    def swa_half(g, W):
        cv = Carver()
        QT = cv.get([128, 4, S], BF16)
        KT = cv.get([128, S], BF16)
        Vt = cv.get([128, NT, 66], BF16)
        szy = cv.get([128, NT, 256], BF16)
        qa = [cv.get([128, 4, 128], BF16) for _ in range(3)]
        ka = [cv.get([128, 128], BF16) for _ in range(3)]
        ptb = [cv.get([128, 512], BF16) for _ in range(3)]
        tmpf = [cv.get([128, 512], F32) for _ in range(2)]
        s8 = [cv.get([128, 8], F32) for _ in range(3)]
        gq = cv.get([128, 1], F32)
        gk = cv.get([128, 1], F32)
        esink = cv.get([128, 8], F32)
        rden = [cv.get([128, 1], F32) for _ in range(8)]
        yT = [cv.get([128, 2, 128], BF16) for _ in range(2)]
        load_gain_col(gq, dq_d, 1.0)
        load_gain_col(gk, dk_d, 8.0)
        dma("sp", esink[:], dsink_d.partition_broadcast(128), "c", writes=["esink"])
        act(esink[:], esink[:], AF.Exp, ["esink"], ["esink"])
        for b in range(3):
            MSET(qa[b][:, :, 64:128], 0.0, [("qa", b)])
            MSET(ka[b][:, 64:128], 0.0, [("ka", b)])
        MSET(Vt[:, :, 64:65], 1.0, ["Vt"])
        s = load_w(W, [(1816 + g * 256, 256), (2328 + 64 * g, 64), (2456 + 64 * g, 64)])
        s2 = load_w(W, [(2584 + g * 256, 256)])
        load_wo(1, 4 + 2 * g, 2)
        def pa(i):
            b2 = i % 2
            pp = pf[b2]
            qai, kai = qa[b2], ka[b2]
            CPY(qai[:, :, 96:100], qal[:, i, g * 16:g * 16 + 16].rearrange("p (h c) -> p h c", c=4), ["const"], [("qa", b2)])
            CPY(kai[:, 64:100], kalp[:, i, :], ["const"], [("ka", b2)])
            for k in range(8):
                mm(pp[:, 0:384], hT[:, k, i * 128:(i + 1) * 128], wbuf[s][:, k, 0:384], k == 0, k == 7, [("hT", i), ("w", s)], [("pf", b2)])
            tf = tmpf[b2]
            head_rstd(pp, ("pf", b2), 320, tf, ("tf", b2), s8[b2], ("s8", b2))
            TT(qai[:, :, 0:64], pp[:, 0:256].rearrange("p (h d) -> p h d", d=64), s8[b2][:, 0:4].unsqueeze(2).to_broadcast([128, 4, 64]),
               ALU.mult, [("pf", b2), ("s8", b2)], [("qa", b2)])
            TS(kai[:, 0:64], pp[:, 256:320], s8[b2][:, 4:5], None, ALU.mult, None, [("pf", b2), ("s8", b2)], [("ka", b2)])
            act(Vt[:, i, 0:64], pp[:, 320:384], AF.Copy, [("pf", b2), "Vt"], [("V", i)])

        def pbk(i):
            b2 = i % 2
            qai, kai = qa[b2], ka[b2]
            pt = pb[b2]
            for h in range(4):
                tr(pt[:, h * 128:(h + 1) * 128], qai[:, h, :], [("qa", b2)], [("pb", b2)])
            tr(pt[:, 512:640], kai[:], [("ka", b2)], [("pb", b2)])
            act(QT[:, :, i * 128:(i + 1) * 128], pt[:, 0:512].rearrange("p (h t) -> p h t", h=4), AF.Copy, [("pb", b2), "gq"], [("QT", i)],
                scale=gq[:, 0:1])
            act(KT[:, i * 128:(i + 1) * 128], pt[:, 512:640], AF.Copy, [("pb", b2), "gq"], [("KT", i)], scale=gk[:, 0:1])

        lagged(NT, pa, pbk)
        for i in range(NT):
            b2 = 2 + i % 2
            pp = pf[b2]
            for k in range(8):
                mm(pp[:, 0:256], hT[:, k, i * 128:(i + 1) * 128], wbuf[s2][:, k, 0:256], k == 0, k == 7, [("hT", i), ("w", s2)], [("pf", b2)])
            act(szy[:, i, :], pp[:, 0:256], AF.Silu, [("pf", b2)], [("szy", i)])
        for r in range(4):
            def fin(i, Ob, okey, r=r):
                ri = (i + 4 * r) % 8
                rd = rden[ri]
                TS(rd[:], Ob[:, 64:65], esink[:, 4 * g + r:4 * g + r + 1], None, ALU.add, None, [okey, "esink"], [("rden", ri)])
                RCP(rd[:], rd[:], [("rden", ri)], [("rden", ri)])
                dst = szy[:, i, r * 64:(r + 1) * 64]
                STT(dst, Ob[:, 0:64], rd[:], dst, ALU.mult, ALU.mult, [okey, ("rden", ri), ("szy", i)], [("szy", i)])

            attention(QT[:, r, :], lambda kt: KT[:, kt * 128:(kt + 1) * 128], 128, lambda kt: Vt[:, kt, 0:65], 65,
                      plan_band(1), fin, lambda qc: [("QT", 4 * qc + u) for u in range(4)], lambda kt: [("KT", kt)],
                      lambda kt: [("V", kt)], ptb, "d")
        outproj_half(szy, yT, store=(g == 1 and stop is None))

    if 0 in layers:
        layer0()
    if 1 in layers:
        layer1()

    if not (1 in layers and stop is None):
        for i in range(NT):
            dma("sp", yv[:, i, :], x_sb[:, i, :], "out", reads=[("x", i)])
    P.finish("sp", list(P.chan_count.keys()))
    P.emit()
    es.close()
    return nc


_CACHE = {}


def kernel(**inputs):
    consts = _consts()
    shared = {}
    shared["norm_g"] = np.ascontiguousarray(inputs["norm_g"], dtype=np.float32)
    shared["w_out"] = np.ascontiguousarray(inputs["w_out"], dtype=np.float32)
    shared["e_w_in"] = np.ascontiguousarray(inputs["e_w_in"][0], dtype=np.float32)
    shared["a_conv_w"] = np.ascontiguousarray(inputs["a_conv_w"][0], dtype=np.float32)
    shared["a_conv_b"] = np.ascontiguousarray(inputs["a_conv_b"][0], dtype=np.float32)
    shared["a_ln_g"] = np.ascontiguousarray(inputs["a_ln_g"][0], dtype=np.float32)
    shared["a_ln_b"] = np.ascontiguousarray(inputs["a_ln_b"][0], dtype=np.float32)
    shared["b_qnorm_g"] = np.ascontiguousarray(inputs["b_qnorm_g"], dtype=np.float32)
    shared["b_knorm_g"] = np.ascontiguousarray(inputs["b_knorm_g"], dtype=np.float32)
    for k in ("ident", "tri", "up", "qal", "kal_moba", "kal_slc", "kal_plain", "kal_cmp", "cmaskneg", "ovl", "impc"):
        shared[k] = consts[k]
    shared["o_w_in"] = np.ascontiguousarray(inputs["o_w_in"][0], dtype=np.float32)
    for k in ("c_qnorm_g", "c_knorm_cmp_g", "c_knorm_slc_g", "c_knorm_win_g", "c_k_b2", "c_v_b2", "d_qnorm_g", "d_knorm_g", "d_sinks"):
        shared[k] = np.ascontiguousarray(inputs[k], dtype=np.float32).reshape(1, -1)
    for k in ("c_pos_k", "c_pos_v", "c_k_w1", "c_k_b1", "c_k_w2", "c_v_w1", "c_v_b1", "c_v_w2"):
        shared[k] = np.ascontiguousarray(inputs[k][0], dtype=np.float32)
    x = np.ascontiguousarray(inputs["x"], dtype=np.float32)
    nb = x.shape[0]
    layers = inputs.get("_layers", (0, 1))
    nc = build_program(layers, inputs.get("_stop"))
    in_maps = [dict(shared, x=x[b]) for b in range(nb)]
    res = run_bass_kernel_spmd(nc, in_maps, core_ids=list(range(nb)))
    return np.stack([np.asarray(r["y"], dtype=np.float32) for r in res.results], axis=0)
```

```python
import contextlib
import os
import numpy as np
import ml_dtypes
import concourse.bass as bass
import concourse.mybir as mybir
from concourse.bass_utils import run_bass_kernel_spmd

F32 = mybir.dt.float32
BF16 = mybir.dt.bfloat16
ALU = mybir.AluOpType
AF = mybir.ActivationFunctionType
AX = mybir.AxisListType

S = 2048
D = 1024
NT = 16
NEGM = -30000.0
EPS = 1e-6


class _Op:
    __slots__ = ("eng", "fn", "deps", "chan", "signal", "val", "dmaval", "chanseq")

    def __init__(self, eng, fn, chan):
        self.eng = eng
        self.fn = fn
        self.deps = {}
        self.chan = chan
        self.signal = False
        self.val = 0
        self.dmaval = None


class Prog:
    ENGS = ("pe", "act", "dve", "pool", "sp")

    def __init__(self, nc):
        self.nc = nc
        self.ops = {e: [] for e in self.ENGS}
        self.res = {}
        self.chan_count = {}
        self.chan_last = {}
        self.all_ops = []
        self.bar = {}
        self.final_waits = {}
        self.pool_ctr = 0
        self.sp_ctr = 0
        self.const_keys = []
        self.pres = {}

    PERS = ("w", "wo")

    def _st(self, k):
        if isinstance(k, tuple) and k[0] in self.PERS or k in self.PERS:
            return self.pres
        return self.res

    def op(self, eng, fn, reads=(), writes=(), chan=None, nobar=False):
        for w in writes:
            if isinstance(w, tuple) and w[0] == "ck" and w not in self.const_keys:
                self.const_keys.append(w)
        if "const" in reads:
            reads = [k for r in reads for k in (self.const_keys if r == "const" else (r,))]
        if chan is not None and eng == "pool":
            chan = "pq%d" % (self.pool_ctr % 12)
            self.pool_ctr += 1
        elif chan == "c":
            chan = "pc%d" % (self.sp_ctr % 16)
            self.sp_ctr += 1
        o = _Op(eng, fn, chan)
        deps = o.deps
        if not nobar:
            deps.update(self.bar)
        if chan is not None and (chan.startswith("pq") or chan.startswith("pc")):
            prev = self.chan_last.get(chan)
            if prev is not None:
                deps[id(prev)] = prev
        for k in reads:
            st = self._st(k).get(k)
            if st is not None and st[0] is not None:
                deps[id(st[0])] = st[0]
        for k in writes:
            st = self._st(k).get(k)
            if st is not None:
                if st[0] is not None:
                    deps[id(st[0])] = st[0]
                for r in st[1]:
                    deps[id(r)] = r
        for k in reads:
            res = self._st(k)
            st = res.get(k)
            if st is None:
                st = [None, []]
                res[k] = st
            st[1].append(o)
        for k in writes:
            self._st(k)[k] = [o, []]
        o.dmaval = dict(self.chan_count)
        if chan is not None:
            self.chan_count[chan] = self.chan_count.get(chan, 0) + 1
            o.chanseq = self.chan_count[chan]
            self.chan_last[chan] = o
            o.signal = True
        self.ops[eng].append(o)
        self.all_ops.append(o)
        return o

    def barrier(self):
        bar = {}
        for e in self.ENGS:
            if self.ops[e]:
                o = self.ops[e][-1]
                bar[id(o)] = o
        for ch, o in self.chan_last.items():
            bar[id(o)] = o
        self.bar = bar
        self.res = {}

    def finish(self, eng, chans):
        self.final_waits = {eng: {ch: self.chan_count[ch] for ch in chans}}

    def emit(self):
        nc = self.nc
        for o in self.all_ops:
            for d in o.deps.values():
                if d.chan is None:
                    if d.eng == "pe" and o.eng == "pe":
                        continue
                    d.signal = True
        for e in self.ENGS:
            c = 0
            for o in self.ops[e]:
                if o.chan is None and o.signal:
                    c += 1
                    o.val = c
        import os
        if os.environ.get("KDBG"):
            print("sem counts", {e: max([o.val for o in self.ops[e]] + [0]) for e in self.ENGS}, {e: len(self.ops[e]) for e in self.ENGS},
                  {c: 16 * v for c, v in self.chan_count.items()})
        stack = contextlib.ExitStack()
        sems = {}
        for e in self.ENGS:
            sems[e] = stack.enter_context(nc.semaphore("s_" + e))
        for ch in self.chan_count:
            sems["c_" + ch] = stack.enter_context(nc.semaphore("c_" + ch))
        block = stack.enter_context(nc.Block())
        engobj = {"pe": "tensor", "act": "scalar", "dve": "vector", "pool": "gpsimd", "sp": "sync"}

        def make(e):
            def body(eng):
                waited = {}
                for o in self.ops[e]:
                    need = {}
                    for d in o.deps.values():
                        if d.chan is not None:
                            k = "c_" + d.chan
                            if d.chan.startswith("pq") or d.chan.startswith("pc"):
                                v = 16 * d.chanseq
                            else:
                                v = 16 * o.dmaval[d.chan]
                        else:
                            if d.eng == "pe" and e == "pe":
                                continue
                            k = d.eng
                            v = d.val
                        if v > need.get(k, 0):
                            need[k] = v
                    for k, v in need.items():
                        if waited.get(k, 0) >= v:
                            continue
                        eng.wait_ge(sems[k], v)
                        waited[k] = v
                    ins = o.fn(eng)
                    if o.chan is not None:
                        ins.then_inc(sems["c_" + o.chan], 16)
                    elif o.signal:
                        ins.then_inc(sems[e], 1)
                for ch, c in self.final_waits.get(e, {}).items():
                    eng.wait_ge(sems["c_" + ch], 16 * c)
            return body

        for e in self.ENGS:
            getattr(block, engobj[e])(make(e))
        stack.close()


def _consts():
    c = {}
    c["ident"] = np.eye(128, dtype=np.float32)
    k = np.arange(128)[:, None]
    q = np.arange(128)[None, :]
    c["tri"] = np.where(k <= q, 0.0, NEGM).astype(np.float32)
    c["up"] = np.where(k > q, 0.0, NEGM).astype(np.float32)
    t = np.arange(S)
    b = (t % 16).astype(np.float32)
    a = (t - t % 16).astype(np.float32)
    slopes = np.power(2.0, -8.0 * np.arange(1, 9) / 8).astype(np.float32)
    qal = np.zeros((S, 8, 4), np.float32)
    qal[:, :, 0] = -slopes[None, :] * a[:, None]
    qal[:, :, 1] = -slopes[None, :] * b[:, None]
    qal[:, :, 2] = slopes[None, :]
    qal[:, :, 3] = slopes[None, :]
    c["qal"] = qal.reshape(NT, 128, 32).transpose(1, 0, 2).copy()

    def kal(onehot_block):
        m = np.zeros((S, 36), np.float32)
        if onehot_block:
            m[t, t // onehot_block] = 1.0
        m[:, 32] = 1.0
        m[:, 33] = 1.0
        m[:, 34] = a
        m[:, 35] = b
        return m.reshape(NT, 128, 36).transpose(1, 0, 2).copy()

    c["kal_moba"] = kal(256)
    c["kal_slc"] = kal(64)
    c["kal_plain"] = kal(0)
    cc = np.arange(128)
    cend = 16 * cc + 31
    kc = np.zeros((128, 36), np.float32)
    kc[:, 32] = 1.0
    kc[:, 33] = 1.0
    kc[:, 34] = cend - cend % 16
    kc[:, 35] = cend % 16
    c["kal_cmp"] = kc
    c["cmaskneg"] = np.where(t[None, :] >= cend[:, None], 0.0, NEGM).astype(np.float32)
    start = np.arange(127)[:, None] * 16
    bs = np.arange(32)[None, :] * 64
    ov = np.zeros((128, 32), np.float32)
    ov[:127] = ((start < bs + 64) & (start + 32 > bs)).astype(np.float32)
    c["ovl"] = ov
    blk = np.arange(32)[None, :]
    cur = (t // 64)[:, None]
    forced = (blk == 0) | (blk == cur) | (blk == cur - 1)
    impc = 1e4 * forced.astype(np.float32) - 1e5 * (blk > cur).astype(np.float32)
    c["impc"] = impc.reshape(NT, 128, 32).transpose(1, 0, 2).copy()
    return c


def build_program(layers=(0, 1), stop=None):
    nc = bass.Bass("TRN2", target_bir_lowering=False)
    es = contextlib.ExitStack()
    dram = {}

    def din(name, shape, dt=F32):
        dram[name] = nc.dram_tensor(name, list(shape), dt, kind="ExternalInput").ap()
        return dram[name]

    x_d = din("x", [S, D])
    normg_d = din("norm_g", [2, D])
    wout_d = din("w_out", [2, D, D])
    ewin_d = din("e_w_in", [D, 3584])
    convw_d = din("a_conv_w", [31, 512])
    convb_d = din("a_conv_b", [512])
    lng_d = din("a_ln_g", [512])
    lnb_d = din("a_ln_b", [512])
    bq_d = din("b_qnorm_g", [1, 64])
    bk_d = din("b_knorm_g", [1, 64])
    ident_d = din("ident", [128, 128])
    tri_d = din("tri", [128, 128])
    up_d = din("up", [128, 128])
    qal_d = din("qal", [128, NT, 32])
    kalm_d = din("kal_moba", [128, NT, 36])
    kals_d = din("kal_slc", [128, NT, 36])
    kalp_d = din("kal_plain", [128, NT, 36])
    kalc_d = din("kal_cmp", [128, 36])
    cmn_d = din("cmaskneg", [128, S])
    ovl_d = din("ovl", [128, 32])
    impc_d = din("impc", [128, NT, 32])
    owin_d = din("o_w_in", [D, 3096])
    cq_d = din("c_qnorm_g", [1, 64])
    ckc_d = din("c_knorm_cmp_g", [1, 64])
    cks_d = din("c_knorm_slc_g", [1, 64])
    ckw_d = din("c_knorm_win_g", [1, 64])
    cposk_d = din("c_pos_k", [32, 64])
    cposv_d = din("c_pos_v", [32, 64])
    ckw1_d = din("c_k_w1", [2048, 256])
    ckb1_d = din("c_k_b1", [256])
    ckw2_d = din("c_k_w2", [256, 64])
    ckb2_d = din("c_k_b2", [1, 64])
    cvw1_d = din("c_v_w1", [2048, 256])
    cvb1_d = din("c_v_b1", [256])
    cvw2_d = din("c_v_w2", [256, 64])
    cvb2_d = din("c_v_b2", [1, 64])
    dq_d = din("d_qnorm_g", [1, 64])
    dk_d = din("d_knorm_g", [1, 64])
    dsink_d = din("d_sinks", [1, 8])
    y_d = nc.dram_tensor("y", [S, D], F32, kind="ExternalOutput").ap()

    def sb(name, shape, dt):
        return es.enter_context(nc.sbuf_tensor(name, list(shape), dt))

    def psum(name, shape, dt):
        return es.enter_context(nc.psum_tensor(name, list(shape), dt))

    P = Prog(nc)

    x_sb = sb("x_sb", [128, NT, D], F32)
    hT = sb("hT", [128, 8, S], BF16)
    wbuf = [sb("wbuf%d" % i, [128, 8, 512], BF16) for i in range(2)]
    wo = sb("wo", [128, 4, D], BF16)
    ident = sb("ident_sb", [128, 128], BF16)
    identf = sb("identf_sb", [128, 128], F32)
    onesf = sb("onesf", [128, 128], F32)
    tri = sb("tri_sb", [128, 128], BF16)
    upm = sb("up_sb", [128, 128], BF16)
    qal = sb("qal_sb", [128, NT, 32], BF16)
    kalm = sb("kalm_sb", [128, NT, 36], BF16)
    KTc = sb("KTc", [128, 2, 128], BF16)
    Vca = sb("Vca", [128, 2, 98], BF16)
    kals = sb("kals_sb", [128, NT, 36], BF16)
    kalp = sb("kalp_sb", [128, NT, 36], BF16)
    ss = sb("ss", [128, NT], F32)
    rstd = sb("rstd", [128, NT], F32)
    ARENA = 80 * 1024
    arena = sb("arena", [128, ARENA // 2], BF16)

    pf = [psum("pf%d" % i, [128, 512], F32) for i in range(6)]
    pb = [psum("pb%d" % i, [128, 1024], BF16) for i in range(2)]

    class Carver:
        def __init__(self, base=0):
            self.off = base

        def get(self, shape, dt):
            n = int(np.prod(shape[1:]))
            nb = n * (4 if dt == F32 else 2)
            nb = (nb + 63) // 64 * 64
            assert self.off + nb <= ARENA, ("arena overflow", self.off + nb, ARENA)
            a = arena[:, self.off // 2:(self.off + nb) // 2]
            self.off += nb
            if os.environ.get("KDBG"):
                print("carve", shape, dt, "->", self.off)
            if dt == F32:
                a = a.bitcast(F32)
            a = a[:, 0:n]
            if len(shape) == 3:
                a = a.rearrange("p (a b) -> p a b", b=shape[2])
            elif len(shape) == 4:
                a = a.rearrange("p (a b c) -> p a b c", b=shape[2], c=shape[3])
            if shape[0] < 128:
                a = a[0:shape[0]]
            return a

    def dma(eng, out, in_, chan, reads=(), writes=(), slow=False, nobar=False):
        if slow:
            P.op(eng, lambda e: e.dma_start(out=out, in_=in_, allow_slow_non_contiguous=True), reads=reads, writes=writes, chan=chan, nobar=nobar)
        else:
            P.op(eng, lambda e: e.dma_start(out=out, in_=in_), reads=reads, writes=writes, chan=chan, nobar=nobar)

    def mm(out, lhsT, rhs, start, stop, reads, writes):
        P.op("pe", lambda e: e.matmul(out, lhsT=lhsT, rhs=rhs, start=start, stop=stop), reads=reads, writes=writes)

    def tr(out, in_, reads, writes, idn=None):
        ck = ("ck", "ident") if idn is None else ("ck", "identf")
        idn = ident[:] if idn is None else idn
        P.op("pe", lambda e: e.transpose(out=out, in_=in_, identity=idn), reads=list(reads) + [ck], writes=writes)

    def act(out, in_, func, reads, writes, **kw):
        P.op("act", lambda e: e.activation(out=out, in_=in_, func=func, **kw), reads=reads, writes=writes)

    def V(fn, reads, writes, eng="dve"):
        P.op(eng, fn, reads=reads, writes=writes)

    def TT(out, in0, in1, op, reads, writes, eng="dve"):
        P.op(eng, lambda e: e.tensor_tensor(out=out, in0=in0, in1=in1, op=op), reads=reads, writes=writes)

    def TS(out, in0, s1, s2, op0, op1, reads, writes, eng="dve"):
        if op1 is None:
            P.op(eng, lambda e: e.tensor_scalar(out=out, in0=in0, scalar1=s1, scalar2=None, op0=op0), reads=reads, writes=writes)
        else:
            P.op(eng, lambda e: e.tensor_scalar(out=out, in0=in0, scalar1=s1, scalar2=s2, op0=op0, op1=op1), reads=reads, writes=writes)

    def STT(out, in0, scalar, in1, op0, op1, reads, writes, eng="dve"):
        P.op(eng, lambda e: e.scalar_tensor_tensor(out=out, in0=in0, scalar=scalar, in1=in1, op0=op0, op1=op1), reads=reads, writes=writes)

    def RED(out, in_, op, reads, writes):
        P.op("dve", lambda e: e.tensor_reduce(out=out, in_=in_, axis=AX.X, op=op), reads=reads, writes=writes)

    def RCP(out, in_, reads, writes):
        P.op("dve", lambda e: e.reciprocal(out=out, in_=in_), reads=reads, writes=writes)

    def CPY(out, in_, reads, writes, eng="dve"):
        P.op(eng, lambda e: e.tensor_copy(out=out, in_=in_), reads=reads, writes=writes)

    def MSET(ap, val, writes, eng="dve"):
        P.op(eng, lambda e: e.memset(ap, val), reads=[], writes=writes)

    import os
    KB = os.environ.get("KBIS", "abcdefg")
    if "a" in KB:
        dma("pool", ident[:], ident_d, "c", writes=[("ck", "ident")])
    if "b" in KB:
        dma("pool", tri[:], tri_d, "c", writes=[("ck", "tri")])
        dma("pool", upm[:], up_d, "c", writes=[("ck", "up")])
    if "c" in KB:
        dma("pool", qal[:], qal_d, "c", writes=[("ck", "qal")])
    if "d" in KB:
        dma("pool", kalm[:], kalm_d, "c", writes=[("ck", "kalm")])
        dma("pool", kals[:], kals_d, "c", writes=[("ck", "kals")])
        dma("pool", kalp[:], kalp_d, "c", writes=[("ck", "kalp")])
    if "e" in KB:
        dma("sp", identf[:], ident_d, "c", writes=[("ck", "identf")])
    if "f" in KB:
        MSET(onesf[:], 1.0, [("ck", "onesf")])

    xv = x_d.rearrange("(i p) d -> p i d", p=128)
    yv = y_d.rearrange("(i p) d -> p i d", p=128)
    for i in range(NT):
        dma("sp", x_sb[:, i, :], xv[:, i, :], "x%d" % i, writes=[("x", i)])

    wslot_ctr = [0]

    def load_w(wd, segs):
        s = wslot_ctr[0] % 2
        wslot_ctr[0] += 1
        wv = wd.rearrange("(k p) n -> p k n", p=128)
        o = 0
        for (c0, n) in segs:
            dma("pool", wbuf[s][:, :, o:o + n], wv[:, :, c0:c0 + n], "w%d" % s, writes=[("w", s)], nobar=True)
            o += n
        return s

    def norm_phase(layer, base=0, barrier=True):
        cvn = Carver(base=base)
        gbc = cvn.get([128, D], F32)
        junk = cvn.get([128, D], BF16)
        hb = [cvn.get([128, D], BF16) for _ in range(2)]
        dma("act", gbc[:], normg_d[layer:layer + 1, :].partition_broadcast(128), "c", writes=["gbc"])
        MSET(ss[:], 0.0, [("ss", gI) for gI in range(4)])

        def stats(gI):
            for i in range(4 * gI, 4 * gI + 4):
                act(junk[:], x_sb[:, i, :], AF.Square, [("x", i), ("ss", gI)], ["junk", ("ss", gI)], accum_out=ss[:, i:i + 1])
            sl = slice(4 * gI, 4 * gI + 4)
            TS(rstd[:, sl], ss[:, sl], 1.0 / D, EPS, ALU.mult, ALU.add, [("ss", gI)], [("rstd", gI)])
            act(rstd[:, sl], rstd[:, sl], AF.Sqrt, [("rstd", gI)], [("rstd", gI)])
            RCP(rstd[:, sl], rstd[:, sl], [("rstd", gI)], [("rstd", gI)])

        def na(i):
            h = hb[i % 2]
            STT(h[:], x_sb[:, i, :], rstd[:, i:i + 1], gbc[:], ALU.mult, ALU.mult, [("x", i), ("rstd", i // 4), "gbc"], [("hb", i % 2)])

        def nb(i):
            h = hb[i % 2]
            pt = pb[i % 2]
            for k in range(8):
                tr(pt[:, k * 128:(k + 1) * 128], h[:, k * 128:(k + 1) * 128], [("hb", i % 2)], [("pb", i % 2)])
            act(hT[:, :, i * 128:(i + 1) * 128], pt[:].rearrange("p (k t) -> p k t", k=8), AF.Copy, [("pb", i % 2)], [("hT", i)])

        pend = []
        for gI in range(4):
            stats(gI)
            for i in range(4 * gI, 4 * gI + 4):
                na(i)
                pend.append(i)
                if len(pend) > 1:
                    nb(pend.pop(0))
        nb(pend.pop(0))
        if barrier:
            P.barrier()

    def load_wo(layer, c0, n):
        wv = wout_d[layer].rearrange("(k p) n -> p k n", p=128)
        for hf in range(2):
            dma("pool", wo[:, 0:n, hf * 512:(hf + 1) * 512], wv[:, c0:c0 + n, hf * 512:(hf + 1) * 512], "wo", writes=["wo"], nobar=True)

    def outproj_tile(i, lhs_list, chunks, yT_reads, pbanks):
        for half in range(2):
            pp = pf[pbanks[half]]
            for n, (c, lhs) in enumerate(zip(chunks, lhs_list)):
                mm(pp[:], lhs, wo[:, c, half * 512:(half + 1) * 512], n == 0, n == len(chunks) - 1,
                   list(yT_reads) + ["wo"], [("pf", pbanks[half])])
            xs = x_sb[:, i, half * 512:(half + 1) * 512]
            TT(xs, xs, pp[:], ALU.add, [("pf", pbanks[half]), ("x", i)], [("x", i)])

    att_ctr = [0, 0]

    def lagged(n, stage_a, stage_b, lag=1):
        for i in range(n + lag):
            if i < n:
                stage_a(i)
            if i >= lag:
                stage_b(i - lag)

    def mm_acc(out, lhsT, rhs, start, stop, reads, writes):
        P.op("pe", lambda e: e.matmul(out, lhsT=lhsT, rhs=rhs, start=start, stop=stop, skip_group_check=True), reads=reads, writes=writes)

    def attention(QT, KT, nk, Vrhs, ncv, plan, finish, qkeys, kkeys, vkeys, ptb, tagk, addmask=None):
        NS = len(ptb)
        work = []
        for qc in range(4):
            items = plan(qc)
            if not items:
                continue
            ob = 3 + att_ctr[1] % 3
            att_ctr[1] += 1
            lastk = {}
            for (kt, jlo, jhi, masks) in items:
                for j in range(jlo, jhi + 1):
                    lastk[j] = kt
            for n, it in enumerate(items):
                work.append((qc, ob, it, n == 0, n == len(items) - 1, lastk))

        def front(w):
            qc, ob, (kt, jlo, jhi, masks), first, last, lastk = w
            sbk = att_ctr[0] % NS
            att_ctr[0] += 1
            Sp = pf[sbk]
            pt = ptb[sbk]
            c0, c1 = jlo * 128, (jhi + 1) * 128
            extra = list(masks.items())
            mm_acc(Sp[0:nk, c0:c1], KT(kt), QT[:, qc * 512 + c0:qc * 512 + c1], True, addmask is None and not extra,
                   list(qkeys(qc)) + list(kkeys(kt)), [("pf", sbk)])
            if addmask is not None:
                mm_acc(Sp[0:nk, c0:c1], ident[0:nk, 0:nk], addmask[0:nk, qc * 512 + c0:qc * 512 + c1], False, not extra,
                       ["const", "addmask"], [("pf", sbk)])
            for n, (j, mk) in enumerate(extra):
                mm_acc(Sp[0:nk, j * 128:(j + 1) * 128], ident[0:nk, 0:nk], mk[0:nk, :], False, n == len(extra) - 1,
                       ["const"], [("pf", sbk)])
            act(pt[0:nk, c0:c1], Sp[0:nk, c0:c1], AF.Exp, [("pf", sbk)], [("pt", tagk, sbk)])
            return sbk

        def back(w, sbk, started):
            qc, ob, (kt, jlo, jhi, masks), first, last, lastk = w
            pt = ptb[sbk]
            for j in range(jlo, jhi + 1):
                mm_acc(pf[ob][:, j * 128:j * 128 + ncv], pt[0:nk, j * 128:(j + 1) * 128], Vrhs(kt), len(started) == 0, lastk[j] == kt,
                       [("pt", tagk, sbk)] + list(vkeys(kt)), [("pf", ob)])
                started.add(j)
            if last:
                for j in sorted(started):
                    finish(qc * 4 + j, pf[ob][:, j * 128:j * 128 + ncv], ("pf", ob))
                started.clear()

        LAG = NS - 1
        started = set()
        pend = []
        for w in work:
            pend.append((w, front(w)))
            if len(pend) > LAG:
                pw, psb = pend.pop(0)
                back(pw, psb, started)
        for (pw, psb) in pend:
            back(pw, psb, started)

    def layer0():
        if stop == "load":
            return
        norm_phase(0, base=70 * 1024, barrier=False)
        if stop == "norm0":
            return
        load_wo(0, 0, 4)
        W = ewin_d
        if stop == "norm":
            return
        cv = Carver()
        yc = cv.get([128, 4, S], F32)
        hc = cv.get([128, S + 32], BF16)
        dg = cv.get([128, 31, 128], BF16)
        cw = cv.get([128, 4, 31], F32)
        cb = cv.get([128, 4], F32)
        lg = cv.get([128, 4], F32)
        lb = cv.get([128, 4], F32)
        Tsq = [cv.get([128, 512], F32) for _ in range(2)]
        mu2 = [cv.get([128, 512], F32) for _ in range(2)]
        msq = cv.get([128, 512], F32)
        rs2 = [cv.get([128, 512], F32) for _ in range(2)]
        sz4 = [cv.get([128, 512], BF16) for _ in range(4)]
        T4 = [cv.get([128, 512], F32) for _ in range(4)]
        yaT2 = [cv.get([128, 4, 512], BF16) for _ in range(2)]
        HO = 32
        stg = Tsq[0][0:32, :]
        stg2 = Tsq[1][0:12, 0:128]
        dma("sp", stg[0:31, :], convw_d, "c", writes=[("Tsq", 0)])
        dma("sp", stg2[0:4, :], convb_d.rearrange("(a c) -> a c", c=128), "c", writes=[("Tsq", 1)])
        dma("sp", stg2[4:8, :], lng_d.rearrange("(a c) -> a c", c=128), "c", writes=[("Tsq", 1)])
        dma("sp", stg2[8:12, :], lnb_d.rearrange("(a c) -> a c", c=128), "c", writes=[("Tsq", 1)])
        for cc in range(4):
            tr(pf[0][:, cc * 32:cc * 32 + 31], stg[0:31, cc * 128:(cc + 1) * 128], [("Tsq", 0)], [("pf", 0)], idn=identf[0:31, 0:31])
        tr(pf[0][:, 128:140], stg2[0:12, :], [("Tsq", 1)], [("pf", 0)], idn=identf[0:12, 0:12])
        CPY(cw[:], pf[0][:, 0:128].rearrange("p (a j) -> p a j", j=32)[:, :, 0:31], [("pf", 0)], ["cw"])
        CPY(cb[:], pf[0][:, 128:132], [("pf", 0)], ["cw"])
        CPY(lg[:], pf[0][:, 132:136], [("pf", 0)], ["cw"])
        CPY(lb[:], pf[0][:, 136:140], [("pf", 0)], ["cw"])
        MSET(hc[:, 0:HO], 0.0, ["hc0"])
        if stop == "convp":
            return
        for cc in range(4):
            if stop == "conv1" and cc == 1:
                return
            s = load_w(W, [(cc * 128, 128), (512 + cc * 128, 128)])
            for j in range(31):
                TS(dg[:, j, :], ident[:], cw[:, cc, j:j + 1], None, ALU.mult, None, ["const", "cw"], [("dg", j)])
            for tq in range(4):
                bv, bg = (tq % 2) * 2, (tq % 2) * 2 + 1
                for which, bk in ((0, bv), (1, bg)):
                    for k in range(8):
                        mm(pf[bk][:], wbuf[s][:, k, which * 128:(which + 1) * 128], hT[:, k, tq * 512:(tq + 1) * 512], k == 0, k == 7,
                           [("w", s)] + [("hT", 4 * tq + u) for u in range(4)], [("pf", bk)])
                act(T4[tq % 2][:], pf[bg][:], AF.Sigmoid, [("pf", bg)], [("T4", tq % 2)])
                TT(hc[:, HO + tq * 512:HO + (tq + 1) * 512], pf[bv][:], T4[tq % 2][:], ALU.mult, [("pf", bv), ("T4", tq % 2)], [("hc", tq)])
            for tq in range(4):
                pc = pf[4 + tq % 2]
                for j in range(31):
                    o = HO - 30 + j + tq * 512
                    mm(pc[:], dg[:, j, :], hc[:, o:o + 512], j == 0, j == 30,
                       [("dg", j), ("hc", tq)] + ([("hc", tq - 1)] if tq > 0 else ["hc0"]), [("pf", 4 + tq % 2)])
                act(yc[:, cc, tq * 512:(tq + 1) * 512], pc[:], AF.Identity, [("pf", 4 + tq % 2), "cw"], [("yc", cc, tq)],
                    bias=cb[:, cc:cc + 1], scale=1.0)
        if stop == "conv4":
            return
        P.barrier()
        sz_slot = load_w(W, [(1024, 512)])

        def st_a(tq):
            ts = slice(tq * 512, (tq + 1) * 512)
            p2 = tq % 2
            for cc in range(4):
                mm(pf[0][:], onesf[:], yc[:, cc, ts], cc == 0, cc == 3, [("yc", cc, tq), "const"], [("pf", 0)])
            for cc in range(4):
                act(Tsq[cc % 2][:], yc[:, cc, ts], AF.Square, [("yc", cc, tq)], [("Tsq", cc % 2)])
                mm(pf[1][:], onesf[:], Tsq[cc % 2][:], cc == 0, cc == 3, [("Tsq", cc % 2), "const"], [("pf", 1)])
            TS(mu2[p2][:], pf[0][:], 1.0 / 512, None, ALU.mult, None, [("pf", 0)], [("mu", p2)])
            TT(msq[:], mu2[p2][:], mu2[p2][:], ALU.mult, [("mu", p2)], ["msq"])
            STT(rs2[p2][:], pf[1][:], 1.0 / 512, msq[:], ALU.mult, ALU.subtract, [("pf", 1), "msq"], [("rs", p2)])
            TS(rs2[p2][:], rs2[p2][:], EPS, None, ALU.add, None, [("rs", p2)], [("rs", p2)])
            act(rs2[p2][:], rs2[p2][:], AF.Sqrt, [("rs", p2)], [("rs", p2)])
            RCP(rs2[p2][:], rs2[p2][:], [("rs", p2)], [("rs", p2)])

        def st_b(tq):
            ts = slice(tq * 512, (tq + 1) * 512)
            p2 = tq % 2
            for cc in range(4):
                pz = pf[2 + cc % 2]
                for k in range(8):
                    mm(pz[:], wbuf[sz_slot][:, k, cc * 128:(cc + 1) * 128], hT[:, k, ts], k == 0, k == 7,
                       [("w", sz_slot)] + [("hT", 4 * tq + u) for u in range(4)], [("pf", 2 + cc % 2)])
                act(sz4[cc][:], pz[:], AF.Silu, [("pf", 2 + cc % 2)], [("sz", cc)])
            for cc in range(4):
                tt = T4[cc]
                TT(tt[:], yc[:, cc, ts], mu2[p2][:], ALU.subtract, [("yc", cc, tq), ("mu", p2)], [("T4", cc)])
                TT(tt[:], tt[:], rs2[p2][:], ALU.mult, [("T4", cc), ("rs", p2)], [("T4", cc)])
            for cc in range(4):
                tt = T4[cc]
                act(tt[:], tt[:], AF.Silu, [("T4", cc), "cw"], [("T4", cc)], scale=lg[:, cc:cc + 1], bias=lb[:, cc:cc + 1])
            for cc in range(4):
                TT(yaT2[p2][:, cc, :], T4[cc][:], sz4[cc][:], ALU.mult, [("T4", cc), ("sz", cc)], [("yaT", p2, cc)])

        def st_c(tq):
            p2 = tq % 2
            for u in range(4):
                i = 4 * tq + u
                outproj_tile(i, [yaT2[p2][:, cc, u * 128:(u + 1) * 128] for cc in range(4)], [0, 1, 2, 3],
                             [("yaT", p2, cc) for cc in range(4)], (4, 5))

        for step in range(6):
            if step < 4:
                st_a(step)
            if 0 <= step - 1 < 4:
                st_b(step - 1)
            if step - 2 >= 0:
                st_c(step - 2)
        P.barrier()
        if stop == "conv":
            return
        for hh in range(2):
            moba_half(hh, W)
            P.barrier()
            if stop is not None:
                return

    def moba_half(hh, W):
        cv = Carver()
        QT = cv.get([128, 4, S], BF16)
        KT = cv.get([128, 4, S], BF16)
        Vt = cv.get([128, NT, 4, 66], BF16)
        szy = cv.get([128, NT, 256], BF16)
        ptb = [cv.get([128, 512], BF16) for _ in range(3)]
        gq = cv.get([128, 1], F32)
        gk = cv.get([128, 1], F32)
        kmf = cv.get([128, 4, 8], F32)
        kmb = cv.get([128, 4, 8], BF16)
        gs = cv.get([128, 4, 8], F32)
        cmp_ = cv.get([128, 4, 8, 8], F32)
        rank = cv.get([128, 4, 8], F32)
        nm = cv.get([128, 4, 32], BF16)
        rden = [cv.get([128, 1], F32) for _ in range(8)]
        yT = [cv.get([128, 2, 128], BF16) for _ in range(2)]
        tmpf = [cv.get([128, 512], F32) for _ in range(2)]
        s8 = [cv.get([128, 8], F32) for _ in range(3)]
        qa = [cv.get([128, 4, 128], BF16) for _ in range(3)]
        ka = [cv.get([128, 4, 128], BF16) for _ in range(3)]
        c_q = 1536 + hh * 256
        c_k = 2048 + hh * 256
        c_v = 2560 + hh * 256
        c_z = 3072 + hh * 256
        s = load_w(W, [(c_q, 256), (c_k, 256)])
        s2 = load_w(W, [(c_v, 256), (c_z, 256)])
        load_wo(0, 4 + 2 * hh, 2)
        load_gain_col(gq, bq_d, 1.0)
        load_gain_col(gk, bk_d, 8.0)
        for b in range(3):
            MSET(qa[b][:, :, 64:128], 0.0, [("qa", b)])
            MSET(ka[b][:, :, 64:128], 0.0, [("ka", b)])
        MSET(Vt[:, :, :, 64:65], 1.0, ["Vt"])
        MSET(nm[:], 0.0, ["nm"])

        def m0(i):
            r = i % 3
            CPY(qa[r][:, :, 96:100], qal[:, i, hh * 16:hh * 16 + 16].rearrange("p (h c) -> p h c", c=4), ["const"], [("qa", r)])
            CPY(ka[r][:, :, 64:100], kalm[:, i, :].unsqueeze(1).to_broadcast([128, 4, 36]), ["const"], [("ka", r)])
            for k in range(8):
                mm(pf[r][:], hT[:, k, i * 128:(i + 1) * 128], wbuf[s][:, k, :], k == 0, k == 7, [("hT", i), ("w", s)], [("pf", r)])

        def m1(i):
            r = i % 3
            rstd_a(pf[r], ("pf", r), 512, tmpf[i % 2], ("tf", i % 2), s8[r], ("s8", r))

        def m2(i):
            r = i % 3
            rstd_b(512, s8[r], ("s8", r))
            TT(qa[r][:, :, 0:64], pf[r][:, 0:256].rearrange("p (h d) -> p h d", d=64), s8[r][:, 0:4].unsqueeze(2).to_broadcast([128, 4, 64]),
               ALU.mult, [("pf", r), ("s8", r)], [("qa", r)])
            TT(ka[r][:, :, 0:64], pf[r][:, 256:512].rearrange("p (h d) -> p h d", d=64), s8[r][:, 4:8].unsqueeze(2).to_broadcast([128, 4, 64]),
               ALU.mult, [("pf", r), ("s8", r)], [("ka", r)])

        def m3(i):
            r = i % 3
            b2 = i % 2
            pt = pb[b2]
            for h in range(4):
                tr(pt[:, h * 128:(h + 1) * 128], qa[r][:, h, :], [("qa", r)], [("pb", b2)])
            for h in range(4):
                tr(pt[:, (4 + h) * 128:(5 + h) * 128], ka[r][:, h, :], [("ka", r)], [("pb", b2)])
            act(QT[:, :, i * 128:(i + 1) * 128], pt[:, 0:512].rearrange("p (h t) -> p h t", h=4), AF.Copy, [("pb", b2), "gq"], [("QT", i)],
                scale=gq[:, 0:1])
            act(KT[:, :, i * 128:(i + 1) * 128], pt[:, 512:1024].rearrange("p (h t) -> p h t", h=4), AF.Copy, [("pb", b2), "gq"], [("KT", i)],
                scale=gk[:, 0:1])

        pipe4(NT, m0, m1, m2, m3)
        if hh == 1 and 1 in layers:
            prefetch_w1([("qa", r) for r in range(3)] + [("ka", r) for r in range(3)] + [("s8", r) for r in range(3)] + [("tf", 0), ("tf", 1)])
        if stop == "m1":
            return
        for i in range(NT):
            b2 = 2 + i % 2
            pp = pf[b2]
            for k in range(8):
                mm(pp[:], hT[:, k, i * 128:(i + 1) * 128], wbuf[s2][:, k, :], k == 0, k == 7, [("hT", i), ("w", s2)], [("pf", b2)])
            act(Vt[:, i, :, 0:64], pp[:, 0:256].rearrange("p (h d) -> p h d", d=64), AF.Copy, [("pf", b2), "Vt"], [("V", i)])
            act(szy[:, i, :], pp[:, 256:512], AF.Silu, [("pf", b2)], [("szy", i)])
        if stop == "m3":
            return
        for h in range(4):
            RED(kmf[:, h, :], KT[:, h, :].rearrange("p (n t) -> p n t", t=256), ALU.add, [("KT", i) for i in range(NT)], ["kmf"])
        TS(kmb[:], kmf[:], 1.0 / 256, None, ALU.mult, None, ["kmf"], ["kmb"])
        if stop == "mk":
            return
        for i in range(8, NT):
            npast = i // 2
            b2 = 4 + i % 2
            pg = pf[b2]
            for h in range(4):
                mm(pg[:, h * 8:h * 8 + 8], QT[0:64, h, i * 128:(i + 1) * 128], kmb[0:64, h, :], True, True, [("QT", i), "kmb"], [("pf", b2)])
            CPY(gs[:], pg[:, 0:32].rearrange("p (h n) -> p h n", n=8), [("pf", b2)], ["gs"])
            TT(cmp_[:, :, 0:npast, 0:npast], gs[:, :, 0:npast].unsqueeze(2).to_broadcast([128, 4, npast, npast]),
               gs[:, :, 0:npast].unsqueeze(3).to_broadcast([128, 4, npast, npast]), ALU.is_gt, ["gs"], ["cmp"])
            RED(rank[:, :, 0:npast], cmp_[:, :, 0:npast, 0:npast], ALU.add, ["cmp"], ["rank"])
            TS(nm[:, :, 0:npast], rank[:, :, 0:npast], 2.5, NEGM, ALU.is_ge, ALU.mult, ["rank"], ["nm"])
            pt = pb[i % 2]
            tr(pt[:, 0:128], nm[:].rearrange("p h n -> p (h n)"), ["nm"], [("pb", i % 2)])
            for h in range(4):
                act(QT[64:96, h, i * 128:(i + 1) * 128], pt[h * 32:(h + 1) * 32, 0:128], AF.Copy, [("pb", i % 2)], [("QT", i)])

        if stop == "m2":
            return

        def plan(qc):
            items = []
            for kt in range(4 * qc + 4):
                if kt < 4 * qc:
                    items.append((kt, 0, 3, {}))
                else:
                    m = kt - 4 * qc
                    items.append((kt, m, 3, {m: tri[:]}))
            return items

        for h in range(4):
            def finish(i, Ob, okey, h=h):
                r = (i + 4 * h) % 8
                rd = rden[r]
                RCP(rd[:], Ob[:, 64:65], [okey], [("rden", r)])
                dst = szy[:, i, h * 64:(h + 1) * 64]
                STT(dst, Ob[:, 0:64], rd[:], dst, ALU.mult, ALU.mult, [okey, ("rden", r), ("szy", i)], [("szy", i)])

            attention(QT[:, h, :], lambda kt, h=h: KT[:, h, kt * 128:(kt + 1) * 128], 128,
                      lambda kt, h=h: Vt[:, kt, h, 0:65], 65, plan, finish,
                      lambda qc: [("QT", 4 * qc + u) for u in range(4)], lambda kt: [("KT", kt)], lambda kt: [("V", kt)],
                      ptb, "m")
        if stop == "ma":
            return
        outproj_half(szy, yT)

    def rstd_a(pp, pkey, ncol, tf, tfkey, s8i, s8key):
        nh = ncol // 64
        act(tf[:, 0:ncol], pp[:, 0:ncol], AF.Square, [pkey], [tfkey])
        RED(s8i[:, 0:nh], tf[:, 0:ncol].rearrange("p (h d) -> p h d", d=64), ALU.add, [tfkey], [s8key])
        TS(s8i[:, 0:nh], s8i[:, 0:nh], 64.0 * EPS, None, ALU.add, None, [s8key], [s8key])
        act(s8i[:, 0:nh], s8i[:, 0:nh], AF.Sqrt, [s8key], [s8key])

    def rstd_b(ncol, s8i, s8key):
        nh = ncol // 64
        RCP(s8i[:, 0:nh], s8i[:, 0:nh], [s8key], [s8key])

    def head_rstd(pp, pkey, ncol, tf, tfkey, s8i, s8key):
        rstd_a(pp, pkey, ncol, tf, tfkey, s8i, s8key)
        rstd_b(ncol, s8i, s8key)

    def pipe4(n, s0, s1, s2, s3):
        for step in range(n + 2):
            if step < n:
                s0(step)
                s1(step)
            if 1 <= step <= n:
                s2(step - 1)
            if step >= 2:
                s3(step - 2)

    def load_gain_col(dst, gd, mult):
        MSET(dst[:], 1.0, ["gq"])
        dma("sp", dst[0:64, :], gd.rearrange("o d -> d o"), "c", writes=["gq"], slow=True)
        if mult != 1.0:
            TS(dst[0:64, :], dst[0:64, :], mult, None, ALU.mult, None, ["gq"], ["gq"])

    def outproj_half(szy, yT, store=False):
        def oa(i):
            pt = pb[i % 2]
            for c in range(2):
                tr(pt[:, c * 128:(c + 1) * 128], szy[:, i, c * 128:(c + 1) * 128], [("szy", i)], [("pb", i % 2)])
            y = yT[i % 2]
            act(y[:], pt[:, 0:256].rearrange("p (c t) -> p c t", c=2), AF.Copy, [("pb", i % 2)], [("yT", i % 2)])

        def ob(i):
            y = yT[i % 2]
            outproj_tile(i, [y[:, 0, :], y[:, 1, :]], [0, 1], [("yT", i % 2)], (4, 5))
            if store:
                dma("sp", yv[:, i, :], x_sb[:, i, :], "out", reads=[("x", i)])

        lagged(NT, oa, ob)

    def plan_causal(qc):
        items = []
        for kt in range(4 * qc + 4):
            if kt < 4 * qc:
                items.append((kt, 0, 3, {}))
            else:
                m = kt - 4 * qc
                items.append((kt, m, 3, {m: tri[:]}))
        return items

    def plan_band(wt):
        def plan(qc):
            items = []
            for kt in range(max(0, 4 * qc - wt), 4 * qc + 4):
                jlo = max(0, kt - 4 * qc)
                jhi = min(3, kt + wt - 4 * qc)
                if jlo > jhi:
                    continue
                masks = {}
                if 0 <= kt - 4 * qc <= 3:
                    masks[kt - 4 * qc] = tri[:]
                if 0 <= kt + wt - 4 * qc <= 3:
                    masks[kt + wt - 4 * qc] = upm[:]
                items.append((kt, jlo, jhi, masks))
            return items
        return plan

    W1_OFF = 64 * 1024

    def prefetch_w1(war_keys=()):
        cvp = Carver(base=W1_OFF)
        wA = cvp.get([128, 32, 256], BF16)
        for kv, wd in enumerate((ckw1_d, cvw1_d)):
            wv = wd.rearrange("(l d) j -> d l j", d=64)
            for lq in range(4):
                dma("pool", wA[kv * 64:(kv + 1) * 64, lq * 8:(lq + 1) * 8, :], wv[:, lq * 8:(lq + 1) * 8, :], "w1", writes=["w1A"] + list(war_keys))
        return wA

    def layer1():
        W = owin_d
        wA = Carver(base=W1_OFF).get([128, 32, 256], BF16)
        if 0 not in layers:
            prefetch_w1()
        cvc = Carver(base=16 * 1024)
        wB = cvc.get([128, 32, 256], BF16)
        dma("sp", wB[64:128, :, :], wA[0:64, :, :], "c", writes=["w1B"])
        dma("sp", wB[0:64, :, :], wA[64:128, :, :], "c", writes=["w1B"])
        w1 = [[wA, wB], [wB, wA]]
        B = compress_loads(W, cvc)
        norm_phase(1)
        compress_stage(W, B, w1)
        P.barrier()
        if stop == "cmp":
            return
        for g in range(2):
            nsa_half(g, W)
            P.barrier()
            if stop == "nsa0":
                return
        if stop == "nsa":
            return
        for g in range(2):
            swa_half(g, W)
            P.barrier()

    def compress_loads(W, cv):
        s = load_w(W, [(512, 128), (640, 128)])
        B = {}
        B["s"] = s
        B["KVD"] = [cv.get([128, 16, 128], BF16) for _ in range(2)]
        B["w2"] = [cv.get([128, 2, 64], BF16) for _ in range(2)]
        B["posn"] = cv.get([32, 2, 64], F32)
        B["posT"] = cv.get([64, 2, 32], BF16)
        B["stgb"] = cv.get([4, 128], F32)
        B["b1sb"] = cv.get([128, 4], F32)
        B["biasj"] = cv.get([128, 4], F32)
        B["b2bc"] = cv.get([128, 2, 64], F32)
        B["gcm"] = cv.get([128, 64], F32)
        B["kalc"] = cv.get([128, 36], BF16)
        B["hid"] = [cv.get([128, 2, 128], BF16) for _ in range(4)]
        B["kcf"] = cv.get([128, 64], F32)
        B["junk2"] = cv.get([128, 64], F32)
        B["ssc"] = cv.get([128, 1], F32)
        B["kaug"] = cv.get([128, 128], BF16)
        for kv, wd in enumerate((ckw2_d, cvw2_d)):
            dma("pool", B["w2"][kv][:], wd.rearrange("(c p) d -> p c d", p=128), "w2", writes=["w2"])
        dma("sp", B["posn"][:, 0, :], cposk_d, "c", writes=["posn"])
        dma("sp", B["posn"][:, 1, :], cposv_d, "c", writes=["posn"])
        dma("sp", B["stgb"][0:2, :], ckb1_d.rearrange("(a c) -> a c", c=128), "c", writes=["stgb"])
        dma("sp", B["stgb"][2:4, :], cvb1_d.rearrange("(a c) -> a c", c=128), "c", writes=["stgb"])
        dma("sp", B["b2bc"][:, 0, :], ckb2_d.partition_broadcast(128), "c", writes=["b2bc"])
        dma("sp", B["b2bc"][:, 1, :], cvb2_d.partition_broadcast(128), "c", writes=["b2bc"])
        dma("sp", B["gcm"][:], ckc_d.partition_broadcast(128), "c", writes=["gcm"])
        dma("pool", B["kalc"][:], kalc_d, "c", writes=["kalc"])
        for g in range(2):
            dma("pool", Vca[:, g, 65:97], ovl_d, "c", writes=[("Vca", g)])
        return B

    def compress_stage(W, B, w1):
        s = B["s"]
        KVD, w2, posn, posT, stgb, b1sb, biasj = B["KVD"], B["w2"], B["posn"], B["posT"], B["stgb"], B["b1sb"], B["biasj"]
        b2bc, gcm, kalc, hid, kcf, junk2, ssc, kaug = B["b2bc"], B["gcm"], B["kalc"], B["hid"], B["kcf"], B["junk2"], B["ssc"], B["kaug"]
        for g in range(2):
            MSET(Vca[:, g, 64:65], 1.0, [("Vca", g)])
        MSET(kaug[:], 0.0, ["kaug"])
        for kv in range(2):
            tr(pf[0][0:64, kv * 32:(kv + 1) * 32], posn[:, kv, :], ["posn"], [("pf", 0)], idn=identf[0:32, 0:32])
        tr(pf[0][:, 64:68], stgb[:], ["stgb"], [("pf", 0)], idn=identf[0:4, 0:4])
        CPY(posT[:], pf[0][0:64, 0:64].rearrange("p (k l) -> p k l", l=32), [("pf", 0)], ["posT"])
        CPY(b1sb[:], pf[0][:, 64:68], [("pf", 0)], ["b1sb"])
        for kv in range(2):
            for jc in range(2):
                col = kv * 2 + jc
                for l in range(32):
                    mm(pf[1][:, col:col + 1], w1[kv][0][0:64, l, jc * 128:(jc + 1) * 128], posT[0:64, kv, l:l + 1], l == 0, l == 31,
                       ["w1B", "posT"], [("pf", 1)])
        TT(biasj[:], pf[1][:, 0:4], b1sb[:], ALU.add, [("pf", 1), "b1sb"], ["biasj"])
        for tq in range(4):
            for which in range(2):
                pp = pf[2 + which]
                for k in range(8):
                    mm(pp[:], wbuf[s][:, k, which * 128:(which + 1) * 128], hT[:, k, tq * 512:(tq + 1) * 512], k == 0, k == 7,
                       [("w", s)] + [("hT", 4 * tq + u) for u in range(4)], [("pf", 2 + which)])
                act(KVD[which][:, :, tq * 32:(tq + 1) * 32].rearrange("p r m -> p m r"), pp[:].rearrange("p (m r) -> p m r", r=16),
                    AF.Copy, [("pf", 2 + which)], [("KVT", which)])
        n = 0
        for kv in range(2):
            for g in range(2):
                hb_ = hid[kv * 2 + g]
                for jc in range(2):
                    pp = pf[n % 2]
                    for l in range(32):
                        mm(pp[:, 0:127], w1[kv][g][g * 64:(g + 1) * 64, l, jc * 128:(jc + 1) * 128],
                           KVD[kv][g * 64:(g + 1) * 64, l % 16, (l // 16):(l // 16) + 127], l == 0, l == 31,
                           ["w1B", ("KVT", kv)], [("pf", n % 2)])
                    act(hb_[:, jc, 0:127], pp[:, 0:127], AF.Silu, [("pf", n % 2), "biasj"], [("hid", kv * 2 + g)],
                        bias=biasj[:, kv * 2 + jc:kv * 2 + jc + 1], scale=1.0)
                    n += 1
                po = pf[2 + g]
                for jc in range(2):
                    mm(po[0:127, 0:64], hb_[:, jc, 0:127], w2[kv][:, jc, :], jc == 0, jc == 1, [("hid", kv * 2 + g), "w2"], [("pf", 2 + g)])
                if kv == 0:
                    TT(kcf[0:127, :], po[0:127, 0:64], b2bc[0:127, 0, :], ALU.add, [("pf", 2 + g), "b2bc"], ["kcf"])
                    act(junk2[0:127, :], kcf[0:127, :], AF.Square, ["kcf"], ["junk2", "ssc"], accum_out=ssc[0:127, :])
                    TS(ssc[0:127, :], ssc[0:127, :], 1.0 / 64, EPS, ALU.mult, ALU.add, ["ssc"], ["ssc"])
                    act(ssc[0:127, :], ssc[0:127, :], AF.Sqrt, ["ssc"], ["ssc"])
                    RCP(ssc[0:127, :], ssc[0:127, :], ["ssc"], ["ssc"])
                    STT(kaug[0:127, 0:64], kcf[0:127, :], ssc[0:127, :], gcm[0:127, :], ALU.mult, ALU.mult, ["kcf", "ssc", "gcm"], ["kaug"])
                    CPY(kaug[:, 64:100], kalc[:], ["kalc"], ["kaug"])
                    tr(pb[g][:, 0:128], kaug[:], ["kaug"], [("pb", g)])
                    act(KTc[:, g, :], pb[g][:, 0:128], AF.Copy, [("pb", g)], [("KTc", g)])
                else:
                    TT(Vca[0:127, g, 0:64], po[0:127, 0:64], b2bc[0:127, 1, :], ALU.add, [("pf", 2 + g), "b2bc"], [("Vca", g)])

    def nsa_half(g, W):
        cv = Carver()
        QT = cv.get([128, 4, S], BF16)
        KTs = cv.get([128, S], BF16)
        KTw = cv.get([128, S], BF16)
        Vs = cv.get([128, NT, 66], BF16)
        Vw = cv.get([128, NT, 66], BF16)
        szy = cv.get([128, NT, 256], BF16)
        gates = cv.get([128, NT, 12], F32)
        oacc = cv.get([128, NT, 256], F32)
        imp = cv.get([128, 8, 32], F32)
        impc = cv.get([128, 8, 32], F32)
        cmn = cv.get([128, S], BF16)
        qa = [cv.get([128, 4, 128], BF16) for _ in range(3)]
        ksa = [cv.get([128, 128], BF16) for _ in range(3)]
        kwa = [cv.get([128, 128], BF16) for _ in range(3)]
        ptb = [cv.get([128, 512], BF16) for _ in range(3)]
        tmpf = [cv.get([128, 512], F32) for _ in range(2)]
        cmpb = cv.get([128, 32, 32], F32)
        rank = cv.get([128, 32], F32)
        nmt = cv.get([128, 128], BF16)
        s8 = [cv.get([128, 8], F32) for _ in range(3)]
        gq = cv.get([128, 1], F32)
        gks = cv.get([128, 1], F32)
        gkw = cv.get([128, 1], F32)
        rden = [cv.get([128, 1], F32) for _ in range(8)]
        yT = [cv.get([128, 2, 128], BF16) for _ in range(2)]
        s = load_w(W, [(g * 256, 256), (768 + 64 * g, 64), (1024 + 64 * g, 64), (896 + 64 * g, 64), (1152 + 64 * g, 64)])
        s2 = load_w(W, [(1304 + g * 256, 256), (1280 + 4 * g, 4), (1288 + 4 * g, 4), (1296 + 4 * g, 4)])
        load_wo(1, 2 * g, 2)
        load_gain_col(gq, cq_d, 1.0)
        load_gain_col(gks, cks_d, 8.0)
        load_gain_col(gkw, ckw_d, 8.0)
        dma("sp", impc[:], impc_d[:, 8:16, :], "c", writes=["impc"])
        dma("pool", cmn[:], cmn_d, "c", writes=["addmask"])
        for b in range(3):
            MSET(qa[b][:, :, 64:128], 0.0, [("qa", b)])
            MSET(ksa[b][:, 64:128], 0.0, [("ksa", b)])
            MSET(kwa[b][:, 64:128], 0.0, [("kwa", b)])
        MSET(Vs[:, :, 64:65], 1.0, ["Vs"])
        MSET(Vw[:, :, 64:65], 1.0, ["Vw"])
        MSET(nmt[:], 0.0, ["nmt"])
        def pa(i):
            b2 = i % 2
            pp = pf[b2]
            qai, ksi, kwi = qa[b2], ksa[b2], kwa[b2]
            CPY(qai[:, :, 96:100], qal[:, i, g * 16:g * 16 + 16].rearrange("p (h c) -> p h c", c=4), ["const"], [("qa", b2)])
            CPY(ksi[:, 64:100], kals[:, i, :], ["const"], [("ksa", b2)])
            CPY(kwi[:, 64:100], kalp[:, i, :], ["const"], [("kwa", b2)])
            for k in range(8):
                mm(pp[:], hT[:, k, i * 128:(i + 1) * 128], wbuf[s][:, k, :], k == 0, k == 7, [("hT", i), ("w", s)], [("pf", b2)])
            tf = tmpf[b2]
            head_rstd(pp, ("pf", b2), 384, tf, ("tf", b2), s8[b2], ("s8", b2))
            TT(qai[:, :, 0:64], pp[:, 0:256].rearrange("p (h d) -> p h d", d=64), s8[b2][:, 0:4].unsqueeze(2).to_broadcast([128, 4, 64]),
               ALU.mult, [("pf", b2), ("s8", b2)], [("qa", b2)])
            TS(ksi[:, 0:64], pp[:, 256:320], s8[b2][:, 4:5], None, ALU.mult, None, [("pf", b2), ("s8", b2)], [("ksa", b2)])
            TS(kwi[:, 0:64], pp[:, 320:384], s8[b2][:, 5:6], None, ALU.mult, None, [("pf", b2), ("s8", b2)], [("kwa", b2)])
            act(Vs[:, i, 0:64], pp[:, 384:448], AF.Copy, [("pf", b2), "Vs"], [("Vs", i)])
            act(Vw[:, i, 0:64], pp[:, 448:512], AF.Copy, [("pf", b2), "Vw"], [("Vw", i)])

        def pbk(i):
            b2 = i % 2
            qai, ksi, kwi = qa[b2], ksa[b2], kwa[b2]
            pt = pb[b2]
            for h in range(4):
                tr(pt[:, h * 128:(h + 1) * 128], qai[:, h, :], [("qa", b2)], [("pb", b2)])
            tr(pt[:, 512:640], ksi[:], [("ksa", b2)], [("pb", b2)])
            tr(pt[:, 640:768], kwi[:], [("kwa", b2)], [("pb", b2)])
            act(QT[:, :, i * 128:(i + 1) * 128], pt[:, 0:512].rearrange("p (h t) -> p h t", h=4), AF.Copy, [("pb", b2), "gq"], [("QT", i)],
                scale=gq[:, 0:1])
            act(KTs[:, i * 128:(i + 1) * 128], pt[:, 512:640], AF.Copy, [("pb", b2), "gq"], [("KTs", i)], scale=gks[:, 0:1])
            act(KTw[:, i * 128:(i + 1) * 128], pt[:, 640:768], AF.Copy, [("pb", b2), "gq"], [("KTw", i)], scale=gkw[:, 0:1])

        lagged(NT, pa, pbk)
        for i in range(NT):
            b2 = 2 + i % 2
            pp = pf[b2]
            for k in range(8):
                mm(pp[:, 0:256], hT[:, k, i * 128:(i + 1) * 128], wbuf[s2][:, k, 0:256], k == 0, k == 7, [("hT", i), ("w", s2)], [("pf", b2)])
            act(szy[:, i, :], pp[:, 0:256], AF.Silu, [("pf", b2)], [("szy", i)])
        for i in range(NT):
            b2 = 2 + i % 2
            pp = pf[b2]
            for k in range(8):
                mm(pp[:, 0:12], hT[:, k, i * 128:(i + 1) * 128], wbuf[s2][:, k, 256:268], k == 0, k == 7, [("hT", i), ("w", s2)], [("pf", b2)])
            act(gates[:, i, :], pp[:, 0:12], AF.Sigmoid, [("pf", b2)], [("gates", i)])
        if stop == "nsaproj":
            return
        qk = lambda qc: [("QT", 4 * qc + u) for u in range(4)]
        for r in range(4):
            def fin_cmp(i, Ob, okey, r=r):
                ri = (i + 4 * r) % 8
                rd = rden[ri]
                TS(rd[:], Ob[:, 64:65], 1e-30, None, ALU.max, None, [okey], [("rden", ri)])
                RCP(rd[:], rd[:], [("rden", ri)], [("rden", ri)])
                TS(oacc[:, i, r * 64:(r + 1) * 64], Ob[:, 0:64], rd[:], gates[:, i, r:r + 1], ALU.mult, ALU.mult,
                   [okey, ("rden", ri), ("gates", i)], [("oacc", i)])
                if i >= 8:
                    if r == 0:
                        TS(imp[:, i - 8, :], Ob[:, 65:97], rd[:], None, ALU.mult, None, [okey, ("rden", ri)], [("imp", i)])
                    else:
                        STT(imp[:, i - 8, :], Ob[:, 65:97], rd[:], imp[:, i - 8, :], ALU.mult, ALU.add, [okey, ("rden", ri), ("imp", i)], [("imp", i)])

            attention(QT[:, r, :], lambda kt: KTc[:, g, 0:127], 127, lambda kt: Vca[0:127, g, 0:97], 97,
                      lambda qc: [(0, 0, 3, {})], fin_cmp, qk, lambda kt: [("KTc", g)], lambda kt: [("Vca", g)], ptb, "n", addmask=cmn)
        if stop == "nsacmp":
            return
        for i in range(8, NT):
            im = imp[:, i - 8, :]
            TT(im, im, impc[:, i - 8, :], ALU.add, [("imp", i), "impc"], [("imp", i)])
            TT(cmpb[:], im.unsqueeze(1).to_broadcast([128, 32, 32]), im.unsqueeze(2).to_broadcast([128, 32, 32]), ALU.is_gt,
               [("imp", i)], ["cmpb"])
            RED(rank[:], cmpb[:], ALU.add, ["cmpb"], ["rank"])
            TS(nmt[:, 0:32], rank[:], 15.5, NEGM, ALU.is_ge, ALU.mult, ["rank"], ["nmt"])
            pt = pb[i % 2]
            tr(pt[:, 0:128], nmt[:], ["nmt"], [("pb", i % 2)])
            for r in range(4):
                act(QT[64:96, r, i * 128:(i + 1) * 128], pt[0:32, 0:128], AF.Copy, [("pb", i % 2)], [("QT", i)])
        for (KTx, Vx, kname, vname, plan, gcol) in ((KTs, Vs, "KTs", "Vs", plan_causal, 4), (KTw, Vw, "KTw", "Vw", plan_band(4), 8)):
            for r in range(4):
                def fin_add(i, Ob, okey, r=r, gcol=gcol):
                    ri = (i + 4 * r) % 8
                    rd = rden[ri]
                    RCP(rd[:], Ob[:, 64:65], [okey], [("rden", ri)])
                    TT(rd[:], rd[:], gates[:, i, gcol + r:gcol + r + 1], ALU.mult, [("rden", ri), ("gates", i)], [("rden", ri)])
                    dst = oacc[:, i, r * 64:(r + 1) * 64]
                    STT(dst, Ob[:, 0:64], rd[:], dst, ALU.mult, ALU.add, [okey, ("rden", ri), ("oacc", i)], [("oacc", i)])

                attention(QT[:, r, :], lambda kt, KTx=KTx: KTx[:, kt * 128:(kt + 1) * 128], 128,
                          lambda kt, Vx=Vx: Vx[:, kt, 0:65], 65, plan, fin_add, qk,
                          lambda kt, kname=kname: [(kname, kt)], lambda kt, vname=vname: [(vname, kt)], ptb, "n")
        for i in range(NT):
            TT(szy[:, i, :], oacc[:, i, :], szy[:, i, :], ALU.mult, [("oacc", i), ("szy", i)], [("szy", i)])
        outproj_half(szy, yT)

    def swa_half(g, W):
        cv = Carver()
        QT = cv.get([128, 4, S], BF16)
        KT = cv.get([128, S], BF16)
        Vt = cv.get([128, NT, 66], BF16)
        szy = cv.get([128, NT, 256], BF16)
        qa = [cv.get([128, 4, 128], BF16) for _ in range(3)]
        ka = [cv.get([128, 128], BF16) for _ in range(3)]
        ptb = [cv.get([128, 512], BF16) for _ in range(3)]
        tmpf = [cv.get([128, 512], F32) for _ in range(2)]
        s8 = [cv.get([128, 8], F32) for _ in range(3)]
        gq = cv.get([128, 1], F32)
        gk = cv.get([128, 1], F32)
        esink = cv.get([128, 8], F32)
        rden = [cv.get([128, 1], F32) for _ in range(8)]
        yT = [cv.get([128, 2, 128], BF16) for _ in range(2)]
        load_gain_col(gq, dq_d, 1.0)
        load_gain_col(gk, dk_d, 8.0)
        dma("sp", esink[:], dsink_d.partition_broadcast(128), "c", writes=["esink"])
        act(esink[:], esink[:], AF.Exp, ["esink"], ["esink"])
        for b in range(3):
            MSET(qa[b][:, :, 64:128], 0.0, [("qa", b)])
            MSET(ka[b][:, 64:128], 0.0, [("ka", b)])
        MSET(Vt[:, :, 64:65], 1.0, ["Vt"])
        s = load_w(W, [(1816 + g * 256, 256), (2328 + 64 * g, 64), (2456 + 64 * g, 64)])
        s2 = load_w(W, [(2584 + g * 256, 256)])
        load_wo(1, 4 + 2 * g, 2)
        def pa(i):
            b2 = i % 2
            pp = pf[b2]
            qai, kai = qa[b2], ka[b2]
            CPY(qai[:, :, 96:100], qal[:, i, g * 16:g * 16 + 16].rearrange("p (h c) -> p h c", c=4), ["const"], [("qa", b2)])
            CPY(kai[:, 64:100], kalp[:, i, :], ["const"], [("ka", b2)])
            for k in range(8):
                mm(pp[:, 0:384], hT[:, k, i * 128:(i + 1) * 128], wbuf[s][:, k, 0:384], k == 0, k == 7, [("hT", i), ("w", s)], [("pf", b2)])
            tf = tmpf[b2]
            head_rstd(pp, ("pf", b2), 320, tf, ("tf", b2), s8[b2], ("s8", b2))
            TT(qai[:, :, 0:64], pp[:, 0:256].rearrange("p (h d) -> p h d", d=64), s8[b2][:, 0:4].unsqueeze(2).to_broadcast([128, 4, 64]),
               ALU.mult, [("pf", b2), ("s8", b2)], [("qa", b2)])
            TS(kai[:, 0:64], pp[:, 256:320], s8[b2][:, 4:5], None, ALU.mult, None, [("pf", b2), ("s8", b2)], [("ka", b2)])
            act(Vt[:, i, 0:64], pp[:, 320:384], AF.Copy, [("pf", b2), "Vt"], [("V", i)])

        def pbk(i):
            b2 = i % 2
            qai, kai = qa[b2], ka[b2]
            pt = pb[b2]
            for h in range(4):
                tr(pt[:, h * 128:(h + 1) * 128], qai[:, h, :], [("qa", b2)], [("pb", b2)])
            tr(pt[:, 512:640], kai[:], [("ka", b2)], [("pb", b2)])
            act(QT[:, :, i * 128:(i + 1) * 128], pt[:, 0:512].rearrange("p (h t) -> p h t", h=4), AF.Copy, [("pb", b2), "gq"], [("QT", i)],
                scale=gq[:, 0:1])
            act(KT[:, i * 128:(i + 1) * 128], pt[:, 512:640], AF.Copy, [("pb", b2), "gq"], [("KT", i)], scale=gk[:, 0:1])

        lagged(NT, pa, pbk)
        for i in range(NT):
            b2 = 2 + i % 2
            pp = pf[b2]
            for k in range(8):
                mm(pp[:, 0:256], hT[:, k, i * 128:(i + 1) * 128], wbuf[s2][:, k, 0:256], k == 0, k == 7, [("hT", i), ("w", s2)], [("pf", b2)])
            act(szy[:, i, :], pp[:, 0:256], AF.Silu, [("pf", b2)], [("szy", i)])
        for r in range(4):
            def fin(i, Ob, okey, r=r):
                ri = (i + 4 * r) % 8
                rd = rden[ri]
                TS(rd[:], Ob[:, 64:65], esink[:, 4 * g + r:4 * g + r + 1], None, ALU.add, None, [okey, "esink"], [("rden", ri)])
                RCP(rd[:], rd[:], [("rden", ri)], [("rden", ri)])
                dst = szy[:, i, r * 64:(r + 1) * 64]
                STT(dst, Ob[:, 0:64], rd[:], dst, ALU.mult, ALU.mult, [okey, ("rden", ri), ("szy", i)], [("szy", i)])

            attention(QT[:, r, :], lambda kt: KT[:, kt * 128:(kt + 1) * 128], 128, lambda kt: Vt[:, kt, 0:65], 65,
                      plan_band(1), fin, lambda qc: [("QT", 4 * qc + u) for u in range(4)], lambda kt: [("KT", kt)],
                      lambda kt: [("V", kt)], ptb, "d")
        outproj_half(szy, yT, store=(g == 1 and stop is None))

    if 0 in layers:
        layer0()
    if 1 in layers:
        layer1()

    if not (1 in layers and stop is None):
        for i in range(NT):
            dma("sp", yv[:, i, :], x_sb[:, i, :], "out", reads=[("x", i)])
    P.finish("sp", list(P.chan_count.keys()))
    P.emit()
    es.close()
    return nc


_CACHE = {}


def kernel(**inputs):
    consts = _consts()
    shared = {}
    shared["norm_g"] = np.ascontiguousarray(inputs["norm_g"], dtype=np.float32)
    shared["w_out"] = np.ascontiguousarray(inputs["w_out"], dtype=np.float32)
    shared["e_w_in"] = np.ascontiguousarray(inputs["e_w_in"][0], dtype=np.float32)
    shared["a_conv_w"] = np.ascontiguousarray(inputs["a_conv_w"][0], dtype=np.float32)
    shared["a_conv_b"] = np.ascontiguousarray(inputs["a_conv_b"][0], dtype=np.float32)
    shared["a_ln_g"] = np.ascontiguousarray(inputs["a_ln_g"][0], dtype=np.float32)
    shared["a_ln_b"] = np.ascontiguousarray(inputs["a_ln_b"][0], dtype=np.float32)
    shared["b_qnorm_g"] = np.ascontiguousarray(inputs["b_qnorm_g"], dtype=np.float32)
    shared["b_knorm_g"] = np.ascontiguousarray(inputs["b_knorm_g"], dtype=np.float32)
    for k in ("ident", "tri", "up", "qal", "kal_moba", "kal_slc", "kal_plain", "kal_cmp", "cmaskneg", "ovl", "impc"):
        shared[k] = consts[k]
    shared["o_w_in"] = np.ascontiguousarray(inputs["o_w_in"][0], dtype=np.float32)
    for k in ("c_qnorm_g", "c_knorm_cmp_g", "c_knorm_slc_g", "c_knorm_win_g", "c_k_b2", "c_v_b2", "d_qnorm_g", "d_knorm_g", "d_sinks"):
        shared[k] = np.ascontiguousarray(inputs[k], dtype=np.float32).reshape(1, -1)
    for k in ("c_pos_k", "c_pos_v", "c_k_w1", "c_k_b1", "c_k_w2", "c_v_w1", "c_v_b1", "c_v_w2"):
        shared[k] = np.ascontiguousarray(inputs[k][0], dtype=np.float32)
    x = np.ascontiguousarray(inputs["x"], dtype=np.float32)
    nb = x.shape[0]
    layers = inputs.get("_layers", (0, 1))
    nc = build_program(layers, inputs.get("_stop"))
    in_maps = [dict(shared, x=x[b]) for b in range(nb)]
    res = run_bass_kernel_spmd(nc, in_maps, core_ids=list(range(nb)))
    return np.stack([np.asarray(r["y"], dtype=np.float32) for r in res.results], axis=0)
```

```python
import contextlib
import os
import numpy as np
import ml_dtypes
import concourse.bass as bass
import concourse.mybir as mybir
from concourse.bass_utils import run_bass_kernel_spmd

F32 = mybir.dt.float32
BF16 = mybir.dt.bfloat16
ALU = mybir.AluOpType
AF = mybir.ActivationFunctionType
AX = mybir.AxisListType

S = 2048
D = 1024
NT = 16
NEGM = -30000.0
EPS = 1e-6


class _Op:
    __slots__ = ("eng", "fn", "deps", "chan", "signal", "val", "dmaval", "chanseq")

    def __init__(self, eng, fn, chan):
        self.eng = eng
        self.fn = fn
        self.deps = {}
        self.chan = chan
        self.signal = False
        self.val = 0
        self.dmaval = None


class Prog:
    ENGS = ("pe", "act", "dve", "pool", "sp")

    def __init__(self, nc):
        self.nc = nc
        self.ops = {e: [] for e in self.ENGS}
        self.res = {}
        self.chan_count = {}
        self.chan_last = {}
        self.all_ops = []
        self.bar = {}
        self.final_waits = {}
        self.pool_ctr = 0
        self.sp_ctr = 0
        self.const_keys = []
        self.pres = {}

    PERS = ("w", "wo")

    def _st(self, k):
        if isinstance(k, tuple) and k[0] in self.PERS or k in self.PERS:
            return self.pres
        return self.res

    def op(self, eng, fn, reads=(), writes=(), chan=None, nobar=False):
        for w in writes:
            if isinstance(w, tuple) and w[0] == "ck" and w not in self.const_keys:
                self.const_keys.append(w)
        if "const" in reads:
            reads = [k for r in reads for k in (self.const_keys if r == "const" else (r,))]
        if chan is not None and eng == "pool":
            chan = "pq%d" % (self.pool_ctr % 12)
            self.pool_ctr += 1
        elif chan == "c":
            chan = "pc%d" % (self.sp_ctr % 16)
            self.sp_ctr += 1
        o = _Op(eng, fn, chan)
        deps = o.deps
        if not nobar:
            deps.update(self.bar)
        if chan is not None and (chan.startswith("pq") or chan.startswith("pc")):
            prev = self.chan_last.get(chan)
            if prev is not None:
                deps[id(prev)] = prev
        for k in reads:
            st = self._st(k).get(k)
            if st is not None and st[0] is not None:
                deps[id(st[0])] = st[0]
        for k in writes:
            st = self._st(k).get(k)
            if st is not None:
                if st[0] is not None:
                    deps[id(st[0])] = st[0]
                for r in st[1]:
                    deps[id(r)] = r
        for k in reads:
            res = self._st(k)
            st = res.get(k)
            if st is None:
                st = [None, []]
                res[k] = st
            st[1].append(o)
        for k in writes:
            self._st(k)[k] = [o, []]
        o.dmaval = dict(self.chan_count)
        if chan is not None:
            self.chan_count[chan] = self.chan_count.get(chan, 0) + 1
            o.chanseq = self.chan_count[chan]
            self.chan_last[chan] = o
            o.signal = True
        self.ops[eng].append(o)
        self.all_ops.append(o)
        return o

    def barrier(self):
        bar = {}
        for e in self.ENGS:
            if self.ops[e]:
                o = self.ops[e][-1]
                bar[id(o)] = o
        for ch, o in self.chan_last.items():
            bar[id(o)] = o
        self.bar = bar
        self.res = {}

    def finish(self, eng, chans):
        self.final_waits = {eng: {ch: self.chan_count[ch] for ch in chans}}

    def emit(self):
        nc = self.nc
        for o in self.all_ops:
            for d in o.deps.values():
                if d.chan is None:
                    if d.eng == "pe" and o.eng == "pe":
                        continue
                    d.signal = True
        for e in self.ENGS:
            c = 0
            for o in self.ops[e]:
                if o.chan is None and o.signal:
                    c += 1
                    o.val = c
        import os
        if os.environ.get("KDBG"):
            print("sem counts", {e: max([o.val for o in self.ops[e]] + [0]) for e in self.ENGS}, {e: len(self.ops[e]) for e in self.ENGS},
                  {c: 16 * v for c, v in self.chan_count.items()})
        stack = contextlib.ExitStack()
        sems = {}
        for e in self.ENGS:
            sems[e] = stack.enter_context(nc.semaphore("s_" + e))
        for ch in self.chan_count:
            sems["c_" + ch] = stack.enter_context(nc.semaphore("c_" + ch))
        block = stack.enter_context(nc.Block())
        engobj = {"pe": "tensor", "act": "scalar", "dve": "vector", "pool": "gpsimd", "sp": "sync"}

        def make(e):
            def body(eng):
                waited = {}
                for o in self.ops[e]:
                    need = {}
                    for d in o.deps.values():
                        if d.chan is not None:
                            k = "c_" + d.chan
                            if d.chan.startswith("pq") or d.chan.startswith("pc"):
                                v = 16 * d.chanseq
                            else:
                                v = 16 * o.dmaval[d.chan]
                        else:
                            if d.eng == "pe" and e == "pe":
                                continue
                            k = d.eng
                            v = d.val
                        if v > need.get(k, 0):
                            need[k] = v
                    for k, v in need.items():
                        if waited.get(k, 0) >= v:
                            continue
                        eng.wait_ge(sems[k], v)
                        waited[k] = v
                    ins = o.fn(eng)
                    if o.chan is not None:
                        ins.then_inc(sems["c_" + o.chan], 16)
                    elif o.signal:
                        ins.then_inc(sems[e], 1)
                for ch, c in self.final_waits.get(e, {}).items():
                    eng.wait_ge(sems["c_" + ch], 16 * c)
            return body

        for e in self.ENGS:
            getattr(block, engobj[e])(make(e))
        stack.close()


def _consts():
    c = {}
    c["ident"] = np.eye(128, dtype=np.float32)
    k = np.arange(128)[:, None]
    q = np.arange(128)[None, :]
    c["tri"] = np.where(k <= q, 0.0, NEGM).astype(np.float32)
    c["up"] = np.where(k > q, 0.0, NEGM).astype(np.float32)
    t = np.arange(S)
    b = (t % 16).astype(np.float32)
    a = (t - t % 16).astype(np.float32)
    slopes = np.power(2.0, -8.0 * np.arange(1, 9) / 8).astype(np.float32)
    qal = np.zeros((S, 8, 4), np.float32)
    qal[:, :, 0] = -slopes[None, :] * a[:, None]
    qal[:, :, 1] = -slopes[None, :] * b[:, None]
    qal[:, :, 2] = slopes[None, :]
    qal[:, :, 3] = slopes[None, :]
    c["qal"] = qal.reshape(NT, 128, 32).transpose(1, 0, 2).copy()

    def kal(onehot_block):
        m = np.zeros((S, 36), np.float32)
        if onehot_block:
            m[t, t // onehot_block] = 1.0
        m[:, 32] = 1.0
        m[:, 33] = 1.0
        m[:, 34] = a
        m[:, 35] = b
        return m.reshape(NT, 128, 36).transpose(1, 0, 2).copy()

    c["kal_moba"] = kal(256)
    c["kal_slc"] = kal(64)
    c["kal_plain"] = kal(0)
    cc = np.arange(128)
    cend = 16 * cc + 31
    kc = np.zeros((128, 36), np.float32)
    kc[:, 32] = 1.0
    kc[:, 33] = 1.0
    kc[:, 34] = cend - cend % 16
    kc[:, 35] = cend % 16
    c["kal_cmp"] = kc
    c["cmaskneg"] = np.where(t[None, :] >= cend[:, None], 0.0, NEGM).astype(np.float32)
    start = np.arange(127)[:, None] * 16
    bs = np.arange(32)[None, :] * 64
    ov = np.zeros((128, 32), np.float32)
    ov[:127] = ((start < bs + 64) & (start + 32 > bs)).astype(np.float32)
    c["ovl"] = ov
    blk = np.arange(32)[None, :]
    cur = (t // 64)[:, None]
    forced = (blk == 0) | (blk == cur) | (blk == cur - 1)
    impc = 1e4 * forced.astype(np.float32) - 1e5 * (blk > cur).astype(np.float32)
    c["impc"] = impc.reshape(NT, 128, 32).transpose(1, 0, 2).copy()
    return c


def build_program(layers=(0, 1), stop=None):
    nc = bass.Bass("TRN2", target_bir_lowering=False)
    es = contextlib.ExitStack()
    dram = {}

    def din(name, shape, dt=F32):
        dram[name] = nc.dram_tensor(name, list(shape), dt, kind="ExternalInput").ap()
        return dram[name]

    x_d = din("x", [S, D])
    normg_d = din("norm_g", [2, D])
    wout_d = din("w_out", [2, D, D])
    ewin_d = din("e_w_in", [D, 3584])
    convw_d = din("a_conv_w", [31, 512])
    convb_d = din("a_conv_b", [512])
    lng_d = din("a_ln_g", [512])
    lnb_d = din("a_ln_b", [512])
    bq_d = din("b_qnorm_g", [1, 64])
    bk_d = din("b_knorm_g", [1, 64])
    ident_d = din("ident", [128, 128])
    tri_d = din("tri", [128, 128])
    up_d = din("up", [128, 128])
    qal_d = din("qal", [128, NT, 32])
    kalm_d = din("kal_moba", [128, NT, 36])
    kals_d = din("kal_slc", [128, NT, 36])
    kalp_d = din("kal_plain", [128, NT, 36])
    kalc_d = din("kal_cmp", [128, 36])
    cmn_d = din("cmaskneg", [128, S])
    ovl_d = din("ovl", [128, 32])
    impc_d = din("impc", [128, NT, 32])
    owin_d = din("o_w_in", [D, 3096])
    cq_d = din("c_qnorm_g", [1, 64])
    ckc_d = din("c_knorm_cmp_g", [1, 64])
    cks_d = din("c_knorm_slc_g", [1, 64])
    ckw_d = din("c_knorm_win_g", [1, 64])
    cposk_d = din("c_pos_k", [32, 64])
    cposv_d = din("c_pos_v", [32, 64])
    ckw1_d = din("c_k_w1", [2048, 256])
    ckb1_d = din("c_k_b1", [256])
    ckw2_d = din("c_k_w2", [256, 64])
    ckb2_d = din("c_k_b2", [1, 64])
    cvw1_d = din("c_v_w1", [2048, 256])
    cvb1_d = din("c_v_b1", [256])
    cvw2_d = din("c_v_w2", [256, 64])
    cvb2_d = din("c_v_b2", [1, 64])
    dq_d = din("d_qnorm_g", [1, 64])
    dk_d = din("d_knorm_g", [1, 64])
    dsink_d = din("d_sinks", [1, 8])
    y_d = nc.dram_tensor("y", [S, D], F32, kind="ExternalOutput").ap()

    def sb(name, shape, dt):
        return es.enter_context(nc.sbuf_tensor(name, list(shape), dt))

    def psum(name, shape, dt):
        return es.enter_context(nc.psum_tensor(name, list(shape), dt))

    P = Prog(nc)

    x_sb = sb("x_sb", [128, NT, D], F32)
    hT = sb("hT", [128, 8, S], BF16)
    wbuf = [sb("wbuf%d" % i, [128, 8, 512], BF16) for i in range(2)]
    wo = sb("wo", [128, 4, D], BF16)
    ident = sb("ident_sb", [128, 128], BF16)
    identf = sb("identf_sb", [128, 128], F32)
    onesf = sb("onesf", [128, 128], F32)
    tri = sb("tri_sb", [128, 128], BF16)
    upm = sb("up_sb", [128, 128], BF16)
    qal = sb("qal_sb", [128, NT, 32], BF16)
    kalm = sb("kalm_sb", [128, NT, 36], BF16)
    KTc = sb("KTc", [128, 2, 128], BF16)
    Vca = sb("Vca", [128, 2, 98], BF16)
    kals = sb("kals_sb", [128, NT, 36], BF16)
    kalp = sb("kalp_sb", [128, NT, 36], BF16)
    ss = sb("ss", [128, NT], F32)
    rstd = sb("rstd", [128, NT], F32)
    ARENA = 80 * 1024
    arena = sb("arena", [128, ARENA // 2], BF16)

    pf = [psum("pf%d" % i, [128, 512], F32) for i in range(6)]
    pb = [psum("pb%d" % i, [128, 1024], BF16) for i in range(2)]

    class Carver:
        def __init__(self, base=0):
            self.off = base

        def get(self, shape, dt):
            n = int(np.prod(shape[1:]))
            nb = n * (4 if dt == F32 else 2)
            nb = (nb + 63) // 64 * 64
            assert self.off + nb <= ARENA, ("arena overflow", self.off + nb, ARENA)
            a = arena[:, self.off // 2:(self.off + nb) // 2]
            self.off += nb
            if os.environ.get("KDBG"):
                print("carve", shape, dt, "->", self.off)
            if dt == F32:
                a = a.bitcast(F32)
            a = a[:, 0:n]
            if len(shape) == 3:
                a = a.rearrange("p (a b) -> p a b", b=shape[2])
            elif len(shape) == 4:
                a = a.rearrange("p (a b c) -> p a b c", b=shape[2], c=shape[3])
            if shape[0] < 128:
                a = a[0:shape[0]]
            return a

    def dma(eng, out, in_, chan, reads=(), writes=(), slow=False, nobar=False):
        if slow:
            P.op(eng, lambda e: e.dma_start(out=out, in_=in_, allow_slow_non_contiguous=True), reads=reads, writes=writes, chan=chan, nobar=nobar)
        else:
            P.op(eng, lambda e: e.dma_start(out=out, in_=in_), reads=reads, writes=writes, chan=chan, nobar=nobar)

    def mm(out, lhsT, rhs, start, stop, reads, writes):
        P.op("pe", lambda e: e.matmul(out, lhsT=lhsT, rhs=rhs, start=start, stop=stop), reads=reads, writes=writes)

    def tr(out, in_, reads, writes, idn=None):
        ck = ("ck", "ident") if idn is None else ("ck", "identf")
        idn = ident[:] if idn is None else idn
        P.op("pe", lambda e: e.transpose(out=out, in_=in_, identity=idn), reads=list(reads) + [ck], writes=writes)

    def act(out, in_, func, reads, writes, **kw):
        P.op("act", lambda e: e.activation(out=out, in_=in_, func=func, **kw), reads=reads, writes=writes)

    def V(fn, reads, writes, eng="dve"):
        P.op(eng, fn, reads=reads, writes=writes)

    def TT(out, in0, in1, op, reads, writes, eng="dve"):
        P.op(eng, lambda e: e.tensor_tensor(out=out, in0=in0, in1=in1, op=op), reads=reads, writes=writes)

    def TS(out, in0, s1, s2, op0, op1, reads, writes, eng="dve"):
        if op1 is None:
            P.op(eng, lambda e: e.tensor_scalar(out=out, in0=in0, scalar1=s1, scalar2=None, op0=op0), reads=reads, writes=writes)
        else:
            P.op(eng, lambda e: e.tensor_scalar(out=out, in0=in0, scalar1=s1, scalar2=s2, op0=op0, op1=op1), reads=reads, writes=writes)

    def STT(out, in0, scalar, in1, op0, op1, reads, writes, eng="dve"):
        P.op(eng, lambda e: e.scalar_tensor_tensor(out=out, in0=in0, scalar=scalar, in1=in1, op0=op0, op1=op1), reads=reads, writes=writes)

    def RED(out, in_, op, reads, writes):
        P.op("dve", lambda e: e.tensor_reduce(out=out, in_=in_, axis=AX.X, op=op), reads=reads, writes=writes)

    def RCP(out, in_, reads, writes):
        P.op("dve", lambda e: e.reciprocal(out=out, in_=in_), reads=reads, writes=writes)

    def CPY(out, in_, reads, writes, eng="dve"):
        P.op(eng, lambda e: e.tensor_copy(out=out, in_=in_), reads=reads, writes=writes)

    def MSET(ap, val, writes, eng="dve"):
        P.op(eng, lambda e: e.memset(ap, val), reads=[], writes=writes)

    import os
    KB = os.environ.get("KBIS", "abcdefg")
    if "a" in KB:
        dma("pool", ident[:], ident_d, "c", writes=[("ck", "ident")])
    if "b" in KB:
        dma("pool", tri[:], tri_d, "c", writes=[("ck", "tri")])
        dma("pool", upm[:], up_d, "c", writes=[("ck", "up")])
    if "c" in KB:
        dma("pool", qal[:], qal_d, "c", writes=[("ck", "qal")])
    if "d" in KB:
        dma("pool", kalm[:], kalm_d, "c", writes=[("ck", "kalm")])
        dma("pool", kals[:], kals_d, "c", writes=[("ck", "kals")])
        dma("pool", kalp[:], kalp_d, "c", writes=[("ck", "kalp")])
    if "e" in KB:
        dma("sp", identf[:], ident_d, "c", writes=[("ck", "identf")])
    if "f" in KB:
        MSET(onesf[:], 1.0, [("ck", "onesf")])

    xv = x_d.rearrange("(i p) d -> p i d", p=128)
    yv = y_d.rearrange("(i p) d -> p i d", p=128)
    for i in range(NT):
        dma("sp", x_sb[:, i, :], xv[:, i, :], "x%d" % i, writes=[("x", i)])

    wslot_ctr = [0]

    def load_w(wd, segs):
        s = wslot_ctr[0] % 2
        wslot_ctr[0] += 1
        wv = wd.rearrange("(k p) n -> p k n", p=128)
        o = 0
        for (c0, n) in segs:
            dma("pool", wbuf[s][:, :, o:o + n], wv[:, :, c0:c0 + n], "w%d" % s, writes=[("w", s)], nobar=True)
            o += n
        return s

    def norm_phase(layer, base=0, barrier=True):
        cvn = Carver(base=base)
        gbc = cvn.get([128, D], F32)
        junk = cvn.get([128, D], BF16)
        hb = [cvn.get([128, D], BF16) for _ in range(2)]
        dma("act", gbc[:], normg_d[layer:layer + 1, :].partition_broadcast(128), "c", writes=["gbc"])
        MSET(ss[:], 0.0, [("ss", gI) for gI in range(4)])

        def stats(gI):
            for i in range(4 * gI, 4 * gI + 4):
                act(junk[:], x_sb[:, i, :], AF.Square, [("x", i), ("ss", gI)], ["junk", ("ss", gI)], accum_out=ss[:, i:i + 1])
            sl = slice(4 * gI, 4 * gI + 4)
            TS(rstd[:, sl], ss[:, sl], 1.0 / D, EPS, ALU.mult, ALU.add, [("ss", gI)], [("rstd", gI)])
            act(rstd[:, sl], rstd[:, sl], AF.Sqrt, [("rstd", gI)], [("rstd", gI)])
            RCP(rstd[:, sl], rstd[:, sl], [("rstd", gI)], [("rstd", gI)])

        def na(i):
            h = hb[i % 2]
            STT(h[:], x_sb[:, i, :], rstd[:, i:i + 1], gbc[:], ALU.mult, ALU.mult, [("x", i), ("rstd", i // 4), "gbc"], [("hb", i % 2)])

        def nb(i):
            h = hb[i % 2]
            pt = pb[i % 2]
            for k in range(8):
                tr(pt[:, k * 128:(k + 1) * 128], h[:, k * 128:(k + 1) * 128], [("hb", i % 2)], [("pb", i % 2)])
            act(hT[:, :, i * 128:(i + 1) * 128], pt[:].rearrange("p (k t) -> p k t", k=8), AF.Copy, [("pb", i % 2)], [("hT", i)])

        pend = []
        if layer == 0:
            for gI in range(4):
                stats(gI)
                for i in range(4 * gI, 4 * gI + 4):
                    na(i)
                    pend.append(i)
                    if len(pend) > 1:
                        nb(pend.pop(0))
        else:
            for gI in range(5):
                if gI < 4:
                    stats(gI)
                if gI >= 1:
                    for i in range(4 * (gI - 1), 4 * gI):
                        na(i)
                        pend.append(i)
                        if len(pend) > 1:
                            nb(pend.pop(0))
        nb(pend.pop(0))
        if barrier:
            P.barrier()

    def load_wo(layer, c0, n):
        wv = wout_d[layer].rearrange("(k p) n -> p k n", p=128)
        for hf in range(2):
            dma("pool", wo[:, 0:n, hf * 512:(hf + 1) * 512], wv[:, c0:c0 + n, hf * 512:(hf + 1) * 512], "wo", writes=["wo"], nobar=True)

    def outproj_tile(i, lhs_list, chunks, yT_reads, pbanks):
        for half in range(2):
            pp = pf[pbanks[half]]
            for n, (c, lhs) in enumerate(zip(chunks, lhs_list)):
                mm(pp[:], lhs, wo[:, c, half * 512:(half + 1) * 512], n == 0, n == len(chunks) - 1,
                   list(yT_reads) + ["wo"], [("pf", pbanks[half])])
            xs = x_sb[:, i, half * 512:(half + 1) * 512]
            TT(xs, xs, pp[:], ALU.add, [("pf", pbanks[half]), ("x", i)], [("x", i)])

    att_ctr = [0, 0]

    def lagged(n, stage_a, stage_b, lag=1):
        for i in range(n + lag):
            if i < n:
                stage_a(i)
            if i >= lag:
                stage_b(i - lag)

    def mm_acc(out, lhsT, rhs, start, stop, reads, writes):
        P.op("pe", lambda e: e.matmul(out, lhsT=lhsT, rhs=rhs, start=start, stop=stop, skip_group_check=True), reads=reads, writes=writes)

    def attention(QT, KT, nk, Vrhs, ncv, plan, finish, qkeys, kkeys, vkeys, ptb, tagk, addmask=None):
        NS = len(ptb)
        work = []
        for qc in range(4):
            items = plan(qc)
            if not items:
                continue
            ob = 3 + att_ctr[1] % 3
            att_ctr[1] += 1
            lastk = {}
            for (kt, jlo, jhi, masks) in items:
                for j in range(jlo, jhi + 1):
                    lastk[j] = kt
            for n, it in enumerate(items):
                work.append((qc, ob, it, n == 0, n == len(items) - 1, lastk))

        def front(w):
            qc, ob, (kt, jlo, jhi, masks), first, last, lastk = w
            sbk = att_ctr[0] % NS
            att_ctr[0] += 1
            Sp = pf[sbk]
            pt = ptb[sbk]
            c0, c1 = jlo * 128, (jhi + 1) * 128
            extra = list(masks.items())
            mm_acc(Sp[0:nk, c0:c1], KT(kt), QT[:, qc * 512 + c0:qc * 512 + c1], True, addmask is None and not extra,
                   list(qkeys(qc)) + list(kkeys(kt)), [("pf", sbk)])
            if addmask is not None:
                mm_acc(Sp[0:nk, c0:c1], ident[0:nk, 0:nk], addmask[0:nk, qc * 512 + c0:qc * 512 + c1], False, not extra,
                       ["const", "addmask"], [("pf", sbk)])
            for n, (j, mk) in enumerate(extra):
                mm_acc(Sp[0:nk, j * 128:(j + 1) * 128], ident[0:nk, 0:nk], mk[0:nk, :], False, n == len(extra) - 1,
                       ["const"], [("pf", sbk)])
            act(pt[0:nk, c0:c1], Sp[0:nk, c0:c1], AF.Exp, [("pf", sbk)], [("pt", tagk, sbk)])
            return sbk

        def back(w, sbk, started):
            qc, ob, (kt, jlo, jhi, masks), first, last, lastk = w
            pt = ptb[sbk]
            for j in range(jlo, jhi + 1):
                mm_acc(pf[ob][:, j * 128:j * 128 + ncv], pt[0:nk, j * 128:(j + 1) * 128], Vrhs(kt), len(started) == 0, lastk[j] == kt,
                       [("pt", tagk, sbk)] + list(vkeys(kt)), [("pf", ob)])
                started.add(j)
            if last:
                for j in sorted(started):
                    finish(qc * 4 + j, pf[ob][:, j * 128:j * 128 + ncv], ("pf", ob))
                started.clear()

        LAG = NS - 1
        started = set()
        pend = []
        for w in work:
            pend.append((w, front(w)))
            if len(pend) > LAG:
                pw, psb = pend.pop(0)
                back(pw, psb, started)
        for (pw, psb) in pend:
            back(pw, psb, started)

    def layer0():
        if stop == "load":
            return
        norm_phase(0, base=70 * 1024, barrier=False)
        if stop == "norm0":
            return
        load_wo(0, 0, 4)
        W = ewin_d
        if stop == "norm":
            return
        cv = Carver()
        yc = cv.get([128, 4, S], F32)
        hc = cv.get([128, S + 32], BF16)
        dg = cv.get([128, 31, 128], BF16)
        cw = cv.get([128, 4, 31], F32)
        cb = cv.get([128, 4], F32)
        lg = cv.get([128, 4], F32)
        lb = cv.get([128, 4], F32)
        Tsq = [cv.get([128, 512], F32) for _ in range(2)]
        mu2 = [cv.get([128, 512], F32) for _ in range(2)]
        msq = cv.get([128, 512], F32)
        rs2 = [cv.get([128, 512], F32) for _ in range(2)]
        sz4 = [cv.get([128, 512], BF16) for _ in range(4)]
        T4 = [cv.get([128, 512], F32) for _ in range(4)]
        yaT2 = [cv.get([128, 4, 512], BF16) for _ in range(2)]
        HO = 32
        stg = Tsq[0][0:32, :]
        stg2 = Tsq[1][0:12, 0:128]
        dma("sp", stg[0:31, :], convw_d, "c", writes=[("Tsq", 0)])
        dma("sp", stg2[0:4, :], convb_d.rearrange("(a c) -> a c", c=128), "c", writes=[("Tsq", 1)])
        dma("sp", stg2[4:8, :], lng_d.rearrange("(a c) -> a c", c=128), "c", writes=[("Tsq", 1)])
        dma("sp", stg2[8:12, :], lnb_d.rearrange("(a c) -> a c", c=128), "c", writes=[("Tsq", 1)])
        for cc in range(4):
            tr(pf[0][:, cc * 32:cc * 32 + 31], stg[0:31, cc * 128:(cc + 1) * 128], [("Tsq", 0)], [("pf", 0)], idn=identf[0:31, 0:31])
        tr(pf[0][:, 128:140], stg2[0:12, :], [("Tsq", 1)], [("pf", 0)], idn=identf[0:12, 0:12])
        CPY(cw[:], pf[0][:, 0:128].rearrange("p (a j) -> p a j", j=32)[:, :, 0:31], [("pf", 0)], ["cw"])
        CPY(cb[:], pf[0][:, 128:132], [("pf", 0)], ["cw"])
        CPY(lg[:], pf[0][:, 132:136], [("pf", 0)], ["cw"])
        CPY(lb[:], pf[0][:, 136:140], [("pf", 0)], ["cw"])
        MSET(hc[:, 0:HO], 0.0, ["hc0"])
        if stop == "convp":
            return
        for cc in range(4):
            if stop == "conv1" and cc == 1:
                return
            s = load_w(W, [(cc * 128, 128), (512 + cc * 128, 128)])
            for j in range(31):
                TS(dg[:, j, :], ident[:], cw[:, cc, j:j + 1], None, ALU.mult, None, ["const", "cw"], [("dg", j)])
            for tq in range(4):
                bv, bg = (tq % 2) * 2, (tq % 2) * 2 + 1
                for which, bk in ((0, bv), (1, bg)):
                    for k in range(8):
                        mm(pf[bk][:], wbuf[s][:, k, which * 128:(which + 1) * 128], hT[:, k, tq * 512:(tq + 1) * 512], k == 0, k == 7,
                           [("w", s)] + [("hT", 4 * tq + u) for u in range(4)], [("pf", bk)])
                act(T4[tq % 2][:], pf[bg][:], AF.Sigmoid, [("pf", bg)], [("T4", tq % 2)])
                TT(hc[:, HO + tq * 512:HO + (tq + 1) * 512], pf[bv][:], T4[tq % 2][:], ALU.mult, [("pf", bv), ("T4", tq % 2)], [("hc", tq)])
            for tq in range(4):
                pc = pf[4 + tq % 2]
                for j in range(31):
                    o = HO - 30 + j + tq * 512
                    mm(pc[:], dg[:, j, :], hc[:, o:o + 512], j == 0, j == 30,
                       [("dg", j), ("hc", tq)] + ([("hc", tq - 1)] if tq > 0 else ["hc0"]), [("pf", 4 + tq % 2)])
                act(yc[:, cc, tq * 512:(tq + 1) * 512], pc[:], AF.Identity, [("pf", 4 + tq % 2), "cw"], [("yc", cc, tq)],
                    bias=cb[:, cc:cc + 1], scale=1.0)
        if stop == "conv4":
            return
        P.barrier()
        sz_slot = load_w(W, [(1024, 512)])

        def st_a(tq):
            ts = slice(tq * 512, (tq + 1) * 512)
            p2 = tq % 2
            for cc in range(4):
                mm(pf[0][:], onesf[:], yc[:, cc, ts], cc == 0, cc == 3, [("yc", cc, tq), "const"], [("pf", 0)])
            for cc in range(4):
                act(Tsq[cc % 2][:], yc[:, cc, ts], AF.Square, [("yc", cc, tq)], [("Tsq", cc % 2)])
                mm(pf[1][:], onesf[:], Tsq[cc % 2][:], cc == 0, cc == 3, [("Tsq", cc % 2), "const"], [("pf", 1)])
            TS(mu2[p2][:], pf[0][:], 1.0 / 512, None, ALU.mult, None, [("pf", 0)], [("mu", p2)])
            TT(msq[:], mu2[p2][:], mu2[p2][:], ALU.mult, [("mu", p2)], ["msq"])
            STT(rs2[p2][:], pf[1][:], 1.0 / 512, msq[:], ALU.mult, ALU.subtract, [("pf", 1), "msq"], [("rs", p2)])
            TS(rs2[p2][:], rs2[p2][:], EPS, None, ALU.add, None, [("rs", p2)], [("rs", p2)])
            act(rs2[p2][:], rs2[p2][:], AF.Sqrt, [("rs", p2)], [("rs", p2)])
            RCP(rs2[p2][:], rs2[p2][:], [("rs", p2)], [("rs", p2)])

        def st_b(tq):
            ts = slice(tq * 512, (tq + 1) * 512)
            p2 = tq % 2
            for cc in range(4):
                pz = pf[2 + cc % 2]
                for k in range(8):
                    mm(pz[:], wbuf[sz_slot][:, k, cc * 128:(cc + 1) * 128], hT[:, k, ts], k == 0, k == 7,
                       [("w", sz_slot)] + [("hT", 4 * tq + u) for u in range(4)], [("pf", 2 + cc % 2)])
                act(sz4[cc][:], pz[:], AF.Silu, [("pf", 2 + cc % 2)], [("sz", cc)])
            for cc in range(4):
                tt = T4[cc]
                TT(tt[:], yc[:, cc, ts], mu2[p2][:], ALU.subtract, [("yc", cc, tq), ("mu", p2)], [("T4", cc)])
                TT(tt[:], tt[:], rs2[p2][:], ALU.mult, [("T4", cc), ("rs", p2)], [("T4", cc)])
            for cc in range(4):
                tt = T4[cc]
                act(tt[:], tt[:], AF.Silu, [("T4", cc), "cw"], [("T4", cc)], scale=lg[:, cc:cc + 1], bias=lb[:, cc:cc + 1])
            for cc in range(4):
                TT(yaT2[p2][:, cc, :], T4[cc][:], sz4[cc][:], ALU.mult, [("T4", cc), ("sz", cc)], [("yaT", p2, cc)])

        def st_c(tq):
            p2 = tq % 2
            for u in range(4):
                i = 4 * tq + u
                outproj_tile(i, [yaT2[p2][:, cc, u * 128:(u + 1) * 128] for cc in range(4)], [0, 1, 2, 3],
                             [("yaT", p2, cc) for cc in range(4)], (4, 5))

        for step in range(6):
            if step < 4:
                st_a(step)
            if 0 <= step - 1 < 4:
                st_b(step - 1)
            if step - 2 >= 0:
                st_c(step - 2)
        P.barrier()
        if stop == "conv":
            return
        for hh in range(2):
            moba_half(hh, W)
            P.barrier()
            if stop is not None:
                return

    def moba_half(hh, W):
        cv = Carver()
        QT = cv.get([128, 4, S], BF16)
        KT = cv.get([128, 4, S], BF16)
        Vt = cv.get([128, NT, 4, 66], BF16)
        szy = cv.get([128, NT, 256], BF16)
        ptb = [cv.get([128, 512], BF16) for _ in range(3)]
        gq = cv.get([128, 1], F32)
        gk = cv.get([128, 1], F32)
        kmf = cv.get([128, 4, 8], F32)
        kmb = cv.get([128, 4, 8], BF16)
        gs = cv.get([128, 4, 8], F32)
        cmp_ = cv.get([128, 4, 8, 8], F32)
        rank = cv.get([128, 4, 8], F32)
        nm = cv.get([128, 4, 32], BF16)
        rden = [cv.get([128, 1], F32) for _ in range(8)]
        yT = [cv.get([128, 2, 128], BF16) for _ in range(2)]
        tmpf = [cv.get([128, 512], F32) for _ in range(2)]
        s8 = [cv.get([128, 8], F32) for _ in range(3)]
        qa = [cv.get([128, 4, 128], BF16) for _ in range(3)]
        ka = [cv.get([128, 4, 128], BF16) for _ in range(3)]
        c_q = 1536 + hh * 256
        c_k = 2048 + hh * 256
        c_v = 2560 + hh * 256
        c_z = 3072 + hh * 256
        s = load_w(W, [(c_q, 256), (c_k, 256)])
        s2 = load_w(W, [(c_v, 256), (c_z, 256)])
        load_wo(0, 4 + 2 * hh, 2)
        load_gain_col(gq, bq_d, 1.0)
        load_gain_col(gk, bk_d, 8.0)
        for b in range(3):
            MSET(qa[b][:, :, 64:128], 0.0, [("qa", b)])
            MSET(ka[b][:, :, 64:128], 0.0, [("ka", b)])
        MSET(Vt[:, :, :, 64:65], 1.0, ["Vt"])
        MSET(nm[:], 0.0, ["nm"])

        def m0(i):
            r = i % 3
            CPY(qa[r][:, :, 96:100], qal[:, i, hh * 16:hh * 16 + 16].rearrange("p (h c) -> p h c", c=4), ["const"], [("qa", r)])
            CPY(ka[r][:, :, 64:100], kalm[:, i, :].unsqueeze(1).to_broadcast([128, 4, 36]), ["const"], [("ka", r)])
            for k in range(8):
                mm(pf[r][:], hT[:, k, i * 128:(i + 1) * 128], wbuf[s][:, k, :], k == 0, k == 7, [("hT", i), ("w", s)], [("pf", r)])

        def m1(i):
            r = i % 3
            rstd_a(pf[r], ("pf", r), 512, tmpf[i % 2], ("tf", i % 2), s8[r], ("s8", r))

        def m2(i):
            r = i % 3
            rstd_b(512, s8[r], ("s8", r))
            TT(qa[r][:, :, 0:64], pf[r][:, 0:256].rearrange("p (h d) -> p h d", d=64), s8[r][:, 0:4].unsqueeze(2).to_broadcast([128, 4, 64]),
               ALU.mult, [("pf", r), ("s8", r)], [("qa", r)])
            TT(ka[r][:, :, 0:64], pf[r][:, 256:512].rearrange("p (h d) -> p h d", d=64), s8[r][:, 4:8].unsqueeze(2).to_broadcast([128, 4, 64]),
               ALU.mult, [("pf", r), ("s8", r)], [("ka", r)])

        def m3(i):
            r = i % 3
            b2 = i % 2
            pt = pb[b2]
            for h in range(4):
                tr(pt[:, h * 128:(h + 1) * 128], qa[r][:, h, :], [("qa", r)], [("pb", b2)])
            for h in range(4):
                tr(pt[:, (4 + h) * 128:(5 + h) * 128], ka[r][:, h, :], [("ka", r)], [("pb", b2)])
            act(QT[:, :, i * 128:(i + 1) * 128], pt[:, 0:512].rearrange("p (h t) -> p h t", h=4), AF.Copy, [("pb", b2), "gq"], [("QT", i)],
                scale=gq[:, 0:1])
            act(KT[:, :, i * 128:(i + 1) * 128], pt[:, 512:1024].rearrange("p (h t) -> p h t", h=4), AF.Copy, [("pb", b2), "gq"], [("KT", i)],
                scale=gk[:, 0:1])

        pipe4(NT, m0, m1, m2, m3)
        if hh == 1 and 1 in layers:
            prefetch_w1([("qa", r) for r in range(3)] + [("ka", r) for r in range(3)] + [("s8", r) for r in range(3)] + [("tf", 0), ("tf", 1)])
        if stop == "m1":
            return
        for i in range(NT):
            b2 = 2 + i % 2
            pp = pf[b2]
            for k in range(8):
                mm(pp[:], hT[:, k, i * 128:(i + 1) * 128], wbuf[s2][:, k, :], k == 0, k == 7, [("hT", i), ("w", s2)], [("pf", b2)])
            act(Vt[:, i, :, 0:64], pp[:, 0:256].rearrange("p (h d) -> p h d", d=64), AF.Copy, [("pf", b2), "Vt"], [("V", i)])
            act(szy[:, i, :], pp[:, 256:512], AF.Silu, [("pf", b2)], [("szy", i)])
        if stop == "m3":
            return
        for h in range(4):
            RED(kmf[:, h, :], KT[:, h, :].rearrange("p (n t) -> p n t", t=256), ALU.add, [("KT", i) for i in range(NT)], ["kmf"])
        TS(kmb[:], kmf[:], 1.0 / 256, None, ALU.mult, None, ["kmf"], ["kmb"])
        if stop == "mk":
            return
        for i in range(8, NT):
            npast = i // 2
            b2 = 4 + i % 2
            pg = pf[b2]
            for h in range(4):
                mm(pg[:, h * 8:h * 8 + 8], QT[0:64, h, i * 128:(i + 1) * 128], kmb[0:64, h, :], True, True, [("QT", i), "kmb"], [("pf", b2)])
            CPY(gs[:], pg[:, 0:32].rearrange("p (h n) -> p h n", n=8), [("pf", b2)], ["gs"])
            TT(cmp_[:, :, 0:npast, 0:npast], gs[:, :, 0:npast].unsqueeze(2).to_broadcast([128, 4, npast, npast]),
               gs[:, :, 0:npast].unsqueeze(3).to_broadcast([128, 4, npast, npast]), ALU.is_gt, ["gs"], ["cmp"])
            RED(rank[:, :, 0:npast], cmp_[:, :, 0:npast, 0:npast], ALU.add, ["cmp"], ["rank"])
            TS(nm[:, :, 0:npast], rank[:, :, 0:npast], 2.5, NEGM, ALU.is_ge, ALU.mult, ["rank"], ["nm"])
            pt = pb[i % 2]
            tr(pt[:, 0:128], nm[:].rearrange("p h n -> p (h n)"), ["nm"], [("pb", i % 2)])
            for h in range(4):
                act(QT[64:96, h, i * 128:(i + 1) * 128], pt[h * 32:(h + 1) * 32, 0:128], AF.Copy, [("pb", i % 2)], [("QT", i)])

        if stop == "m2":
            return

        def plan(qc):
            items = []
            for kt in range(4 * qc + 4):
                if kt < 4 * qc:
                    items.append((kt, 0, 3, {}))
                else:
                    m = kt - 4 * qc
                    items.append((kt, m, 3, {m: tri[:]}))
            return items

        for h in range(4):
            def finish(i, Ob, okey, h=h):
                r = (i + 4 * h) % 8
                rd = rden[r]
                RCP(rd[:], Ob[:, 64:65], [okey], [("rden", r)])
                dst = szy[:, i, h * 64:(h + 1) * 64]
                STT(dst, Ob[:, 0:64], rd[:], dst, ALU.mult, ALU.mult, [okey, ("rden", r), ("szy", i)], [("szy", i)])

            attention(QT[:, h, :], lambda kt, h=h: KT[:, h, kt * 128:(kt + 1) * 128], 128,
                      lambda kt, h=h: Vt[:, kt, h, 0:65], 65, plan, finish,
                      lambda qc: [("QT", 4 * qc + u) for u in range(4)], lambda kt: [("KT", kt)], lambda kt: [("V", kt)],
                      ptb, "m")
        if stop == "ma":
            return
        outproj_half(szy, yT)

    def rstd_a(pp, pkey, ncol, tf, tfkey, s8i, s8key):
        nh = ncol // 64
        act(tf[:, 0:ncol], pp[:, 0:ncol], AF.Square, [pkey], [tfkey])
        RED(s8i[:, 0:nh], tf[:, 0:ncol].rearrange("p (h d) -> p h d", d=64), ALU.add, [tfkey], [s8key])
        TS(s8i[:, 0:nh], s8i[:, 0:nh], 64.0 * EPS, None, ALU.add, None, [s8key], [s8key])
        act(s8i[:, 0:nh], s8i[:, 0:nh], AF.Sqrt, [s8key], [s8key])

    def rstd_b(ncol, s8i, s8key):
        nh = ncol // 64
        RCP(s8i[:, 0:nh], s8i[:, 0:nh], [s8key], [s8key])

    def head_rstd(pp, pkey, ncol, tf, tfkey, s8i, s8key):
        rstd_a(pp, pkey, ncol, tf, tfkey, s8i, s8key)
        rstd_b(ncol, s8i, s8key)

    def pipe4(n, s0, s1, s2, s3):
        for step in range(n + 2):
            if step < n:
                s0(step)
                s1(step)
            if 1 <= step <= n:
                s2(step - 1)
            if step >= 2:
                s3(step - 2)

    def load_gain_col(dst, gd, mult):
        MSET(dst[:], 1.0, ["gq"])
        dma("sp", dst[0:64, :], gd.rearrange("o d -> d o"), "c", writes=["gq"], slow=True)
        if mult != 1.0:
            TS(dst[0:64, :], dst[0:64, :], mult, None, ALU.mult, None, ["gq"], ["gq"])

    def outproj_half(szy, yT, store=False):
        def oa(i):
            pt = pb[i % 2]
            for c in range(2):
                tr(pt[:, c * 128:(c + 1) * 128], szy[:, i, c * 128:(c + 1) * 128], [("szy", i)], [("pb", i % 2)])
            y = yT[i % 2]
            act(y[:], pt[:, 0:256].rearrange("p (c t) -> p c t", c=2), AF.Copy, [("pb", i % 2)], [("yT", i % 2)])

        def ob(i):
            y = yT[i % 2]
            outproj_tile(i, [y[:, 0, :], y[:, 1, :]], [0, 1], [("yT", i % 2)], (4, 5))
            if store:
                dma("sp", yv[:, i, :], x_sb[:, i, :], "out", reads=[("x", i)])

        lagged(NT, oa, ob)

    def plan_causal(qc):
        items = []
        for kt in range(4 * qc + 4):
            if kt < 4 * qc:
                items.append((kt, 0, 3, {}))
            else:
                m = kt - 4 * qc
                items.append((kt, m, 3, {m: tri[:]}))
        return items

    def plan_band(wt):
        def plan(qc):
            items = []
            for kt in range(max(0, 4 * qc - wt), 4 * qc + 4):
                jlo = max(0, kt - 4 * qc)
                jhi = min(3, kt + wt - 4 * qc)
                if jlo > jhi:
                    continue
                masks = {}
                if 0 <= kt - 4 * qc <= 3:
                    masks[kt - 4 * qc] = tri[:]
                if 0 <= kt + wt - 4 * qc <= 3:
                    masks[kt + wt - 4 * qc] = upm[:]
                items.append((kt, jlo, jhi, masks))
            return items
        return plan

    W1_OFF = 64 * 1024

    def prefetch_w1(war_keys=()):
        cvp = Carver(base=W1_OFF)
        wA = cvp.get([128, 32, 256], BF16)
        for kv, wd in enumerate((ckw1_d, cvw1_d)):
            wv = wd.rearrange("(l d) j -> d l j", d=64)
            for lq in range(4):
                dma("pool", wA[kv * 64:(kv + 1) * 64, lq * 8:(lq + 1) * 8, :], wv[:, lq * 8:(lq + 1) * 8, :], "w1", writes=["w1A"] + list(war_keys))
        return wA

    def layer1():
        W = owin_d
        wA = Carver(base=W1_OFF).get([128, 32, 256], BF16)
        if 0 not in layers:
            prefetch_w1()
        cvc = Carver(base=16 * 1024)
        wB = cvc.get([128, 32, 256], BF16)
        dma("sp", wB[64:128, :, :], wA[0:64, :, :], "c", writes=["w1B"])
        dma("sp", wB[0:64, :, :], wA[64:128, :, :], "c", writes=["w1B"])
        w1 = [[wA, wB], [wB, wA]]
        B = compress_loads(W, cvc)
        norm_phase(1)
        compress_stage(W, B, w1)
        P.barrier()
        if stop == "cmp":
            return
        for g in range(2):
            nsa_half(g, W)
            P.barrier()
            if stop == "nsa0":
                return
        if stop == "nsa":
            return
        for g in range(2):
            swa_half(g, W)
            P.barrier()

    def compress_loads(W, cv):
        s = load_w(W, [(512, 128), (640, 128)])
        B = {}
        B["s"] = s
        B["KVD"] = [cv.get([128, 16, 128], BF16) for _ in range(2)]
        B["w2"] = [cv.get([128, 2, 64], BF16) for _ in range(2)]
        B["posn"] = cv.get([32, 2, 64], F32)
        B["posT"] = cv.get([64, 2, 32], BF16)
        B["stgb"] = cv.get([4, 128], F32)
        B["b1sb"] = cv.get([128, 4], F32)
        B["biasj"] = cv.get([128, 4], F32)
        B["b2bc"] = cv.get([128, 2, 64], F32)
        B["gcm"] = cv.get([128, 64], F32)
        B["kalc"] = cv.get([128, 36], BF16)
        B["hid"] = [cv.get([128, 2, 128], BF16) for _ in range(4)]
        B["kcf"] = cv.get([128, 64], F32)
        B["junk2"] = cv.get([128, 64], F32)
        B["ssc"] = cv.get([128, 1], F32)
        B["kaug"] = cv.get([128, 128], BF16)
        for kv, wd in enumerate((ckw2_d, cvw2_d)):
            dma("pool", B["w2"][kv][:], wd.rearrange("(c p) d -> p c d", p=128), "w2", writes=["w2"])
        dma("sp", B["posn"][:, 0, :], cposk_d, "c", writes=["posn"])
        dma("sp", B["posn"][:, 1, :], cposv_d, "c", writes=["posn"])
        dma("sp", B["stgb"][0:2, :], ckb1_d.rearrange("(a c) -> a c", c=128), "c", writes=["stgb"])
        dma("sp", B["stgb"][2:4, :], cvb1_d.rearrange("(a c) -> a c", c=128), "c", writes=["stgb"])
        dma("sp", B["b2bc"][:, 0, :], ckb2_d.partition_broadcast(128), "c", writes=["b2bc"])
        dma("sp", B["b2bc"][:, 1, :], cvb2_d.partition_broadcast(128), "c", writes=["b2bc"])
        dma("sp", B["gcm"][:], ckc_d.partition_broadcast(128), "c", writes=["gcm"])
        dma("pool", B["kalc"][:], kalc_d, "c", writes=["kalc"])
        for g in range(2):
            dma("pool", Vca[:, g, 65:97], ovl_d, "c", writes=[("Vca", g)])
        return B

    def compress_stage(W, B, w1):
        s = B["s"]
        KVD, w2, posn, posT, stgb, b1sb, biasj = B["KVD"], B["w2"], B["posn"], B["posT"], B["stgb"], B["b1sb"], B["biasj"]
        b2bc, gcm, kalc, hid, kcf, junk2, ssc, kaug = B["b2bc"], B["gcm"], B["kalc"], B["hid"], B["kcf"], B["junk2"], B["ssc"], B["kaug"]
        for g in range(2):
            MSET(Vca[:, g, 64:65], 1.0, [("Vca", g)])
        MSET(kaug[:], 0.0, ["kaug"])
        for kv in range(2):
            tr(pf[0][0:64, kv * 32:(kv + 1) * 32], posn[:, kv, :], ["posn"], [("pf", 0)], idn=identf[0:32, 0:32])
        tr(pf[0][:, 64:68], stgb[:], ["stgb"], [("pf", 0)], idn=identf[0:4, 0:4])
        CPY(posT[:], pf[0][0:64, 0:64].rearrange("p (k l) -> p k l", l=32), [("pf", 0)], ["posT"])
        CPY(b1sb[:], pf[0][:, 64:68], [("pf", 0)], ["b1sb"])
        for kv in range(2):
            for jc in range(2):
                col = kv * 2 + jc
                for l in range(32):
                    mm(pf[1][:, col:col + 1], w1[kv][0][0:64, l, jc * 128:(jc + 1) * 128], posT[0:64, kv, l:l + 1], l == 0, l == 31,
                       ["w1B", "posT"], [("pf", 1)])
        TT(biasj[:], pf[1][:, 0:4], b1sb[:], ALU.add, [("pf", 1), "b1sb"], ["biasj"])
        for tq in range(4):
            for which in range(2):
                pp = pf[2 + which]
                for k in range(8):
                    mm(pp[:], wbuf[s][:, k, which * 128:(which + 1) * 128], hT[:, k, tq * 512:(tq + 1) * 512], k == 0, k == 7,
                       [("w", s)] + [("hT", 4 * tq + u) for u in range(4)], [("pf", 2 + which)])
                act(KVD[which][:, :, tq * 32:(tq + 1) * 32].rearrange("p r m -> p m r"), pp[:].rearrange("p (m r) -> p m r", r=16),
                    AF.Copy, [("pf", 2 + which)], [("KVT", which)])
        n = 0
        for kv in range(2):
            for g in range(2):
                hb_ = hid[kv * 2 + g]
                for jc in range(2):
                    pp = pf[n % 2]
                    for l in range(32):
                        mm(pp[:, 0:127], w1[kv][g][g * 64:(g + 1) * 64, l, jc * 128:(jc + 1) * 128],
                           KVD[kv][g * 64:(g + 1) * 64, l % 16, (l // 16):(l // 16) + 127], l == 0, l == 31,
                           ["w1B", ("KVT", kv)], [("pf", n % 2)])
                    act(hb_[:, jc, 0:127], pp[:, 0:127], AF.Silu, [("pf", n % 2), "biasj"], [("hid", kv * 2 + g)],
                        bias=biasj[:, kv * 2 + jc:kv * 2 + jc + 1], scale=1.0)
                    n += 1
                po = pf[2 + g]
                for jc in range(2):
                    mm(po[0:127, 0:64], hb_[:, jc, 0:127], w2[kv][:, jc, :], jc == 0, jc == 1, [("hid", kv * 2 + g), "w2"], [("pf", 2 + g)])
                if kv == 0:
                    TT(kcf[0:127, :], po[0:127, 0:64], b2bc[0:127, 0, :], ALU.add, [("pf", 2 + g), "b2bc"], ["kcf"])
                    act(junk2[0:127, :], kcf[0:127, :], AF.Square, ["kcf"], ["junk2", "ssc"], accum_out=ssc[0:127, :])
                    TS(ssc[0:127, :], ssc[0:127, :], 1.0 / 64, EPS, ALU.mult, ALU.add, ["ssc"], ["ssc"])
                    act(ssc[0:127, :], ssc[0:127, :], AF.Sqrt, ["ssc"], ["ssc"])
                    RCP(ssc[0:127, :], ssc[0:127, :], ["ssc"], ["ssc"])
                    STT(kaug[0:127, 0:64], kcf[0:127, :], ssc[0:127, :], gcm[0:127, :], ALU.mult, ALU.mult, ["kcf", "ssc", "gcm"], ["kaug"])
                    CPY(kaug[:, 64:100], kalc[:], ["kalc"], ["kaug"])
                    tr(pb[g][:, 0:128], kaug[:], ["kaug"], [("pb", g)])
                    act(KTc[:, g, :], pb[g][:, 0:128], AF.Copy, [("pb", g)], [("KTc", g)])
                else:
                    TT(Vca[0:127, g, 0:64], po[0:127, 0:64], b2bc[0:127, 1, :], ALU.add, [("pf", 2 + g), "b2bc"], [("Vca", g)])

    def nsa_half(g, W):
        cv = Carver()
        QT = cv.get([128, 4, S], BF16)
        KTs = cv.get([128, S], BF16)
        KTw = cv.get([128, S], BF16)
        Vs = cv.get([128, NT, 66], BF16)
        Vw = cv.get([128, NT, 66], BF16)
        szy = cv.get([128, NT, 256], BF16)
        gates = cv.get([128, NT, 12], F32)
        oacc = cv.get([128, NT, 256], F32)
        imp = cv.get([128, 8, 32], F32)
        impc = cv.get([128, 8, 32], F32)
        cmn = cv.get([128, S], BF16)
        qa = [cv.get([128, 4, 128], BF16) for _ in range(3)]
        ksa = [cv.get([128, 128], BF16) for _ in range(3)]
        kwa = [cv.get([128, 128], BF16) for _ in range(3)]
        ptb = [cv.get([128, 512], BF16) for _ in range(3)]
        tmpf = [cv.get([128, 512], F32) for _ in range(2)]
        cmpb = cv.get([128, 32, 32], F32)
        rank = cv.get([128, 32], F32)
        nmt = cv.get([128, 128], BF16)
        s8 = [cv.get([128, 8], F32) for _ in range(3)]
        gq = cv.get([128, 1], F32)
        gks = cv.get([128, 1], F32)
        gkw = cv.get([128, 1], F32)
        rden = [cv.get([128, 1], F32) for _ in range(8)]
        yT = [cv.get([128, 2, 128], BF16) for _ in range(2)]
        s = load_w(W, [(g * 256, 256), (768 + 64 * g, 64), (1024 + 64 * g, 64), (896 + 64 * g, 64), (1152 + 64 * g, 64)])
        s2 = load_w(W, [(1304 + g * 256, 256), (1280 + 4 * g, 4), (1288 + 4 * g, 4), (1296 + 4 * g, 4)])
        load_wo(1, 2 * g, 2)
        load_gain_col(gq, cq_d, 1.0)
        load_gain_col(gks, cks_d, 8.0)
        load_gain_col(gkw, ckw_d, 8.0)
        dma("sp", impc[:], impc_d[:, 8:16, :], "c", writes=["impc"])
        dma("pool", cmn[:], cmn_d, "c", writes=["addmask"])
        for b in range(3):
            MSET(qa[b][:, :, 64:128], 0.0, [("qa", b)])
            MSET(ksa[b][:, 64:128], 0.0, [("ksa", b)])
            MSET(kwa[b][:, 64:128], 0.0, [("kwa", b)])
        MSET(Vs[:, :, 64:65], 1.0, ["Vs"])
        MSET(Vw[:, :, 64:65], 1.0, ["Vw"])
        MSET(nmt[:], 0.0, ["nmt"])
        def pa(i):
            b2 = i % 2
            pp = pf[b2]
            qai, ksi, kwi = qa[b2], ksa[b2], kwa[b2]
            CPY(qai[:, :, 96:100], qal[:, i, g * 16:g * 16 + 16].rearrange("p (h c) -> p h c", c=4), ["const"], [("qa", b2)])
            CPY(ksi[:, 64:100], kals[:, i, :], ["const"], [("ksa", b2)])
            CPY(kwi[:, 64:100], kalp[:, i, :], ["const"], [("kwa", b2)])
            for k in range(8):
                mm(pp[:], hT[:, k, i * 128:(i + 1) * 128], wbuf[s][:, k, :], k == 0, k == 7, [("hT", i), ("w", s)], [("pf", b2)])
            tf = tmpf[b2]
            head_rstd(pp, ("pf", b2), 384, tf, ("tf", b2), s8[b2], ("s8", b2))
            TT(qai[:, :, 0:64], pp[:, 0:256].rearrange("p (h d) -> p h d", d=64), s8[b2][:, 0:4].unsqueeze(2).to_broadcast([128, 4, 64]),
               ALU.mult, [("pf", b2), ("s8", b2)], [("qa", b2)])
            TS(ksi[:, 0:64], pp[:, 256:320], s8[b2][:, 4:5], None, ALU.mult, None, [("pf", b2), ("s8", b2)], [("ksa", b2)])
            TS(kwi[:, 0:64], pp[:, 320:384], s8[b2][:, 5:6], None, ALU.mult, None, [("pf", b2), ("s8", b2)], [("kwa", b2)])
            act(Vs[:, i, 0:64], pp[:, 384:448], AF.Copy, [("pf", b2), "Vs"], [("Vs", i)])
            act(Vw[:, i, 0:64], pp[:, 448:512], AF.Copy, [("pf", b2), "Vw"], [("Vw", i)])

        def pbk(i):
            b2 = i % 2
            qai, ksi, kwi = qa[b2], ksa[b2], kwa[b2]
            pt = pb[b2]
            for h in range(4):
                tr(pt[:, h * 128:(h + 1) * 128], qai[:, h, :], [("qa", b2)], [("pb", b2)])
            tr(pt[:, 512:640], ksi[:], [("ksa", b2)], [("pb", b2)])
            tr(pt[:, 640:768], kwi[:], [("kwa", b2)], [("pb", b2)])
            act(QT[:, :, i * 128:(i + 1) * 128], pt[:, 0:512].rearrange("p (h t) -> p h t", h=4), AF.Copy, [("pb", b2), "gq"], [("QT", i)],
                scale=gq[:, 0:1])
            act(KTs[:, i * 128:(i + 1) * 128], pt[:, 512:640], AF.Copy, [("pb", b2), "gq"], [("KTs", i)], scale=gks[:, 0:1])
            act(KTw[:, i * 128:(i + 1) * 128], pt[:, 640:768], AF.Copy, [("pb", b2), "gq"], [("KTw", i)], scale=gkw[:, 0:1])

        lagged(NT, pa, pbk)
        for i in range(NT):
            b2 = 2 + i % 2
            pp = pf[b2]
            for k in range(8):
                mm(pp[:, 0:256], hT[:, k, i * 128:(i + 1) * 128], wbuf[s2][:, k, 0:256], k == 0, k == 7, [("hT", i), ("w", s2)], [("pf", b2)])
            act(szy[:, i, :], pp[:, 0:256], AF.Silu, [("pf", b2)], [("szy", i)])
        for i in range(NT):
            b2 = 2 + i % 2
            pp = pf[b2]
            for k in range(8):
                mm(pp[:, 0:12], hT[:, k, i * 128:(i + 1) * 128], wbuf[s2][:, k, 256:268], k == 0, k == 7, [("hT", i), ("w", s2)], [("pf", b2)])
            act(gates[:, i, :], pp[:, 0:12], AF.Sigmoid, [("pf", b2)], [("gates", i)])
        if stop == "nsaproj":
            return
        qk = lambda qc: [("QT", 4 * qc + u) for u in range(4)]
        for r in range(4):
            def fin_cmp(i, Ob, okey, r=r):
                ri = (i + 4 * r) % 8
                rd = rden[ri]
                TS(rd[:], Ob[:, 64:65], 1e-30, None, ALU.max, None, [okey], [("rden", ri)])
                RCP(rd[:], rd[:], [("rden", ri)], [("rden", ri)])
                TS(oacc[:, i, r * 64:(r + 1) * 64], Ob[:, 0:64], rd[:], gates[:, i, r:r + 1], ALU.mult, ALU.mult,
                   [okey, ("rden", ri), ("gates", i)], [("oacc", i)])
                if i >= 8:
                    if r == 0:
                        TS(imp[:, i - 8, :], Ob[:, 65:97], rd[:], None, ALU.mult, None, [okey, ("rden", ri)], [("imp", i)])
                    else:
                        STT(imp[:, i - 8, :], Ob[:, 65:97], rd[:], imp[:, i - 8, :], ALU.mult, ALU.add, [okey, ("rden", ri), ("imp", i)], [("imp", i)])

            attention(QT[:, r, :], lambda kt: KTc[:, g, 0:127], 127, lambda kt: Vca[0:127, g, 0:97], 97,
                      lambda qc: [(0, 0, 3, {})], fin_cmp, qk, lambda kt: [("KTc", g)], lambda kt: [("Vca", g)], ptb, "n", addmask=cmn)
        if stop == "nsacmp":
            return
        for i in range(8, NT):
            im = imp[:, i - 8, :]
            TT(im, im, impc[:, i - 8, :], ALU.add, [("imp", i), "impc"], [("imp", i)])
            TT(cmpb[:], im.unsqueeze(1).to_broadcast([128, 32, 32]), im.unsqueeze(2).to_broadcast([128, 32, 32]), ALU.is_gt,
               [("imp", i)], ["cmpb"])
            RED(rank[:], cmpb[:], ALU.add, ["cmpb"], ["rank"])
            TS(nmt[:, 0:32], rank[:], 15.5, NEGM, ALU.is_ge, ALU.mult, ["rank"], ["nmt"])
            pt = pb[i % 2]
            tr(pt[:, 0:128], nmt[:], ["nmt"], [("pb", i % 2)])
            for r in range(4):
                act(QT[64:96, r, i * 128:(i + 1) * 128], pt[0:32, 0:128], AF.Copy, [("pb", i % 2)], [("QT", i)])
        for (KTx, Vx, kname, vname, plan, gcol) in ((KTs, Vs, "KTs", "Vs", plan_causal, 4), (KTw, Vw, "KTw", "Vw", plan_band(4), 8)):
            for r in range(4):
                def fin_add(i, Ob, okey, r=r, gcol=gcol):
                    ri = (i + 4 * r) % 8
                    rd = rden[ri]
                    RCP(rd[:], Ob[:, 64:65], [okey], [("rden", ri)])
                    TT(rd[:], rd[:], gates[:, i, gcol + r:gcol + r + 1], ALU.mult, [("rden", ri), ("gates", i)], [("rden", ri)])
                    dst = oacc[:, i, r * 64:(r + 1) * 64]
                    STT(dst, Ob[:, 0:64], rd[:], dst, ALU.mult, ALU.add, [okey, ("rden", ri), ("oacc", i)], [("oacc", i)])

                attention(QT[:, r, :], lambda kt, KTx=KTx: KTx[:, kt * 128:(kt + 1) * 128], 128,
                          lambda kt, Vx=Vx: Vx[:, kt, 0:65], 65, plan, fin_add, qk,
                          lambda kt, kname=kname: [(kname, kt)], lambda kt, vname=vname: [(vname, kt)], ptb, "n")
        for i in range(NT):
            TT(szy[:, i, :], oacc[:, i, :], szy[:, i, :], ALU.mult, [("oacc", i), ("szy", i)], [("szy", i)])
        outproj_half(szy, yT)

    def swa_half(g, W):
        cv = Carver()
        QT = cv.get([128, 4, S], BF16)
        KT = cv.get([128, S], BF16)
        Vt = cv.get([128, NT, 66], BF16)
        szy = cv.get([128, NT, 256], BF16)
        qa = [cv.get([128, 4, 128], BF16) for _ in range(3)]
        ka = [cv.get([128, 128], BF16) for _ in range(3)]
        ptb = [cv.get([128, 512], BF16) for _ in range(3)]
        tmpf = [cv.get([128, 512], F32) for _ in range(2)]
        s8 = [cv.get([128, 8], F32) for _ in range(3)]
        gq = cv.get([128, 1], F32)
        gk = cv.get([128, 1], F32)
        esink = cv.get([128, 8], F32)
        rden = [cv.get([128, 1], F32) for _ in range(8)]
        yT = [cv.get([128, 2, 128], BF16) for _ in range(2)]
        load_gain_col(gq, dq_d, 1.0)
        load_gain_col(gk, dk_d, 8.0)
        dma("sp", esink[:], dsink_d.partition_broadcast(128), "c", writes=["esink"])
        act(esink[:], esink[:], AF.Exp, ["esink"], ["esink"])
        for b in range(3):
            MSET(qa[b][:, :, 64:128], 0.0, [("qa", b)])
            MSET(ka[b][:, 64:128], 0.0, [("ka", b)])
        MSET(Vt[:, :, 64:65], 1.0, ["Vt"])
        s = load_w(W, [(1816 + g * 256, 256), (2328 + 64 * g, 64), (2456 + 64 * g, 64)])
        s2 = load_w(W, [(2584 + g * 256, 256)])
        load_wo(1, 4 + 2 * g, 2)
        def pa(i):
            b2 = i % 2
            pp = pf[b2]
            qai, kai = qa[b2], ka[b2]
            CPY(qai[:, :, 96:100], qal[:, i, g * 16:g * 16 + 16].rearrange("p (h c) -> p h c", c=4), ["const"], [("qa", b2)])
            CPY(kai[:, 64:100], kalp[:, i, :], ["const"], [("ka", b2)])
            for k in range(8):
                mm(pp[:, 0:384], hT[:, k, i * 128:(i + 1) * 128], wbuf[s][:, k, 0:384], k == 0, k == 7, [("hT", i), ("w", s)], [("pf", b2)])
            tf = tmpf[b2]
            head_rstd(pp, ("pf", b2), 320, tf, ("tf", b2), s8[b2], ("s8", b2))
            TT(qai[:, :, 0:64], pp[:, 0:256].rearrange("p (h d) -> p h d", d=64), s8[b2][:, 0:4].unsqueeze(2).to_broadcast([128, 4, 64]),
               ALU.mult, [("pf", b2), ("s8", b2)], [("qa", b2)])
            TS(kai[:, 0:64], pp[:, 256:320], s8[b2][:, 4:5], None, ALU.mult, None, [("pf", b2), ("s8", b2)], [("ka", b2)])
            act(Vt[:, i, 0:64], pp[:, 320:384], AF.Copy, [("pf", b2), "Vt"], [("V", i)])

        def pbk(i):
            b2 = i % 2
            qai, kai = qa[b2], ka[b2]
            pt = pb[b2]
            for h in range(4):
                tr(pt[:, h * 128:(h + 1) * 128], qai[:, h, :], [("qa", b2)], [("pb", b2)])
            tr(pt[:, 512:640], kai[:], [("ka", b2)], [("pb", b2)])
            act(QT[:, :, i * 128:(i + 1) * 128], pt[:, 0:512].rearrange("p (h t) -> p h t", h=4), AF.Copy, [("pb", b2), "gq"], [("QT", i)],
                scale=gq[:, 0:1])
            act(KT[:, i * 128:(i + 1) * 128], pt[:, 512:640], AF.Copy, [("pb", b2), "gq"], [("KT", i)], scale=gk[:, 0:1])

        lagged(NT, pa, pbk)
        for i in range(NT):
            b2 = 2 + i % 2
            pp = pf[b2]
            for k in range(8):
                mm(pp[:, 0:256], hT[:, k, i * 128:(i + 1) * 128], wbuf[s2][:, k, 0:256], k == 0, k == 7, [("hT", i), ("w", s2)], [("pf", b2)])
            act(szy[:, i, :], pp[:, 0:256], AF.Silu, [("pf", b2)], [("szy", i)])
        for r in range(4):
            def fin(i, Ob, okey, r=r):
                ri = (i + 4 * r) % 8
                rd = rden[ri]
                TS(rd[:], Ob[:, 64:65], esink[:, 4 * g + r:4 * g + r + 1], None, ALU.add, None, [okey, "esink"], [("rden", ri)])
                RCP(rd[:], rd[:], [("rden", ri)], [("rden", ri)])
                dst = szy[:, i, r * 64:(r + 1) * 64]
                STT(dst, Ob[:, 0:64], rd[:], dst, ALU.mult, ALU.mult, [okey, ("rden", ri), ("szy", i)], [("szy", i)])

            attention(QT[:, r, :], lambda kt: KT[:, kt * 128:(kt + 1) * 128], 128, lambda kt: Vt[:, kt, 0:65], 65,
                      plan_band(1), fin, lambda qc: [("QT", 4 * qc + u) for u in range(4)], lambda kt: [("KT", kt)],
                      lambda kt: [("V", kt)], ptb, "d")
        outproj_half(szy, yT, store=(g == 1 and stop is None))

    if 0 in layers:
        layer0()
    if 1 in layers:
        layer1()

    if not (1 in layers and stop is None):
        for i in range(NT):
            dma("sp", yv[:, i, :], x_sb[:, i, :], "out", reads=[("x", i)])
    P.finish("sp", list(P.chan_count.keys()))
    P.emit()
    es.close()
    return nc


_CACHE = {}


def kernel(**inputs):
    consts = _consts()
    shared = {}
    shared["norm_g"] = np.ascontiguousarray(inputs["norm_g"], dtype=np.float32)
    shared["w_out"] = np.ascontiguousarray(inputs["w_out"], dtype=np.float32)
    shared["e_w_in"] = np.ascontiguousarray(inputs["e_w_in"][0], dtype=np.float32)
    shared["a_conv_w"] = np.ascontiguousarray(inputs["a_conv_w"][0], dtype=np.float32)
    shared["a_conv_b"] = np.ascontiguousarray(inputs["a_conv_b"][0], dtype=np.float32)
    shared["a_ln_g"] = np.ascontiguousarray(inputs["a_ln_g"][0], dtype=np.float32)
    shared["a_ln_b"] = np.ascontiguousarray(inputs["a_ln_b"][0], dtype=np.float32)
    shared["b_qnorm_g"] = np.ascontiguousarray(inputs["b_qnorm_g"], dtype=np.float32)
    shared["b_knorm_g"] = np.ascontiguousarray(inputs["b_knorm_g"], dtype=np.float32)
    for k in ("ident", "tri", "up", "qal", "kal_moba", "kal_slc", "kal_plain", "kal_cmp", "cmaskneg", "ovl", "impc"):
        shared[k] = consts[k]
    shared["o_w_in"] = np.ascontiguousarray(inputs["o_w_in"][0], dtype=np.float32)
    for k in ("c_qnorm_g", "c_knorm_cmp_g", "c_knorm_slc_g", "c_knorm_win_g", "c_k_b2", "c_v_b2", "d_qnorm_g", "d_knorm_g", "d_sinks"):
        shared[k] = np.ascontiguousarray(inputs[k], dtype=np.float32).reshape(1, -1)
    for k in ("c_pos_k", "c_pos_v", "c_k_w1", "c_k_b1", "c_k_w2", "c_v_w1", "c_v_b1", "c_v_w2"):
        shared[k] = np.ascontiguousarray(inputs[k][0], dtype=np.float32)
    x = np.ascontiguousarray(inputs["x"], dtype=np.float32)
    nb = x.shape[0]
    layers = inputs.get("_layers", (0, 1))
    nc = build_program(layers, inputs.get("_stop"))
    in_maps = [dict(shared, x=x[b]) for b in range(nb)]
    res = run_bass_kernel_spmd(nc, in_maps, core_ids=list(range(nb)))
    return np.stack([np.asarray(r["y"], dtype=np.float32) for r in res.results], axis=0)
```

```python
import contextlib
import os
import numpy as np
import ml_dtypes
import concourse.bass as bass
import concourse.mybir as mybir
from concourse.bass_utils import run_bass_kernel_spmd

F32 = mybir.dt.float32
BF16 = mybir.dt.bfloat16
ALU = mybir.AluOpType
AF = mybir.ActivationFunctionType
AX = mybir.AxisListType

S = 2048
D = 1024
NT = 16
NEGM = -30000.0
EPS = 1e-6


class _Op:
    __slots__ = ("eng", "fn", "deps", "chan", "signal", "val", "dmaval", "chanseq")

    def __init__(self, eng, fn, chan):
        self.eng = eng
        self.fn = fn
        self.deps = {}
        self.chan = chan
        self.signal = False
        self.val = 0
        self.dmaval = None


class Prog:
    ENGS = ("pe", "act", "dve", "pool", "sp")

    def __init__(self, nc):
        self.nc = nc
        self.ops = {e: [] for e in self.ENGS}
        self.res = {}
        self.chan_count = {}
        self.chan_last = {}
        self.all_ops = []
        self.bar = {}
        self.final_waits = {}
        self.pool_ctr = 0
        self.sp_ctr = 0
        self.const_keys = []
        self.pres = {}

    PERS = ("w", "wo")

    def _st(self, k):
        if isinstance(k, tuple) and k[0] in self.PERS or k in self.PERS:
            return self.pres
        return self.res

    def op(self, eng, fn, reads=(), writes=(), chan=None, nobar=False):
        for w in writes:
            if isinstance(w, tuple) and w[0] == "ck" and w not in self.const_keys:
                self.const_keys.append(w)
        if "const" in reads:
            reads = [k for r in reads for k in (self.const_keys if r == "const" else (r,))]
        if chan is not None and eng == "pool":
            chan = "pq%d" % (self.pool_ctr % 12)
            self.pool_ctr += 1
        elif chan == "c":
            chan = "pc%d" % (self.sp_ctr % 16)
            self.sp_ctr += 1
        o = _Op(eng, fn, chan)
        deps = o.deps
        if not nobar:
            deps.update(self.bar)
        if chan is not None and (chan.startswith("pq") or chan.startswith("pc")):
            prev = self.chan_last.get(chan)
            if prev is not None:
                deps[id(prev)] = prev
        for k in reads:
            st = self._st(k).get(k)
            if st is not None and st[0] is not None:
                deps[id(st[0])] = st[0]
        for k in writes:
            st = self._st(k).get(k)
            if st is not None:
                if st[0] is not None:
                    deps[id(st[0])] = st[0]
                for r in st[1]:
                    deps[id(r)] = r
        for k in reads:
            res = self._st(k)
            st = res.get(k)
            if st is None:
                st = [None, []]
                res[k] = st
            st[1].append(o)
        for k in writes:
            self._st(k)[k] = [o, []]
        o.dmaval = dict(self.chan_count)
        if chan is not None:
            self.chan_count[chan] = self.chan_count.get(chan, 0) + 1
            o.chanseq = self.chan_count[chan]
            self.chan_last[chan] = o
            o.signal = True
        self.ops[eng].append(o)
        self.all_ops.append(o)
        return o

    def barrier(self):
        bar = {}
        for e in self.ENGS:
            if self.ops[e]:
                o = self.ops[e][-1]
                bar[id(o)] = o
        for ch, o in self.chan_last.items():
            bar[id(o)] = o
        self.bar = bar
        self.res = {}

    def finish(self, eng, chans):
        self.final_waits = {eng: {ch: self.chan_count[ch] for ch in chans}}

    def emit(self):
        nc = self.nc
        for o in self.all_ops:
            for d in o.deps.values():
                if d.chan is None:
                    if d.eng == "pe" and o.eng == "pe":
                        continue
                    d.signal = True
        for e in self.ENGS:
            c = 0
            for o in self.ops[e]:
                if o.chan is None and o.signal:
                    c += 1
                    o.val = c
        import os
        if os.environ.get("KDBG"):
            print("sem counts", {e: max([o.val for o in self.ops[e]] + [0]) for e in self.ENGS}, {e: len(self.ops[e]) for e in self.ENGS},
                  {c: 16 * v for c, v in self.chan_count.items()})
        stack = contextlib.ExitStack()
        sems = {}
        for e in self.ENGS:
            sems[e] = stack.enter_context(nc.semaphore("s_" + e))
        for ch in self.chan_count:
            sems["c_" + ch] = stack.enter_context(nc.semaphore("c_" + ch))
        block = stack.enter_context(nc.Block())
        engobj = {"pe": "tensor", "act": "scalar", "dve": "vector", "pool": "gpsimd", "sp": "sync"}

        def make(e):
            def body(eng):
                waited = {}
                for o in self.ops[e]:
                    need = {}
                    for d in o.deps.values():
                        if d.chan is not None:
                            k = "c_" + d.chan
                            if d.chan.startswith("pq") or d.chan.startswith("pc"):
                                v = 16 * d.chanseq
                            else:
                                v = 16 * o.dmaval[d.chan]
                        else:
                            if d.eng == "pe" and e == "pe":
                                continue
                            k = d.eng
                            v = d.val
                        if v > need.get(k, 0):
                            need[k] = v
                    for k, v in need.items():
                        if waited.get(k, 0) >= v:
                            continue
                        eng.wait_ge(sems[k], v)
                        waited[k] = v
                    ins = o.fn(eng)
                    if o.chan is not None:
                        ins.then_inc(sems["c_" + o.chan], 16)
                    elif o.signal:
                        ins.then_inc(sems[e], 1)
                for ch, c in self.final_waits.get(e, {}).items():
                    eng.wait_ge(sems["c_" + ch], 16 * c)
            return body

        for e in self.ENGS:
            getattr(block, engobj[e])(make(e))
        stack.close()


def _consts():
    c = {}
    c["ident"] = np.eye(128, dtype=np.float32)
    k = np.arange(128)[:, None]
    q = np.arange(128)[None, :]
    c["tri"] = np.where(k <= q, 0.0, NEGM).astype(np.float32)
    c["up"] = np.where(k > q, 0.0, NEGM).astype(np.float32)
    t = np.arange(S)
    b = (t % 16).astype(np.float32)
    a = (t - t % 16).astype(np.float32)
    slopes = np.power(2.0, -8.0 * np.arange(1, 9) / 8).astype(np.float32)
    qal = np.zeros((S, 8, 4), np.float32)
    qal[:, :, 0] = -slopes[None, :] * a[:, None]
    qal[:, :, 1] = -slopes[None, :] * b[:, None]
    qal[:, :, 2] = slopes[None, :]
    qal[:, :, 3] = slopes[None, :]
    c["qal"] = qal.reshape(NT, 128, 32).transpose(1, 0, 2).copy()

    def kal(onehot_block):
        m = np.zeros((S, 36), np.float32)
        if onehot_block:
            m[t, t // onehot_block] = 1.0
        m[:, 32] = 1.0
        m[:, 33] = 1.0
        m[:, 34] = a
        m[:, 35] = b
        return m.reshape(NT, 128, 36).transpose(1, 0, 2).copy()

    c["kal_moba"] = kal(256)
    c["kal_slc"] = kal(64)
    c["kal_plain"] = kal(0)
    cc = np.arange(128)
    cend = 16 * cc + 31
    kc = np.zeros((128, 36), np.float32)
    kc[:, 32] = 1.0
    kc[:, 33] = 1.0
    kc[:, 34] = cend - cend % 16
    kc[:, 35] = cend % 16
    c["kal_cmp"] = kc
    c["cmaskneg"] = np.where(t[None, :] >= cend[:, None], 0.0, NEGM).astype(np.float32)
    start = np.arange(127)[:, None] * 16
    bs = np.arange(32)[None, :] * 64
    ov = np.zeros((128, 32), np.float32)
    ov[:127] = ((start < bs + 64) & (start + 32 > bs)).astype(np.float32)
    c["ovl"] = ov
    blk = np.arange(32)[None, :]
    cur = (t // 64)[:, None]
    forced = (blk == 0) | (blk == cur) | (blk == cur - 1)
    impc = 1e4 * forced.astype(np.float32) - 1e5 * (blk > cur).astype(np.float32)
    c["impc"] = impc.reshape(NT, 128, 32).transpose(1, 0, 2).copy()
    return c


def build_program(layers=(0, 1), stop=None):
    nc = bass.Bass("TRN2", target_bir_lowering=False)
    es = contextlib.ExitStack()
    dram = {}

    def din(name, shape, dt=F32):
        dram[name] = nc.dram_tensor(name, list(shape), dt, kind="ExternalInput").ap()
        return dram[name]

    x_d = din("x", [S, D])
    normg_d = din("norm_g", [2, D])
    wout_d = din("w_out", [2, D, D])
    ewin_d = din("e_w_in", [D, 3584])
    convw_d = din("a_conv_w", [31, 512])
    convb_d = din("a_conv_b", [512])
    lng_d = din("a_ln_g", [512])
    lnb_d = din("a_ln_b", [512])
    bq_d = din("b_qnorm_g", [1, 64])
    bk_d = din("b_knorm_g", [1, 64])
    ident_d = din("ident", [128, 128])
    tri_d = din("tri", [128, 128])
    up_d = din("up", [128, 128])
    qal_d = din("qal", [128, NT, 32])
    kalm_d = din("kal_moba", [128, NT, 36])
    kals_d = din("kal_slc", [128, NT, 36])
    kalp_d = din("kal_plain", [128, NT, 36])
    kalc_d = din("kal_cmp", [128, 36])
    cmn_d = din("cmaskneg", [128, S])
    ovl_d = din("ovl", [128, 32])
    impc_d = din("impc", [128, NT, 32])
    owin_d = din("o_w_in", [D, 3096])
    cq_d = din("c_qnorm_g", [1, 64])
    ckc_d = din("c_knorm_cmp_g", [1, 64])
    cks_d = din("c_knorm_slc_g", [1, 64])
    ckw_d = din("c_knorm_win_g", [1, 64])
    cposk_d = din("c_pos_k", [32, 64])
    cposv_d = din("c_pos_v", [32, 64])
    ckw1_d = din("c_k_w1", [2048, 256])
    ckb1_d = din("c_k_b1", [256])
    ckw2_d = din("c_k_w2", [256, 64])
    ckb2_d = din("c_k_b2", [1, 64])
    cvw1_d = din("c_v_w1", [2048, 256])
    cvb1_d = din("c_v_b1", [256])
    cvw2_d = din("c_v_w2", [256, 64])
    cvb2_d = din("c_v_b2", [1, 64])
    dq_d = din("d_qnorm_g", [1, 64])
    dk_d = din("d_knorm_g", [1, 64])
    dsink_d = din("d_sinks", [1, 8])
    y_d = nc.dram_tensor("y", [S, D], F32, kind="ExternalOutput").ap()

    def sb(name, shape, dt):
        return es.enter_context(nc.sbuf_tensor(name, list(shape), dt))

    def psum(name, shape, dt):
        return es.enter_context(nc.psum_tensor(name, list(shape), dt))

    P = Prog(nc)

    x_sb = sb("x_sb", [128, NT, D], F32)
    hT = sb("hT", [128, 8, S], BF16)
    wbuf = [sb("wbuf%d" % i, [128, 8, 512], BF16) for i in range(2)]
    wo = sb("wo", [128, 4, D], BF16)
    ident = sb("ident_sb", [128, 128], BF16)
    identf = sb("identf_sb", [128, 128], F32)
    onesf = sb("onesf", [128, 128], F32)
    tri = sb("tri_sb", [128, 128], BF16)
    upm = sb("up_sb", [128, 128], BF16)
    qal = sb("qal_sb", [128, NT, 32], BF16)
    kalm = sb("kalm_sb", [128, NT, 36], BF16)
    KTc = sb("KTc", [128, 2, 128], BF16)
    Vca = sb("Vca", [128, 2, 98], BF16)
    kals = sb("kals_sb", [128, NT, 36], BF16)
    kalp = sb("kalp_sb", [128, NT, 36], BF16)
    ss = sb("ss", [128, NT], F32)
    rstd = sb("rstd", [128, NT], F32)
    ARENA = 80 * 1024
    arena = sb("arena", [128, ARENA // 2], BF16)

    pf = [psum("pf%d" % i, [128, 512], F32) for i in range(6)]
    pb = [psum("pb%d" % i, [128, 1024], BF16) for i in range(2)]

    class Carver:
        def __init__(self, base=0):
            self.off = base

        def get(self, shape, dt):
            n = int(np.prod(shape[1:]))
            nb = n * (4 if dt == F32 else 2)
            nb = (nb + 63) // 64 * 64
            assert self.off + nb <= ARENA, ("arena overflow", self.off + nb, ARENA)
            a = arena[:, self.off // 2:(self.off + nb) // 2]
            self.off += nb
            if os.environ.get("KDBG"):
                print("carve", shape, dt, "->", self.off)
            if dt == F32:
                a = a.bitcast(F32)
            a = a[:, 0:n]
            if len(shape) == 3:
                a = a.rearrange("p (a b) -> p a b", b=shape[2])
            elif len(shape) == 4:
                a = a.rearrange("p (a b c) -> p a b c", b=shape[2], c=shape[3])
            if shape[0] < 128:
                a = a[0:shape[0]]
            return a

    def dma(eng, out, in_, chan, reads=(), writes=(), slow=False, nobar=False):
        if slow:
            P.op(eng, lambda e: e.dma_start(out=out, in_=in_, allow_slow_non_contiguous=True), reads=reads, writes=writes, chan=chan, nobar=nobar)
        else:
            P.op(eng, lambda e: e.dma_start(out=out, in_=in_), reads=reads, writes=writes, chan=chan, nobar=nobar)

    def mm(out, lhsT, rhs, start, stop, reads, writes):
        P.op("pe", lambda e: e.matmul(out, lhsT=lhsT, rhs=rhs, start=start, stop=stop), reads=reads, writes=writes)

    def tr(out, in_, reads, writes, idn=None):
        ck = ("ck", "ident") if idn is None else ("ck", "identf")
        idn = ident[:] if idn is None else idn
        P.op("pe", lambda e: e.transpose(out=out, in_=in_, identity=idn), reads=list(reads) + [ck], writes=writes)

    def act(out, in_, func, reads, writes, **kw):
        P.op("act", lambda e: e.activation(out=out, in_=in_, func=func, **kw), reads=reads, writes=writes)

    def V(fn, reads, writes, eng="dve"):
        P.op(eng, fn, reads=reads, writes=writes)

    def TT(out, in0, in1, op, reads, writes, eng="dve"):
        P.op(eng, lambda e: e.tensor_tensor(out=out, in0=in0, in1=in1, op=op), reads=reads, writes=writes)

    def TS(out, in0, s1, s2, op0, op1, reads, writes, eng="dve"):
        if op1 is None:
            P.op(eng, lambda e: e.tensor_scalar(out=out, in0=in0, scalar1=s1, scalar2=None, op0=op0), reads=reads, writes=writes)
        else:
            P.op(eng, lambda e: e.tensor_scalar(out=out, in0=in0, scalar1=s1, scalar2=s2, op0=op0, op1=op1), reads=reads, writes=writes)

    def STT(out, in0, scalar, in1, op0, op1, reads, writes, eng="dve"):
        P.op(eng, lambda e: e.scalar_tensor_tensor(out=out, in0=in0, scalar=scalar, in1=in1, op0=op0, op1=op1), reads=reads, writes=writes)

    def RED(out, in_, op, reads, writes):
        P.op("dve", lambda e: e.tensor_reduce(out=out, in_=in_, axis=AX.X, op=op), reads=reads, writes=writes)

    def RCP(out, in_, reads, writes):
        P.op("dve", lambda e: e.reciprocal(out=out, in_=in_), reads=reads, writes=writes)

    def CPY(out, in_, reads, writes, eng="dve"):
        P.op(eng, lambda e: e.tensor_copy(out=out, in_=in_), reads=reads, writes=writes)

    def MSET(ap, val, writes, eng="dve"):
        P.op(eng, lambda e: e.memset(ap, val), reads=[], writes=writes)

    import os
    KB = os.environ.get("KBIS", "abcdefg")
    if "a" in KB:
        dma("pool", ident[:], ident_d, "c", writes=[("ck", "ident")])
    if "b" in KB:
        dma("pool", tri[:], tri_d, "c", writes=[("ck", "tri")])
        dma("pool", upm[:], up_d, "c", writes=[("ck", "up")])
    if "c" in KB:
        dma("pool", qal[:], qal_d, "c", writes=[("ck", "qal")])
    if "d" in KB:
        dma("pool", kalm[:], kalm_d, "c", writes=[("ck", "kalm")])
        dma("pool", kals[:], kals_d, "c", writes=[("ck", "kals")])
        dma("pool", kalp[:], kalp_d, "c", writes=[("ck", "kalp")])
    if "e" in KB:
        dma("sp", identf[:], ident_d, "c", writes=[("ck", "identf")])
    if "f" in KB:
        MSET(onesf[:], 1.0, [("ck", "onesf")])

    xv = x_d.rearrange("(i p) d -> p i d", p=128)
    yv = y_d.rearrange("(i p) d -> p i d", p=128)
    for i in range(NT):
        dma("sp", x_sb[:, i, :], xv[:, i, :], "x%d" % i, writes=[("x", i)])

    wslot_ctr = [0]

    def load_w(wd, segs):
        s = wslot_ctr[0] % 2
        wslot_ctr[0] += 1
        wv = wd.rearrange("(k p) n -> p k n", p=128)
        o = 0
        for (c0, n) in segs:
            dma("pool", wbuf[s][:, :, o:o + n], wv[:, :, c0:c0 + n], "w%d" % s, writes=[("w", s)], nobar=True)
            o += n
        return s

    def norm_phase(layer, base=0, barrier=True):
        cvn = Carver(base=base)
        gbc = cvn.get([128, D], F32)
        junk = cvn.get([128, D], BF16)
        hb = [cvn.get([128, D], BF16) for _ in range(2)]
        dma("act", gbc[:], normg_d[layer:layer + 1, :].partition_broadcast(128), "c", writes=["gbc"])
        MSET(ss[:], 0.0, [("ss", gI) for gI in range(4)])

        def stats(gI):
            for i in range(4 * gI, 4 * gI + 4):
                act(junk[:], x_sb[:, i, :], AF.Square, [("x", i), ("ss", gI)], ["junk", ("ss", gI)], accum_out=ss[:, i:i + 1])
            sl = slice(4 * gI, 4 * gI + 4)
            TS(rstd[:, sl], ss[:, sl], 1.0 / D, EPS, ALU.mult, ALU.add, [("ss", gI)], [("rstd", gI)])
            act(rstd[:, sl], rstd[:, sl], AF.Sqrt, [("rstd", gI)], [("rstd", gI)])
            RCP(rstd[:, sl], rstd[:, sl], [("rstd", gI)], [("rstd", gI)])

        def na(i):
            h = hb[i % 2]
            STT(h[:], x_sb[:, i, :], rstd[:, i:i + 1], gbc[:], ALU.mult, ALU.mult, [("x", i), ("rstd", i // 4), "gbc"], [("hb", i % 2)])

        def nb(i):
            h = hb[i % 2]
            pt = pb[i % 2]
            for k in range(8):
                tr(pt[:, k * 128:(k + 1) * 128], h[:, k * 128:(k + 1) * 128], [("hb", i % 2)], [("pb", i % 2)])
            act(hT[:, :, i * 128:(i + 1) * 128], pt[:].rearrange("p (k t) -> p k t", k=8), AF.Copy, [("pb", i % 2)], [("hT", i)])

        pend = []
        if layer == 0:
            for gI in range(4):
                stats(gI)
                for i in range(4 * gI, 4 * gI + 4):
                    na(i)
                    pend.append(i)
                    if len(pend) > 1:
                        nb(pend.pop(0))
        else:
            for gI in range(5):
                if gI < 4:
                    stats(gI)
                if gI >= 1:
                    for i in range(4 * (gI - 1), 4 * gI):
                        na(i)
                        pend.append(i)
                        if len(pend) > 1:
                            nb(pend.pop(0))
        nb(pend.pop(0))
        if barrier:
            P.barrier()

    def load_wo(layer, c0, n):
        wv = wout_d[layer].rearrange("(k p) n -> p k n", p=128)
        for hf in range(2):
            dma("pool", wo[:, 0:n, hf * 512:(hf + 1) * 512], wv[:, c0:c0 + n, hf * 512:(hf + 1) * 512], "wo", writes=["wo"], nobar=True)

    def outproj_tile(i, lhs_list, chunks, yT_reads, pbanks):
        for half in range(2):
            pp = pf[pbanks[half]]
            for n, (c, lhs) in enumerate(zip(chunks, lhs_list)):
                mm(pp[:], lhs, wo[:, c, half * 512:(half + 1) * 512], n == 0, n == len(chunks) - 1,
                   list(yT_reads) + ["wo"], [("pf", pbanks[half])])
            xs = x_sb[:, i, half * 512:(half + 1) * 512]
            TT(xs, xs, pp[:], ALU.add, [("pf", pbanks[half]), ("x", i)], [("x", i)])

    att_ctr = [0, 0]

    def lagged(n, stage_a, stage_b, lag=1):
        for i in range(n + lag):
            if i < n:
                stage_a(i)
            if i >= lag:
                stage_b(i - lag)

    def mm_acc(out, lhsT, rhs, start, stop, reads, writes):
        P.op("pe", lambda e: e.matmul(out, lhsT=lhsT, rhs=rhs, start=start, stop=stop, skip_group_check=True), reads=reads, writes=writes)

    def attention(QT, KT, nk, Vrhs, ncv, plan, finish, qkeys, kkeys, vkeys, ptb, tagk, addmask=None, finish_chunk=None):
        NS = len(ptb)
        work = []
        for qc in range(4):
            items = plan(qc)
            if not items:
                continue
            ob = 3 + att_ctr[1] % 3
            att_ctr[1] += 1
            lastk = {}
            for (kt, jlo, jhi, masks) in items:
                for j in range(jlo, jhi + 1):
                    lastk[j] = kt
            for n, it in enumerate(items):
                work.append((qc, ob, it, n == 0, n == len(items) - 1, lastk))

        def front(w):
            qc, ob, (kt, jlo, jhi, masks), first, last, lastk = w
            sbk = att_ctr[0] % NS
            att_ctr[0] += 1
            Sp = pf[sbk]
            pt = ptb[sbk]
            c0, c1 = jlo * 128, (jhi + 1) * 128
            extra = list(masks.items())
            mm_acc(Sp[0:nk, c0:c1], KT(kt), QT[:, qc * 512 + c0:qc * 512 + c1], True, addmask is None and not extra,
                   list(qkeys(qc)) + list(kkeys(kt)), [("pf", sbk)])
            if addmask is not None:
                mm_acc(Sp[0:nk, c0:c1], ident[0:nk, 0:nk], addmask[0:nk, qc * 512 + c0:qc * 512 + c1], False, not extra,
                       ["const", "addmask"], [("pf", sbk)])
            for n, (j, mk) in enumerate(extra):
                mm_acc(Sp[0:nk, j * 128:(j + 1) * 128], ident[0:nk, 0:nk], mk[0:nk, :], False, n == len(extra) - 1,
                       ["const"], [("pf", sbk)])
            act(pt[0:nk, c0:c1], Sp[0:nk, c0:c1], AF.Exp, [("pf", sbk)], [("pt", tagk, sbk)])
            return sbk

        def back(w, sbk, started):
            qc, ob, (kt, jlo, jhi, masks), first, last, lastk = w
            pt = ptb[sbk]
            for j in range(jlo, jhi + 1):
                mm_acc(pf[ob][:, j * 128:j * 128 + ncv], pt[0:nk, j * 128:(j + 1) * 128], Vrhs(kt), len(started) == 0, lastk[j] == kt,
                       [("pt", tagk, sbk)] + list(vkeys(kt)), [("pf", ob)])
                started.add(j)
            if last:
                if finish_chunk is not None:
                    finish_chunk(qc, pf[ob], ("pf", ob))
                else:
                    for j in sorted(started):
                        finish(qc * 4 + j, pf[ob][:, j * 128:j * 128 + ncv], ("pf", ob))
                started.clear()

        LAG = NS - 1
        started = set()
        pend = []
        for w in work:
            pend.append((w, front(w)))
            if len(pend) > LAG:
                pw, psb = pend.pop(0)
                back(pw, psb, started)
        for (pw, psb) in pend:
            back(pw, psb, started)

    def layer0():
        if stop == "load":
            return
        norm_phase(0, base=70 * 1024, barrier=False)
        if stop == "norm0":
            return
        load_wo(0, 0, 4)
        W = ewin_d
        if stop == "norm":
            return
        cv = Carver()
        yc = cv.get([128, 4, S], F32)
        hc = cv.get([128, S + 32], BF16)
        dg = cv.get([128, 31, 128], BF16)
        cw = cv.get([128, 4, 31], F32)
        cb = cv.get([128, 4], F32)
        lg = cv.get([128, 4], F32)
        lb = cv.get([128, 4], F32)
        Tsq = [cv.get([128, 512], F32) for _ in range(2)]
        mu2 = [cv.get([128, 512], F32) for _ in range(2)]
        msq = cv.get([128, 512], F32)
        rs2 = [cv.get([128, 512], F32) for _ in range(2)]
        sz4 = [cv.get([128, 512], BF16) for _ in range(4)]
        T4 = [cv.get([128, 512], F32) for _ in range(4)]
        yaT2 = [cv.get([128, 4, 512], BF16) for _ in range(2)]
        HO = 32
        stg = Tsq[0][0:32, :]
        stg2 = Tsq[1][0:12, 0:128]
        dma("sp", stg[0:31, :], convw_d, "c", writes=[("Tsq", 0)])
        dma("sp", stg2[0:4, :], convb_d.rearrange("(a c) -> a c", c=128), "c", writes=[("Tsq", 1)])
        dma("sp", stg2[4:8, :], lng_d.rearrange("(a c) -> a c", c=128), "c", writes=[("Tsq", 1)])
        dma("sp", stg2[8:12, :], lnb_d.rearrange("(a c) -> a c", c=128), "c", writes=[("Tsq", 1)])
        for cc in range(4):
            tr(pf[0][:, cc * 32:cc * 32 + 31], stg[0:31, cc * 128:(cc + 1) * 128], [("Tsq", 0)], [("pf", 0)], idn=identf[0:31, 0:31])
        tr(pf[0][:, 128:140], stg2[0:12, :], [("Tsq", 1)], [("pf", 0)], idn=identf[0:12, 0:12])
        CPY(cw[:], pf[0][:, 0:128].rearrange("p (a j) -> p a j", j=32)[:, :, 0:31], [("pf", 0)], ["cw"])
        CPY(cb[:], pf[0][:, 128:132], [("pf", 0)], ["cw"])
        CPY(lg[:], pf[0][:, 132:136], [("pf", 0)], ["cw"])
        CPY(lb[:], pf[0][:, 136:140], [("pf", 0)], ["cw"])
        MSET(hc[:, 0:HO], 0.0, ["hc0"])
        if stop == "convp":
            return
        for cc in range(4):
            if stop == "conv1" and cc == 1:
                return
            s = load_w(W, [(cc * 128, 128), (512 + cc * 128, 128)])
            for j in range(31):
                TS(dg[:, j, :], ident[:], cw[:, cc, j:j + 1], None, ALU.mult, None, ["const", "cw"], [("dg", j)])
            for tq in range(4):
                bv, bg = (tq % 2) * 2, (tq % 2) * 2 + 1
                for which, bk in ((0, bv), (1, bg)):
                    for k in range(8):
                        mm(pf[bk][:], wbuf[s][:, k, which * 128:(which + 1) * 128], hT[:, k, tq * 512:(tq + 1) * 512], k == 0, k == 7,
                           [("w", s)] + [("hT", 4 * tq + u) for u in range(4)], [("pf", bk)])
                act(T4[tq % 2][:], pf[bg][:], AF.Sigmoid, [("pf", bg)], [("T4", tq % 2)])
                TT(hc[:, HO + tq * 512:HO + (tq + 1) * 512], pf[bv][:], T4[tq % 2][:], ALU.mult, [("pf", bv), ("T4", tq % 2)], [("hc", tq)])
            for tq in range(4):
                pc = pf[4 + tq % 2]
                for j in range(31):
                    o = HO - 30 + j + tq * 512
                    mm(pc[:], dg[:, j, :], hc[:, o:o + 512], j == 0, j == 30,
                       [("dg", j), ("hc", tq)] + ([("hc", tq - 1)] if tq > 0 else ["hc0"]), [("pf", 4 + tq % 2)])
                act(yc[:, cc, tq * 512:(tq + 1) * 512], pc[:], AF.Identity, [("pf", 4 + tq % 2), "cw"], [("yc", cc, tq)],
                    bias=cb[:, cc:cc + 1], scale=1.0)
        if stop == "conv4":
            return
        P.barrier()
        sz_slot = load_w(W, [(1024, 512)])

        def st_a(tq):
            ts = slice(tq * 512, (tq + 1) * 512)
            p2 = tq % 2
            for cc in range(4):
                mm(pf[0][:], onesf[:], yc[:, cc, ts], cc == 0, cc == 3, [("yc", cc, tq), "const"], [("pf", 0)])
            for cc in range(4):
                act(Tsq[cc % 2][:], yc[:, cc, ts], AF.Square, [("yc", cc, tq)], [("Tsq", cc % 2)])
                mm(pf[1][:], onesf[:], Tsq[cc % 2][:], cc == 0, cc == 3, [("Tsq", cc % 2), "const"], [("pf", 1)])
            TS(mu2[p2][:], pf[0][:], 1.0 / 512, None, ALU.mult, None, [("pf", 0)], [("mu", p2)])
            TT(msq[:], mu2[p2][:], mu2[p2][:], ALU.mult, [("mu", p2)], ["msq"])
            STT(rs2[p2][:], pf[1][:], 1.0 / 512, msq[:], ALU.mult, ALU.subtract, [("pf", 1), "msq"], [("rs", p2)])
            TS(rs2[p2][:], rs2[p2][:], EPS, None, ALU.add, None, [("rs", p2)], [("rs", p2)])
            act(rs2[p2][:], rs2[p2][:], AF.Sqrt, [("rs", p2)], [("rs", p2)])
            RCP(rs2[p2][:], rs2[p2][:], [("rs", p2)], [("rs", p2)])

        def st_b(tq):
            ts = slice(tq * 512, (tq + 1) * 512)
            p2 = tq % 2
            for cc in range(4):
                pz = pf[2 + cc % 2]
                for k in range(8):
                    mm(pz[:], wbuf[sz_slot][:, k, cc * 128:(cc + 1) * 128], hT[:, k, ts], k == 0, k == 7,
                       [("w", sz_slot)] + [("hT", 4 * tq + u) for u in range(4)], [("pf", 2 + cc % 2)])
                act(sz4[cc][:], pz[:], AF.Silu, [("pf", 2 + cc % 2)], [("sz", cc)])
            for cc in range(4):
                tt = T4[cc]
                TT(tt[:], yc[:, cc, ts], mu2[p2][:], ALU.subtract, [("yc", cc, tq), ("mu", p2)], [("T4", cc)])
                TT(tt[:], tt[:], rs2[p2][:], ALU.mult, [("T4", cc), ("rs", p2)], [("T4", cc)])
            for cc in range(4):
                tt = T4[cc]
                act(tt[:], tt[:], AF.Silu, [("T4", cc), "cw"], [("T4", cc)], scale=lg[:, cc:cc + 1], bias=lb[:, cc:cc + 1])
            for cc in range(4):
                TT(yaT2[p2][:, cc, :], T4[cc][:], sz4[cc][:], ALU.mult, [("T4", cc), ("sz", cc)], [("yaT", p2, cc)])

        def st_c(tq):
            p2 = tq % 2
            for u in range(4):
                i = 4 * tq + u
                outproj_tile(i, [yaT2[p2][:, cc, u * 128:(u + 1) * 128] for cc in range(4)], [0, 1, 2, 3],
                             [("yaT", p2, cc) for cc in range(4)], (4, 5))

        for step in range(6):
            if step < 4:
                st_a(step)
            if 0 <= step - 1 < 4:
                st_b(step - 1)
            if step - 2 >= 0:
                st_c(step - 2)
        P.barrier()
        if stop == "conv":
            return
        for hh in range(2):
            moba_half(hh, W)
            P.barrier()
            if stop is not None:
                return

    def moba_half(hh, W):
        cv = Carver()
        QT = cv.get([128, 4, S], BF16)
        KT = cv.get([128, 4, S], BF16)
        Vt = cv.get([128, NT, 4, 66], BF16)
        szy = cv.get([128, NT, 256], BF16)
        ptb = [cv.get([128, 512], BF16) for _ in range(3)]
        gq = cv.get([128, 1], F32)
        gk = cv.get([128, 1], F32)
        kmf = cv.get([128, 4, 8], F32)
        kmb = cv.get([128, 4, 8], BF16)
        gs = cv.get([128, 4, 8], F32)
        cmp_ = cv.get([128, 4, 8, 8], F32)
        rank = cv.get([128, 4, 8], F32)
        nm = cv.get([128, 4, 32], BF16)
        rden = [cv.get([128, 1], F32) for _ in range(8)]
        yT = [cv.get([128, 2, 128], BF16) for _ in range(2)]
        tmpf = [cv.get([128, 512], F32) for _ in range(2)]
        s8 = [cv.get([128, 8], F32) for _ in range(3)]
        qa = [cv.get([128, 4, 128], BF16) for _ in range(3)]
        ka = [cv.get([128, 4, 128], BF16) for _ in range(3)]
        c_q = 1536 + hh * 256
        c_k = 2048 + hh * 256
        c_v = 2560 + hh * 256
        c_z = 3072 + hh * 256
        s = load_w(W, [(c_q, 256), (c_k, 256)])
        s2 = load_w(W, [(c_v, 256), (c_z, 256)])
        load_wo(0, 4 + 2 * hh, 2)
        load_gain_col(gq, bq_d, 1.0)
        load_gain_col(gk, bk_d, 8.0)
        for b in range(3):
            MSET(qa[b][:, :, 64:128], 0.0, [("qa", b)])
            MSET(ka[b][:, :, 64:128], 0.0, [("ka", b)])
        MSET(Vt[:, :, :, 64:65], 1.0, ["Vt"])
        MSET(nm[:], 0.0, ["nm"])

        def m0(i):
            r = i % 3
            CPY(qa[r][:, :, 96:100], qal[:, i, hh * 16:hh * 16 + 16].rearrange("p (h c) -> p h c", c=4), ["const"], [("qa", r)])
            CPY(ka[r][:, :, 64:100], kalm[:, i, :].unsqueeze(1).to_broadcast([128, 4, 36]), ["const"], [("ka", r)])
            for k in range(8):
                mm(pf[r][:], hT[:, k, i * 128:(i + 1) * 128], wbuf[s][:, k, :], k == 0, k == 7, [("hT", i), ("w", s)], [("pf", r)])

        def m1(i):
            r = i % 3
            rstd_a(pf[r], ("pf", r), 512, tmpf[i % 2], ("tf", i % 2), s8[r], ("s8", r))

        def m2(i):
            r = i % 3
            rstd_b(512, s8[r], ("s8", r))
            TT(qa[r][:, :, 0:64], pf[r][:, 0:256].rearrange("p (h d) -> p h d", d=64), s8[r][:, 0:4].unsqueeze(2).to_broadcast([128, 4, 64]),
               ALU.mult, [("pf", r), ("s8", r)], [("qa", r)])
            TT(ka[r][:, :, 0:64], pf[r][:, 256:512].rearrange("p (h d) -> p h d", d=64), s8[r][:, 4:8].unsqueeze(2).to_broadcast([128, 4, 64]),
               ALU.mult, [("pf", r), ("s8", r)], [("ka", r)])

        def m3(i):
            r = i % 3
            b2 = i % 2
            pt = pb[b2]
            for h in range(4):
                tr(pt[:, h * 128:(h + 1) * 128], qa[r][:, h, :], [("qa", r)], [("pb", b2)])
            for h in range(4):
                tr(pt[:, (4 + h) * 128:(5 + h) * 128], ka[r][:, h, :], [("ka", r)], [("pb", b2)])
            act(QT[:, :, i * 128:(i + 1) * 128], pt[:, 0:512].rearrange("p (h t) -> p h t", h=4), AF.Copy, [("pb", b2), "gq"], [("QT", i)],
                scale=gq[:, 0:1])
            act(KT[:, :, i * 128:(i + 1) * 128], pt[:, 512:1024].rearrange("p (h t) -> p h t", h=4), AF.Copy, [("pb", b2), "gq"], [("KT", i)],
                scale=gk[:, 0:1])

        pipe4(NT, m0, m1, m2, m3)
        if hh == 1 and 1 in layers:
            prefetch_w1([("qa", r) for r in range(3)] + [("ka", r) for r in range(3)] + [("s8", r) for r in range(3)] + [("tf", 0), ("tf", 1)])
        if stop == "m1":
            return
        for i in range(NT):
            b2 = 2 + i % 2
            pp = pf[b2]
            for k in range(8):
                mm(pp[:], hT[:, k, i * 128:(i + 1) * 128], wbuf[s2][:, k, :], k == 0, k == 7, [("hT", i), ("w", s2)], [("pf", b2)])
            act(Vt[:, i, :, 0:64], pp[:, 0:256].rearrange("p (h d) -> p h d", d=64), AF.Copy, [("pf", b2), "Vt"], [("V", i)])
            act(szy[:, i, :], pp[:, 256:512], AF.Silu, [("pf", b2)], [("szy", i)])
        if stop == "m3":
            return
        for h in range(4):
            RED(kmf[:, h, :], KT[:, h, :].rearrange("p (n t) -> p n t", t=256), ALU.add, [("KT", i) for i in range(NT)], ["kmf"])
        TS(kmb[:], kmf[:], 1.0 / 256, None, ALU.mult, None, ["kmf"], ["kmb"])
        if stop == "mk":
            return
        for i in range(8, NT):
            npast = i // 2
            b2 = 4 + i % 2
            pg = pf[b2]
            for h in range(4):
                mm(pg[:, h * 8:h * 8 + 8], QT[0:64, h, i * 128:(i + 1) * 128], kmb[0:64, h, :], True, True, [("QT", i), "kmb"], [("pf", b2)])
            CPY(gs[:], pg[:, 0:32].rearrange("p (h n) -> p h n", n=8), [("pf", b2)], ["gs"])
            TT(cmp_[:, :, 0:npast, 0:npast], gs[:, :, 0:npast].unsqueeze(2).to_broadcast([128, 4, npast, npast]),
               gs[:, :, 0:npast].unsqueeze(3).to_broadcast([128, 4, npast, npast]), ALU.is_gt, ["gs"], ["cmp"])
            RED(rank[:, :, 0:npast], cmp_[:, :, 0:npast, 0:npast], ALU.add, ["cmp"], ["rank"])
            TS(nm[:, :, 0:npast], rank[:, :, 0:npast], 2.5, NEGM, ALU.is_ge, ALU.mult, ["rank"], ["nm"])
            pt = pb[i % 2]
            tr(pt[:, 0:128], nm[:].rearrange("p h n -> p (h n)"), ["nm"], [("pb", i % 2)])
            for h in range(4):
                act(QT[64:96, h, i * 128:(i + 1) * 128], pt[h * 32:(h + 1) * 32, 0:128], AF.Copy, [("pb", i % 2)], [("QT", i)])

        if stop == "m2":
            return

        def plan(qc):
            items = []
            for kt in range(4 * qc + 4):
                if kt < 4 * qc:
                    items.append((kt, 0, 3, {}))
                else:
                    m = kt - 4 * qc
                    items.append((kt, m, 3, {m: tri[:]}))
            return items

        for h in range(4):
            def finish(i, Ob, okey, h=h):
                r = (i + 4 * h) % 8
                rd = rden[r]
                RCP(rd[:], Ob[:, 64:65], [okey], [("rden", r)])
                dst = szy[:, i, h * 64:(h + 1) * 64]
                STT(dst, Ob[:, 0:64], rd[:], dst, ALU.mult, ALU.mult, [okey, ("rden", r), ("szy", i)], [("szy", i)])

            attention(QT[:, h, :], lambda kt, h=h: KT[:, h, kt * 128:(kt + 1) * 128], 128,
                      lambda kt, h=h: Vt[:, kt, h, 0:65], 65, plan, finish,
                      lambda qc: [("QT", 4 * qc + u) for u in range(4)], lambda kt: [("KT", kt)], lambda kt: [("V", kt)],
                      ptb, "m")
        if stop == "ma":
            return
        outproj_half(szy, yT)

    def rstd_a(pp, pkey, ncol, tf, tfkey, s8i, s8key):
        nh = ncol // 64
        act(tf[:, 0:ncol], pp[:, 0:ncol], AF.Square, [pkey], [tfkey])
        RED(s8i[:, 0:nh], tf[:, 0:ncol].rearrange("p (h d) -> p h d", d=64), ALU.add, [tfkey], [s8key])
        TS(s8i[:, 0:nh], s8i[:, 0:nh], 64.0 * EPS, None, ALU.add, None, [s8key], [s8key])
        act(s8i[:, 0:nh], s8i[:, 0:nh], AF.Sqrt, [s8key], [s8key])

    def rstd_b(ncol, s8i, s8key):
        nh = ncol // 64
        RCP(s8i[:, 0:nh], s8i[:, 0:nh], [s8key], [s8key])

    def head_rstd(pp, pkey, ncol, tf, tfkey, s8i, s8key):
        rstd_a(pp, pkey, ncol, tf, tfkey, s8i, s8key)
        rstd_b(ncol, s8i, s8key)

    def pipe4(n, s0, s1, s2, s3):
        for step in range(n + 2):
            if step < n:
                s0(step)
                s1(step)
            if 1 <= step <= n:
                s2(step - 1)
            if step >= 2:
                s3(step - 2)

    def load_gain_col(dst, gd, mult):
        MSET(dst[:], 1.0, ["gq"])
        dma("sp", dst[0:64, :], gd.rearrange("o d -> d o"), "c", writes=["gq"], slow=True)
        if mult != 1.0:
            TS(dst[0:64, :], dst[0:64, :], mult, None, ALU.mult, None, ["gq"], ["gq"])

    def outproj_half(szy, yT, store=False):
        def oa(i):
            pt = pb[i % 2]
            for c in range(2):
                tr(pt[:, c * 128:(c + 1) * 128], szy[:, i, c * 128:(c + 1) * 128], [("szy", i)], [("pb", i % 2)])
            y = yT[i % 2]
            act(y[:], pt[:, 0:256].rearrange("p (c t) -> p c t", c=2), AF.Copy, [("pb", i % 2)], [("yT", i % 2)])

        def ob(i):
            y = yT[i % 2]
            outproj_tile(i, [y[:, 0, :], y[:, 1, :]], [0, 1], [("yT", i % 2)], (4, 5))
            if store:
                dma("sp", yv[:, i, :], x_sb[:, i, :], "out", reads=[("x", i)])

        lagged(NT, oa, ob)

    def plan_causal(qc):
        items = []
        for kt in range(4 * qc + 4):
            if kt < 4 * qc:
                items.append((kt, 0, 3, {}))
            else:
                m = kt - 4 * qc
                items.append((kt, m, 3, {m: tri[:]}))
        return items

    def plan_band(wt):
        def plan(qc):
            items = []
            for kt in range(max(0, 4 * qc - wt), 4 * qc + 4):
                jlo = max(0, kt - 4 * qc)
                jhi = min(3, kt + wt - 4 * qc)
                if jlo > jhi:
                    continue
                masks = {}
                if 0 <= kt - 4 * qc <= 3:
                    masks[kt - 4 * qc] = tri[:]
                if 0 <= kt + wt - 4 * qc <= 3:
                    masks[kt + wt - 4 * qc] = upm[:]
                items.append((kt, jlo, jhi, masks))
            return items
        return plan

    W1_OFF = 64 * 1024

    def prefetch_w1(war_keys=()):
        cvp = Carver(base=W1_OFF)
        wA = cvp.get([128, 32, 256], BF16)
        for kv, wd in enumerate((ckw1_d, cvw1_d)):
            wv = wd.rearrange("(l d) j -> d l j", d=64)
            for lq in range(4):
                dma("pool", wA[kv * 64:(kv + 1) * 64, lq * 8:(lq + 1) * 8, :], wv[:, lq * 8:(lq + 1) * 8, :], "w1", writes=["w1A"] + list(war_keys))
        return wA

    def layer1():
        W = owin_d
        wA = Carver(base=W1_OFF).get([128, 32, 256], BF16)
        if 0 not in layers:
            prefetch_w1()
        cvc = Carver(base=16 * 1024)
        wB = cvc.get([128, 32, 256], BF16)
        dma("sp", wB[64:128, :, :], wA[0:64, :, :], "c", writes=["w1B"])
        dma("sp", wB[0:64, :, :], wA[64:128, :, :], "c", writes=["w1B"])
        w1 = [[wA, wB], [wB, wA]]
        B = compress_loads(W, cvc)
        norm_phase(1)
        compress_stage(W, B, w1)
        P.barrier()
        if stop == "cmp":
            return
        for g in range(2):
            nsa_half(g, W)
            P.barrier()
            if stop == "nsa0":
                return
        if stop == "nsa":
            return
        for g in range(2):
            swa_half(g, W)
            P.barrier()

    def compress_loads(W, cv):
        s = load_w(W, [(512, 128), (640, 128)])
        B = {}
        B["s"] = s
        B["KVD"] = [cv.get([128, 16, 128], BF16) for _ in range(2)]
        B["w2"] = [cv.get([128, 2, 64], BF16) for _ in range(2)]
        B["posn"] = cv.get([32, 2, 64], F32)
        B["posT"] = cv.get([64, 2, 32], BF16)
        B["stgb"] = cv.get([4, 128], F32)
        B["b1sb"] = cv.get([128, 4], F32)
        B["biasj"] = cv.get([128, 4], F32)
        B["b2bc"] = cv.get([128, 2, 64], F32)
        B["gcm"] = cv.get([128, 64], F32)
        B["kalc"] = cv.get([128, 36], BF16)
        B["hid"] = [cv.get([128, 2, 128], BF16) for _ in range(4)]
        B["kcf"] = cv.get([128, 64], F32)
        B["junk2"] = cv.get([128, 64], F32)
        B["ssc"] = cv.get([128, 1], F32)
        B["kaug"] = cv.get([128, 128], BF16)
        for kv, wd in enumerate((ckw2_d, cvw2_d)):
            dma("pool", B["w2"][kv][:], wd.rearrange("(c p) d -> p c d", p=128), "w2", writes=["w2"])
        dma("sp", B["posn"][:, 0, :], cposk_d, "c", writes=["posn"])
        dma("sp", B["posn"][:, 1, :], cposv_d, "c", writes=["posn"])
        dma("sp", B["stgb"][0:2, :], ckb1_d.rearrange("(a c) -> a c", c=128), "c", writes=["stgb"])
        dma("sp", B["stgb"][2:4, :], cvb1_d.rearrange("(a c) -> a c", c=128), "c", writes=["stgb"])
        dma("sp", B["b2bc"][:, 0, :], ckb2_d.partition_broadcast(128), "c", writes=["b2bc"])
        dma("sp", B["b2bc"][:, 1, :], cvb2_d.partition_broadcast(128), "c", writes=["b2bc"])
        dma("sp", B["gcm"][:], ckc_d.partition_broadcast(128), "c", writes=["gcm"])
        dma("pool", B["kalc"][:], kalc_d, "c", writes=["kalc"])
        for g in range(2):
            dma("pool", Vca[:, g, 65:97], ovl_d, "c", writes=[("Vca", g)])
        return B

    def compress_stage(W, B, w1):
        s = B["s"]
        KVD, w2, posn, posT, stgb, b1sb, biasj = B["KVD"], B["w2"], B["posn"], B["posT"], B["stgb"], B["b1sb"], B["biasj"]
        b2bc, gcm, kalc, hid, kcf, junk2, ssc, kaug = B["b2bc"], B["gcm"], B["kalc"], B["hid"], B["kcf"], B["junk2"], B["ssc"], B["kaug"]
        for g in range(2):
            MSET(Vca[:, g, 64:65], 1.0, [("Vca", g)])
        MSET(kaug[:], 0.0, ["kaug"])
        for kv in range(2):
            tr(pf[0][0:64, kv * 32:(kv + 1) * 32], posn[:, kv, :], ["posn"], [("pf", 0)], idn=identf[0:32, 0:32])
        tr(pf[0][:, 64:68], stgb[:], ["stgb"], [("pf", 0)], idn=identf[0:4, 0:4])
        CPY(posT[:], pf[0][0:64, 0:64].rearrange("p (k l) -> p k l", l=32), [("pf", 0)], ["posT"])
        CPY(b1sb[:], pf[0][:, 64:68], [("pf", 0)], ["b1sb"])
        for kv in range(2):
            for jc in range(2):
                col = kv * 2 + jc
                for l in range(32):
                    mm(pf[1][:, col:col + 1], w1[kv][0][0:64, l, jc * 128:(jc + 1) * 128], posT[0:64, kv, l:l + 1], l == 0, l == 31,
                       ["w1B", "posT"], [("pf", 1)])
        TT(biasj[:], pf[1][:, 0:4], b1sb[:], ALU.add, [("pf", 1), "b1sb"], ["biasj"])
        for tq in range(4):
            for which in range(2):
                pp = pf[2 + which]
                for k in range(8):
                    mm(pp[:], wbuf[s][:, k, which * 128:(which + 1) * 128], hT[:, k, tq * 512:(tq + 1) * 512], k == 0, k == 7,
                       [("w", s)] + [("hT", 4 * tq + u) for u in range(4)], [("pf", 2 + which)])
                act(KVD[which][:, :, tq * 32:(tq + 1) * 32].rearrange("p r m -> p m r"), pp[:].rearrange("p (m r) -> p m r", r=16),
                    AF.Copy, [("pf", 2 + which)], [("KVT", which)])
        n = 0
        for kv in range(2):
            for g in range(2):
                hb_ = hid[kv * 2 + g]
                for jc in range(2):
                    pp = pf[n % 2]
                    for l in range(32):
                        mm(pp[:, 0:127], w1[kv][g][g * 64:(g + 1) * 64, l, jc * 128:(jc + 1) * 128],
                           KVD[kv][g * 64:(g + 1) * 64, l % 16, (l // 16):(l // 16) + 127], l == 0, l == 31,
                           ["w1B", ("KVT", kv)], [("pf", n % 2)])
                    act(hb_[:, jc, 0:127], pp[:, 0:127], AF.Silu, [("pf", n % 2), "biasj"], [("hid", kv * 2 + g)],
                        bias=biasj[:, kv * 2 + jc:kv * 2 + jc + 1], scale=1.0)
                    n += 1
                po = pf[2 + g]
                for jc in range(2):
                    mm(po[0:127, 0:64], hb_[:, jc, 0:127], w2[kv][:, jc, :], jc == 0, jc == 1, [("hid", kv * 2 + g), "w2"], [("pf", 2 + g)])
                if kv == 0:
                    TT(kcf[0:127, :], po[0:127, 0:64], b2bc[0:127, 0, :], ALU.add, [("pf", 2 + g), "b2bc"], ["kcf"])
                    act(junk2[0:127, :], kcf[0:127, :], AF.Square, ["kcf"], ["junk2", "ssc"], accum_out=ssc[0:127, :])
                    TS(ssc[0:127, :], ssc[0:127, :], 1.0 / 64, EPS, ALU.mult, ALU.add, ["ssc"], ["ssc"])
                    act(ssc[0:127, :], ssc[0:127, :], AF.Sqrt, ["ssc"], ["ssc"])
                    RCP(ssc[0:127, :], ssc[0:127, :], ["ssc"], ["ssc"])
                    STT(kaug[0:127, 0:64], kcf[0:127, :], ssc[0:127, :], gcm[0:127, :], ALU.mult, ALU.mult, ["kcf", "ssc", "gcm"], ["kaug"])
                    CPY(kaug[:, 64:100], kalc[:], ["kalc"], ["kaug"])
                    tr(pb[g][:, 0:128], kaug[:], ["kaug"], [("pb", g)])
                    act(KTc[:, g, :], pb[g][:, 0:128], AF.Copy, [("pb", g)], [("KTc", g)])
                else:
                    TT(Vca[0:127, g, 0:64], po[0:127, 0:64], b2bc[0:127, 1, :], ALU.add, [("pf", 2 + g), "b2bc"], [("Vca", g)])

    def nsa_half(g, W):
        cv = Carver()
        QT = cv.get([128, 4, S], BF16)
        KTs = cv.get([128, S], BF16)
        KTw = cv.get([128, S], BF16)
        Vs = cv.get([128, NT, 66], BF16)
        Vw = cv.get([128, NT, 66], BF16)
        szy = cv.get([128, NT, 256], BF16)
        gates = cv.get([128, NT, 12], F32)
        oacc = cv.get([128, NT, 256], F32)
        imp = cv.get([128, 8, 32], F32)
        impc = cv.get([128, 8, 32], F32)
        cmn = cv.get([128, S], BF16)
        qa = [cv.get([128, 4, 128], BF16) for _ in range(3)]
        ksa = [cv.get([128, 128], BF16) for _ in range(3)]
        kwa = [cv.get([128, 128], BF16) for _ in range(3)]
        ptb = [cv.get([128, 512], BF16) for _ in range(3)]
        tmpf = [cv.get([128, 512], F32) for _ in range(2)]
        cmpb = cv.get([128, 32, 32], F32)
        rank = cv.get([128, 32], F32)
        nmt = cv.get([128, 128], BF16)
        s8 = [cv.get([128, 8], F32) for _ in range(3)]
        gq = cv.get([128, 1], F32)
        gks = cv.get([128, 1], F32)
        gkw = cv.get([128, 1], F32)
        rden = [cv.get([128, 1], F32) for _ in range(8)]
        rden4 = [cv.get([128, 4], F32) for _ in range(8)]
        tmp32 = cv.get([128, 4, 32], F32)
        yT = [cv.get([128, 2, 128], BF16) for _ in range(2)]
        s = load_w(W, [(g * 256, 256), (768 + 64 * g, 64), (1024 + 64 * g, 64), (896 + 64 * g, 64), (1152 + 64 * g, 64)])
        s2 = load_w(W, [(1304 + g * 256, 256), (1280 + 4 * g, 4), (1288 + 4 * g, 4), (1296 + 4 * g, 4)])
        load_wo(1, 2 * g, 2)
        load_gain_col(gq, cq_d, 1.0)
        load_gain_col(gks, cks_d, 8.0)
        load_gain_col(gkw, ckw_d, 8.0)
        dma("sp", impc[:], impc_d[:, 8:16, :], "c", writes=["impc"])
        dma("pool", cmn[:], cmn_d, "c", writes=["addmask"])
        for b in range(3):
            MSET(qa[b][:, :, 64:128], 0.0, [("qa", b)])
            MSET(ksa[b][:, 64:128], 0.0, [("ksa", b)])
            MSET(kwa[b][:, 64:128], 0.0, [("kwa", b)])
        MSET(Vs[:, :, 64:65], 1.0, ["Vs"])
        MSET(Vw[:, :, 64:65], 1.0, ["Vw"])
        MSET(nmt[:], 0.0, ["nmt"])
        def pa(i):
            b2 = i % 2
            pp = pf[b2]
            qai, ksi, kwi = qa[b2], ksa[b2], kwa[b2]
            CPY(qai[:, :, 96:100], qal[:, i, g * 16:g * 16 + 16].rearrange("p (h c) -> p h c", c=4), ["const"], [("qa", b2)])
            CPY(ksi[:, 64:100], kals[:, i, :], ["const"], [("ksa", b2)])
            CPY(kwi[:, 64:100], kalp[:, i, :], ["const"], [("kwa", b2)])
            for k in range(8):
                mm(pp[:], hT[:, k, i * 128:(i + 1) * 128], wbuf[s][:, k, :], k == 0, k == 7, [("hT", i), ("w", s)], [("pf", b2)])
            tf = tmpf[b2]
            head_rstd(pp, ("pf", b2), 384, tf, ("tf", b2), s8[b2], ("s8", b2))
            TT(qai[:, :, 0:64], pp[:, 0:256].rearrange("p (h d) -> p h d", d=64), s8[b2][:, 0:4].unsqueeze(2).to_broadcast([128, 4, 64]),
               ALU.mult, [("pf", b2), ("s8", b2)], [("qa", b2)])
            TS(ksi[:, 0:64], pp[:, 256:320], s8[b2][:, 4:5], None, ALU.mult, None, [("pf", b2), ("s8", b2)], [("ksa", b2)])
            TS(kwi[:, 0:64], pp[:, 320:384], s8[b2][:, 5:6], None, ALU.mult, None, [("pf", b2), ("s8", b2)], [("kwa", b2)])
            act(Vs[:, i, 0:64], pp[:, 384:448], AF.Copy, [("pf", b2), "Vs"], [("Vs", i)])
            act(Vw[:, i, 0:64], pp[:, 448:512], AF.Copy, [("pf", b2), "Vw"], [("Vw", i)])

        def pbk(i):
            b2 = i % 2
            qai, ksi, kwi = qa[b2], ksa[b2], kwa[b2]
            pt = pb[b2]
            for h in range(4):
                tr(pt[:, h * 128:(h + 1) * 128], qai[:, h, :], [("qa", b2)], [("pb", b2)])
            tr(pt[:, 512:640], ksi[:], [("ksa", b2)], [("pb", b2)])
            tr(pt[:, 640:768], kwi[:], [("kwa", b2)], [("pb", b2)])
            act(QT[:, :, i * 128:(i + 1) * 128], pt[:, 0:512].rearrange("p (h t) -> p h t", h=4), AF.Copy, [("pb", b2), "gq"], [("QT", i)],
                scale=gq[:, 0:1])
            act(KTs[:, i * 128:(i + 1) * 128], pt[:, 512:640], AF.Copy, [("pb", b2), "gq"], [("KTs", i)], scale=gks[:, 0:1])
            act(KTw[:, i * 128:(i + 1) * 128], pt[:, 640:768], AF.Copy, [("pb", b2), "gq"], [("KTw", i)], scale=gkw[:, 0:1])

        lagged(NT, pa, pbk)
        for i in range(NT):
            b2 = 2 + i % 2
            pp = pf[b2]
            for k in range(8):
                mm(pp[:, 0:256], hT[:, k, i * 128:(i + 1) * 128], wbuf[s2][:, k, 0:256], k == 0, k == 7, [("hT", i), ("w", s2)], [("pf", b2)])
            act(szy[:, i, :], pp[:, 0:256], AF.Silu, [("pf", b2)], [("szy", i)])
        for i in range(NT):
            b2 = 2 + i % 2
            pp = pf[b2]
            for k in range(8):
                mm(pp[:, 0:12], hT[:, k, i * 128:(i + 1) * 128], wbuf[s2][:, k, 256:268], k == 0, k == 7, [("hT", i), ("w", s2)], [("pf", b2)])
            act(gates[:, i, :], pp[:, 0:12], AF.Sigmoid, [("pf", b2)], [("gates", i)])
        if stop == "nsaproj":
            return
        qk = lambda qc: [("QT", 4 * qc + u) for u in range(4)]
        for r in range(4):
            def fin_cmp_chunk(qc, Obank, okey, r=r):
                ri = (qc + 4 * r) % 8
                rd = rden4[ri]
                Ov = Obank[:].rearrange("p (j c) -> p j c", c=128)
                tl = slice(4 * qc, 4 * qc + 4)
                gk_ = [("gates", i) for i in range(4 * qc, 4 * qc + 4)]
                ok_ = [("oacc", i) for i in range(4 * qc, 4 * qc + 4)]
                TS(rd[:], Ov[:, :, 64:65].rearrange("p j c -> p (j c)"), 1e-30, None, ALU.max, None, [okey], [("rden4", ri)])
                RCP(rd[:], rd[:], [("rden4", ri)], [("rden4", ri)])
                if qc >= 2:
                    ik_ = [("imp", i) for i in range(4 * qc, 4 * qc + 4)]
                    dst = imp[:, 4 * qc - 8:4 * qc - 4, :]
                    if r == 0:
                        TT(dst, Ov[:, :, 65:97], rd[:].unsqueeze(2).to_broadcast([128, 4, 32]), ALU.mult, [okey, ("rden4", ri)], ik_)
                    else:
                        TT(tmp32[:], Ov[:, :, 65:97], rd[:].unsqueeze(2).to_broadcast([128, 4, 32]), ALU.mult, [okey, ("rden4", ri)], ["tmp32"])
                        TT(dst, dst, tmp32[:], ALU.add, ["tmp32"] + ik_, ik_)
                TT(rd[:], rd[:], gates[:, tl, r:r + 1].rearrange("p j c -> p (j c)"), ALU.mult, [("rden4", ri)] + gk_, [("rden4", ri)])
                TT(oacc[:, tl, r * 64:(r + 1) * 64], Ov[:, :, 0:64], rd[:].unsqueeze(2).to_broadcast([128, 4, 64]), ALU.mult,
                   [okey, ("rden4", ri)], ok_)

            attention(QT[:, r, :], lambda kt: KTc[:, g, 0:127], 127, lambda kt: Vca[0:127, g, 0:97], 97,
                      lambda qc: [(0, 0, 3, {})], None, qk, lambda kt: [("KTc", g)], lambda kt: [("Vca", g)], ptb, "n", addmask=cmn,
                      finish_chunk=fin_cmp_chunk)
        if stop == "nsacmp":
            return
        for i in range(8, NT):
            im = imp[:, i - 8, :]
            TT(im, im, impc[:, i - 8, :], ALU.add, [("imp", i), "impc"], [("imp", i)])
            TT(cmpb[:], im.unsqueeze(1).to_broadcast([128, 32, 32]), im.unsqueeze(2).to_broadcast([128, 32, 32]), ALU.is_gt,
               [("imp", i)], ["cmpb"])
            RED(rank[:], cmpb[:], ALU.add, ["cmpb"], ["rank"])
            TS(nmt[:, 0:32], rank[:], 15.5, NEGM, ALU.is_ge, ALU.mult, ["rank"], ["nmt"])
            pt = pb[i % 2]
            tr(pt[:, 0:128], nmt[:], ["nmt"], [("pb", i % 2)])
            for r in range(4):
                act(QT[64:96, r, i * 128:(i + 1) * 128], pt[0:32, 0:128], AF.Copy, [("pb", i % 2)], [("QT", i)])
        for (KTx, Vx, kname, vname, plan, gcol) in ((KTs, Vs, "KTs", "Vs", plan_causal, 4), (KTw, Vw, "KTw", "Vw", plan_band(4), 8)):
            for r in range(4):
                def fin_add(i, Ob, okey, r=r, gcol=gcol):
                    ri = (i + 4 * r) % 8
                    rd = rden[ri]
                    RCP(rd[:], Ob[:, 64:65], [okey], [("rden", ri)])
                    TT(rd[:], rd[:], gates[:, i, gcol + r:gcol + r + 1], ALU.mult, [("rden", ri), ("gates", i)], [("rden", ri)])
                    dst = oacc[:, i, r * 64:(r + 1) * 64]
                    STT(dst, Ob[:, 0:64], rd[:], dst, ALU.mult, ALU.add, [okey, ("rden", ri), ("oacc", i)], [("oacc", i)])

                attention(QT[:, r, :], lambda kt, KTx=KTx: KTx[:, kt * 128:(kt + 1) * 128], 128,
                          lambda kt, Vx=Vx: Vx[:, kt, 0:65], 65, plan, fin_add, qk,
                          lambda kt, kname=kname: [(kname, kt)], lambda kt, vname=vname: [(vname, kt)], ptb, "n")
        for i in range(NT):
            TT(szy[:, i, :], oacc[:, i, :], szy[:, i, :], ALU.mult, [("oacc", i), ("szy", i)], [("szy", i)])
        outproj_half(szy, yT)

    def swa_half(g, W):
        cv = Carver()
        QT = cv.get([128, 4, S], BF16)
        KT = cv.get([128, S], BF16)
        Vt = cv.get([128, NT, 66], BF16)
        szy = cv.get([128, NT, 256], BF16)
        qa = [cv.get([128, 4, 128], BF16) for _ in range(3)]
        ka = [cv.get([128, 128], BF16) for _ in range(3)]
        ptb = [cv.get([128, 512], BF16) for _ in range(3)]
        tmpf = [cv.get([128, 512], F32) for _ in range(2)]
        s8 = [cv.get([128, 8], F32) for _ in range(3)]
        gq = cv.get([128, 1], F32)
        gk = cv.get([128, 1], F32)
        esink = cv.get([128, 8], F32)
        rden = [cv.get([128, 1], F32) for _ in range(8)]
        yT = [cv.get([128, 2, 128], BF16) for _ in range(2)]
        load_gain_col(gq, dq_d, 1.0)
        load_gain_col(gk, dk_d, 8.0)
        dma("sp", esink[:], dsink_d.partition_broadcast(128), "c", writes=["esink"])
        act(esink[:], esink[:], AF.Exp, ["esink"], ["esink"])
        for b in range(3):
            MSET(qa[b][:, :, 64:128], 0.0, [("qa", b)])
            MSET(ka[b][:, 64:128], 0.0, [("ka", b)])
        MSET(Vt[:, :, 64:65], 1.0, ["Vt"])
        s = load_w(W, [(1816 + g * 256, 256), (2328 + 64 * g, 64), (2456 + 64 * g, 64)])
        s2 = load_w(W, [(2584 + g * 256, 256)])
        load_wo(1, 4 + 2 * g, 2)
        def pa(i):
            b2 = i % 2
            pp = pf[b2]
            qai, kai = qa[b2], ka[b2]
            CPY(qai[:, :, 96:100], qal[:, i, g * 16:g * 16 + 16].rearrange("p (h c) -> p h c", c=4), ["const"], [("qa", b2)])
            CPY(kai[:, 64:100], kalp[:, i, :], ["const"], [("ka", b2)])
            for k in range(8):
                mm(pp[:, 0:384], hT[:, k, i * 128:(i + 1) * 128], wbuf[s][:, k, 0:384], k == 0, k == 7, [("hT", i), ("w", s)], [("pf", b2)])
            tf = tmpf[b2]
            head_rstd(pp, ("pf", b2), 320, tf, ("tf", b2), s8[b2], ("s8", b2))
            TT(qai[:, :, 0:64], pp[:, 0:256].rearrange("p (h d) -> p h d", d=64), s8[b2][:, 0:4].unsqueeze(2).to_broadcast([128, 4, 64]),
               ALU.mult, [("pf", b2), ("s8", b2)], [("qa", b2)])
            TS(kai[:, 0:64], pp[:, 256:320], s8[b2][:, 4:5], None, ALU.mult, None, [("pf", b2), ("s8", b2)], [("ka", b2)])
            act(Vt[:, i, 0:64], pp[:, 320:384], AF.Copy, [("pf", b2), "Vt"], [("V", i)])

        def pbk(i):
            b2 = i % 2
            qai, kai = qa[b2], ka[b2]
            pt = pb[b2]
            for h in range(4):
                tr(pt[:, h * 128:(h + 1) * 128], qai[:, h, :], [("qa", b2)], [("pb", b2)])
            tr(pt[:, 512:640], kai[:], [("ka", b2)], [("pb", b2)])
            act(QT[:, :, i * 128:(i + 1) * 128], pt[:, 0:512].rearrange("p (h t) -> p h t", h=4), AF.Copy, [("pb", b2), "gq"], [("QT", i)],
                scale=gq[:, 0:1])
            act(KT[:, i * 128:(i + 1) * 128], pt[:, 512:640], AF.Copy, [("pb", b2), "gq"], [("KT", i)], scale=gk[:, 0:1])

        lagged(NT, pa, pbk)
        for i in range(NT):
            b2 = 2 + i % 2
            pp = pf[b2]
            for k in range(8):
                mm(pp[:, 0:256], hT[:, k, i * 128:(i + 1) * 128], wbuf[s2][:, k, 0:256], k == 0, k == 7, [("hT", i), ("w", s2)], [("pf", b2)])
            act(szy[:, i, :], pp[:, 0:256], AF.Silu, [("pf", b2)], [("szy", i)])
        for r in range(4):
            def fin(i, Ob, okey, r=r):
                ri = (i + 4 * r) % 8
                rd = rden[ri]
                TS(rd[:], Ob[:, 64:65], esink[:, 4 * g + r:4 * g + r + 1], None, ALU.add, None, [okey, "esink"], [("rden", ri)])
                RCP(rd[:], rd[:], [("rden", ri)], [("rden", ri)])
                dst = szy[:, i, r * 64:(r + 1) * 64]
                STT(dst, Ob[:, 0:64], rd[:], dst, ALU.mult, ALU.mult, [okey, ("rden", ri), ("szy", i)], [("szy", i)])

            attention(QT[:, r, :], lambda kt: KT[:, kt * 128:(kt + 1) * 128], 128, lambda kt: Vt[:, kt, 0:65], 65,
                      plan_band(1), fin, lambda qc: [("QT", 4 * qc + u) for u in range(4)], lambda kt: [("KT", kt)],
                      lambda kt: [("V", kt)], ptb, "d")
        outproj_half(szy, yT, store=(g == 1 and stop is None))

    if 0 in layers:
        layer0()
    if 1 in layers:
        layer1()

    if not (1 in layers and stop is None):
        for i in range(NT):
            dma("sp", yv[:, i, :], x_sb[:, i, :], "out", reads=[("x", i)])
    P.finish("sp", list(P.chan_count.keys()))
    P.emit()
    es.close()
    return nc


_CACHE = {}


def kernel(**inputs):
    consts = _consts()
    shared = {}
    shared["norm_g"] = np.ascontiguousarray(inputs["norm_g"], dtype=np.float32)
    shared["w_out"] = np.ascontiguousarray(inputs["w_out"], dtype=np.float32)
    shared["e_w_in"] = np.ascontiguousarray(inputs["e_w_in"][0], dtype=np.float32)
    shared["a_conv_w"] = np.ascontiguousarray(inputs["a_conv_w"][0], dtype=np.float32)
    shared["a_conv_b"] = np.ascontiguousarray(inputs["a_conv_b"][0], dtype=np.float32)
    shared["a_ln_g"] = np.ascontiguousarray(inputs["a_ln_g"][0], dtype=np.float32)
    shared["a_ln_b"] = np.ascontiguousarray(inputs["a_ln_b"][0], dtype=np.float32)
    shared["b_qnorm_g"] = np.ascontiguousarray(inputs["b_qnorm_g"], dtype=np.float32)
    shared["b_knorm_g"] = np.ascontiguousarray(inputs["b_knorm_g"], dtype=np.float32)
    for k in ("ident", "tri", "up", "qal", "kal_moba", "kal_slc", "kal_plain", "kal_cmp", "cmaskneg", "ovl", "impc"):
        shared[k] = consts[k]
    shared["o_w_in"] = np.ascontiguousarray(inputs["o_w_in"][0], dtype=np.float32)
    for k in ("c_qnorm_g", "c_knorm_cmp_g", "c_knorm_slc_g", "c_knorm_win_g", "c_k_b2", "c_v_b2", "d_qnorm_g", "d_knorm_g", "d_sinks"):
        shared[k] = np.ascontiguousarray(inputs[k], dtype=np.float32).reshape(1, -1)
    for k in ("c_pos_k", "c_pos_v", "c_k_w1", "c_k_b1", "c_k_w2", "c_v_w1", "c_v_b1", "c_v_w2"):
        shared[k] = np.ascontiguousarray(inputs[k][0], dtype=np.float32)
    x = np.ascontiguousarray(inputs["x"], dtype=np.float32)
    nb = x.shape[0]
    layers = inputs.get("_layers", (0, 1))
    nc = build_program(layers, inputs.get("_stop"))
    in_maps = [dict(shared, x=x[b]) for b in range(nb)]
    res = run_bass_kernel_spmd(nc, in_maps, core_ids=list(range(nb)))
    return np.stack([np.asarray(r["y"], dtype=np.float32) for r in res.results], axis=0)
```

```python
import contextlib
import os
import numpy as np
import ml_dtypes
import concourse.bass as bass
import concourse.mybir as mybir
from concourse.bass_utils import run_bass_kernel_spmd

F32 = mybir.dt.float32
BF16 = mybir.dt.bfloat16
ALU = mybir.AluOpType
AF = mybir.ActivationFunctionType
AX = mybir.AxisListType

S = 2048
D = 1024
NT = 16
NEGM = -30000.0
EPS = 1e-6


class _Op:
    __slots__ = ("eng", "fn", "deps", "chan", "signal", "val", "dmaval", "chanseq")

    def __init__(self, eng, fn, chan):
        self.eng = eng
        self.fn = fn
        self.deps = {}
        self.chan = chan
        self.signal = False
        self.val = 0
        self.dmaval = None


class Prog:
    ENGS = ("pe", "act", "dve", "pool", "sp")

    def __init__(self, nc):
        self.nc = nc
        self.ops = {e: [] for e in self.ENGS}
        self.res = {}
        self.chan_count = {}
        self.chan_last = {}
        self.all_ops = []
        self.bar = {}
        self.final_waits = {}
        self.pool_ctr = 0
        self.sp_ctr = 0
        self.const_keys = []
        self.pres = {}

    PERS = ("w", "wo")

    def _st(self, k):
        if isinstance(k, tuple) and k[0] in self.PERS or k in self.PERS:
            return self.pres
        return self.res

    def op(self, eng, fn, reads=(), writes=(), chan=None, nobar=False):
        for w in writes:
            if isinstance(w, tuple) and w[0] == "ck" and w not in self.const_keys:
                self.const_keys.append(w)
        if "const" in reads:
            reads = [k for r in reads for k in (self.const_keys if r == "const" else (r,))]
        if chan is not None and eng == "pool":
            chan = "pq%d" % (self.pool_ctr % 12)
            self.pool_ctr += 1
        elif chan == "c":
            chan = "pc%d" % (self.sp_ctr % 16)
            self.sp_ctr += 1
        o = _Op(eng, fn, chan)
        deps = o.deps
        if not nobar:
            deps.update(self.bar)
        if chan is not None and (chan.startswith("pq") or chan.startswith("pc")):
            prev = self.chan_last.get(chan)
            if prev is not None:
                deps[id(prev)] = prev
        for k in reads:
            st = self._st(k).get(k)
            if st is not None and st[0] is not None:
                deps[id(st[0])] = st[0]
        for k in writes:
            st = self._st(k).get(k)
            if st is not None:
                if st[0] is not None:
                    deps[id(st[0])] = st[0]
                for r in st[1]:
                    deps[id(r)] = r
        for k in reads:
            res = self._st(k)
            st = res.get(k)
            if st is None:
                st = [None, []]
                res[k] = st
            st[1].append(o)
        for k in writes:
            self._st(k)[k] = [o, []]
        o.dmaval = dict(self.chan_count)
        if chan is not None:
            self.chan_count[chan] = self.chan_count.get(chan, 0) + 1
            o.chanseq = self.chan_count[chan]
            self.chan_last[chan] = o
            o.signal = True
        self.ops[eng].append(o)
        self.all_ops.append(o)
        return o

    def barrier(self):
        bar = {}
        for e in self.ENGS:
            if self.ops[e]:
                o = self.ops[e][-1]
                bar[id(o)] = o
        for ch, o in self.chan_last.items():
            bar[id(o)] = o
        self.bar = bar
        self.res = {}

    def finish(self, eng, chans):
        self.final_waits = {eng: {ch: self.chan_count[ch] for ch in chans}}

    def emit(self):
        nc = self.nc
        for o in self.all_ops:
            for d in o.deps.values():
                if d.chan is None:
                    if d.eng == "pe" and o.eng == "pe":
                        continue
                    d.signal = True
        for e in self.ENGS:
            c = 0
            for o in self.ops[e]:
                if o.chan is None and o.signal:
                    c += 1
                    o.val = c
        import os
        if os.environ.get("KDBG"):
            print("sem counts", {e: max([o.val for o in self.ops[e]] + [0]) for e in self.ENGS}, {e: len(self.ops[e]) for e in self.ENGS},
                  {c: 16 * v for c, v in self.chan_count.items()})
        stack = contextlib.ExitStack()
        sems = {}
        for e in self.ENGS:
            sems[e] = stack.enter_context(nc.semaphore("s_" + e))
        for ch in self.chan_count:
            sems["c_" + ch] = stack.enter_context(nc.semaphore("c_" + ch))
        block = stack.enter_context(nc.Block())
        engobj = {"pe": "tensor", "act": "scalar", "dve": "vector", "pool": "gpsimd", "sp": "sync"}

        def make(e):
            def body(eng):
                waited = {}
                for o in self.ops[e]:
                    need = {}
                    for d in o.deps.values():
                        if d.chan is not None:
                            k = "c_" + d.chan
                            if d.chan.startswith("pq") or d.chan.startswith("pc"):
                                v = 16 * d.chanseq
                            else:
                                v = 16 * o.dmaval[d.chan]
                        else:
                            if d.eng == "pe" and e == "pe":
                                continue
                            k = d.eng
                            v = d.val
                        if v > need.get(k, 0):
                            need[k] = v
                    for k, v in need.items():
                        if waited.get(k, 0) >= v:
                            continue
                        eng.wait_ge(sems[k], v)
                        waited[k] = v
                    ins = o.fn(eng)
                    if o.chan is not None:
                        ins.then_inc(sems["c_" + o.chan], 16)
                    elif o.signal:
                        ins.then_inc(sems[e], 1)
                for ch, c in self.final_waits.get(e, {}).items():
                    eng.wait_ge(sems["c_" + ch], 16 * c)
            return body

        for e in self.ENGS:
            getattr(block, engobj[e])(make(e))
        stack.close()


def _consts():
    c = {}
    c["ident"] = np.eye(128, dtype=np.float32)
    k = np.arange(128)[:, None]
    q = np.arange(128)[None, :]
    c["tri"] = np.where(k <= q, 0.0, NEGM).astype(np.float32)
    c["up"] = np.where(k > q, 0.0, NEGM).astype(np.float32)
    t = np.arange(S)
    b = (t % 16).astype(np.float32)
    a = (t - t % 16).astype(np.float32)
    slopes = np.power(2.0, -8.0 * np.arange(1, 9) / 8).astype(np.float32)
    qal = np.zeros((S, 8, 4), np.float32)
    qal[:, :, 0] = -slopes[None, :] * a[:, None]
    qal[:, :, 1] = -slopes[None, :] * b[:, None]
    qal[:, :, 2] = slopes[None, :]
    qal[:, :, 3] = slopes[None, :]
    c["qal"] = qal.reshape(NT, 128, 32).transpose(1, 0, 2).copy()

    def kal(onehot_block):
        m = np.zeros((S, 36), np.float32)
        if onehot_block:
            m[t, t // onehot_block] = 1.0
        m[:, 32] = 1.0
        m[:, 33] = 1.0
        m[:, 34] = a
        m[:, 35] = b
        return m.reshape(NT, 128, 36).transpose(1, 0, 2).copy()

    c["kal_moba"] = kal(256)
    c["kal_slc"] = kal(64)
    c["kal_plain"] = kal(0)
    cc = np.arange(128)
    cend = 16 * cc + 31
    kc = np.zeros((128, 36), np.float32)
    kc[:, 32] = 1.0
    kc[:, 33] = 1.0
    kc[:, 34] = cend - cend % 16
    kc[:, 35] = cend % 16
    c["kal_cmp"] = kc
    c["cmaskneg"] = np.where(t[None, :] >= cend[:, None], 0.0, NEGM).astype(np.float32)
    start = np.arange(127)[:, None] * 16
    bs = np.arange(32)[None, :] * 64
    ov = np.zeros((128, 32), np.float32)
    ov[:127] = ((start < bs + 64) & (start + 32 > bs)).astype(np.float32)
    c["ovl"] = ov
    blk = np.arange(32)[None, :]
    cur = (t // 64)[:, None]
    forced = (blk == 0) | (blk == cur) | (blk == cur - 1)
    impc = 1e4 * forced.astype(np.float32) - 1e5 * (blk > cur).astype(np.float32)
    c["impc"] = impc.reshape(NT, 128, 32).transpose(1, 0, 2).copy()
    return c


def build_program(layers=(0, 1), stop=None):
    nc = bass.Bass("TRN2", target_bir_lowering=False)
    es = contextlib.ExitStack()
    dram = {}

    def din(name, shape, dt=F32):
        dram[name] = nc.dram_tensor(name, list(shape), dt, kind="ExternalInput").ap()
        return dram[name]

    x_d = din("x", [S, D])
    normg_d = din("norm_g", [2, D])
    wout_d = din("w_out", [2, D, D])
    ewin_d = din("e_w_in", [D, 3584])
    convw_d = din("a_conv_w", [31, 512])
    convb_d = din("a_conv_b", [512])
    lng_d = din("a_ln_g", [512])
    lnb_d = din("a_ln_b", [512])
    bq_d = din("b_qnorm_g", [1, 64])
    bk_d = din("b_knorm_g", [1, 64])
    ident_d = din("ident", [128, 128])
    tri_d = din("tri", [128, 128])
    up_d = din("up", [128, 128])
    qal_d = din("qal", [128, NT, 32])
    kalm_d = din("kal_moba", [128, NT, 36])
    kals_d = din("kal_slc", [128, NT, 36])
    kalp_d = din("kal_plain", [128, NT, 36])
    kalc_d = din("kal_cmp", [128, 36])
    cmn_d = din("cmaskneg", [128, S])
    ovl_d = din("ovl", [128, 32])
    impc_d = din("impc", [128, NT, 32])
    owin_d = din("o_w_in", [D, 3096])
    cq_d = din("c_qnorm_g", [1, 64])
    ckc_d = din("c_knorm_cmp_g", [1, 64])
    cks_d = din("c_knorm_slc_g", [1, 64])
    ckw_d = din("c_knorm_win_g", [1, 64])
    cposk_d = din("c_pos_k", [32, 64])
    cposv_d = din("c_pos_v", [32, 64])
    ckw1_d = din("c_k_w1", [2048, 256])
    ckb1_d = din("c_k_b1", [256])
    ckw2_d = din("c_k_w2", [256, 64])
    ckb2_d = din("c_k_b2", [1, 64])
    cvw1_d = din("c_v_w1", [2048, 256])
    cvb1_d = din("c_v_b1", [256])
    cvw2_d = din("c_v_w2", [256, 64])
    cvb2_d = din("c_v_b2", [1, 64])
    dq_d = din("d_qnorm_g", [1, 64])
    dk_d = din("d_knorm_g", [1, 64])
    dsink_d = din("d_sinks", [1, 8])
    y_d = nc.dram_tensor("y", [S, D], F32, kind="ExternalOutput").ap()

    def sb(name, shape, dt):
        return es.enter_context(nc.sbuf_tensor(name, list(shape), dt))

    def psum(name, shape, dt):
        return es.enter_context(nc.psum_tensor(name, list(shape), dt))

    P = Prog(nc)

    x_sb = sb("x_sb", [128, NT, D], F32)
    hT = sb("hT", [128, 8, S], BF16)
    wbuf = [sb("wbuf%d" % i, [128, 8, 512], BF16) for i in range(2)]
    wo = sb("wo", [128, 4, D], BF16)
    ident = sb("ident_sb", [128, 128], BF16)
    identf = sb("identf_sb", [128, 128], F32)
    onesf = sb("onesf", [128, 128], F32)
    tri = sb("tri_sb", [128, 128], BF16)
    upm = sb("up_sb", [128, 128], BF16)
    qal = sb("qal_sb", [128, NT, 32], BF16)
    kalm = sb("kalm_sb", [128, NT, 36], BF16)
    KTc = sb("KTc", [128, 2, 128], BF16)
    Vca = sb("Vca", [128, 2, 98], BF16)
    kals = sb("kals_sb", [128, NT, 36], BF16)
    kalp = sb("kalp_sb", [128, NT, 36], BF16)
    ss = sb("ss", [128, NT], F32)
    rstd = sb("rstd", [128, NT], F32)
    ARENA = 80 * 1024
    arena = sb("arena", [128, ARENA // 2], BF16)

    pf = [psum("pf%d" % i, [128, 512], F32) for i in range(6)]
    pb = [psum("pb%d" % i, [128, 1024], BF16) for i in range(2)]

    class Carver:
        def __init__(self, base=0):
            self.off = base

        def get(self, shape, dt):
            n = int(np.prod(shape[1:]))
            nb = n * (4 if dt == F32 else 2)
            nb = (nb + 63) // 64 * 64
            assert self.off + nb <= ARENA, ("arena overflow", self.off + nb, ARENA)
            a = arena[:, self.off // 2:(self.off + nb) // 2]
            self.off += nb
            if os.environ.get("KDBG"):
                print("carve", shape, dt, "->", self.off)
            if dt == F32:
                a = a.bitcast(F32)
            a = a[:, 0:n]
            if len(shape) == 3:
                a = a.rearrange("p (a b) -> p a b", b=shape[2])
            elif len(shape) == 4:
                a = a.rearrange("p (a b c) -> p a b c", b=shape[2], c=shape[3])
            if shape[0] < 128:
                a = a[0:shape[0]]
            return a

    def dma(eng, out, in_, chan, reads=(), writes=(), slow=False, nobar=False):
        if slow:
            P.op(eng, lambda e: e.dma_start(out=out, in_=in_, allow_slow_non_contiguous=True), reads=reads, writes=writes, chan=chan, nobar=nobar)
        else:
            P.op(eng, lambda e: e.dma_start(out=out, in_=in_), reads=reads, writes=writes, chan=chan, nobar=nobar)

    def mm(out, lhsT, rhs, start, stop, reads, writes):
        P.op("pe", lambda e: e.matmul(out, lhsT=lhsT, rhs=rhs, start=start, stop=stop), reads=reads, writes=writes)

    def tr(out, in_, reads, writes, idn=None):
        ck = ("ck", "ident") if idn is None else ("ck", "identf")
        idn = ident[:] if idn is None else idn
        P.op("pe", lambda e: e.transpose(out=out, in_=in_, identity=idn), reads=list(reads) + [ck], writes=writes)

    def act(out, in_, func, reads, writes, **kw):
        P.op("act", lambda e: e.activation(out=out, in_=in_, func=func, **kw), reads=reads, writes=writes)

    def V(fn, reads, writes, eng="dve"):
        P.op(eng, fn, reads=reads, writes=writes)

    def TT(out, in0, in1, op, reads, writes, eng="dve"):
        P.op(eng, lambda e: e.tensor_tensor(out=out, in0=in0, in1=in1, op=op), reads=reads, writes=writes)

    def TS(out, in0, s1, s2, op0, op1, reads, writes, eng="dve"):
        if op1 is None:
            P.op(eng, lambda e: e.tensor_scalar(out=out, in0=in0, scalar1=s1, scalar2=None, op0=op0), reads=reads, writes=writes)
        else:
            P.op(eng, lambda e: e.tensor_scalar(out=out, in0=in0, scalar1=s1, scalar2=s2, op0=op0, op1=op1), reads=reads, writes=writes)

    def STT(out, in0, scalar, in1, op0, op1, reads, writes, eng="dve"):
        P.op(eng, lambda e: e.scalar_tensor_tensor(out=out, in0=in0, scalar=scalar, in1=in1, op0=op0, op1=op1), reads=reads, writes=writes)

    def RED(out, in_, op, reads, writes):
        P.op("dve", lambda e: e.tensor_reduce(out=out, in_=in_, axis=AX.X, op=op), reads=reads, writes=writes)

    def RCP(out, in_, reads, writes):
        P.op("dve", lambda e: e.reciprocal(out=out, in_=in_), reads=reads, writes=writes)

    def CPY(out, in_, reads, writes, eng="dve"):
        P.op(eng, lambda e: e.tensor_copy(out=out, in_=in_), reads=reads, writes=writes)

    def MSET(ap, val, writes, eng="dve"):
        P.op(eng, lambda e: e.memset(ap, val), reads=[], writes=writes)

    import os
    KB = os.environ.get("KBIS", "abcdefg")
    if "a" in KB:
        dma("pool", ident[:], ident_d, "c", writes=[("ck", "ident")])
    if "b" in KB:
        dma("pool", tri[:], tri_d, "c", writes=[("ck", "tri")])
        dma("pool", upm[:], up_d, "c", writes=[("ck", "up")])
    if "c" in KB:
        dma("pool", qal[:], qal_d, "c", writes=[("ck", "qal")])
    if "d" in KB:
        dma("pool", kalm[:], kalm_d, "c", writes=[("ck", "kalm")])
        dma("pool", kals[:], kals_d, "c", writes=[("ck", "kals")])
        dma("pool", kalp[:], kalp_d, "c", writes=[("ck", "kalp")])
    if "e" in KB:
        dma("sp", identf[:], ident_d, "c", writes=[("ck", "identf")])
    if "f" in KB:
        MSET(onesf[:], 1.0, [("ck", "onesf")])

    xv = x_d.rearrange("(i p) d -> p i d", p=128)
    yv = y_d.rearrange("(i p) d -> p i d", p=128)
    for i in range(NT):
        dma("sp", x_sb[:, i, :], xv[:, i, :], "x%d" % i, writes=[("x", i)])

    wslot_ctr = [0]

    def load_w(wd, segs):
        s = wslot_ctr[0] % 2
        wslot_ctr[0] += 1
        wv = wd.rearrange("(k p) n -> p k n", p=128)
        o = 0
        for (c0, n) in segs:
            dma("pool", wbuf[s][:, :, o:o + n], wv[:, :, c0:c0 + n], "w%d" % s, writes=[("w", s)], nobar=True)
            o += n
        return s

    def norm_phase(layer, base=0, barrier=True):
        cvn = Carver(base=base)
        gbc = cvn.get([128, D], F32)
        junk = cvn.get([128, D], BF16)
        hb = [cvn.get([128, D], BF16) for _ in range(2)]
        dma("act", gbc[:], normg_d[layer:layer + 1, :].partition_broadcast(128), "c", writes=["gbc"])
        MSET(ss[:], 0.0, [("ss", gI) for gI in range(4)])

        def stats(gI):
            for i in range(4 * gI, 4 * gI + 4):
                act(junk[:], x_sb[:, i, :], AF.Square, [("x", i), ("ss", gI)], ["junk", ("ss", gI)], accum_out=ss[:, i:i + 1])
            sl = slice(4 * gI, 4 * gI + 4)
            TS(rstd[:, sl], ss[:, sl], 1.0 / D, EPS, ALU.mult, ALU.add, [("ss", gI)], [("rstd", gI)])
            act(rstd[:, sl], rstd[:, sl], AF.Sqrt, [("rstd", gI)], [("rstd", gI)])
            RCP(rstd[:, sl], rstd[:, sl], [("rstd", gI)], [("rstd", gI)])

        def na(i):
            h = hb[i % 2]
            STT(h[:], x_sb[:, i, :], rstd[:, i:i + 1], gbc[:], ALU.mult, ALU.mult, [("x", i), ("rstd", i // 4), "gbc"], [("hb", i % 2)])

        def nb(i):
            h = hb[i % 2]
            pt = pb[i % 2]
            for k in range(8):
                tr(pt[:, k * 128:(k + 1) * 128], h[:, k * 128:(k + 1) * 128], [("hb", i % 2)], [("pb", i % 2)])
            act(hT[:, :, i * 128:(i + 1) * 128], pt[:].rearrange("p (k t) -> p k t", k=8), AF.Copy, [("pb", i % 2)], [("hT", i)])

        pend = []
        if layer == 0:
            for gI in range(4):
                stats(gI)
                for i in range(4 * gI, 4 * gI + 4):
                    na(i)
                    pend.append(i)
                    if len(pend) > 1:
                        nb(pend.pop(0))
        else:
            for gI in range(5):
                if gI < 4:
                    stats(gI)
                if gI >= 1:
                    for i in range(4 * (gI - 1), 4 * gI):
                        na(i)
                        pend.append(i)
                        if len(pend) > 1:
                            nb(pend.pop(0))
        nb(pend.pop(0))
        if barrier:
            P.barrier()

    def load_wo(layer, c0, n):
        wv = wout_d[layer].rearrange("(k p) n -> p k n", p=128)
        for hf in range(2):
            dma("pool", wo[:, 0:n, hf * 512:(hf + 1) * 512], wv[:, c0:c0 + n, hf * 512:(hf + 1) * 512], "wo", writes=["wo"], nobar=True)

    def outproj_tile(i, lhs_list, chunks, yT_reads, pbanks):
        for half in range(2):
            pp = pf[pbanks[half]]
            for n, (c, lhs) in enumerate(zip(chunks, lhs_list)):
                mm(pp[:], lhs, wo[:, c, half * 512:(half + 1) * 512], n == 0, n == len(chunks) - 1,
                   list(yT_reads) + ["wo"], [("pf", pbanks[half])])
            xs = x_sb[:, i, half * 512:(half + 1) * 512]
            TT(xs, xs, pp[:], ALU.add, [("pf", pbanks[half]), ("x", i)], [("x", i)])

    att_ctr = [0, 0]

    def lagged(n, stage_a, stage_b, lag=1):
        for i in range(n + lag):
            if i < n:
                stage_a(i)
            if i >= lag:
                stage_b(i - lag)

    def mm_acc(out, lhsT, rhs, start, stop, reads, writes):
        P.op("pe", lambda e: e.matmul(out, lhsT=lhsT, rhs=rhs, start=start, stop=stop, skip_group_check=True), reads=reads, writes=writes)

    def attention(QT, KT, nk, Vrhs, ncv, plan, finish, qkeys, kkeys, vkeys, ptb, tagk, addmask=None, finish_chunk=None):
        NS = len(ptb)
        work = []
        for qc in range(4):
            items = plan(qc)
            if not items:
                continue
            ob = 3 + att_ctr[1] % 3
            att_ctr[1] += 1
            lastk = {}
            for (kt, jlo, jhi, masks) in items:
                for j in range(jlo, jhi + 1):
                    lastk[j] = kt
            for n, it in enumerate(items):
                work.append((qc, ob, it, n == 0, n == len(items) - 1, lastk))

        def front(w):
            qc, ob, (kt, jlo, jhi, masks), first, last, lastk = w
            sbk = att_ctr[0] % NS
            att_ctr[0] += 1
            Sp = pf[sbk]
            pt = ptb[sbk]
            c0, c1 = jlo * 128, (jhi + 1) * 128
            extra = list(masks.items())
            mm_acc(Sp[0:nk, c0:c1], KT(kt), QT[:, qc * 512 + c0:qc * 512 + c1], True, addmask is None and not extra,
                   list(qkeys(qc)) + list(kkeys(kt)), [("pf", sbk)])
            if addmask is not None:
                mm_acc(Sp[0:nk, c0:c1], ident[0:nk, 0:nk], addmask[0:nk, qc * 512 + c0:qc * 512 + c1], False, not extra,
                       ["const", "addmask"], [("pf", sbk)])
            for n, (j, mk) in enumerate(extra):
                mm_acc(Sp[0:nk, j * 128:(j + 1) * 128], ident[0:nk, 0:nk], mk[0:nk, :], False, n == len(extra) - 1,
                       ["const"], [("pf", sbk)])
            act(pt[0:nk, c0:c1], Sp[0:nk, c0:c1], AF.Exp, [("pf", sbk)], [("pt", tagk, sbk)])
            return sbk

        def back(w, sbk, started):
            qc, ob, (kt, jlo, jhi, masks), first, last, lastk = w
            pt = ptb[sbk]
            for j in range(jlo, jhi + 1):
                mm_acc(pf[ob][:, j * 128:j * 128 + ncv], pt[0:nk, j * 128:(j + 1) * 128], Vrhs(kt), len(started) == 0, lastk[j] == kt,
                       [("pt", tagk, sbk)] + list(vkeys(kt)), [("pf", ob)])
                started.add(j)
            if last:
                if finish_chunk is not None:
                    finish_chunk(qc, pf[ob], ("pf", ob))
                else:
                    for j in sorted(started):
                        finish(qc * 4 + j, pf[ob][:, j * 128:j * 128 + ncv], ("pf", ob))
                started.clear()

        LAG = NS - 1
        started = set()
        pend = []
        for w in work:
            pend.append((w, front(w)))
            if len(pend) > LAG:
                pw, psb = pend.pop(0)
                back(pw, psb, started)
        for (pw, psb) in pend:
            back(pw, psb, started)

    def layer0():
        if stop == "load":
            return
        norm_phase(0, base=70 * 1024, barrier=False)
        if stop == "norm0":
            return
        load_wo(0, 0, 4)
        W = ewin_d
        if stop == "norm":
            return
        cv = Carver()
        yc = cv.get([128, 4, S], F32)
        hc = cv.get([128, S + 32], BF16)
        dg = cv.get([128, 31, 128], BF16)
        cw = cv.get([128, 4, 31], F32)
        cb = cv.get([128, 4], F32)
        lg = cv.get([128, 4], F32)
        lb = cv.get([128, 4], F32)
        Tsq = [cv.get([128, 512], F32) for _ in range(2)]
        mu2 = [cv.get([128, 512], F32) for _ in range(2)]
        msq = cv.get([128, 512], F32)
        rs2 = [cv.get([128, 512], F32) for _ in range(2)]
        sz4 = [cv.get([128, 512], BF16) for _ in range(4)]
        T4 = [cv.get([128, 512], F32) for _ in range(4)]
        yaT2 = [cv.get([128, 4, 512], BF16) for _ in range(2)]
        HO = 32
        stg = Tsq[0][0:32, :]
        stg2 = Tsq[1][0:12, 0:128]
        dma("sp", stg[0:31, :], convw_d, "c", writes=[("Tsq", 0)])
        dma("sp", stg2[0:4, :], convb_d.rearrange("(a c) -> a c", c=128), "c", writes=[("Tsq", 1)])
        dma("sp", stg2[4:8, :], lng_d.rearrange("(a c) -> a c", c=128), "c", writes=[("Tsq", 1)])
        dma("sp", stg2[8:12, :], lnb_d.rearrange("(a c) -> a c", c=128), "c", writes=[("Tsq", 1)])
        for cc in range(4):
            tr(pf[0][:, cc * 32:cc * 32 + 31], stg[0:31, cc * 128:(cc + 1) * 128], [("Tsq", 0)], [("pf", 0)], idn=identf[0:31, 0:31])
        tr(pf[0][:, 128:140], stg2[0:12, :], [("Tsq", 1)], [("pf", 0)], idn=identf[0:12, 0:12])
        CPY(cw[:], pf[0][:, 0:128].rearrange("p (a j) -> p a j", j=32)[:, :, 0:31], [("pf", 0)], ["cw"])
        CPY(cb[:], pf[0][:, 128:132], [("pf", 0)], ["cw"])
        CPY(lg[:], pf[0][:, 132:136], [("pf", 0)], ["cw"])
        CPY(lb[:], pf[0][:, 136:140], [("pf", 0)], ["cw"])
        MSET(hc[:, 0:HO], 0.0, ["hc0"])
        if stop == "convp":
            return
        for cc in range(4):
            if stop == "conv1" and cc == 1:
                return
            s = load_w(W, [(cc * 128, 128), (512 + cc * 128, 128)])
            for j in range(31):
                TS(dg[:, j, :], ident[:], cw[:, cc, j:j + 1], None, ALU.mult, None, ["const", "cw"], [("dg", j)])
            for tq in range(4):
                bv, bg = (tq % 2) * 2, (tq % 2) * 2 + 1
                for which, bk in ((0, bv), (1, bg)):
                    for k in range(8):
                        mm(pf[bk][:], wbuf[s][:, k, which * 128:(which + 1) * 128], hT[:, k, tq * 512:(tq + 1) * 512], k == 0, k == 7,
                           [("w", s)] + [("hT", 4 * tq + u) for u in range(4)], [("pf", bk)])
                act(T4[tq % 2][:], pf[bg][:], AF.Sigmoid, [("pf", bg)], [("T4", tq % 2)])
                TT(hc[:, HO + tq * 512:HO + (tq + 1) * 512], pf[bv][:], T4[tq % 2][:], ALU.mult, [("pf", bv), ("T4", tq % 2)], [("hc", tq)])
            for tq in range(4):
                pc = pf[4 + tq % 2]
                for j in range(31):
                    o = HO - 30 + j + tq * 512
                    mm(pc[:], dg[:, j, :], hc[:, o:o + 512], j == 0, j == 30,
                       [("dg", j), ("hc", tq)] + ([("hc", tq - 1)] if tq > 0 else ["hc0"]), [("pf", 4 + tq % 2)])
                act(yc[:, cc, tq * 512:(tq + 1) * 512], pc[:], AF.Identity, [("pf", 4 + tq % 2), "cw"], [("yc", cc, tq)],
                    bias=cb[:, cc:cc + 1], scale=1.0)
        if stop == "conv4":
            return
        P.barrier()
        sz_slot = load_w(W, [(1024, 512)])

        def st_a(tq):
            ts = slice(tq * 512, (tq + 1) * 512)
            p2 = tq % 2
            for cc in range(4):
                mm(pf[0][:], onesf[:], yc[:, cc, ts], cc == 0, cc == 3, [("yc", cc, tq), "const"], [("pf", 0)])
            for cc in range(4):
                act(Tsq[cc % 2][:], yc[:, cc, ts], AF.Square, [("yc", cc, tq)], [("Tsq", cc % 2)])
                mm(pf[1][:], onesf[:], Tsq[cc % 2][:], cc == 0, cc == 3, [("Tsq", cc % 2), "const"], [("pf", 1)])
            TS(mu2[p2][:], pf[0][:], 1.0 / 512, None, ALU.mult, None, [("pf", 0)], [("mu", p2)])
            TT(msq[:], mu2[p2][:], mu2[p2][:], ALU.mult, [("mu", p2)], ["msq"])
            STT(rs2[p2][:], pf[1][:], 1.0 / 512, msq[:], ALU.mult, ALU.subtract, [("pf", 1), "msq"], [("rs", p2)])
            TS(rs2[p2][:], rs2[p2][:], EPS, None, ALU.add, None, [("rs", p2)], [("rs", p2)])
            act(rs2[p2][:], rs2[p2][:], AF.Sqrt, [("rs", p2)], [("rs", p2)])
            RCP(rs2[p2][:], rs2[p2][:], [("rs", p2)], [("rs", p2)])

        def st_b(tq):
            ts = slice(tq * 512, (tq + 1) * 512)
            p2 = tq % 2
            for cc in range(4):
                pz = pf[2 + cc % 2]
                for k in range(8):
                    mm(pz[:], wbuf[sz_slot][:, k, cc * 128:(cc + 1) * 128], hT[:, k, ts], k == 0, k == 7,
                       [("w", sz_slot)] + [("hT", 4 * tq + u) for u in range(4)], [("pf", 2 + cc % 2)])
                act(sz4[cc][:], pz[:], AF.Silu, [("pf", 2 + cc % 2)], [("sz", cc)])
            for cc in range(4):
                tt = T4[cc]
                TT(tt[:], yc[:, cc, ts], mu2[p2][:], ALU.subtract, [("yc", cc, tq), ("mu", p2)], [("T4", cc)])
                TT(tt[:], tt[:], rs2[p2][:], ALU.mult, [("T4", cc), ("rs", p2)], [("T4", cc)])
            for cc in range(4):
                tt = T4[cc]
                act(tt[:], tt[:], AF.Silu, [("T4", cc), "cw"], [("T4", cc)], scale=lg[:, cc:cc + 1], bias=lb[:, cc:cc + 1])
            for cc in range(4):
                TT(yaT2[p2][:, cc, :], T4[cc][:], sz4[cc][:], ALU.mult, [("T4", cc), ("sz", cc)], [("yaT", p2, cc)])

        def st_c(tq):
            p2 = tq % 2
            for u in range(4):
                i = 4 * tq + u
                outproj_tile(i, [yaT2[p2][:, cc, u * 128:(u + 1) * 128] for cc in range(4)], [0, 1, 2, 3],
                             [("yaT", p2, cc) for cc in range(4)], (4, 5))

        for step in range(6):
            if step < 4:
                st_a(step)
            if 0 <= step - 1 < 4:
                st_b(step - 1)
            if step - 2 >= 0:
                st_c(step - 2)
        P.barrier()
        if stop == "conv":
            return
        for hh in range(2):
            moba_half(hh, W)
            P.barrier()
            if stop is not None:
                return

    def moba_half(hh, W):
        cv = Carver()
        QT = cv.get([128, 4, S], BF16)
        KT = cv.get([128, 4, S], BF16)
        Vt = cv.get([128, NT, 4, 66], BF16)
        szy = cv.get([128, NT, 256], BF16)
        ptb = [cv.get([128, 512], BF16) for _ in range(3)]
        gq = cv.get([128, 1], F32)
        gk = cv.get([128, 1], F32)
        kmf = cv.get([128, 4, 8], F32)
        kmb = cv.get([128, 4, 8], BF16)
        gs = cv.get([128, 4, 8], F32)
        cmp_ = cv.get([128, 4, 8, 8], F32)
        rank = cv.get([128, 4, 8], F32)
        nm = cv.get([128, 4, 32], BF16)
        rden = [cv.get([128, 1], F32) for _ in range(8)]
        yT = [cv.get([128, 2, 128], BF16) for _ in range(2)]
        tmpf = [cv.get([128, 512], F32) for _ in range(2)]
        s8 = [cv.get([128, 8], F32) for _ in range(3)]
        qa = [cv.get([128, 4, 128], BF16) for _ in range(3)]
        ka = [cv.get([128, 4, 128], BF16) for _ in range(3)]
        c_q = 1536 + hh * 256
        c_k = 2048 + hh * 256
        c_v = 2560 + hh * 256
        c_z = 3072 + hh * 256
        s = load_w(W, [(c_q, 256), (c_k, 256)])
        s2 = load_w(W, [(c_v, 256), (c_z, 256)])
        load_wo(0, 4 + 2 * hh, 2)
        load_gain_col(gq, bq_d, 1.0)
        load_gain_col(gk, bk_d, 8.0)
        for b in range(3):
            MSET(qa[b][:, :, 64:128], 0.0, [("qa", b)])
            MSET(ka[b][:, :, 64:128], 0.0, [("ka", b)])
        MSET(Vt[:, :, :, 64:65], 1.0, ["Vt"])
        MSET(nm[:], 0.0, ["nm"])

        def m0(i):
            r = i % 3
            CPY(qa[r][:, :, 96:100], qal[:, i, hh * 16:hh * 16 + 16].rearrange("p (h c) -> p h c", c=4), ["const"], [("qa", r)])
            CPY(ka[r][:, :, 64:100], kalm[:, i, :].unsqueeze(1).to_broadcast([128, 4, 36]), ["const"], [("ka", r)])
            for k in range(8):
                mm(pf[r][:], hT[:, k, i * 128:(i + 1) * 128], wbuf[s][:, k, :], k == 0, k == 7, [("hT", i), ("w", s)], [("pf", r)])

        def m1(i):
            r = i % 3
            rstd_a(pf[r], ("pf", r), 512, tmpf[i % 2], ("tf", i % 2), s8[r], ("s8", r))

        def m2(i):
            r = i % 3
            rstd_b(512, s8[r], ("s8", r))
            TT(qa[r][:, :, 0:64], pf[r][:, 0:256].rearrange("p (h d) -> p h d", d=64), s8[r][:, 0:4].unsqueeze(2).to_broadcast([128, 4, 64]),
               ALU.mult, [("pf", r), ("s8", r)], [("qa", r)])
            TT(ka[r][:, :, 0:64], pf[r][:, 256:512].rearrange("p (h d) -> p h d", d=64), s8[r][:, 4:8].unsqueeze(2).to_broadcast([128, 4, 64]),
               ALU.mult, [("pf", r), ("s8", r)], [("ka", r)])

        def m3(i):
            r = i % 3
            b2 = i % 2
            pt = pb[b2]
            for h in range(4):
                tr(pt[:, h * 128:(h + 1) * 128], qa[r][:, h, :], [("qa", r)], [("pb", b2)])
            for h in range(4):
                tr(pt[:, (4 + h) * 128:(5 + h) * 128], ka[r][:, h, :], [("ka", r)], [("pb", b2)])
            act(QT[:, :, i * 128:(i + 1) * 128], pt[:, 0:512].rearrange("p (h t) -> p h t", h=4), AF.Copy, [("pb", b2), "gq"], [("QT", i)],
                scale=gq[:, 0:1])
            act(KT[:, :, i * 128:(i + 1) * 128], pt[:, 512:1024].rearrange("p (h t) -> p h t", h=4), AF.Copy, [("pb", b2), "gq"], [("KT", i)],
                scale=gk[:, 0:1])

        pipe4(NT, m0, m1, m2, m3)
        if hh == 1 and 1 in layers:
            prefetch_w1([("qa", r) for r in range(3)] + [("ka", r) for r in range(3)] + [("s8", r) for r in range(3)] + [("tf", 0), ("tf", 1)])
        if stop == "m1":
            return
        for i in range(NT):
            b2 = 2 + i % 2
            pp = pf[b2]
            for k in range(8):
                mm(pp[:], hT[:, k, i * 128:(i + 1) * 128], wbuf[s2][:, k, :], k == 0, k == 7, [("hT", i), ("w", s2)], [("pf", b2)])
            act(Vt[:, i, :, 0:64], pp[:, 0:256].rearrange("p (h d) -> p h d", d=64), AF.Copy, [("pf", b2), "Vt"], [("V", i)])
            act(szy[:, i, :], pp[:, 256:512], AF.Silu, [("pf", b2)], [("szy", i)])
        if stop == "m3":
            return
        for h in range(4):
            RED(kmf[:, h, :], KT[:, h, :].rearrange("p (n t) -> p n t", t=256), ALU.add, [("KT", i) for i in range(NT)], ["kmf"])
        TS(kmb[:], kmf[:], 1.0 / 256, None, ALU.mult, None, ["kmf"], ["kmb"])
        if stop == "mk":
            return
        for i in range(8, NT):
            npast = i // 2
            b2 = 4 + i % 2
            pg = pf[b2]
            for h in range(4):
                mm(pg[:, h * 8:h * 8 + 8], QT[0:64, h, i * 128:(i + 1) * 128], kmb[0:64, h, :], True, True, [("QT", i), "kmb"], [("pf", b2)])
            CPY(gs[:], pg[:, 0:32].rearrange("p (h n) -> p h n", n=8), [("pf", b2)], ["gs"])
            TT(cmp_[:, :, 0:npast, 0:npast], gs[:, :, 0:npast].unsqueeze(2).to_broadcast([128, 4, npast, npast]),
               gs[:, :, 0:npast].unsqueeze(3).to_broadcast([128, 4, npast, npast]), ALU.is_gt, ["gs"], ["cmp"])
            RED(rank[:, :, 0:npast], cmp_[:, :, 0:npast, 0:npast], ALU.add, ["cmp"], ["rank"])
            TS(nm[:, :, 0:npast], rank[:, :, 0:npast], 2.5, NEGM, ALU.is_ge, ALU.mult, ["rank"], ["nm"])
            pt = pb[i % 2]
            tr(pt[:, 0:128], nm[:].rearrange("p h n -> p (h n)"), ["nm"], [("pb", i % 2)])
            for h in range(4):
                act(QT[64:96, h, i * 128:(i + 1) * 128], pt[h * 32:(h + 1) * 32, 0:128], AF.Copy, [("pb", i % 2)], [("QT", i)])

        if stop == "m2":
            return

        def plan(qc):
            items = []
            for kt in range(4 * qc + 4):
                if kt < 4 * qc:
                    items.append((kt, 0, 3, {}))
                else:
                    m = kt - 4 * qc
                    items.append((kt, m, 3, {m: tri[:]}))
            return items

        for h in range(4):
            def finish(i, Ob, okey, h=h):
                r = (i + 4 * h) % 8
                rd = rden[r]
                RCP(rd[:], Ob[:, 64:65], [okey], [("rden", r)])
                dst = szy[:, i, h * 64:(h + 1) * 64]
                STT(dst, Ob[:, 0:64], rd[:], dst, ALU.mult, ALU.mult, [okey, ("rden", r), ("szy", i)], [("szy", i)])

            attention(QT[:, h, :], lambda kt, h=h: KT[:, h, kt * 128:(kt + 1) * 128], 128,
                      lambda kt, h=h: Vt[:, kt, h, 0:65], 65, plan, finish,
                      lambda qc: [("QT", 4 * qc + u) for u in range(4)], lambda kt: [("KT", kt)], lambda kt: [("V", kt)],
                      ptb, "m")
        if stop == "ma":
            return
        outproj_half(szy, yT)

    def rstd_a(pp, pkey, ncol, tf, tfkey, s8i, s8key):
        nh = ncol // 64
        act(tf[:, 0:ncol], pp[:, 0:ncol], AF.Square, [pkey], [tfkey])
        RED(s8i[:, 0:nh], tf[:, 0:ncol].rearrange("p (h d) -> p h d", d=64), ALU.add, [tfkey], [s8key])
        TS(s8i[:, 0:nh], s8i[:, 0:nh], 64.0 * EPS, None, ALU.add, None, [s8key], [s8key])
        act(s8i[:, 0:nh], s8i[:, 0:nh], AF.Sqrt, [s8key], [s8key])

    def rstd_b(ncol, s8i, s8key):
        nh = ncol // 64
        RCP(s8i[:, 0:nh], s8i[:, 0:nh], [s8key], [s8key])

    def head_rstd(pp, pkey, ncol, tf, tfkey, s8i, s8key):
        rstd_a(pp, pkey, ncol, tf, tfkey, s8i, s8key)
        rstd_b(ncol, s8i, s8key)

    def pipe4(n, s0, s1, s2, s3):
        for step in range(n + 2):
            if step < n:
                s0(step)
                s1(step)
            if 1 <= step <= n:
                s2(step - 1)
            if step >= 2:
                s3(step - 2)

    def load_gain_col(dst, gd, mult):
        MSET(dst[:], 1.0, ["gq"])
        dma("sp", dst[0:64, :], gd.rearrange("o d -> d o"), "c", writes=["gq"], slow=True)
        if mult != 1.0:
            TS(dst[0:64, :], dst[0:64, :], mult, None, ALU.mult, None, ["gq"], ["gq"])

    def outproj_half(szy, yT, store=False):
        def oa(i):
            pt = pb[i % 2]
            for c in range(2):
                tr(pt[:, c * 128:(c + 1) * 128], szy[:, i, c * 128:(c + 1) * 128], [("szy", i)], [("pb", i % 2)])
            y = yT[i % 2]
            act(y[:], pt[:, 0:256].rearrange("p (c t) -> p c t", c=2), AF.Copy, [("pb", i % 2)], [("yT", i % 2)])

        def ob(i):
            y = yT[i % 2]
            outproj_tile(i, [y[:, 0, :], y[:, 1, :]], [0, 1], [("yT", i % 2)], (4, 5))
            if store:
                dma("sp", yv[:, i, :], x_sb[:, i, :], "out", reads=[("x", i)])

        lagged(NT, oa, ob)

    def plan_causal(qc):
        items = []
        for kt in range(4 * qc + 4):
            if kt < 4 * qc:
                items.append((kt, 0, 3, {}))
            else:
                m = kt - 4 * qc
                items.append((kt, m, 3, {m: tri[:]}))
        return items

    def plan_band(wt):
        def plan(qc):
            items = []
            for kt in range(max(0, 4 * qc - wt), 4 * qc + 4):
                jlo = max(0, kt - 4 * qc)
                jhi = min(3, kt + wt - 4 * qc)
                if jlo > jhi:
                    continue
                masks = {}
                if 0 <= kt - 4 * qc <= 3:
                    masks[kt - 4 * qc] = tri[:]
                if 0 <= kt + wt - 4 * qc <= 3:
                    masks[kt + wt - 4 * qc] = upm[:]
                items.append((kt, jlo, jhi, masks))
            return items
        return plan

    W1_OFF = 64 * 1024

    def prefetch_w1(war_keys=()):
        cvp = Carver(base=W1_OFF)
        wA = cvp.get([128, 32, 256], BF16)
        for kv, wd in enumerate((ckw1_d, cvw1_d)):
            wv = wd.rearrange("(l d) j -> d l j", d=64)
            for lq in range(4):
                dma("pool", wA[kv * 64:(kv + 1) * 64, lq * 8:(lq + 1) * 8, :], wv[:, lq * 8:(lq + 1) * 8, :], "w1", writes=["w1A"] + list(war_keys))
        return wA

    def layer1():
        W = owin_d
        wA = Carver(base=W1_OFF).get([128, 32, 256], BF16)
        if 0 not in layers:
            prefetch_w1()
        cvc = Carver(base=16 * 1024)
        wB = cvc.get([128, 32, 256], BF16)
        dma("sp", wB[64:128, :, :], wA[0:64, :, :], "c", writes=["w1B"])
        dma("sp", wB[0:64, :, :], wA[64:128, :, :], "c", writes=["w1B"])
        w1 = [[wA, wB], [wB, wA]]
        B = compress_loads(W, cvc)
        norm_phase(1)
        compress_stage(W, B, w1)
        P.barrier()
        if stop == "cmp":
            return
        for g in range(2):
            nsa_half(g, W)
            P.barrier()
            if stop == "nsa0":
                return
        if stop == "nsa":
            return
        for g in range(2):
            swa_half(g, W)
            P.barrier()

    def compress_loads(W, cv):
        s = load_w(W, [(512, 128), (640, 128)])
        B = {}
        B["s"] = s
        B["KVD"] = [cv.get([128, 16, 128], BF16) for _ in range(2)]
        B["w2"] = [cv.get([128, 2, 64], BF16) for _ in range(2)]
        B["posn"] = cv.get([32, 2, 64], F32)
        B["posT"] = cv.get([64, 2, 32], BF16)
        B["stgb"] = cv.get([4, 128], F32)
        B["b1sb"] = cv.get([128, 4], F32)
        B["biasj"] = cv.get([128, 4], F32)
        B["b2bc"] = cv.get([128, 2, 64], F32)
        B["gcm"] = cv.get([128, 64], F32)
        B["kalc"] = cv.get([128, 36], BF16)
        B["hid"] = [cv.get([128, 2, 128], BF16) for _ in range(4)]
        B["kcf"] = cv.get([128, 64], F32)
        B["junk2"] = cv.get([128, 64], F32)
        B["ssc"] = cv.get([128, 1], F32)
        B["kaug"] = cv.get([128, 128], BF16)
        for kv, wd in enumerate((ckw2_d, cvw2_d)):
            dma("pool", B["w2"][kv][:], wd.rearrange("(c p) d -> p c d", p=128), "w2", writes=["w2"])
        dma("sp", B["posn"][:, 0, :], cposk_d, "c", writes=["posn"])
        dma("sp", B["posn"][:, 1, :], cposv_d, "c", writes=["posn"])
        dma("sp", B["stgb"][0:2, :], ckb1_d.rearrange("(a c) -> a c", c=128), "c", writes=["stgb"])
        dma("sp", B["stgb"][2:4, :], cvb1_d.rearrange("(a c) -> a c", c=128), "c", writes=["stgb"])
        dma("sp", B["b2bc"][:, 0, :], ckb2_d.partition_broadcast(128), "c", writes=["b2bc"])
        dma("sp", B["b2bc"][:, 1, :], cvb2_d.partition_broadcast(128), "c", writes=["b2bc"])
        dma("sp", B["gcm"][:], ckc_d.partition_broadcast(128), "c", writes=["gcm"])
        dma("pool", B["kalc"][:], kalc_d, "c", writes=["kalc"])
        for g in range(2):
            dma("pool", Vca[:, g, 65:97], ovl_d, "c", writes=[("Vca", g)])
        return B

    def compress_stage(W, B, w1):
        s = B["s"]
        KVD, w2, posn, posT, stgb, b1sb, biasj = B["KVD"], B["w2"], B["posn"], B["posT"], B["stgb"], B["b1sb"], B["biasj"]
        b2bc, gcm, kalc, hid, kcf, junk2, ssc, kaug = B["b2bc"], B["gcm"], B["kalc"], B["hid"], B["kcf"], B["junk2"], B["ssc"], B["kaug"]
        for g in range(2):
            MSET(Vca[:, g, 64:65], 1.0, [("Vca", g)])
        MSET(kaug[:], 0.0, ["kaug"])
        for kv in range(2):
            tr(pf[0][0:64, kv * 32:(kv + 1) * 32], posn[:, kv, :], ["posn"], [("pf", 0)], idn=identf[0:32, 0:32])
        tr(pf[0][:, 64:68], stgb[:], ["stgb"], [("pf", 0)], idn=identf[0:4, 0:4])
        CPY(posT[:], pf[0][0:64, 0:64].rearrange("p (k l) -> p k l", l=32), [("pf", 0)], ["posT"])
        CPY(b1sb[:], pf[0][:, 64:68], [("pf", 0)], ["b1sb"])
        for kv in range(2):
            for jc in range(2):
                col = kv * 2 + jc
                for l in range(32):
                    mm(pf[1][:, col:col + 1], w1[kv][0][0:64, l, jc * 128:(jc + 1) * 128], posT[0:64, kv, l:l + 1], l == 0, l == 31,
                       ["w1B", "posT"], [("pf", 1)])
        TT(biasj[:], pf[1][:, 0:4], b1sb[:], ALU.add, [("pf", 1), "b1sb"], ["biasj"])
        for tq in range(4):
            for which in range(2):
                pp = pf[2 + which]
                for k in range(8):
                    mm(pp[:], wbuf[s][:, k, which * 128:(which + 1) * 128], hT[:, k, tq * 512:(tq + 1) * 512], k == 0, k == 7,
                       [("w", s)] + [("hT", 4 * tq + u) for u in range(4)], [("pf", 2 + which)])
                act(KVD[which][:, :, tq * 32:(tq + 1) * 32].rearrange("p r m -> p m r"), pp[:].rearrange("p (m r) -> p m r", r=16),
                    AF.Copy, [("pf", 2 + which)], [("KVT", which)])
        n = 0
        for kv in range(2):
            for g in range(2):
                hb_ = hid[kv * 2 + g]
                for jc in range(2):
                    pp = pf[n % 2]
                    for l in range(32):
                        mm(pp[:, 0:127], w1[kv][g][g * 64:(g + 1) * 64, l, jc * 128:(jc + 1) * 128],
                           KVD[kv][g * 64:(g + 1) * 64, l % 16, (l // 16):(l // 16) + 127], l == 0, l == 31,
                           ["w1B", ("KVT", kv)], [("pf", n % 2)])
                    act(hb_[:, jc, 0:127], pp[:, 0:127], AF.Silu, [("pf", n % 2), "biasj"], [("hid", kv * 2 + g)],
                        bias=biasj[:, kv * 2 + jc:kv * 2 + jc + 1], scale=1.0)
                    n += 1
                po = pf[2 + g]
                for jc in range(2):
                    mm(po[0:127, 0:64], hb_[:, jc, 0:127], w2[kv][:, jc, :], jc == 0, jc == 1, [("hid", kv * 2 + g), "w2"], [("pf", 2 + g)])
                if kv == 0:
                    TT(kcf[0:127, :], po[0:127, 0:64], b2bc[0:127, 0, :], ALU.add, [("pf", 2 + g), "b2bc"], ["kcf"])
                    act(junk2[0:127, :], kcf[0:127, :], AF.Square, ["kcf"], ["junk2", "ssc"], accum_out=ssc[0:127, :])
                    TS(ssc[0:127, :], ssc[0:127, :], 1.0 / 64, EPS, ALU.mult, ALU.add, ["ssc"], ["ssc"])
                    act(ssc[0:127, :], ssc[0:127, :], AF.Sqrt, ["ssc"], ["ssc"])
                    RCP(ssc[0:127, :], ssc[0:127, :], ["ssc"], ["ssc"])
                    STT(kaug[0:127, 0:64], kcf[0:127, :], ssc[0:127, :], gcm[0:127, :], ALU.mult, ALU.mult, ["kcf", "ssc", "gcm"], ["kaug"])
                    CPY(kaug[:, 64:100], kalc[:], ["kalc"], ["kaug"])
                    tr(pb[g][:, 0:128], kaug[:], ["kaug"], [("pb", g)])
                    act(KTc[:, g, :], pb[g][:, 0:128], AF.Copy, [("pb", g)], [("KTc", g)])
                else:
                    TT(Vca[0:127, g, 0:64], po[0:127, 0:64], b2bc[0:127, 1, :], ALU.add, [("pf", 2 + g), "b2bc"], [("Vca", g)])

    def nsa_half(g, W):
        cv = Carver()
        QT = cv.get([128, 4, S], BF16)
        KTs = cv.get([128, S], BF16)
        KTw = cv.get([128, S], BF16)
        Vs = cv.get([128, NT, 66], BF16)
        Vw = cv.get([128, NT, 66], BF16)
        szy = cv.get([128, NT, 256], BF16)
        gates = cv.get([128, NT, 12], F32)
        oacc = cv.get([128, NT, 256], F32)
        imp = cv.get([128, 8, 32], F32)
        impc = cv.get([128, 8, 32], F32)
        cmn = cv.get([128, S], BF16)
        qa = [cv.get([128, 4, 128], BF16) for _ in range(3)]
        ksa = [cv.get([128, 128], BF16) for _ in range(3)]
        kwa = [cv.get([128, 128], BF16) for _ in range(3)]
        ptb = [cv.get([128, 512], BF16) for _ in range(3)]
        tmpf = [cv.get([128, 512], F32) for _ in range(2)]
        cmpb = cv.get([128, 32, 32], F32)
        rank = cv.get([128, 32], F32)
        nmt = cv.get([128, 128], BF16)
        s8 = [cv.get([128, 8], F32) for _ in range(3)]
        gq = cv.get([128, 1], F32)
        gks = cv.get([128, 1], F32)
        gkw = cv.get([128, 1], F32)
        rden = [cv.get([128, 1], F32) for _ in range(8)]
        rden4 = [cv.get([128, 4], F32) for _ in range(8)]
        tmp32 = cv.get([128, 4, 32], F32)
        yT = [cv.get([128, 2, 128], BF16) for _ in range(2)]
        s = load_w(W, [(g * 256, 256), (768 + 64 * g, 64), (1024 + 64 * g, 64), (896 + 64 * g, 64), (1152 + 64 * g, 64)])
        s2 = load_w(W, [(1304 + g * 256, 256), (1280 + 4 * g, 4), (1288 + 4 * g, 4), (1296 + 4 * g, 4)])
        load_wo(1, 2 * g, 2)
        load_gain_col(gq, cq_d, 1.0)
        load_gain_col(gks, cks_d, 8.0)
        load_gain_col(gkw, ckw_d, 8.0)
        dma("sp", impc[:], impc_d[:, 8:16, :], "c", writes=["impc"])
        dma("pool", cmn[:], cmn_d, "c", writes=["addmask"])
        for b in range(3):
            MSET(qa[b][:, :, 64:128], 0.0, [("qa", b)])
            MSET(ksa[b][:, 64:128], 0.0, [("ksa", b)])
            MSET(kwa[b][:, 64:128], 0.0, [("kwa", b)])
        MSET(Vs[:, :, 64:65], 1.0, ["Vs"])
        MSET(Vw[:, :, 64:65], 1.0, ["Vw"])
        MSET(nmt[:], 0.0, ["nmt"])
        def pa(i):
            b2 = i % 2
            pp = pf[b2]
            qai, ksi, kwi = qa[b2], ksa[b2], kwa[b2]
            CPY(qai[:, :, 96:100], qal[:, i, g * 16:g * 16 + 16].rearrange("p (h c) -> p h c", c=4), ["const"], [("qa", b2)])
            CPY(ksi[:, 64:100], kals[:, i, :], ["const"], [("ksa", b2)])
            CPY(kwi[:, 64:100], kalp[:, i, :], ["const"], [("kwa", b2)])
            for k in range(8):
                mm(pp[:], hT[:, k, i * 128:(i + 1) * 128], wbuf[s][:, k, :], k == 0, k == 7, [("hT", i), ("w", s)], [("pf", b2)])
            tf = tmpf[b2]
            head_rstd(pp, ("pf", b2), 384, tf, ("tf", b2), s8[b2], ("s8", b2))
            TT(qai[:, :, 0:64], pp[:, 0:256].rearrange("p (h d) -> p h d", d=64), s8[b2][:, 0:4].unsqueeze(2).to_broadcast([128, 4, 64]),
               ALU.mult, [("pf", b2), ("s8", b2)], [("qa", b2)])
            TS(ksi[:, 0:64], pp[:, 256:320], s8[b2][:, 4:5], None, ALU.mult, None, [("pf", b2), ("s8", b2)], [("ksa", b2)])
            TS(kwi[:, 0:64], pp[:, 320:384], s8[b2][:, 5:6], None, ALU.mult, None, [("pf", b2), ("s8", b2)], [("kwa", b2)])
            act(Vs[:, i, 0:64], pp[:, 384:448], AF.Copy, [("pf", b2), "Vs"], [("Vs", i)])
            act(Vw[:, i, 0:64], pp[:, 448:512], AF.Copy, [("pf", b2), "Vw"], [("Vw", i)])

        def pbk(i):
            b2 = i % 2
            qai, ksi, kwi = qa[b2], ksa[b2], kwa[b2]
            pt = pb[b2]
            for h in range(4):
                tr(pt[:, h * 128:(h + 1) * 128], qai[:, h, :], [("qa", b2)], [("pb", b2)])
            tr(pt[:, 512:640], ksi[:], [("ksa", b2)], [("pb", b2)])
            tr(pt[:, 640:768], kwi[:], [("kwa", b2)], [("pb", b2)])
            act(QT[:, :, i * 128:(i + 1) * 128], pt[:, 0:512].rearrange("p (h t) -> p h t", h=4), AF.Copy, [("pb", b2), "gq"], [("QT", i)],
                scale=gq[:, 0:1])
            act(KTs[:, i * 128:(i + 1) * 128], pt[:, 512:640], AF.Copy, [("pb", b2), "gq"], [("KTs", i)], scale=gks[:, 0:1])
            act(KTw[:, i * 128:(i + 1) * 128], pt[:, 640:768], AF.Copy, [("pb", b2), "gq"], [("KTw", i)], scale=gkw[:, 0:1])

        lagged(NT, pa, pbk)
        for i in range(NT):
            b2 = 2 + i % 2
            pp = pf[b2]
            for k in range(8):
                mm(pp[:, 0:256], hT[:, k, i * 128:(i + 1) * 128], wbuf[s2][:, k, 0:256], k == 0, k == 7, [("hT", i), ("w", s2)], [("pf", b2)])
            act(szy[:, i, :], pp[:, 0:256], AF.Silu, [("pf", b2)], [("szy", i)])
        for i in range(NT):
            b2 = 2 + i % 2
            pp = pf[b2]
            for k in range(8):
                mm(pp[:, 0:12], hT[:, k, i * 128:(i + 1) * 128], wbuf[s2][:, k, 256:268], k == 0, k == 7, [("hT", i), ("w", s2)], [("pf", b2)])
            act(gates[:, i, :], pp[:, 0:12], AF.Sigmoid, [("pf", b2)], [("gates", i)])
        if stop == "nsaproj":
            return
        qk = lambda qc: [("QT", 4 * qc + u) for u in range(4)]
        for r in range(4):
            def fin_cmp_chunk(qc, Obank, okey, r=r):
                ri = (qc + 4 * r) % 8
                rd = rden4[ri]
                Ov = Obank[:].rearrange("p (j c) -> p j c", c=128)
                tl = slice(4 * qc, 4 * qc + 4)
                gk_ = [("gates", i) for i in range(4 * qc, 4 * qc + 4)]
                ok_ = [("oacc", i) for i in range(4 * qc, 4 * qc + 4)]
                TS(rd[:], Ov[:, :, 64:65].rearrange("p j c -> p (j c)"), 1e-30, None, ALU.max, None, [okey], [("rden4", ri)])
                RCP(rd[:], rd[:], [("rden4", ri)], [("rden4", ri)])
                if qc >= 2:
                    ik_ = [("imp", i) for i in range(4 * qc, 4 * qc + 4)]
                    dst = imp[:, 4 * qc - 8:4 * qc - 4, :]
                    if r == 0:
                        TT(dst, Ov[:, :, 65:97], rd[:].unsqueeze(2).to_broadcast([128, 4, 32]), ALU.mult, [okey, ("rden4", ri)], ik_)
                    else:
                        TT(tmp32[:], Ov[:, :, 65:97], rd[:].unsqueeze(2).to_broadcast([128, 4, 32]), ALU.mult, [okey, ("rden4", ri)], ["tmp32"])
                        TT(dst, dst, tmp32[:], ALU.add, ["tmp32"] + ik_, ik_)
                TT(rd[:], rd[:], gates[:, tl, r:r + 1].rearrange("p j c -> p (j c)"), ALU.mult, [("rden4", ri)] + gk_, [("rden4", ri)])
                TT(oacc[:, tl, r * 64:(r + 1) * 64], Ov[:, :, 0:64], rd[:].unsqueeze(2).to_broadcast([128, 4, 64]), ALU.mult,
                   [okey, ("rden4", ri)], ok_)

            attention(QT[:, r, :], lambda kt: KTc[:, g, 0:127], 127, lambda kt: Vca[0:127, g, 0:97], 97,
                      lambda qc: [(0, 0, 3, {})], None, qk, lambda kt: [("KTc", g)], lambda kt: [("Vca", g)], ptb, "n", addmask=cmn,
                      finish_chunk=fin_cmp_chunk)
        if stop == "nsacmp":
            return
        for i in range(8, NT):
            im = imp[:, i - 8, :]
            TT(im, im, impc[:, i - 8, :], ALU.add, [("imp", i), "impc"], [("imp", i)])
            TT(cmpb[:], im.unsqueeze(1).to_broadcast([128, 32, 32]), im.unsqueeze(2).to_broadcast([128, 32, 32]), ALU.is_gt,
               [("imp", i)], ["cmpb"])
            RED(rank[:], cmpb[:], ALU.add, ["cmpb"], ["rank"])
            TS(nmt[:, 0:32], rank[:], 15.5, NEGM, ALU.is_ge, ALU.mult, ["rank"], ["nmt"])
            pt = pb[i % 2]
            tr(pt[:, 0:128], nmt[:], ["nmt"], [("pb", i % 2)])
            for r in range(4):
                act(QT[64:96, r, i * 128:(i + 1) * 128], pt[0:32, 0:128], AF.Copy, [("pb", i % 2)], [("QT", i)])
        for (KTx, Vx, kname, vname, plan, gcol) in ((KTs, Vs, "KTs", "Vs", plan_causal, 4), (KTw, Vw, "KTw", "Vw", plan_band(4), 8)):
            for r in range(4):
                def fin_add_chunk(qc, Obank, okey, r=r, gcol=gcol):
                    ri = (qc + 4 * r) % 8
                    rd = rden4[ri]
                    tb = ri % 2
                    tmp = tmpf[tb][:, 0:256].rearrange("p (j c) -> p j c", c=64)
                    Ov = Obank[:].rearrange("p (j c) -> p j c", c=128)
                    tl = slice(4 * qc, 4 * qc + 4)
                    gk_ = [("gates", i) for i in range(4 * qc, 4 * qc + 4)]
                    ok_ = [("oacc", i) for i in range(4 * qc, 4 * qc + 4)]
                    RCP(rd[:], Ov[:, :, 64:65].rearrange("p j c -> p (j c)"), [okey], [("rden4", ri)])
                    TT(rd[:], rd[:], gates[:, tl, gcol + r:gcol + r + 1].rearrange("p j c -> p (j c)"), ALU.mult,
                       [("rden4", ri)] + gk_, [("rden4", ri)])
                    TT(tmp, Ov[:, :, 0:64], rd[:].unsqueeze(2).to_broadcast([128, 4, 64]), ALU.mult, [okey, ("rden4", ri)], [("tf", tb)])
                    dst = oacc[:, tl, r * 64:(r + 1) * 64]
                    TT(dst, dst, tmp, ALU.add, [("tf", tb)] + ok_, ok_)

                attention(QT[:, r, :], lambda kt, KTx=KTx: KTx[:, kt * 128:(kt + 1) * 128], 128,
                          lambda kt, Vx=Vx: Vx[:, kt, 0:65], 65, plan, None, qk,
                          lambda kt, kname=kname: [(kname, kt)], lambda kt, vname=vname: [(vname, kt)], ptb, "n",
                          finish_chunk=fin_add_chunk)
        for i in range(NT):
            TT(szy[:, i, :], oacc[:, i, :], szy[:, i, :], ALU.mult, [("oacc", i), ("szy", i)], [("szy", i)])
        outproj_half(szy, yT)

    def swa_half(g, W):
        cv = Carver()
        QT = cv.get([128, 4, S], BF16)
        KT = cv.get([128, S], BF16)
        Vt = cv.get([128, NT, 66], BF16)
        szy = cv.get([128, NT, 256], BF16)
        qa = [cv.get([128, 4, 128], BF16) for _ in range(3)]
        ka = [cv.get([128, 128], BF16) for _ in range(3)]
        ptb = [cv.get([128, 512], BF16) for _ in range(3)]
        tmpf = [cv.get([128, 512], F32) for _ in range(2)]
        s8 = [cv.get([128, 8], F32) for _ in range(3)]
        gq = cv.get([128, 1], F32)
        gk = cv.get([128, 1], F32)
        esink = cv.get([128, 8], F32)
        rden = [cv.get([128, 1], F32) for _ in range(8)]
        yT = [cv.get([128, 2, 128], BF16) for _ in range(2)]
        load_gain_col(gq, dq_d, 1.0)
        load_gain_col(gk, dk_d, 8.0)
        dma("sp", esink[:], dsink_d.partition_broadcast(128), "c", writes=["esink"])
        act(esink[:], esink[:], AF.Exp, ["esink"], ["esink"])
        for b in range(3):
            MSET(qa[b][:, :, 64:128], 0.0, [("qa", b)])
            MSET(ka[b][:, 64:128], 0.0, [("ka", b)])
        MSET(Vt[:, :, 64:65], 1.0, ["Vt"])
        s = load_w(W, [(1816 + g * 256, 256), (2328 + 64 * g, 64), (2456 + 64 * g, 64)])
        s2 = load_w(W, [(2584 + g * 256, 256)])
        load_wo(1, 4 + 2 * g, 2)
        def pa(i):
            b2 = i % 2
            pp = pf[b2]
            qai, kai = qa[b2], ka[b2]
            CPY(qai[:, :, 96:100], qal[:, i, g * 16:g * 16 + 16].rearrange("p (h c) -> p h c", c=4), ["const"], [("qa", b2)])
            CPY(kai[:, 64:100], kalp[:, i, :], ["const"], [("ka", b2)])
            for k in range(8):
                mm(pp[:, 0:384], hT[:, k, i * 128:(i + 1) * 128], wbuf[s][:, k, 0:384], k == 0, k == 7, [("hT", i), ("w", s)], [("pf", b2)])
            tf = tmpf[b2]
            head_rstd(pp, ("pf", b2), 320, tf, ("tf", b2), s8[b2], ("s8", b2))
            TT(qai[:, :, 0:64], pp[:, 0:256].rearrange("p (h d) -> p h d", d=64), s8[b2][:, 0:4].unsqueeze(2).to_broadcast([128, 4, 64]),
               ALU.mult, [("pf", b2), ("s8", b2)], [("qa", b2)])
            TS(kai[:, 0:64], pp[:, 256:320], s8[b2][:, 4:5], None, ALU.mult, None, [("pf", b2), ("s8", b2)], [("ka", b2)])
            act(Vt[:, i, 0:64], pp[:, 320:384], AF.Copy, [("pf", b2), "Vt"], [("V", i)])

        def pbk(i):
            b2 = i % 2
            qai, kai = qa[b2], ka[b2]
            pt = pb[b2]
            for h in range(4):
                tr(pt[:, h * 128:(h + 1) * 128], qai[:, h, :], [("qa", b2)], [("pb", b2)])
            tr(pt[:, 512:640], kai[:], [("ka", b2)], [("pb", b2)])
            act(QT[:, :, i * 128:(i + 1) * 128], pt[:, 0:512].rearrange("p (h t) -> p h t", h=4), AF.Copy, [("pb", b2), "gq"], [("QT", i)],
                scale=gq[:, 0:1])
            act(KT[:, i * 128:(i + 1) * 128], pt[:, 512:640], AF.Copy, [("pb", b2), "gq"], [("KT", i)], scale=gk[:, 0:1])

        lagged(NT, pa, pbk)
        for i in range(NT):
            b2 = 2 + i % 2
            pp = pf[b2]
            for k in range(8):
                mm(pp[:, 0:256], hT[:, k, i * 128:(i + 1) * 128], wbuf[s2][:, k, 0:256], k == 0, k == 7, [("hT", i), ("w", s2)], [("pf", b2)])
            act(szy[:, i, :], pp[:, 0:256], AF.Silu, [("pf", b2)], [("szy", i)])
        for r in range(4):
            def fin(i, Ob, okey, r=r):
                ri = (i + 4 * r) % 8
                rd = rden[ri]
                TS(rd[:], Ob[:, 64:65], esink[:, 4 * g + r:4 * g + r + 1], None, ALU.add, None, [okey, "esink"], [("rden", ri)])
                RCP(rd[:], rd[:], [("rden", ri)], [("rden", ri)])
                dst = szy[:, i, r * 64:(r + 1) * 64]
                STT(dst, Ob[:, 0:64], rd[:], dst, ALU.mult, ALU.mult, [okey, ("rden", ri), ("szy", i)], [("szy", i)])

            attention(QT[:, r, :], lambda kt: KT[:, kt * 128:(kt + 1) * 128], 128, lambda kt: Vt[:, kt, 0:65], 65,
                      plan_band(1), fin, lambda qc: [("QT", 4 * qc + u) for u in range(4)], lambda kt: [("KT", kt)],
                      lambda kt: [("V", kt)], ptb, "d")
        outproj_half(szy, yT, store=(g == 1 and stop is None))

    if 0 in layers:
        layer0()
    if 1 in layers:
        layer1()

    if not (1 in layers and stop is None):
        for i in range(NT):
            dma("sp", yv[:, i, :], x_sb[:, i, :], "out", reads=[("x", i)])
    P.finish("sp", list(P.chan_count.keys()))
    P.emit()
    es.close()
    return nc


_CACHE = {}


def kernel(**inputs):
    consts = _consts()
    shared = {}
    shared["norm_g"] = np.ascontiguousarray(inputs["norm_g"], dtype=np.float32)
    shared["w_out"] = np.ascontiguousarray(inputs["w_out"], dtype=np.float32)
    shared["e_w_in"] = np.ascontiguousarray(inputs["e_w_in"][0], dtype=np.float32)
    shared["a_conv_w"] = np.ascontiguousarray(inputs["a_conv_w"][0], dtype=np.float32)
    shared["a_conv_b"] = np.ascontiguousarray(inputs["a_conv_b"][0], dtype=np.float32)
    shared["a_ln_g"] = np.ascontiguousarray(inputs["a_ln_g"][0], dtype=np.float32)
    shared["a_ln_b"] = np.ascontiguousarray(inputs["a_ln_b"][0], dtype=np.float32)
    shared["b_qnorm_g"] = np.ascontiguousarray(inputs["b_qnorm_g"], dtype=np.float32)
    shared["b_knorm_g"] = np.ascontiguousarray(inputs["b_knorm_g"], dtype=np.float32)
    for k in ("ident", "tri", "up", "qal", "kal_moba", "kal_slc", "kal_plain", "kal_cmp", "cmaskneg", "ovl", "impc"):
        shared[k] = consts[k]
    shared["o_w_in"] = np.ascontiguousarray(inputs["o_w_in"][0], dtype=np.float32)
    for k in ("c_qnorm_g", "c_knorm_cmp_g", "c_knorm_slc_g", "c_knorm_win_g", "c_k_b2", "c_v_b2", "d_qnorm_g", "d_knorm_g", "d_sinks"):
        shared[k] = np.ascontiguousarray(inputs[k], dtype=np.float32).reshape(1, -1)
    for k in ("c_pos_k", "c_pos_v", "c_k_w1", "c_k_b1", "c_k_w2", "c_v_w1", "c_v_b1", "c_v_w2"):
        shared[k] = np.ascontiguousarray(inputs[k][0], dtype=np.float32)
    x = np.ascontiguousarray(inputs["x"], dtype=np.float32)
    nb = x.shape[0]
    layers = inputs.get("_layers", (0, 1))
    nc = build_program(layers, inputs.get("_stop"))
    in_maps = [dict(shared, x=x[b]) for b in range(nb)]
    res = run_bass_kernel_spmd(nc, in_maps, core_ids=list(range(nb)))
    return np.stack([np.asarray(r["y"], dtype=np.float32) for r in res.results], axis=0)
```

```python
import contextlib
import os
import numpy as np
import ml_dtypes
import concourse.bass as bass
import concourse.mybir as mybir
from concourse.bass_utils import run_bass_kernel_spmd

F32 = mybir.dt.float32
BF16 = mybir.dt.bfloat16
ALU = mybir.AluOpType
AF = mybir.ActivationFunctionType
AX = mybir.AxisListType

S = 2048
D = 1024
NT = 16
NEGM = -30000.0
EPS = 1e-6


class _Op:
    __slots__ = ("eng", "fn", "deps", "chan", "signal", "val", "dmaval", "chanseq")

    def __init__(self, eng, fn, chan):
        self.eng = eng
        self.fn = fn
        self.deps = {}
        self.chan = chan
        self.signal = False
        self.val = 0
        self.dmaval = None


class Prog:
    ENGS = ("pe", "act", "dve", "pool", "sp")

    def __init__(self, nc):
        self.nc = nc
        self.ops = {e: [] for e in self.ENGS}
        self.res = {}
        self.chan_count = {}
        self.chan_last = {}
        self.all_ops = []
        self.bar = {}
        self.final_waits = {}
        self.pool_ctr = 0
        self.sp_ctr = 0
        self.const_keys = []
        self.pres = {}

    PERS = ("w", "wo")

    def _st(self, k):
        if isinstance(k, tuple) and k[0] in self.PERS or k in self.PERS:
            return self.pres
        return self.res

    def op(self, eng, fn, reads=(), writes=(), chan=None, nobar=False):
        for w in writes:
            if isinstance(w, tuple) and w[0] == "ck" and w not in self.const_keys:
                self.const_keys.append(w)
        if "const" in reads:
            reads = [k for r in reads for k in (self.const_keys if r == "const" else (r,))]
        if chan is not None and eng == "pool":
            chan = "pq%d" % (self.pool_ctr % 12)
            self.pool_ctr += 1
        elif chan == "c":
            chan = "pc%d" % (self.sp_ctr % 16)
            self.sp_ctr += 1
        o = _Op(eng, fn, chan)
        deps = o.deps
        if not nobar:
            deps.update(self.bar)
        if chan is not None and (chan.startswith("pq") or chan.startswith("pc")):
            prev = self.chan_last.get(chan)
            if prev is not None:
                deps[id(prev)] = prev
        for k in reads:
            st = self._st(k).get(k)
            if st is not None and st[0] is not None:
                deps[id(st[0])] = st[0]
        for k in writes:
            st = self._st(k).get(k)
            if st is not None:
                if st[0] is not None:
                    deps[id(st[0])] = st[0]
                for r in st[1]:
                    deps[id(r)] = r
        for k in reads:
            res = self._st(k)
            st = res.get(k)
            if st is None:
                st = [None, []]
                res[k] = st
            st[1].append(o)
        for k in writes:
            self._st(k)[k] = [o, []]
        o.dmaval = dict(self.chan_count)
        if chan is not None:
            self.chan_count[chan] = self.chan_count.get(chan, 0) + 1
            o.chanseq = self.chan_count[chan]
            self.chan_last[chan] = o
            o.signal = True
        self.ops[eng].append(o)
        self.all_ops.append(o)
        return o

    def barrier(self):
        bar = {}
        for e in self.ENGS:
            if self.ops[e]:
                o = self.ops[e][-1]
                bar[id(o)] = o
        for ch, o in self.chan_last.items():
            bar[id(o)] = o
        self.bar = bar
        self.res = {}

    def finish(self, eng, chans):
        self.final_waits = {eng: {ch: self.chan_count[ch] for ch in chans}}

    def emit(self):
        nc = self.nc
        for o in self.all_ops:
            for d in o.deps.values():
                if d.chan is None:
                    if d.eng == "pe" and o.eng == "pe":
                        continue
                    d.signal = True
        for e in self.ENGS:
            c = 0
            for o in self.ops[e]:
                if o.chan is None and o.signal:
                    c += 1
                    o.val = c
        import os
        if os.environ.get("KDBG"):
            print("sem counts", {e: max([o.val for o in self.ops[e]] + [0]) for e in self.ENGS}, {e: len(self.ops[e]) for e in self.ENGS},
                  {c: 16 * v for c, v in self.chan_count.items()})
        stack = contextlib.ExitStack()
        sems = {}
        for e in self.ENGS:
            sems[e] = stack.enter_context(nc.semaphore("s_" + e))
        for ch in self.chan_count:
            sems["c_" + ch] = stack.enter_context(nc.semaphore("c_" + ch))
        block = stack.enter_context(nc.Block())
        engobj = {"pe": "tensor", "act": "scalar", "dve": "vector", "pool": "gpsimd", "sp": "sync"}

        def make(e):
            def body(eng):
                waited = {}
                for o in self.ops[e]:
                    need = {}
                    for d in o.deps.values():
                        if d.chan is not None:
                            k = "c_" + d.chan
                            if d.chan.startswith("pq") or d.chan.startswith("pc"):
                                v = 16 * d.chanseq
                            else:
                                v = 16 * o.dmaval[d.chan]
                        else:
                            if d.eng == "pe" and e == "pe":
                                continue
                            k = d.eng
                            v = d.val
                        if v > need.get(k, 0):
                            need[k] = v
                    for k, v in need.items():
                        if waited.get(k, 0) >= v:
                            continue
                        eng.wait_ge(sems[k], v)
                        waited[k] = v
                    ins = o.fn(eng)
                    if o.chan is not None:
                        ins.then_inc(sems["c_" + o.chan], 16)
                    elif o.signal:
                        ins.then_inc(sems[e], 1)
                for ch, c in self.final_waits.get(e, {}).items():
                    eng.wait_ge(sems["c_" + ch], 16 * c)
            return body

        for e in self.ENGS:
            getattr(block, engobj[e])(make(e))
        stack.close()


def _consts():
    c = {}
    c["ident"] = np.eye(128, dtype=np.float32)
    k = np.arange(128)[:, None]
    q = np.arange(128)[None, :]
    c["tri"] = np.where(k <= q, 0.0, NEGM).astype(np.float32)
    c["up"] = np.where(k > q, 0.0, NEGM).astype(np.float32)
    t = np.arange(S)
    b = (t % 16).astype(np.float32)
    a = (t - t % 16).astype(np.float32)
    slopes = np.power(2.0, -8.0 * np.arange(1, 9) / 8).astype(np.float32)
    qal = np.zeros((S, 8, 4), np.float32)
    qal[:, :, 0] = -slopes[None, :] * a[:, None]
    qal[:, :, 1] = -slopes[None, :] * b[:, None]
    qal[:, :, 2] = slopes[None, :]
    qal[:, :, 3] = slopes[None, :]
    c["qal"] = qal.reshape(NT, 128, 32).transpose(1, 0, 2).copy()

    def kal(onehot_block):
        m = np.zeros((S, 36), np.float32)
        if onehot_block:
            m[t, t // onehot_block] = 1.0
        m[:, 32] = 1.0
        m[:, 33] = 1.0
        m[:, 34] = a
        m[:, 35] = b
        return m.reshape(NT, 128, 36).transpose(1, 0, 2).copy()

    c["kal_moba"] = kal(256)
    c["kal_slc"] = kal(64)
    c["kal_plain"] = kal(0)
    cc = np.arange(128)
    cend = 16 * cc + 31
    kc = np.zeros((128, 36), np.float32)
    kc[:, 32] = 1.0
    kc[:, 33] = 1.0
    kc[:, 34] = cend - cend % 16
    kc[:, 35] = cend % 16
    c["kal_cmp"] = kc
    c["cmaskneg"] = np.where(t[None, :] >= cend[:, None], 0.0, NEGM).astype(np.float32)
    start = np.arange(127)[:, None] * 16
    bs = np.arange(32)[None, :] * 64
    ov = np.zeros((128, 32), np.float32)
    ov[:127] = ((start < bs + 64) & (start + 32 > bs)).astype(np.float32)
    c["ovl"] = ov
    blk = np.arange(32)[None, :]
    cur = (t // 64)[:, None]
    forced = (blk == 0) | (blk == cur) | (blk == cur - 1)
    impc = 1e4 * forced.astype(np.float32) - 1e5 * (blk > cur).astype(np.float32)
    c["impc"] = impc.reshape(NT, 128, 32).transpose(1, 0, 2).copy()
    return c


def build_program(layers=(0, 1), stop=None):
    nc = bass.Bass("TRN2", target_bir_lowering=False)
    es = contextlib.ExitStack()
    dram = {}

    def din(name, shape, dt=F32):
        dram[name] = nc.dram_tensor(name, list(shape), dt, kind="ExternalInput").ap()
        return dram[name]

    x_d = din("x", [S, D])
    normg_d = din("norm_g", [2, D])
    wout_d = din("w_out", [2, D, D])
    ewin_d = din("e_w_in", [D, 3584])
    convw_d = din("a_conv_w", [31, 512])
    convb_d = din("a_conv_b", [512])
    lng_d = din("a_ln_g", [512])
    lnb_d = din("a_ln_b", [512])
    bq_d = din("b_qnorm_g", [1, 64])
    bk_d = din("b_knorm_g", [1, 64])
    ident_d = din("ident", [128, 128])
    tri_d = din("tri", [128, 128])
    up_d = din("up", [128, 128])
    qal_d = din("qal", [128, NT, 32])
    kalm_d = din("kal_moba", [128, NT, 36])
    kals_d = din("kal_slc", [128, NT, 36])
    kalp_d = din("kal_plain", [128, NT, 36])
    kalc_d = din("kal_cmp", [128, 36])
    cmn_d = din("cmaskneg", [128, S])
    ovl_d = din("ovl", [128, 32])
    impc_d = din("impc", [128, NT, 32])
    owin_d = din("o_w_in", [D, 3096])
    cq_d = din("c_qnorm_g", [1, 64])
    ckc_d = din("c_knorm_cmp_g", [1, 64])
    cks_d = din("c_knorm_slc_g", [1, 64])
    ckw_d = din("c_knorm_win_g", [1, 64])
    cposk_d = din("c_pos_k", [32, 64])
    cposv_d = din("c_pos_v", [32, 64])
    ckw1_d = din("c_k_w1", [2048, 256])
    ckb1_d = din("c_k_b1", [256])
    ckw2_d = din("c_k_w2", [256, 64])
    ckb2_d = din("c_k_b2", [1, 64])
    cvw1_d = din("c_v_w1", [2048, 256])
    cvb1_d = din("c_v_b1", [256])
    cvw2_d = din("c_v_w2", [256, 64])
    cvb2_d = din("c_v_b2", [1, 64])
    dq_d = din("d_qnorm_g", [1, 64])
    dk_d = din("d_knorm_g", [1, 64])
    dsink_d = din("d_sinks", [1, 8])
    y_d = nc.dram_tensor("y", [S, D], F32, kind="ExternalOutput").ap()

    def sb(name, shape, dt):
        return es.enter_context(nc.sbuf_tensor(name, list(shape), dt))

    def psum(name, shape, dt):
        return es.enter_context(nc.psum_tensor(name, list(shape), dt))

    P = Prog(nc)

    x_sb = sb("x_sb", [128, NT, D], F32)
    hT = sb("hT", [128, 8, S], BF16)
    wbuf = [sb("wbuf%d" % i, [128, 8, 512], BF16) for i in range(2)]
    wo = sb("wo", [128, 4, D], BF16)
    ident = sb("ident_sb", [128, 128], BF16)
    identf = sb("identf_sb", [128, 128], F32)
    onesf = sb("onesf", [128, 128], F32)
    tri = sb("tri_sb", [128, 128], BF16)
    upm = sb("up_sb", [128, 128], BF16)
    qal = sb("qal_sb", [128, NT, 32], BF16)
    kalm = sb("kalm_sb", [128, NT, 36], BF16)
    KTc = sb("KTc", [128, 2, 128], BF16)
    Vca = sb("Vca", [128, 2, 98], BF16)
    kals = sb("kals_sb", [128, NT, 36], BF16)
    kalp = sb("kalp_sb", [128, NT, 36], BF16)
    ss = sb("ss", [128, NT], F32)
    rstd = sb("rstd", [128, NT], F32)
    ARENA = 80 * 1024
    arena = sb("arena", [128, ARENA // 2], BF16)

    pf = [psum("pf%d" % i, [128, 512], F32) for i in range(6)]
    pb = [psum("pb%d" % i, [128, 1024], BF16) for i in range(2)]

    class Carver:
        def __init__(self, base=0):
            self.off = base

        def get(self, shape, dt):
            n = int(np.prod(shape[1:]))
            nb = n * (4 if dt == F32 else 2)
            nb = (nb + 63) // 64 * 64
            assert self.off + nb <= ARENA, ("arena overflow", self.off + nb, ARENA)
            a = arena[:, self.off // 2:(self.off + nb) // 2]
            self.off += nb
            if os.environ.get("KDBG"):
                print("carve", shape, dt, "->", self.off)
            if dt == F32:
                a = a.bitcast(F32)
            a = a[:, 0:n]
            if len(shape) == 3:
                a = a.rearrange("p (a b) -> p a b", b=shape[2])
            elif len(shape) == 4:
                a = a.rearrange("p (a b c) -> p a b c", b=shape[2], c=shape[3])
            if shape[0] < 128:
                a = a[0:shape[0]]
            return a

    def dma(eng, out, in_, chan, reads=(), writes=(), slow=False, nobar=False):
        if slow:
            P.op(eng, lambda e: e.dma_start(out=out, in_=in_, allow_slow_non_contiguous=True), reads=reads, writes=writes, chan=chan, nobar=nobar)
        else:
            P.op(eng, lambda e: e.dma_start(out=out, in_=in_), reads=reads, writes=writes, chan=chan, nobar=nobar)

    def mm(out, lhsT, rhs, start, stop, reads, writes):
        P.op("pe", lambda e: e.matmul(out, lhsT=lhsT, rhs=rhs, start=start, stop=stop), reads=reads, writes=writes)

    def tr(out, in_, reads, writes, idn=None):
        ck = ("ck", "ident") if idn is None else ("ck", "identf")
        idn = ident[:] if idn is None else idn
        P.op("pe", lambda e: e.transpose(out=out, in_=in_, identity=idn), reads=list(reads) + [ck], writes=writes)

    def act(out, in_, func, reads, writes, **kw):
        P.op("act", lambda e: e.activation(out=out, in_=in_, func=func, **kw), reads=reads, writes=writes)

    def V(fn, reads, writes, eng="dve"):
        P.op(eng, fn, reads=reads, writes=writes)

    def TT(out, in0, in1, op, reads, writes, eng="dve"):
        P.op(eng, lambda e: e.tensor_tensor(out=out, in0=in0, in1=in1, op=op), reads=reads, writes=writes)

    def TS(out, in0, s1, s2, op0, op1, reads, writes, eng="dve"):
        if op1 is None:
            P.op(eng, lambda e: e.tensor_scalar(out=out, in0=in0, scalar1=s1, scalar2=None, op0=op0), reads=reads, writes=writes)
        else:
            P.op(eng, lambda e: e.tensor_scalar(out=out, in0=in0, scalar1=s1, scalar2=s2, op0=op0, op1=op1), reads=reads, writes=writes)

    def STT(out, in0, scalar, in1, op0, op1, reads, writes, eng="dve"):
        P.op(eng, lambda e: e.scalar_tensor_tensor(out=out, in0=in0, scalar=scalar, in1=in1, op0=op0, op1=op1), reads=reads, writes=writes)

    def RED(out, in_, op, reads, writes):
        P.op("dve", lambda e: e.tensor_reduce(out=out, in_=in_, axis=AX.X, op=op), reads=reads, writes=writes)

    def RCP(out, in_, reads, writes):
        P.op("dve", lambda e: e.reciprocal(out=out, in_=in_), reads=reads, writes=writes)

    def CPY(out, in_, reads, writes, eng="dve"):
        P.op(eng, lambda e: e.tensor_copy(out=out, in_=in_), reads=reads, writes=writes)

    def MSET(ap, val, writes, eng="dve"):
        P.op(eng, lambda e: e.memset(ap, val), reads=[], writes=writes)

    import os
    KB = os.environ.get("KBIS", "abcdefg")
    if "a" in KB:
        dma("pool", ident[:], ident_d, "c", writes=[("ck", "ident")])
    if "b" in KB:
        dma("pool", tri[:], tri_d, "c", writes=[("ck", "tri")])
        dma("pool", upm[:], up_d, "c", writes=[("ck", "up")])
    if "c" in KB:
        dma("pool", qal[:], qal_d, "c", writes=[("ck", "qal")])
    if "d" in KB:
        dma("pool", kalm[:], kalm_d, "c", writes=[("ck", "kalm")])
        dma("pool", kals[:], kals_d, "c", writes=[("ck", "kals")])
        dma("pool", kalp[:], kalp_d, "c", writes=[("ck", "kalp")])
    if "e" in KB:
        dma("sp", identf[:], ident_d, "c", writes=[("ck", "identf")])
    if "f" in KB:
        MSET(onesf[:], 1.0, [("ck", "onesf")])

    xv = x_d.rearrange("(i p) d -> p i d", p=128)
    yv = y_d.rearrange("(i p) d -> p i d", p=128)
    for i in range(NT):
        dma("sp", x_sb[:, i, :], xv[:, i, :], "x%d" % i, writes=[("x", i)])

    wslot_ctr = [0]

    def load_w(wd, segs):
        s = wslot_ctr[0] % 2
        wslot_ctr[0] += 1
        wv = wd.rearrange("(k p) n -> p k n", p=128)
        o = 0
        for (c0, n) in segs:
            dma("pool", wbuf[s][:, :, o:o + n], wv[:, :, c0:c0 + n], "w%d" % s, writes=[("w", s)], nobar=True)
            o += n
        return s

    def norm_phase(layer, base=0, barrier=True):
        cvn = Carver(base=base)
        gbc = cvn.get([128, D], F32)
        junk = cvn.get([128, D], BF16)
        hb = [cvn.get([128, D], BF16) for _ in range(2)]
        dma("act", gbc[:], normg_d[layer:layer + 1, :].partition_broadcast(128), "c", writes=["gbc"])
        MSET(ss[:], 0.0, [("ss", gI) for gI in range(4)])

        def stats(gI):
            for i in range(4 * gI, 4 * gI + 4):
                act(junk[:], x_sb[:, i, :], AF.Square, [("x", i), ("ss", gI)], ["junk", ("ss", gI)], accum_out=ss[:, i:i + 1])
            sl = slice(4 * gI, 4 * gI + 4)
            TS(rstd[:, sl], ss[:, sl], 1.0 / D, EPS, ALU.mult, ALU.add, [("ss", gI)], [("rstd", gI)])
            act(rstd[:, sl], rstd[:, sl], AF.Sqrt, [("rstd", gI)], [("rstd", gI)])
            RCP(rstd[:, sl], rstd[:, sl], [("rstd", gI)], [("rstd", gI)])

        def na(i):
            h = hb[i % 2]
            STT(h[:], x_sb[:, i, :], rstd[:, i:i + 1], gbc[:], ALU.mult, ALU.mult, [("x", i), ("rstd", i // 4), "gbc"], [("hb", i % 2)])

        def nb(i):
            h = hb[i % 2]
            pt = pb[i % 2]
            for k in range(8):
                tr(pt[:, k * 128:(k + 1) * 128], h[:, k * 128:(k + 1) * 128], [("hb", i % 2)], [("pb", i % 2)])
            act(hT[:, :, i * 128:(i + 1) * 128], pt[:].rearrange("p (k t) -> p k t", k=8), AF.Copy, [("pb", i % 2)], [("hT", i)])

        pend = []
        if layer == 0:
            for gI in range(4):
                stats(gI)
                for i in range(4 * gI, 4 * gI + 4):
                    na(i)
                    pend.append(i)
                    if len(pend) > 1:
                        nb(pend.pop(0))
        else:
            for gI in range(5):
                if gI < 4:
                    stats(gI)
                if gI >= 1:
                    for i in range(4 * (gI - 1), 4 * gI):
                        na(i)
                        pend.append(i)
                        if len(pend) > 1:
                            nb(pend.pop(0))
        nb(pend.pop(0))
        if barrier:
            P.barrier()

    def load_wo(layer, c0, n):
        wv = wout_d[layer].rearrange("(k p) n -> p k n", p=128)
        for hf in range(2):
            dma("pool", wo[:, 0:n, hf * 512:(hf + 1) * 512], wv[:, c0:c0 + n, hf * 512:(hf + 1) * 512], "wo", writes=["wo"], nobar=True)

    def outproj_tile(i, lhs_list, chunks, yT_reads, pbanks):
        for half in range(2):
            pp = pf[pbanks[half]]
            for n, (c, lhs) in enumerate(zip(chunks, lhs_list)):
                mm(pp[:], lhs, wo[:, c, half * 512:(half + 1) * 512], n == 0, n == len(chunks) - 1,
                   list(yT_reads) + ["wo"], [("pf", pbanks[half])])
            xs = x_sb[:, i, half * 512:(half + 1) * 512]
            TT(xs, xs, pp[:], ALU.add, [("pf", pbanks[half]), ("x", i)], [("x", i)])

    att_ctr = [0, 0]

    def lagged(n, stage_a, stage_b, lag=1):
        for i in range(n + lag):
            if i < n:
                stage_a(i)
            if i >= lag:
                stage_b(i - lag)

    def mm_acc(out, lhsT, rhs, start, stop, reads, writes):
        P.op("pe", lambda e: e.matmul(out, lhsT=lhsT, rhs=rhs, start=start, stop=stop, skip_group_check=True), reads=reads, writes=writes)

    def attention(QT, KT, nk, Vrhs, ncv, plan, finish, qkeys, kkeys, vkeys, ptb, tagk, addmask=None, finish_chunk=None):
        NS = len(ptb)
        work = []
        for qc in range(4):
            items = plan(qc)
            if not items:
                continue
            ob = 3 + att_ctr[1] % 3
            att_ctr[1] += 1
            lastk = {}
            for (kt, jlo, jhi, masks) in items:
                for j in range(jlo, jhi + 1):
                    lastk[j] = kt
            for n, it in enumerate(items):
                work.append((qc, ob, it, n == 0, n == len(items) - 1, lastk))

        def front(w):
            qc, ob, (kt, jlo, jhi, masks), first, last, lastk = w
            sbk = att_ctr[0] % NS
            att_ctr[0] += 1
            Sp = pf[sbk]
            pt = ptb[sbk]
            c0, c1 = jlo * 128, (jhi + 1) * 128
            extra = list(masks.items())
            mm_acc(Sp[0:nk, c0:c1], KT(kt), QT[:, qc * 512 + c0:qc * 512 + c1], True, addmask is None and not extra,
                   list(qkeys(qc)) + list(kkeys(kt)), [("pf", sbk)])
            if addmask is not None:
                mm_acc(Sp[0:nk, c0:c1], ident[0:nk, 0:nk], addmask[0:nk, qc * 512 + c0:qc * 512 + c1], False, not extra,
                       ["const", "addmask"], [("pf", sbk)])
            for n, (j, mk) in enumerate(extra):
                mm_acc(Sp[0:nk, j * 128:(j + 1) * 128], ident[0:nk, 0:nk], mk[0:nk, :], False, n == len(extra) - 1,
                       ["const"], [("pf", sbk)])
            act(pt[0:nk, c0:c1], Sp[0:nk, c0:c1], AF.Exp, [("pf", sbk)], [("pt", tagk, sbk)])
            return sbk

        def back(w, sbk, started):
            qc, ob, (kt, jlo, jhi, masks), first, last, lastk = w
            pt = ptb[sbk]
            for j in range(jlo, jhi + 1):
                mm_acc(pf[ob][:, j * 128:j * 128 + ncv], pt[0:nk, j * 128:(j + 1) * 128], Vrhs(kt), len(started) == 0, lastk[j] == kt,
                       [("pt", tagk, sbk)] + list(vkeys(kt)), [("pf", ob)])
                started.add(j)
            if last:
                if finish_chunk is not None:
                    finish_chunk(qc, pf[ob], ("pf", ob))
                else:
                    for j in sorted(started):
                        finish(qc * 4 + j, pf[ob][:, j * 128:j * 128 + ncv], ("pf", ob))
                started.clear()

        LAG = NS - 1
        started = set()
        pend = []
        for w in work:
            pend.append((w, front(w)))
            if len(pend) > LAG:
                pw, psb = pend.pop(0)
                back(pw, psb, started)
        for (pw, psb) in pend:
            back(pw, psb, started)

    def layer0():
        if stop == "load":
            return
        norm_phase(0, base=70 * 1024, barrier=False)
        if stop == "norm0":
            return
        load_wo(0, 0, 4)
        W = ewin_d
        if stop == "norm":
            return
        cv = Carver()
        yc = cv.get([128, 4, S], F32)
        hc = cv.get([128, S + 32], BF16)
        dg = cv.get([128, 31, 128], BF16)
        cw = cv.get([128, 4, 31], F32)
        cb = cv.get([128, 4], F32)
        lg = cv.get([128, 4], F32)
        lb = cv.get([128, 4], F32)
        Tsq = [cv.get([128, 512], F32) for _ in range(2)]
        mu2 = [cv.get([128, 512], F32) for _ in range(2)]
        msq = cv.get([128, 512], F32)
        rs2 = [cv.get([128, 512], F32) for _ in range(2)]
        sz4 = [cv.get([128, 512], BF16) for _ in range(4)]
        T4 = [cv.get([128, 512], F32) for _ in range(4)]
        yaT2 = [cv.get([128, 4, 512], BF16) for _ in range(2)]
        HO = 32
        stg = Tsq[0][0:32, :]
        stg2 = Tsq[1][0:12, 0:128]
        dma("sp", stg[0:31, :], convw_d, "c", writes=[("Tsq", 0)])
        dma("sp", stg2[0:4, :], convb_d.rearrange("(a c) -> a c", c=128), "c", writes=[("Tsq", 1)])
        dma("sp", stg2[4:8, :], lng_d.rearrange("(a c) -> a c", c=128), "c", writes=[("Tsq", 1)])
        dma("sp", stg2[8:12, :], lnb_d.rearrange("(a c) -> a c", c=128), "c", writes=[("Tsq", 1)])
        for cc in range(4):
            tr(pf[0][:, cc * 32:cc * 32 + 31], stg[0:31, cc * 128:(cc + 1) * 128], [("Tsq", 0)], [("pf", 0)], idn=identf[0:31, 0:31])
        tr(pf[0][:, 128:140], stg2[0:12, :], [("Tsq", 1)], [("pf", 0)], idn=identf[0:12, 0:12])
        CPY(cw[:], pf[0][:, 0:128].rearrange("p (a j) -> p a j", j=32)[:, :, 0:31], [("pf", 0)], ["cw"])
        CPY(cb[:], pf[0][:, 128:132], [("pf", 0)], ["cw"])
        CPY(lg[:], pf[0][:, 132:136], [("pf", 0)], ["cw"])
        CPY(lb[:], pf[0][:, 136:140], [("pf", 0)], ["cw"])
        MSET(hc[:, 0:HO], 0.0, ["hc0"])
        if stop == "convp":
            return
        for cc in range(4):
            if stop == "conv1" and cc == 1:
                return
            s = load_w(W, [(cc * 128, 128), (512 + cc * 128, 128)])
            for j in range(31):
                TS(dg[:, j, :], ident[:], cw[:, cc, j:j + 1], None, ALU.mult, None, ["const", "cw"], [("dg", j)])
            for tq in range(4):
                bv, bg = (tq % 2) * 2, (tq % 2) * 2 + 1
                for which, bk in ((0, bv), (1, bg)):
                    for k in range(8):
                        mm(pf[bk][:], wbuf[s][:, k, which * 128:(which + 1) * 128], hT[:, k, tq * 512:(tq + 1) * 512], k == 0, k == 7,
                           [("w", s)] + [("hT", 4 * tq + u) for u in range(4)], [("pf", bk)])
                act(T4[tq % 2][:], pf[bg][:], AF.Sigmoid, [("pf", bg)], [("T4", tq % 2)])
                TT(hc[:, HO + tq * 512:HO + (tq + 1) * 512], pf[bv][:], T4[tq % 2][:], ALU.mult, [("pf", bv), ("T4", tq % 2)], [("hc", tq)])
            for tq in range(4):
                pc = pf[4 + tq % 2]
                for j in range(31):
                    o = HO - 30 + j + tq * 512
                    mm(pc[:], dg[:, j, :], hc[:, o:o + 512], j == 0, j == 30,
                       [("dg", j), ("hc", tq)] + ([("hc", tq - 1)] if tq > 0 else ["hc0"]), [("pf", 4 + tq % 2)])
                act(yc[:, cc, tq * 512:(tq + 1) * 512], pc[:], AF.Identity, [("pf", 4 + tq % 2), "cw"], [("yc", cc, tq)],
                    bias=cb[:, cc:cc + 1], scale=1.0)
        if stop == "conv4":
            return
        P.barrier()
        sz_slot = load_w(W, [(1024, 512)])

        def st_a(tq):
            ts = slice(tq * 512, (tq + 1) * 512)
            p2 = tq % 2
            for cc in range(4):
                mm(pf[0][:], onesf[:], yc[:, cc, ts], cc == 0, cc == 3, [("yc", cc, tq), "const"], [("pf", 0)])
            for cc in range(4):
                act(Tsq[cc % 2][:], yc[:, cc, ts], AF.Square, [("yc", cc, tq)], [("Tsq", cc % 2)])
                mm(pf[1][:], onesf[:], Tsq[cc % 2][:], cc == 0, cc == 3, [("Tsq", cc % 2), "const"], [("pf", 1)])
            TS(mu2[p2][:], pf[0][:], 1.0 / 512, None, ALU.mult, None, [("pf", 0)], [("mu", p2)])
            TT(msq[:], mu2[p2][:], mu2[p2][:], ALU.mult, [("mu", p2)], ["msq"])
            STT(rs2[p2][:], pf[1][:], 1.0 / 512, msq[:], ALU.mult, ALU.subtract, [("pf", 1), "msq"], [("rs", p2)])
            TS(rs2[p2][:], rs2[p2][:], EPS, None, ALU.add, None, [("rs", p2)], [("rs", p2)])
            act(rs2[p2][:], rs2[p2][:], AF.Sqrt, [("rs", p2)], [("rs", p2)])
            RCP(rs2[p2][:], rs2[p2][:], [("rs", p2)], [("rs", p2)])

        def st_b(tq):
            ts = slice(tq * 512, (tq + 1) * 512)
            p2 = tq % 2
            for cc in range(4):
                pz = pf[2 + cc % 2]
                for k in range(8):
                    mm(pz[:], wbuf[sz_slot][:, k, cc * 128:(cc + 1) * 128], hT[:, k, ts], k == 0, k == 7,
                       [("w", sz_slot)] + [("hT", 4 * tq + u) for u in range(4)], [("pf", 2 + cc % 2)])
                act(sz4[cc][:], pz[:], AF.Silu, [("pf", 2 + cc % 2)], [("sz", cc)])
            for cc in range(4):
                tt = T4[cc]
                TT(tt[:], yc[:, cc, ts], mu2[p2][:], ALU.subtract, [("yc", cc, tq), ("mu", p2)], [("T4", cc)])
                TT(tt[:], tt[:], rs2[p2][:], ALU.mult, [("T4", cc), ("rs", p2)], [("T4", cc)])
            for cc in range(4):
                tt = T4[cc]
                act(tt[:], tt[:], AF.Silu, [("T4", cc), "cw"], [("T4", cc)], scale=lg[:, cc:cc + 1], bias=lb[:, cc:cc + 1])
            for cc in range(4):
                TT(yaT2[p2][:, cc, :], T4[cc][:], sz4[cc][:], ALU.mult, [("T4", cc), ("sz", cc)], [("yaT", p2, cc)])

        def st_c(tq):
            p2 = tq % 2
            for u in range(4):
                i = 4 * tq + u
                outproj_tile(i, [yaT2[p2][:, cc, u * 128:(u + 1) * 128] for cc in range(4)], [0, 1, 2, 3],
                             [("yaT", p2, cc) for cc in range(4)], (4, 5))

        for step in range(6):
            if step < 4:
                st_a(step)
            if 0 <= step - 1 < 4:
                st_b(step - 1)
            if step - 2 >= 0:
                st_c(step - 2)
        P.barrier()
        if stop == "conv":
            return
        for hh in range(2):
            moba_half(hh, W)
            P.barrier()
            if stop is not None:
                return

    def moba_half(hh, W):
        cv = Carver()
        QT = cv.get([128, 4, S], BF16)
        KT = cv.get([128, 4, S], BF16)
        Vt = cv.get([128, NT, 4, 66], BF16)
        szy = cv.get([128, NT, 256], BF16)
        ptb = [cv.get([128, 512], BF16) for _ in range(3)]
        gq = cv.get([128, 1], F32)
        gk = cv.get([128, 1], F32)
        kmf = cv.get([128, 4, 8], F32)
        kmb = cv.get([128, 4, 8], BF16)
        gs = cv.get([128, 4, 8], F32)
        cmp_ = cv.get([128, 4, 8, 8], F32)
        rank = cv.get([128, 4, 8], F32)
        nm = cv.get([128, 4, 32], BF16)
        rden = [cv.get([128, 1], F32) for _ in range(8)]
        rden4 = [cv.get([128, 4], F32) for _ in range(8)]
        tmpm = [cv.get([128, 4, 64], F32) for _ in range(2)]
        yT = [cv.get([128, 2, 128], BF16) for _ in range(2)]
        tmpf = [cv.get([128, 512], F32) for _ in range(2)]
        s8 = [cv.get([128, 8], F32) for _ in range(3)]
        qa = [cv.get([128, 4, 128], BF16) for _ in range(3)]
        ka = [cv.get([128, 4, 128], BF16) for _ in range(3)]
        c_q = 1536 + hh * 256
        c_k = 2048 + hh * 256
        c_v = 2560 + hh * 256
        c_z = 3072 + hh * 256
        s = load_w(W, [(c_q, 256), (c_k, 256)])
        s2 = load_w(W, [(c_v, 256), (c_z, 256)])
        load_wo(0, 4 + 2 * hh, 2)
        load_gain_col(gq, bq_d, 1.0)
        load_gain_col(gk, bk_d, 8.0)
        for b in range(3):
            MSET(qa[b][:, :, 64:128], 0.0, [("qa", b)])
            MSET(ka[b][:, :, 64:128], 0.0, [("ka", b)])
        MSET(Vt[:, :, :, 64:65], 1.0, ["Vt"])
        MSET(nm[:], 0.0, ["nm"])

        def m0(i):
            r = i % 3
            CPY(qa[r][:, :, 96:100], qal[:, i, hh * 16:hh * 16 + 16].rearrange("p (h c) -> p h c", c=4), ["const"], [("qa", r)])
            CPY(ka[r][:, :, 64:100], kalm[:, i, :].unsqueeze(1).to_broadcast([128, 4, 36]), ["const"], [("ka", r)])
            for k in range(8):
                mm(pf[r][:], hT[:, k, i * 128:(i + 1) * 128], wbuf[s][:, k, :], k == 0, k == 7, [("hT", i), ("w", s)], [("pf", r)])

        def m1(i):
            r = i % 3
            rstd_a(pf[r], ("pf", r), 512, tmpf[i % 2], ("tf", i % 2), s8[r], ("s8", r))

        def m2(i):
            r = i % 3
            rstd_b(512, s8[r], ("s8", r))
            TT(qa[r][:, :, 0:64], pf[r][:, 0:256].rearrange("p (h d) -> p h d", d=64), s8[r][:, 0:4].unsqueeze(2).to_broadcast([128, 4, 64]),
               ALU.mult, [("pf", r), ("s8", r)], [("qa", r)])
            TT(ka[r][:, :, 0:64], pf[r][:, 256:512].rearrange("p (h d) -> p h d", d=64), s8[r][:, 4:8].unsqueeze(2).to_broadcast([128, 4, 64]),
               ALU.mult, [("pf", r), ("s8", r)], [("ka", r)])

        def m3(i):
            r = i % 3
            b2 = i % 2
            pt = pb[b2]
            for h in range(4):
                tr(pt[:, h * 128:(h + 1) * 128], qa[r][:, h, :], [("qa", r)], [("pb", b2)])
            for h in range(4):
                tr(pt[:, (4 + h) * 128:(5 + h) * 128], ka[r][:, h, :], [("ka", r)], [("pb", b2)])
            act(QT[:, :, i * 128:(i + 1) * 128], pt[:, 0:512].rearrange("p (h t) -> p h t", h=4), AF.Copy, [("pb", b2), "gq"], [("QT", i)],
                scale=gq[:, 0:1])
            act(KT[:, :, i * 128:(i + 1) * 128], pt[:, 512:1024].rearrange("p (h t) -> p h t", h=4), AF.Copy, [("pb", b2), "gq"], [("KT", i)],
                scale=gk[:, 0:1])

        pipe4(NT, m0, m1, m2, m3)
        if hh == 1 and 1 in layers:
            prefetch_w1([("qa", r) for r in range(3)] + [("ka", r) for r in range(3)] + [("s8", r) for r in range(3)] + [("tf", 0), ("tf", 1)])
        if stop == "m1":
            return
        for i in range(NT):
            b2 = 2 + i % 2
            pp = pf[b2]
            for k in range(8):
                mm(pp[:], hT[:, k, i * 128:(i + 1) * 128], wbuf[s2][:, k, :], k == 0, k == 7, [("hT", i), ("w", s2)], [("pf", b2)])
            act(Vt[:, i, :, 0:64], pp[:, 0:256].rearrange("p (h d) -> p h d", d=64), AF.Copy, [("pf", b2), "Vt"], [("V", i)])
            act(szy[:, i, :], pp[:, 256:512], AF.Silu, [("pf", b2)], [("szy", i)])
        if stop == "m3":
            return
        for h in range(4):
            RED(kmf[:, h, :], KT[:, h, :].rearrange("p (n t) -> p n t", t=256), ALU.add, [("KT", i) for i in range(NT)], ["kmf"])
        TS(kmb[:], kmf[:], 1.0 / 256, None, ALU.mult, None, ["kmf"], ["kmb"])
        if stop == "mk":
            return
        for i in range(8, NT):
            npast = i // 2
            b2 = 4 + i % 2
            pg = pf[b2]
            for h in range(4):
                mm(pg[:, h * 8:h * 8 + 8], QT[0:64, h, i * 128:(i + 1) * 128], kmb[0:64, h, :], True, True, [("QT", i), "kmb"], [("pf", b2)])
            CPY(gs[:], pg[:, 0:32].rearrange("p (h n) -> p h n", n=8), [("pf", b2)], ["gs"])
            TT(cmp_[:, :, 0:npast, 0:npast], gs[:, :, 0:npast].unsqueeze(2).to_broadcast([128, 4, npast, npast]),
               gs[:, :, 0:npast].unsqueeze(3).to_broadcast([128, 4, npast, npast]), ALU.is_gt, ["gs"], ["cmp"])
            RED(rank[:, :, 0:npast], cmp_[:, :, 0:npast, 0:npast], ALU.add, ["cmp"], ["rank"])
            TS(nm[:, :, 0:npast], rank[:, :, 0:npast], 2.5, NEGM, ALU.is_ge, ALU.mult, ["rank"], ["nm"])
            pt = pb[i % 2]
            tr(pt[:, 0:128], nm[:].rearrange("p h n -> p (h n)"), ["nm"], [("pb", i % 2)])
            for h in range(4):
                act(QT[64:96, h, i * 128:(i + 1) * 128], pt[h * 32:(h + 1) * 32, 0:128], AF.Copy, [("pb", i % 2)], [("QT", i)])

        if stop == "m2":
            return

        def plan(qc):
            items = []
            for kt in range(4 * qc + 4):
                if kt < 4 * qc:
                    items.append((kt, 0, 3, {}))
                else:
                    m = kt - 4 * qc
                    items.append((kt, m, 3, {m: tri[:]}))
            return items

        for h in range(4):
            def finish_chunk(qc, Obank, okey, h=h):
                ri = (qc + 4 * h) % 8
                rd = rden4[ri]
                tb = ri % 2
                Ov = Obank[:].rearrange("p (j c) -> p j c", c=128)
                tl = slice(4 * qc, 4 * qc + 4)
                sk_ = [("szy", i) for i in range(4 * qc, 4 * qc + 4)]
                RCP(rd[:], Ov[:, :, 64:65].rearrange("p j c -> p (j c)"), [okey], [("rden4", ri)])
                TT(tmpm[tb][:], Ov[:, :, 0:64], rd[:].unsqueeze(2).to_broadcast([128, 4, 64]), ALU.mult, [okey, ("rden4", ri)], [("tmpm", tb)])
                dst = szy[:, tl, h * 64:(h + 1) * 64]
                TT(dst, dst, tmpm[tb][:], ALU.mult, [("tmpm", tb)] + sk_, sk_)

            attention(QT[:, h, :], lambda kt, h=h: KT[:, h, kt * 128:(kt + 1) * 128], 128,
                      lambda kt, h=h: Vt[:, kt, h, 0:65], 65, plan, None,
                      lambda qc: [("QT", 4 * qc + u) for u in range(4)], lambda kt: [("KT", kt)], lambda kt: [("V", kt)],
                      ptb, "m", finish_chunk=finish_chunk)
        if stop == "ma":
            return
        outproj_half(szy, yT)

    def rstd_a(pp, pkey, ncol, tf, tfkey, s8i, s8key):
        nh = ncol // 64
        act(tf[:, 0:ncol], pp[:, 0:ncol], AF.Square, [pkey], [tfkey])
        RED(s8i[:, 0:nh], tf[:, 0:ncol].rearrange("p (h d) -> p h d", d=64), ALU.add, [tfkey], [s8key])
        TS(s8i[:, 0:nh], s8i[:, 0:nh], 64.0 * EPS, None, ALU.add, None, [s8key], [s8key])
        act(s8i[:, 0:nh], s8i[:, 0:nh], AF.Sqrt, [s8key], [s8key])

    def rstd_b(ncol, s8i, s8key):
        nh = ncol // 64
        RCP(s8i[:, 0:nh], s8i[:, 0:nh], [s8key], [s8key])

    def head_rstd(pp, pkey, ncol, tf, tfkey, s8i, s8key):
        rstd_a(pp, pkey, ncol, tf, tfkey, s8i, s8key)
        rstd_b(ncol, s8i, s8key)

    def pipe4(n, s0, s1, s2, s3):
        for step in range(n + 2):
            if step < n:
                s0(step)
                s1(step)
            if 1 <= step <= n:
                s2(step - 1)
            if step >= 2:
                s3(step - 2)

    def load_gain_col(dst, gd, mult):
        MSET(dst[:], 1.0, ["gq"])
        dma("sp", dst[0:64, :], gd.rearrange("o d -> d o"), "c", writes=["gq"], slow=True)
        if mult != 1.0:
            TS(dst[0:64, :], dst[0:64, :], mult, None, ALU.mult, None, ["gq"], ["gq"])

    def outproj_half(szy, yT, store=False):
        def oa(i):
            pt = pb[i % 2]
            for c in range(2):
                tr(pt[:, c * 128:(c + 1) * 128], szy[:, i, c * 128:(c + 1) * 128], [("szy", i)], [("pb", i % 2)])
            y = yT[i % 2]
            act(y[:], pt[:, 0:256].rearrange("p (c t) -> p c t", c=2), AF.Copy, [("pb", i % 2)], [("yT", i % 2)])

        def ob(i):
            y = yT[i % 2]
            outproj_tile(i, [y[:, 0, :], y[:, 1, :]], [0, 1], [("yT", i % 2)], (4, 5))
            if store:
                dma("sp", yv[:, i, :], x_sb[:, i, :], "out", reads=[("x", i)])

        lagged(NT, oa, ob)

    def plan_causal(qc):
        items = []
        for kt in range(4 * qc + 4):
            if kt < 4 * qc:
                items.append((kt, 0, 3, {}))
            else:
                m = kt - 4 * qc
                items.append((kt, m, 3, {m: tri[:]}))
        return items

    def plan_band(wt):
        def plan(qc):
            items = []
            for kt in range(max(0, 4 * qc - wt), 4 * qc + 4):
                jlo = max(0, kt - 4 * qc)
                jhi = min(3, kt + wt - 4 * qc)
                if jlo > jhi:
                    continue
                masks = {}
                if 0 <= kt - 4 * qc <= 3:
                    masks[kt - 4 * qc] = tri[:]
                if 0 <= kt + wt - 4 * qc <= 3:
                    masks[kt + wt - 4 * qc] = upm[:]
                items.append((kt, jlo, jhi, masks))
            return items
        return plan

    W1_OFF = 64 * 1024

    def prefetch_w1(war_keys=()):
        cvp = Carver(base=W1_OFF)
        wA = cvp.get([128, 32, 256], BF16)
        for kv, wd in enumerate((ckw1_d, cvw1_d)):
            wv = wd.rearrange("(l d) j -> d l j", d=64)
            for lq in range(4):
                dma("pool", wA[kv * 64:(kv + 1) * 64, lq * 8:(lq + 1) * 8, :], wv[:, lq * 8:(lq + 1) * 8, :], "w1", writes=["w1A"] + list(war_keys))
        return wA

    def layer1():
        W = owin_d
        wA = Carver(base=W1_OFF).get([128, 32, 256], BF16)
        if 0 not in layers:
            prefetch_w1()
        cvc = Carver(base=16 * 1024)
        wB = cvc.get([128, 32, 256], BF16)
        dma("sp", wB[64:128, :, :], wA[0:64, :, :], "c", writes=["w1B"])
        dma("sp", wB[0:64, :, :], wA[64:128, :, :], "c", writes=["w1B"])
        w1 = [[wA, wB], [wB, wA]]
        B = compress_loads(W, cvc)
        norm_phase(1)
        compress_stage(W, B, w1)
        P.barrier()
        if stop == "cmp":
            return
        for g in range(2):
            nsa_half(g, W)
            P.barrier()
            if stop == "nsa0":
                return
        if stop == "nsa":
            return
        for g in range(2):
            swa_half(g, W)
            P.barrier()

    def compress_loads(W, cv):
        s = load_w(W, [(512, 128), (640, 128)])
        B = {}
        B["s"] = s
        B["KVD"] = [cv.get([128, 16, 128], BF16) for _ in range(2)]
        B["w2"] = [cv.get([128, 2, 64], BF16) for _ in range(2)]
        B["posn"] = cv.get([32, 2, 64], F32)
        B["posT"] = cv.get([64, 2, 32], BF16)
        B["stgb"] = cv.get([4, 128], F32)
        B["b1sb"] = cv.get([128, 4], F32)
        B["biasj"] = cv.get([128, 4], F32)
        B["b2bc"] = cv.get([128, 2, 64], F32)
        B["gcm"] = cv.get([128, 64], F32)
        B["kalc"] = cv.get([128, 36], BF16)
        B["hid"] = [cv.get([128, 2, 128], BF16) for _ in range(4)]
        B["kcf"] = cv.get([128, 64], F32)
        B["junk2"] = cv.get([128, 64], F32)
        B["ssc"] = cv.get([128, 1], F32)
        B["kaug"] = cv.get([128, 128], BF16)
        for kv, wd in enumerate((ckw2_d, cvw2_d)):
            dma("pool", B["w2"][kv][:], wd.rearrange("(c p) d -> p c d", p=128), "w2", writes=["w2"])
        dma("sp", B["posn"][:, 0, :], cposk_d, "c", writes=["posn"])
        dma("sp", B["posn"][:, 1, :], cposv_d, "c", writes=["posn"])
        dma("sp", B["stgb"][0:2, :], ckb1_d.rearrange("(a c) -> a c", c=128), "c", writes=["stgb"])
        dma("sp", B["stgb"][2:4, :], cvb1_d.rearrange("(a c) -> a c", c=128), "c", writes=["stgb"])
        dma("sp", B["b2bc"][:, 0, :], ckb2_d.partition_broadcast(128), "c", writes=["b2bc"])
        dma("sp", B["b2bc"][:, 1, :], cvb2_d.partition_broadcast(128), "c", writes=["b2bc"])
        dma("sp", B["gcm"][:], ckc_d.partition_broadcast(128), "c", writes=["gcm"])
        dma("pool", B["kalc"][:], kalc_d, "c", writes=["kalc"])
        for g in range(2):
            dma("pool", Vca[:, g, 65:97], ovl_d, "c", writes=[("Vca", g)])
        return B

    def compress_stage(W, B, w1):
        s = B["s"]
        KVD, w2, posn, posT, stgb, b1sb, biasj = B["KVD"], B["w2"], B["posn"], B["posT"], B["stgb"], B["b1sb"], B["biasj"]
        b2bc, gcm, kalc, hid, kcf, junk2, ssc, kaug = B["b2bc"], B["gcm"], B["kalc"], B["hid"], B["kcf"], B["junk2"], B["ssc"], B["kaug"]
        for g in range(2):
            MSET(Vca[:, g, 64:65], 1.0, [("Vca", g)])
        MSET(kaug[:], 0.0, ["kaug"])
        for kv in range(2):
            tr(pf[0][0:64, kv * 32:(kv + 1) * 32], posn[:, kv, :], ["posn"], [("pf", 0)], idn=identf[0:32, 0:32])
        tr(pf[0][:, 64:68], stgb[:], ["stgb"], [("pf", 0)], idn=identf[0:4, 0:4])
        CPY(posT[:], pf[0][0:64, 0:64].rearrange("p (k l) -> p k l", l=32), [("pf", 0)], ["posT"])
        CPY(b1sb[:], pf[0][:, 64:68], [("pf", 0)], ["b1sb"])
        for kv in range(2):
            for jc in range(2):
                col = kv * 2 + jc
                for l in range(32):
                    mm(pf[1][:, col:col + 1], w1[kv][0][0:64, l, jc * 128:(jc + 1) * 128], posT[0:64, kv, l:l + 1], l == 0, l == 31,
                       ["w1B", "posT"], [("pf", 1)])
        TT(biasj[:], pf[1][:, 0:4], b1sb[:], ALU.add, [("pf", 1), "b1sb"], ["biasj"])
        for tq in range(4):
            for which in range(2):
                pp = pf[2 + which]
                for k in range(8):
                    mm(pp[:], wbuf[s][:, k, which * 128:(which + 1) * 128], hT[:, k, tq * 512:(tq + 1) * 512], k == 0, k == 7,
                       [("w", s)] + [("hT", 4 * tq + u) for u in range(4)], [("pf", 2 + which)])
                act(KVD[which][:, :, tq * 32:(tq + 1) * 32].rearrange("p r m -> p m r"), pp[:].rearrange("p (m r) -> p m r", r=16),
                    AF.Copy, [("pf", 2 + which)], [("KVT", which)])
        n = 0
        for kv in range(2):
            for g in range(2):
                hb_ = hid[kv * 2 + g]
                for jc in range(2):
                    pp = pf[n % 2]
                    for l in range(32):
                        mm(pp[:, 0:127], w1[kv][g][g * 64:(g + 1) * 64, l, jc * 128:(jc + 1) * 128],
                           KVD[kv][g * 64:(g + 1) * 64, l % 16, (l // 16):(l // 16) + 127], l == 0, l == 31,
                           ["w1B", ("KVT", kv)], [("pf", n % 2)])
                    act(hb_[:, jc, 0:127], pp[:, 0:127], AF.Silu, [("pf", n % 2), "biasj"], [("hid", kv * 2 + g)],
                        bias=biasj[:, kv * 2 + jc:kv * 2 + jc + 1], scale=1.0)
                    n += 1
                po = pf[2 + g]
                for jc in range(2):
                    mm(po[0:127, 0:64], hb_[:, jc, 0:127], w2[kv][:, jc, :], jc == 0, jc == 1, [("hid", kv * 2 + g), "w2"], [("pf", 2 + g)])
                if kv == 0:
                    TT(kcf[0:127, :], po[0:127, 0:64], b2bc[0:127, 0, :], ALU.add, [("pf", 2 + g), "b2bc"], ["kcf"])
                    act(junk2[0:127, :], kcf[0:127, :], AF.Square, ["kcf"], ["junk2", "ssc"], accum_out=ssc[0:127, :])
                    TS(ssc[0:127, :], ssc[0:127, :], 1.0 / 64, EPS, ALU.mult, ALU.add, ["ssc"], ["ssc"])
                    act(ssc[0:127, :], ssc[0:127, :], AF.Sqrt, ["ssc"], ["ssc"])
                    RCP(ssc[0:127, :], ssc[0:127, :], ["ssc"], ["ssc"])
                    STT(kaug[0:127, 0:64], kcf[0:127, :], ssc[0:127, :], gcm[0:127, :], ALU.mult, ALU.mult, ["kcf", "ssc", "gcm"], ["kaug"])
                    CPY(kaug[:, 64:100], kalc[:], ["kalc"], ["kaug"])
                    tr(pb[g][:, 0:128], kaug[:], ["kaug"], [("pb", g)])
                    act(KTc[:, g, :], pb[g][:, 0:128], AF.Copy, [("pb", g)], [("KTc", g)])
                else:
                    TT(Vca[0:127, g, 0:64], po[0:127, 0:64], b2bc[0:127, 1, :], ALU.add, [("pf", 2 + g), "b2bc"], [("Vca", g)])

    def nsa_half(g, W):
        cv = Carver()
        QT = cv.get([128, 4, S], BF16)
        KTs = cv.get([128, S], BF16)
        KTw = cv.get([128, S], BF16)
        Vs = cv.get([128, NT, 66], BF16)
        Vw = cv.get([128, NT, 66], BF16)
        szy = cv.get([128, NT, 256], BF16)
        gates = cv.get([128, NT, 12], F32)
        oacc = cv.get([128, NT, 256], F32)
        imp = cv.get([128, 8, 32], F32)
        impc = cv.get([128, 8, 32], F32)
        cmn = cv.get([128, S], BF16)
        qa = [cv.get([128, 4, 128], BF16) for _ in range(3)]
        ksa = [cv.get([128, 128], BF16) for _ in range(3)]
        kwa = [cv.get([128, 128], BF16) for _ in range(3)]
        ptb = [cv.get([128, 512], BF16) for _ in range(3)]
        tmpf = [cv.get([128, 512], F32) for _ in range(2)]
        cmpb = cv.get([128, 32, 32], F32)
        rank = cv.get([128, 32], F32)
        nmt = cv.get([128, 128], BF16)
        s8 = [cv.get([128, 8], F32) for _ in range(3)]
        gq = cv.get([128, 1], F32)
        gks = cv.get([128, 1], F32)
        gkw = cv.get([128, 1], F32)
        rden = [cv.get([128, 1], F32) for _ in range(8)]
        rden4 = [cv.get([128, 4], F32) for _ in range(8)]
        tmp32 = cv.get([128, 4, 32], F32)
        yT = [cv.get([128, 2, 128], BF16) for _ in range(2)]
        s = load_w(W, [(g * 256, 256), (768 + 64 * g, 64), (1024 + 64 * g, 64), (896 + 64 * g, 64), (1152 + 64 * g, 64)])
        s2 = load_w(W, [(1304 + g * 256, 256), (1280 + 4 * g, 4), (1288 + 4 * g, 4), (1296 + 4 * g, 4)])
        load_wo(1, 2 * g, 2)
        load_gain_col(gq, cq_d, 1.0)
        load_gain_col(gks, cks_d, 8.0)
        load_gain_col(gkw, ckw_d, 8.0)
        dma("sp", impc[:], impc_d[:, 8:16, :], "c", writes=["impc"])
        dma("pool", cmn[:], cmn_d, "c", writes=["addmask"])
        for b in range(3):
            MSET(qa[b][:, :, 64:128], 0.0, [("qa", b)])
            MSET(ksa[b][:, 64:128], 0.0, [("ksa", b)])
            MSET(kwa[b][:, 64:128], 0.0, [("kwa", b)])
        MSET(Vs[:, :, 64:65], 1.0, ["Vs"])
        MSET(Vw[:, :, 64:65], 1.0, ["Vw"])
        MSET(nmt[:], 0.0, ["nmt"])
        def pa(i):
            b2 = i % 2
            pp = pf[b2]
            qai, ksi, kwi = qa[b2], ksa[b2], kwa[b2]
            CPY(qai[:, :, 96:100], qal[:, i, g * 16:g * 16 + 16].rearrange("p (h c) -> p h c", c=4), ["const"], [("qa", b2)])
            CPY(ksi[:, 64:100], kals[:, i, :], ["const"], [("ksa", b2)])
            CPY(kwi[:, 64:100], kalp[:, i, :], ["const"], [("kwa", b2)])
            for k in range(8):
                mm(pp[:], hT[:, k, i * 128:(i + 1) * 128], wbuf[s][:, k, :], k == 0, k == 7, [("hT", i), ("w", s)], [("pf", b2)])
            tf = tmpf[b2]
            head_rstd(pp, ("pf", b2), 384, tf, ("tf", b2), s8[b2], ("s8", b2))
            TT(qai[:, :, 0:64], pp[:, 0:256].rearrange("p (h d) -> p h d", d=64), s8[b2][:, 0:4].unsqueeze(2).to_broadcast([128, 4, 64]),
               ALU.mult, [("pf", b2), ("s8", b2)], [("qa", b2)])
            TS(ksi[:, 0:64], pp[:, 256:320], s8[b2][:, 4:5], None, ALU.mult, None, [("pf", b2), ("s8", b2)], [("ksa", b2)])
            TS(kwi[:, 0:64], pp[:, 320:384], s8[b2][:, 5:6], None, ALU.mult, None, [("pf", b2), ("s8", b2)], [("kwa", b2)])
            act(Vs[:, i, 0:64], pp[:, 384:448], AF.Copy, [("pf", b2), "Vs"], [("Vs", i)])
            act(Vw[:, i, 0:64], pp[:, 448:512], AF.Copy, [("pf", b2), "Vw"], [("Vw", i)])

        def pbk(i):
            b2 = i % 2
            qai, ksi, kwi = qa[b2], ksa[b2], kwa[b2]
            pt = pb[b2]
            for h in range(4):
                tr(pt[:, h * 128:(h + 1) * 128], qai[:, h, :], [("qa", b2)], [("pb", b2)])
            tr(pt[:, 512:640], ksi[:], [("ksa", b2)], [("pb", b2)])
            tr(pt[:, 640:768], kwi[:], [("kwa", b2)], [("pb", b2)])
            act(QT[:, :, i * 128:(i + 1) * 128], pt[:, 0:512].rearrange("p (h t) -> p h t", h=4), AF.Copy, [("pb", b2), "gq"], [("QT", i)],
                scale=gq[:, 0:1])
            act(KTs[:, i * 128:(i + 1) * 128], pt[:, 512:640], AF.Copy, [("pb", b2), "gq"], [("KTs", i)], scale=gks[:, 0:1])
            act(KTw[:, i * 128:(i + 1) * 128], pt[:, 640:768], AF.Copy, [("pb", b2), "gq"], [("KTw", i)], scale=gkw[:, 0:1])

        lagged(NT, pa, pbk)
        for i in range(NT):
            b2 = 2 + i % 2
            pp = pf[b2]
            for k in range(8):
                mm(pp[:, 0:256], hT[:, k, i * 128:(i + 1) * 128], wbuf[s2][:, k, 0:256], k == 0, k == 7, [("hT", i), ("w", s2)], [("pf", b2)])
            act(szy[:, i, :], pp[:, 0:256], AF.Silu, [("pf", b2)], [("szy", i)])
        for i in range(NT):
            b2 = 2 + i % 2
            pp = pf[b2]
            for k in range(8):
                mm(pp[:, 0:12], hT[:, k, i * 128:(i + 1) * 128], wbuf[s2][:, k, 256:268], k == 0, k == 7, [("hT", i), ("w", s2)], [("pf", b2)])
            act(gates[:, i, :], pp[:, 0:12], AF.Sigmoid, [("pf", b2)], [("gates", i)])
        if stop == "nsaproj":
            return
        qk = lambda qc: [("QT", 4 * qc + u) for u in range(4)]
        for r in range(4):
            def fin_cmp_chunk(qc, Obank, okey, r=r):
                ri = (qc + 4 * r) % 8
                rd = rden4[ri]
                Ov = Obank[:].rearrange("p (j c) -> p j c", c=128)
                tl = slice(4 * qc, 4 * qc + 4)
                gk_ = [("gates", i) for i in range(4 * qc, 4 * qc + 4)]
                ok_ = [("oacc", i) for i in range(4 * qc, 4 * qc + 4)]
                TS(rd[:], Ov[:, :, 64:65].rearrange("p j c -> p (j c)"), 1e-30, None, ALU.max, None, [okey], [("rden4", ri)])
                RCP(rd[:], rd[:], [("rden4", ri)], [("rden4", ri)])
                if qc >= 2:
                    ik_ = [("imp", i) for i in range(4 * qc, 4 * qc + 4)]
                    dst = imp[:, 4 * qc - 8:4 * qc - 4, :]
                    if r == 0:
                        TT(dst, Ov[:, :, 65:97], rd[:].unsqueeze(2).to_broadcast([128, 4, 32]), ALU.mult, [okey, ("rden4", ri)], ik_)
                    else:
                        TT(tmp32[:], Ov[:, :, 65:97], rd[:].unsqueeze(2).to_broadcast([128, 4, 32]), ALU.mult, [okey, ("rden4", ri)], ["tmp32"])
                        TT(dst, dst, tmp32[:], ALU.add, ["tmp32"] + ik_, ik_)
                TT(rd[:], rd[:], gates[:, tl, r:r + 1].rearrange("p j c -> p (j c)"), ALU.mult, [("rden4", ri)] + gk_, [("rden4", ri)])
                TT(oacc[:, tl, r * 64:(r + 1) * 64], Ov[:, :, 0:64], rd[:].unsqueeze(2).to_broadcast([128, 4, 64]), ALU.mult,
                   [okey, ("rden4", ri)], ok_)

            attention(QT[:, r, :], lambda kt: KTc[:, g, 0:127], 127, lambda kt: Vca[0:127, g, 0:97], 97,
                      lambda qc: [(0, 0, 3, {})], None, qk, lambda kt: [("KTc", g)], lambda kt: [("Vca", g)], ptb, "n", addmask=cmn,
                      finish_chunk=fin_cmp_chunk)
        if stop == "nsacmp":
            return
        for i in range(8, NT):
            im = imp[:, i - 8, :]
            TT(im, im, impc[:, i - 8, :], ALU.add, [("imp", i), "impc"], [("imp", i)])
            TT(cmpb[:], im.unsqueeze(1).to_broadcast([128, 32, 32]), im.unsqueeze(2).to_broadcast([128, 32, 32]), ALU.is_gt,
               [("imp", i)], ["cmpb"])
            RED(rank[:], cmpb[:], ALU.add, ["cmpb"], ["rank"])
            TS(nmt[:, 0:32], rank[:], 15.5, NEGM, ALU.is_ge, ALU.mult, ["rank"], ["nmt"])
            pt = pb[i % 2]
            tr(pt[:, 0:128], nmt[:], ["nmt"], [("pb", i % 2)])
            for r in range(4):
                act(QT[64:96, r, i * 128:(i + 1) * 128], pt[0:32, 0:128], AF.Copy, [("pb", i % 2)], [("QT", i)])
        for (KTx, Vx, kname, vname, plan, gcol) in ((KTs, Vs, "KTs", "Vs", plan_causal, 4), (KTw, Vw, "KTw", "Vw", plan_band(4), 8)):
            for r in range(4):
                def fin_add_chunk(qc, Obank, okey, r=r, gcol=gcol):
                    ri = (qc + 4 * r) % 8
                    rd = rden4[ri]
                    tb = ri % 2
                    tmp = tmpf[tb][:, 0:256].rearrange("p (j c) -> p j c", c=64)
                    Ov = Obank[:].rearrange("p (j c) -> p j c", c=128)
                    tl = slice(4 * qc, 4 * qc + 4)
                    gk_ = [("gates", i) for i in range(4 * qc, 4 * qc + 4)]
                    ok_ = [("oacc", i) for i in range(4 * qc, 4 * qc + 4)]
                    RCP(rd[:], Ov[:, :, 64:65].rearrange("p j c -> p (j c)"), [okey], [("rden4", ri)])
                    TT(rd[:], rd[:], gates[:, tl, gcol + r:gcol + r + 1].rearrange("p j c -> p (j c)"), ALU.mult,
                       [("rden4", ri)] + gk_, [("rden4", ri)])
                    TT(tmp, Ov[:, :, 0:64], rd[:].unsqueeze(2).to_broadcast([128, 4, 64]), ALU.mult, [okey, ("rden4", ri)], [("tf", tb)])
                    dst = oacc[:, tl, r * 64:(r + 1) * 64]
                    TT(dst, dst, tmp, ALU.add, [("tf", tb)] + ok_, ok_)

                attention(QT[:, r, :], lambda kt, KTx=KTx: KTx[:, kt * 128:(kt + 1) * 128], 128,
                          lambda kt, Vx=Vx: Vx[:, kt, 0:65], 65, plan, None, qk,
                          lambda kt, kname=kname: [(kname, kt)], lambda kt, vname=vname: [(vname, kt)], ptb, "n",
                          finish_chunk=fin_add_chunk)
        for i in range(NT):
            TT(szy[:, i, :], oacc[:, i, :], szy[:, i, :], ALU.mult, [("oacc", i), ("szy", i)], [("szy", i)])
        outproj_half(szy, yT)

    def swa_half(g, W):
        cv = Carver()
        QT = cv.get([128, 4, S], BF16)
        KT = cv.get([128, S], BF16)
        Vt = cv.get([128, NT, 66], BF16)
        szy = cv.get([128, NT, 256], BF16)
        qa = [cv.get([128, 4, 128], BF16) for _ in range(3)]
        ka = [cv.get([128, 128], BF16) for _ in range(3)]
        ptb = [cv.get([128, 512], BF16) for _ in range(3)]
        tmpf = [cv.get([128, 512], F32) for _ in range(2)]
        s8 = [cv.get([128, 8], F32) for _ in range(3)]
        gq = cv.get([128, 1], F32)
        gk = cv.get([128, 1], F32)
        esink = cv.get([128, 8], F32)
        rden = [cv.get([128, 1], F32) for _ in range(8)]
        rden4 = [cv.get([128, 4], F32) for _ in range(8)]
        yT = [cv.get([128, 2, 128], BF16) for _ in range(2)]
        load_gain_col(gq, dq_d, 1.0)
        load_gain_col(gk, dk_d, 8.0)
        dma("sp", esink[:], dsink_d.partition_broadcast(128), "c", writes=["esink"])
        act(esink[:], esink[:], AF.Exp, ["esink"], ["esink"])
        for b in range(3):
            MSET(qa[b][:, :, 64:128], 0.0, [("qa", b)])
            MSET(ka[b][:, 64:128], 0.0, [("ka", b)])
        MSET(Vt[:, :, 64:65], 1.0, ["Vt"])
        s = load_w(W, [(1816 + g * 256, 256), (2328 + 64 * g, 64), (2456 + 64 * g, 64)])
        s2 = load_w(W, [(2584 + g * 256, 256)])
        load_wo(1, 4 + 2 * g, 2)
        def pa(i):
            b2 = i % 2
            pp = pf[b2]
            qai, kai = qa[b2], ka[b2]
            CPY(qai[:, :, 96:100], qal[:, i, g * 16:g * 16 + 16].rearrange("p (h c) -> p h c", c=4), ["const"], [("qa", b2)])
            CPY(kai[:, 64:100], kalp[:, i, :], ["const"], [("ka", b2)])
            for k in range(8):
                mm(pp[:, 0:384], hT[:, k, i * 128:(i + 1) * 128], wbuf[s][:, k, 0:384], k == 0, k == 7, [("hT", i), ("w", s)], [("pf", b2)])
            tf = tmpf[b2]
            head_rstd(pp, ("pf", b2), 320, tf, ("tf", b2), s8[b2], ("s8", b2))
            TT(qai[:, :, 0:64], pp[:, 0:256].rearrange("p (h d) -> p h d", d=64), s8[b2][:, 0:4].unsqueeze(2).to_broadcast([128, 4, 64]),
               ALU.mult, [("pf", b2), ("s8", b2)], [("qa", b2)])
            TS(kai[:, 0:64], pp[:, 256:320], s8[b2][:, 4:5], None, ALU.mult, None, [("pf", b2), ("s8", b2)], [("ka", b2)])
            act(Vt[:, i, 0:64], pp[:, 320:384], AF.Copy, [("pf", b2), "Vt"], [("V", i)])

        def pbk(i):
            b2 = i % 2
            qai, kai = qa[b2], ka[b2]
            pt = pb[b2]
            for h in range(4):
                tr(pt[:, h * 128:(h + 1) * 128], qai[:, h, :], [("qa", b2)], [("pb", b2)])
            tr(pt[:, 512:640], kai[:], [("ka", b2)], [("pb", b2)])
            act(QT[:, :, i * 128:(i + 1) * 128], pt[:, 0:512].rearrange("p (h t) -> p h t", h=4), AF.Copy, [("pb", b2), "gq"], [("QT", i)],
                scale=gq[:, 0:1])
            act(KT[:, i * 128:(i + 1) * 128], pt[:, 512:640], AF.Copy, [("pb", b2), "gq"], [("KT", i)], scale=gk[:, 0:1])

        lagged(NT, pa, pbk)
        for i in range(NT):
            b2 = 2 + i % 2
            pp = pf[b2]
            for k in range(8):
                mm(pp[:, 0:256], hT[:, k, i * 128:(i + 1) * 128], wbuf[s2][:, k, 0:256], k == 0, k == 7, [("hT", i), ("w", s2)], [("pf", b2)])
            act(szy[:, i, :], pp[:, 0:256], AF.Silu, [("pf", b2)], [("szy", i)])
        for r in range(4):
            def fin_chunk(qc, Obank, okey, r=r):
                ri = (qc + 4 * r) % 8
                rd = rden4[ri]
                tb = ri % 2
                tmp = tmpf[tb][:, 0:256].rearrange("p (j c) -> p j c", c=64)
                Ov = Obank[:].rearrange("p (j c) -> p j c", c=128)
                tl = slice(4 * qc, 4 * qc + 4)
                sk_ = [("szy", i) for i in range(4 * qc, 4 * qc + 4)]
                TS(rd[:], Ov[:, :, 64:65].rearrange("p j c -> p (j c)"), esink[:, 4 * g + r:4 * g + r + 1], None, ALU.add, None,
                   [okey, "esink"], [("rden4", ri)])
                RCP(rd[:], rd[:], [("rden4", ri)], [("rden4", ri)])
                TT(tmp, Ov[:, :, 0:64], rd[:].unsqueeze(2).to_broadcast([128, 4, 64]), ALU.mult, [okey, ("rden4", ri)], [("tf", tb)])
                dst = szy[:, tl, r * 64:(r + 1) * 64]
                TT(dst, dst, tmp, ALU.mult, [("tf", tb)] + sk_, sk_)

            attention(QT[:, r, :], lambda kt: KT[:, kt * 128:(kt + 1) * 128], 128, lambda kt: Vt[:, kt, 0:65], 65,
                      plan_band(1), None, lambda qc: [("QT", 4 * qc + u) for u in range(4)], lambda kt: [("KT", kt)],
                      lambda kt: [("V", kt)], ptb, "d", finish_chunk=fin_chunk)
        outproj_half(szy, yT, store=(g == 1 and stop is None))

    if 0 in layers:
        layer0()
    if 1 in layers:
        layer1()

    if not (1 in layers and stop is None):
        for i in range(NT):
            dma("sp", yv[:, i, :], x_sb[:, i, :], "out", reads=[("x", i)])
    P.finish("sp", list(P.chan_count.keys()))
    P.emit()
    es.close()
    return nc


_CACHE = {}


def kernel(**inputs):
    consts = _consts()
    shared = {}
    shared["norm_g"] = np.ascontiguousarray(inputs["norm_g"], dtype=np.float32)
    shared["w_out"] = np.ascontiguousarray(inputs["w_out"], dtype=np.float32)
    shared["e_w_in"] = np.ascontiguousarray(inputs["e_w_in"][0], dtype=np.float32)
    shared["a_conv_w"] = np.ascontiguousarray(inputs["a_conv_w"][0], dtype=np.float32)
    shared["a_conv_b"] = np.ascontiguousarray(inputs["a_conv_b"][0], dtype=np.float32)
    shared["a_ln_g"] = np.ascontiguousarray(inputs["a_ln_g"][0], dtype=np.float32)
    shared["a_ln_b"] = np.ascontiguousarray(inputs["a_ln_b"][0], dtype=np.float32)
    shared["b_qnorm_g"] = np.ascontiguousarray(inputs["b_qnorm_g"], dtype=np.float32)
    shared["b_knorm_g"] = np.ascontiguousarray(inputs["b_knorm_g"], dtype=np.float32)
    for k in ("ident", "tri", "up", "qal", "kal_moba", "kal_slc", "kal_plain", "kal_cmp", "cmaskneg", "ovl", "impc"):
        shared[k] = consts[k]
    shared["o_w_in"] = np.ascontiguousarray(inputs["o_w_in"][0], dtype=np.float32)
    for k in ("c_qnorm_g", "c_knorm_cmp_g", "c_knorm_slc_g", "c_knorm_win_g", "c_k_b2", "c_v_b2", "d_qnorm_g", "d_knorm_g", "d_sinks"):
        shared[k] = np.ascontiguousarray(inputs[k], dtype=np.float32).reshape(1, -1)
    for k in ("c_pos_k", "c_pos_v", "c_k_w1", "c_k_b1", "c_k_w2", "c_v_w1", "c_v_b1", "c_v_w2"):
        shared[k] = np.ascontiguousarray(inputs[k][0], dtype=np.float32)
    x = np.ascontiguousarray(inputs["x"], dtype=np.float32)
    nb = x.shape[0]
    layers = inputs.get("_layers", (0, 1))
    nc = build_program(layers, inputs.get("_stop"))
    in_maps = [dict(shared, x=x[b]) for b in range(nb)]
    res = run_bass_kernel_spmd(nc, in_maps, core_ids=list(range(nb)))
    return np.stack([np.asarray(r["y"], dtype=np.float32) for r in res.results], axis=0)
```

```python
import contextlib
import os
import numpy as np
import ml_dtypes
import concourse.bass as bass
import concourse.mybir as mybir
from concourse.bass_utils import run_bass_kernel_spmd

F32 = mybir.dt.float32
BF16 = mybir.dt.bfloat16
ALU = mybir.AluOpType
AF = mybir.ActivationFunctionType
AX = mybir.AxisListType

S = 2048
D = 1024
NT = 16
NEGM = -30000.0
EPS = 1e-6


class _Op:
    __slots__ = ("eng", "fn", "deps", "chan", "signal", "val", "dmaval", "chanseq")

    def __init__(self, eng, fn, chan):
        self.eng = eng
        self.fn = fn
        self.deps = {}
        self.chan = chan
        self.signal = False
        self.val = 0
        self.dmaval = None


class Prog:
    ENGS = ("pe", "act", "dve", "pool", "sp")

    def __init__(self, nc):
        self.nc = nc
        self.ops = {e: [] for e in self.ENGS}
        self.res = {}
        self.chan_count = {}
        self.chan_last = {}
        self.all_ops = []
        self.bar = {}
        self.final_waits = {}
        self.pool_ctr = 0
        self.sp_ctr = 0
        self.const_keys = []
        self.pres = {}

    PERS = ("w", "wo")

    def _st(self, k):
        if isinstance(k, tuple) and k[0] in self.PERS or k in self.PERS:
            return self.pres
        return self.res

    def op(self, eng, fn, reads=(), writes=(), chan=None, nobar=False):
        for w in writes:
            if isinstance(w, tuple) and w[0] == "ck" and w not in self.const_keys:
                self.const_keys.append(w)
        if "const" in reads:
            reads = [k for r in reads for k in (self.const_keys if r == "const" else (r,))]
        if chan is not None and eng == "pool":
            chan = "pq%d" % (self.pool_ctr % 12)
            self.pool_ctr += 1
        elif chan == "c":
            chan = "pc%d" % (self.sp_ctr % 16)
            self.sp_ctr += 1
        o = _Op(eng, fn, chan)
        deps = o.deps
        if not nobar:
            deps.update(self.bar)
        if chan is not None and (chan.startswith("pq") or chan.startswith("pc")):
            prev = self.chan_last.get(chan)
            if prev is not None:
                deps[id(prev)] = prev
        for k in reads:
            st = self._st(k).get(k)
            if st is not None and st[0] is not None:
                deps[id(st[0])] = st[0]
        for k in writes:
            st = self._st(k).get(k)
            if st is not None:
                if st[0] is not None:
                    deps[id(st[0])] = st[0]
                for r in st[1]:
                    deps[id(r)] = r
        for k in reads:
            res = self._st(k)
            st = res.get(k)
            if st is None:
                st = [None, []]
                res[k] = st
            st[1].append(o)
        for k in writes:
            self._st(k)[k] = [o, []]
        o.dmaval = dict(self.chan_count)
        if chan is not None:
            self.chan_count[chan] = self.chan_count.get(chan, 0) + 1
            o.chanseq = self.chan_count[chan]
            self.chan_last[chan] = o
            o.signal = True
        self.ops[eng].append(o)
        self.all_ops.append(o)
        return o

    def barrier(self):
        bar = {}
        for e in self.ENGS:
            if self.ops[e]:
                o = self.ops[e][-1]
                bar[id(o)] = o
        for ch, o in self.chan_last.items():
            bar[id(o)] = o
        self.bar = bar
        self.res = {}

    def finish(self, eng, chans):
        self.final_waits = {eng: {ch: self.chan_count[ch] for ch in chans}}

    def emit(self):
        nc = self.nc
        for o in self.all_ops:
            for d in o.deps.values():
                if d.chan is None:
                    if d.eng == "pe" and o.eng == "pe":
                        continue
                    d.signal = True
        for e in self.ENGS:
            c = 0
            for o in self.ops[e]:
                if o.chan is None and o.signal:
                    c += 1
                    o.val = c
        import os
        if os.environ.get("KDBG"):
            print("sem counts", {e: max([o.val for o in self.ops[e]] + [0]) for e in self.ENGS}, {e: len(self.ops[e]) for e in self.ENGS},
                  {c: 16 * v for c, v in self.chan_count.items()})
        stack = contextlib.ExitStack()
        sems = {}
        for e in self.ENGS:
            sems[e] = stack.enter_context(nc.semaphore("s_" + e))
        for ch in self.chan_count:
            sems["c_" + ch] = stack.enter_context(nc.semaphore("c_" + ch))
        block = stack.enter_context(nc.Block())
        engobj = {"pe": "tensor", "act": "scalar", "dve": "vector", "pool": "gpsimd", "sp": "sync"}

        def make(e):
            def body(eng):
                waited = {}
                for o in self.ops[e]:
                    need = {}
                    for d in o.deps.values():
                        if d.chan is not None:
                            k = "c_" + d.chan
                            if d.chan.startswith("pq") or d.chan.startswith("pc"):
                                v = 16 * d.chanseq
                            else:
                                v = 16 * o.dmaval[d.chan]
                        else:
                            if d.eng == "pe" and e == "pe":
                                continue
                            k = d.eng
                            v = d.val
                        if v > need.get(k, 0):
                            need[k] = v
                    for k, v in need.items():
                        if waited.get(k, 0) >= v:
                            continue
                        eng.wait_ge(sems[k], v)
                        waited[k] = v
                    ins = o.fn(eng)
                    if o.chan is not None:
                        ins.then_inc(sems["c_" + o.chan], 16)
                    elif o.signal:
                        ins.then_inc(sems[e], 1)
                for ch, c in self.final_waits.get(e, {}).items():
                    eng.wait_ge(sems["c_" + ch], 16 * c)
            return body

        for e in self.ENGS:
            getattr(block, engobj[e])(make(e))
        stack.close()


def _consts():
    c = {}
    c["ident"] = np.eye(128, dtype=np.float32)
    k = np.arange(128)[:, None]
    q = np.arange(128)[None, :]
    c["tri"] = np.where(k <= q, 0.0, NEGM).astype(np.float32)
    c["up"] = np.where(k > q, 0.0, NEGM).astype(np.float32)
    t = np.arange(S)
    b = (t % 16).astype(np.float32)
    a = (t - t % 16).astype(np.float32)
    slopes = np.power(2.0, -8.0 * np.arange(1, 9) / 8).astype(np.float32)
    qal = np.zeros((S, 8, 4), np.float32)
    qal[:, :, 0] = -slopes[None, :] * a[:, None]
    qal[:, :, 1] = -slopes[None, :] * b[:, None]
    qal[:, :, 2] = slopes[None, :]
    qal[:, :, 3] = slopes[None, :]
    c["qal"] = qal.reshape(NT, 128, 32).transpose(1, 0, 2).copy()

    def kal(onehot_block):
        m = np.zeros((S, 36), np.float32)
        if onehot_block:
            m[t, t // onehot_block] = 1.0
        m[:, 32] = 1.0
        m[:, 33] = 1.0
        m[:, 34] = a
        m[:, 35] = b
        return m.reshape(NT, 128, 36).transpose(1, 0, 2).copy()

    c["kal_moba"] = kal(256)
    c["kal_slc"] = kal(64)
    c["kal_plain"] = kal(0)
    cc = np.arange(128)
    cend = 16 * cc + 31
    kc = np.zeros((128, 36), np.float32)
    kc[:, 32] = 1.0
    kc[:, 33] = 1.0
    kc[:, 34] = cend - cend % 16
    kc[:, 35] = cend % 16
    c["kal_cmp"] = kc
    c["cmaskneg"] = np.where(t[None, :] >= cend[:, None], 0.0, NEGM).astype(np.float32)
    start = np.arange(127)[:, None] * 16
    bs = np.arange(32)[None, :] * 64
    ov = np.zeros((128, 32), np.float32)
    ov[:127] = ((start < bs + 64) & (start + 32 > bs)).astype(np.float32)
    c["ovl"] = ov
    blk = np.arange(32)[None, :]
    cur = (t // 64)[:, None]
    forced = (blk == 0) | (blk == cur) | (blk == cur - 1)
    impc = 1e4 * forced.astype(np.float32) - 1e5 * (blk > cur).astype(np.float32)
    c["impc"] = impc.reshape(NT, 128, 32).transpose(1, 0, 2).copy()
    return c


def build_program(layers=(0, 1), stop=None):
    nc = bass.Bass("TRN2", target_bir_lowering=False)
    es = contextlib.ExitStack()
    dram = {}

    def din(name, shape, dt=F32):
        dram[name] = nc.dram_tensor(name, list(shape), dt, kind="ExternalInput").ap()
        return dram[name]

    x_d = din("x", [S, D])
    normg_d = din("norm_g", [2, D])
    wout_d = din("w_out", [2, D, D])
    ewin_d = din("e_w_in", [D, 3584])
    convw_d = din("a_conv_w", [31, 512])
    convb_d = din("a_conv_b", [512])
    lng_d = din("a_ln_g", [512])
    lnb_d = din("a_ln_b", [512])
    bq_d = din("b_qnorm_g", [1, 64])
    bk_d = din("b_knorm_g", [1, 64])
    ident_d = din("ident", [128, 128])
    tri_d = din("tri", [128, 128])
    up_d = din("up", [128, 128])
    qal_d = din("qal", [128, NT, 32])
    kalm_d = din("kal_moba", [128, NT, 36])
    kals_d = din("kal_slc", [128, NT, 36])
    kalp_d = din("kal_plain", [128, NT, 36])
    kalc_d = din("kal_cmp", [128, 36])
    cmn_d = din("cmaskneg", [128, S])
    ovl_d = din("ovl", [128, 32])
    impc_d = din("impc", [128, NT, 32])
    owin_d = din("o_w_in", [D, 3096])
    cq_d = din("c_qnorm_g", [1, 64])
    ckc_d = din("c_knorm_cmp_g", [1, 64])
    cks_d = din("c_knorm_slc_g", [1, 64])
    ckw_d = din("c_knorm_win_g", [1, 64])
    cposk_d = din("c_pos_k", [32, 64])
    cposv_d = din("c_pos_v", [32, 64])
    ckw1_d = din("c_k_w1", [2048, 256])
    ckb1_d = din("c_k_b1", [256])
    ckw2_d = din("c_k_w2", [256, 64])
    ckb2_d = din("c_k_b2", [1, 64])
    cvw1_d = din("c_v_w1", [2048, 256])
    cvb1_d = din("c_v_b1", [256])
    cvw2_d = din("c_v_w2", [256, 64])
    cvb2_d = din("c_v_b2", [1, 64])
    dq_d = din("d_qnorm_g", [1, 64])
    dk_d = din("d_knorm_g", [1, 64])
    dsink_d = din("d_sinks", [1, 8])
    y_d = nc.dram_tensor("y", [S, D], F32, kind="ExternalOutput").ap()

    def sb(name, shape, dt):
        return es.enter_context(nc.sbuf_tensor(name, list(shape), dt))

    def psum(name, shape, dt):
        return es.enter_context(nc.psum_tensor(name, list(shape), dt))

    P = Prog(nc)

    x_sb = sb("x_sb", [128, NT, D], F32)
    hT = sb("hT", [128, 8, S], BF16)
    wbuf = [sb("wbuf%d" % i, [128, 8, 512], BF16) for i in range(2)]
    wo = sb("wo", [128, 4, D], BF16)
    ident = sb("ident_sb", [128, 128], BF16)
    identf = sb("identf_sb", [128, 128], F32)
    onesf = sb("onesf", [128, 128], F32)
    tri = sb("tri_sb", [128, 128], BF16)
    upm = sb("up_sb", [128, 128], BF16)
    qal = sb("qal_sb", [128, NT, 32], BF16)
    kalm = sb("kalm_sb", [128, NT, 36], BF16)
    KTc = sb("KTc", [128, 2, 128], BF16)
    Vca = sb("Vca", [128, 2, 98], BF16)
    kals = sb("kals_sb", [128, NT, 36], BF16)
    kalp = sb("kalp_sb", [128, NT, 36], BF16)
    ss = sb("ss", [128, NT], F32)
    rstd = sb("rstd", [128, NT], F32)
    ARENA = 80 * 1024
    arena = sb("arena", [128, ARENA // 2], BF16)

    pf = [psum("pf%d" % i, [128, 512], F32) for i in range(6)]
    pb = [psum("pb%d" % i, [128, 1024], BF16) for i in range(2)]

    class Carver:
        def __init__(self, base=0):
            self.off = base

        def get(self, shape, dt):
            n = int(np.prod(shape[1:]))
            nb = n * (4 if dt == F32 else 2)
            nb = (nb + 63) // 64 * 64
            assert self.off + nb <= ARENA, ("arena overflow", self.off + nb, ARENA)
            a = arena[:, self.off // 2:(self.off + nb) // 2]
            self.off += nb
            if os.environ.get("KDBG"):
                print("carve", shape, dt, "->", self.off)
            if dt == F32:
                a = a.bitcast(F32)
            a = a[:, 0:n]
            if len(shape) == 3:
                a = a.rearrange("p (a b) -> p a b", b=shape[2])
            elif len(shape) == 4:
                a = a.rearrange("p (a b c) -> p a b c", b=shape[2], c=shape[3])
            if shape[0] < 128:
                a = a[0:shape[0]]
            return a

    def dma(eng, out, in_, chan, reads=(), writes=(), slow=False, nobar=False):
        if slow:
            P.op(eng, lambda e: e.dma_start(out=out, in_=in_, allow_slow_non_contiguous=True), reads=reads, writes=writes, chan=chan, nobar=nobar)
        else:
            P.op(eng, lambda e: e.dma_start(out=out, in_=in_), reads=reads, writes=writes, chan=chan, nobar=nobar)

    def mm(out, lhsT, rhs, start, stop, reads, writes):
        P.op("pe", lambda e: e.matmul(out, lhsT=lhsT, rhs=rhs, start=start, stop=stop), reads=reads, writes=writes)

    def tr(out, in_, reads, writes, idn=None):
        ck = ("ck", "ident") if idn is None else ("ck", "identf")
        idn = ident[:] if idn is None else idn
        P.op("pe", lambda e: e.transpose(out=out, in_=in_, identity=idn), reads=list(reads) + [ck], writes=writes)

    def act(out, in_, func, reads, writes, **kw):
        P.op("act", lambda e: e.activation(out=out, in_=in_, func=func, **kw), reads=reads, writes=writes)

    def V(fn, reads, writes, eng="dve"):
        P.op(eng, fn, reads=reads, writes=writes)

    def TT(out, in0, in1, op, reads, writes, eng="dve"):
        P.op(eng, lambda e: e.tensor_tensor(out=out, in0=in0, in1=in1, op=op), reads=reads, writes=writes)

    def TS(out, in0, s1, s2, op0, op1, reads, writes, eng="dve"):
        if op1 is None:
            P.op(eng, lambda e: e.tensor_scalar(out=out, in0=in0, scalar1=s1, scalar2=None, op0=op0), reads=reads, writes=writes)
        else:
            P.op(eng, lambda e: e.tensor_scalar(out=out, in0=in0, scalar1=s1, scalar2=s2, op0=op0, op1=op1), reads=reads, writes=writes)

    def STT(out, in0, scalar, in1, op0, op1, reads, writes, eng="dve"):
        P.op(eng, lambda e: e.scalar_tensor_tensor(out=out, in0=in0, scalar=scalar, in1=in1, op0=op0, op1=op1), reads=reads, writes=writes)

    def RED(out, in_, op, reads, writes):
        P.op("dve", lambda e: e.tensor_reduce(out=out, in_=in_, axis=AX.X, op=op), reads=reads, writes=writes)

    def RCP(out, in_, reads, writes):
        P.op("dve", lambda e: e.reciprocal(out=out, in_=in_), reads=reads, writes=writes)

    def CPY(out, in_, reads, writes, eng="dve"):
        P.op(eng, lambda e: e.tensor_copy(out=out, in_=in_), reads=reads, writes=writes)

    def MSET(ap, val, writes, eng="dve"):
        P.op(eng, lambda e: e.memset(ap, val), reads=[], writes=writes)

    import os
    KB = os.environ.get("KBIS", "abcdefg")
    if "a" in KB:
        dma("pool", ident[:], ident_d, "c", writes=[("ck", "ident")])
    if "b" in KB:
        dma("pool", tri[:], tri_d, "c", writes=[("ck", "tri")])
        dma("pool", upm[:], up_d, "c", writes=[("ck", "up")])
    if "c" in KB:
        dma("pool", qal[:], qal_d, "c", writes=[("ck", "qal")])
    if "d" in KB:
        dma("pool", kalm[:], kalm_d, "c", writes=[("ck", "kalm")])
        dma("pool", kals[:], kals_d, "c", writes=[("ck", "kals")])
        dma("pool", kalp[:], kalp_d, "c", writes=[("ck", "kalp")])
    if "e" in KB:
        dma("sp", identf[:], ident_d, "c", writes=[("ck", "identf")])
    if "f" in KB:
        MSET(onesf[:], 1.0, [("ck", "onesf")])

    xv = x_d.rearrange("(i p) d -> p i d", p=128)
    yv = y_d.rearrange("(i p) d -> p i d", p=128)
    for i in range(NT):
        dma("sp", x_sb[:, i, :], xv[:, i, :], "x%d" % i, writes=[("x", i)])

    wslot_ctr = [0]

    def load_w(wd, segs):
        s = wslot_ctr[0] % 2
        wslot_ctr[0] += 1
        wv = wd.rearrange("(k p) n -> p k n", p=128)
        o = 0
        for (c0, n) in segs:
            dma("pool", wbuf[s][:, :, o:o + n], wv[:, :, c0:c0 + n], "w%d" % s, writes=[("w", s)], nobar=True)
            o += n
        return s

    def norm_phase(layer, base=0, barrier=True):
        cvn = Carver(base=base)
        gbc = cvn.get([128, D], F32)
        junk = cvn.get([128, D], BF16)
        hb = [cvn.get([128, D], BF16) for _ in range(2)]
        dma("act", gbc[:], normg_d[layer:layer + 1, :].partition_broadcast(128), "c", writes=["gbc"])
        MSET(ss[:], 0.0, [("ss", gI) for gI in range(4)])

        def stats(gI):
            for i in range(4 * gI, 4 * gI + 4):
                act(junk[:], x_sb[:, i, :], AF.Square, [("x", i), ("ss", gI)], ["junk", ("ss", gI)], accum_out=ss[:, i:i + 1])
            sl = slice(4 * gI, 4 * gI + 4)
            TS(rstd[:, sl], ss[:, sl], 1.0 / D, EPS, ALU.mult, ALU.add, [("ss", gI)], [("rstd", gI)])
            act(rstd[:, sl], rstd[:, sl], AF.Sqrt, [("rstd", gI)], [("rstd", gI)])
            RCP(rstd[:, sl], rstd[:, sl], [("rstd", gI)], [("rstd", gI)])

        def na(i):
            h = hb[i % 2]
            STT(h[:], x_sb[:, i, :], rstd[:, i:i + 1], gbc[:], ALU.mult, ALU.mult, [("x", i), ("rstd", i // 4), "gbc"], [("hb", i % 2)])

        def nb(i):
            h = hb[i % 2]
            pt = pb[i % 2]
            for k in range(8):
                tr(pt[:, k * 128:(k + 1) * 128], h[:, k * 128:(k + 1) * 128], [("hb", i % 2)], [("pb", i % 2)])
            act(hT[:, :, i * 128:(i + 1) * 128], pt[:].rearrange("p (k t) -> p k t", k=8), AF.Copy, [("pb", i % 2)], [("hT", i)])

        pend = []
        if layer == 0:
            for gI in range(4):
                stats(gI)
                for i in range(4 * gI, 4 * gI + 4):
                    na(i)
                    pend.append(i)
                    if len(pend) > 1:
                        nb(pend.pop(0))
        else:
            for gI in range(5):
                if gI < 4:
                    stats(gI)
                if gI >= 1:
                    for i in range(4 * (gI - 1), 4 * gI):
                        na(i)
                        pend.append(i)
                        if len(pend) > 1:
                            nb(pend.pop(0))
        nb(pend.pop(0))
        if barrier:
            P.barrier()

    def load_wo(layer, c0, n):
        wv = wout_d[layer].rearrange("(k p) n -> p k n", p=128)
        for hf in range(2):
            dma("pool", wo[:, 0:n, hf * 512:(hf + 1) * 512], wv[:, c0:c0 + n, hf * 512:(hf + 1) * 512], "wo", writes=["wo"], nobar=True)

    def outproj_tile(i, lhs_list, chunks, yT_reads, pbanks):
        for half in range(2):
            pp = pf[pbanks[half]]
            for n, (c, lhs) in enumerate(zip(chunks, lhs_list)):
                mm(pp[:], lhs, wo[:, c, half * 512:(half + 1) * 512], n == 0, n == len(chunks) - 1,
                   list(yT_reads) + ["wo"], [("pf", pbanks[half])])
            xs = x_sb[:, i, half * 512:(half + 1) * 512]
            TT(xs, xs, pp[:], ALU.add, [("pf", pbanks[half]), ("x", i)], [("x", i)])

    att_ctr = [0, 0]

    def lagged(n, stage_a, stage_b, lag=1):
        for i in range(n + lag):
            if i < n:
                stage_a(i)
            if i >= lag:
                stage_b(i - lag)

    def mm_acc(out, lhsT, rhs, start, stop, reads, writes):
        P.op("pe", lambda e: e.matmul(out, lhsT=lhsT, rhs=rhs, start=start, stop=stop, skip_group_check=True), reads=reads, writes=writes)

    def attention(QT, KT, nk, Vrhs, ncv, plan, finish, qkeys, kkeys, vkeys, ptb, tagk, addmask=None, finish_chunk=None):
        NS = len(ptb)
        work = []
        for qc in range(4):
            items = plan(qc)
            if not items:
                continue
            ob = 3 + att_ctr[1] % 3
            att_ctr[1] += 1
            lastk = {}
            for (kt, jlo, jhi, masks) in items:
                for j in range(jlo, jhi + 1):
                    lastk[j] = kt
            for n, it in enumerate(items):
                work.append((qc, ob, it, n == 0, n == len(items) - 1, lastk))

        def front(w):
            qc, ob, (kt, jlo, jhi, masks), first, last, lastk = w
            sbk = att_ctr[0] % NS
            att_ctr[0] += 1
            Sp = pf[sbk]
            pt = ptb[sbk]
            c0, c1 = jlo * 128, (jhi + 1) * 128
            extra = list(masks.items())
            mm_acc(Sp[0:nk, c0:c1], KT(kt), QT[:, qc * 512 + c0:qc * 512 + c1], True, addmask is None and not extra,
                   list(qkeys(qc)) + list(kkeys(kt)), [("pf", sbk)])
            if addmask is not None:
                mm_acc(Sp[0:nk, c0:c1], ident[0:nk, 0:nk], addmask[0:nk, qc * 512 + c0:qc * 512 + c1], False, not extra,
                       ["const", "addmask"], [("pf", sbk)])
            for n, (j, mk) in enumerate(extra):
                mm_acc(Sp[0:nk, j * 128:(j + 1) * 128], ident[0:nk, 0:nk], mk[0:nk, :], False, n == len(extra) - 1,
                       ["const"], [("pf", sbk)])
            act(pt[0:nk, c0:c1], Sp[0:nk, c0:c1], AF.Exp, [("pf", sbk)], [("pt", tagk, sbk)])
            return sbk

        def back(w, sbk, started):
            qc, ob, (kt, jlo, jhi, masks), first, last, lastk = w
            pt = ptb[sbk]
            for j in range(jlo, jhi + 1):
                mm_acc(pf[ob][:, j * 128:j * 128 + ncv], pt[0:nk, j * 128:(j + 1) * 128], Vrhs(kt), len(started) == 0, lastk[j] == kt,
                       [("pt", tagk, sbk)] + list(vkeys(kt)), [("pf", ob)])
                started.add(j)
            if last:
                if finish_chunk is not None:
                    finish_chunk(qc, pf[ob], ("pf", ob))
                else:
                    for j in sorted(started):
                        finish(qc * 4 + j, pf[ob][:, j * 128:j * 128 + ncv], ("pf", ob))
                started.clear()

        LAG = NS - 1
        started = set()
        pend = []
        for w in work:
            pend.append((w, front(w)))
            if len(pend) > LAG:
                pw, psb = pend.pop(0)
                back(pw, psb, started)
        for (pw, psb) in pend:
            back(pw, psb, started)

    def layer0():
        if stop == "load":
            return
        norm_phase(0, base=70 * 1024, barrier=False)
        if stop == "norm0":
            return
        load_wo(0, 0, 4)
        W = ewin_d
        if stop == "norm":
            return
        cv = Carver()
        yc = cv.get([128, 4, S], F32)
        hc = cv.get([128, S + 32], BF16)
        dg = cv.get([128, 31, 128], BF16)
        cw = cv.get([128, 4, 31], F32)
        cb = cv.get([128, 4], F32)
        lg = cv.get([128, 4], F32)
        lb = cv.get([128, 4], F32)
        Tsq = [cv.get([128, 512], F32) for _ in range(2)]
        mu2 = [cv.get([128, 512], F32) for _ in range(2)]
        msq = cv.get([128, 512], F32)
        rs2 = [cv.get([128, 512], F32) for _ in range(2)]
        sz4 = [cv.get([128, 512], BF16) for _ in range(4)]
        T4 = [cv.get([128, 512], F32) for _ in range(4)]
        yaT2 = [cv.get([128, 4, 512], BF16) for _ in range(2)]
        HO = 32
        stg = Tsq[0][0:32, :]
        stg2 = Tsq[1][0:12, 0:128]
        dma("sp", stg[0:31, :], convw_d, "c", writes=[("Tsq", 0)])
        dma("sp", stg2[0:4, :], convb_d.rearrange("(a c) -> a c", c=128), "c", writes=[("Tsq", 1)])
        dma("sp", stg2[4:8, :], lng_d.rearrange("(a c) -> a c", c=128), "c", writes=[("Tsq", 1)])
        dma("sp", stg2[8:12, :], lnb_d.rearrange("(a c) -> a c", c=128), "c", writes=[("Tsq", 1)])
        for cc in range(4):
            tr(pf[0][:, cc * 32:cc * 32 + 31], stg[0:31, cc * 128:(cc + 1) * 128], [("Tsq", 0)], [("pf", 0)], idn=identf[0:31, 0:31])
        tr(pf[0][:, 128:140], stg2[0:12, :], [("Tsq", 1)], [("pf", 0)], idn=identf[0:12, 0:12])
        CPY(cw[:], pf[0][:, 0:128].rearrange("p (a j) -> p a j", j=32)[:, :, 0:31], [("pf", 0)], ["cw"])
        CPY(cb[:], pf[0][:, 128:132], [("pf", 0)], ["cw"])
        CPY(lg[:], pf[0][:, 132:136], [("pf", 0)], ["cw"])
        CPY(lb[:], pf[0][:, 136:140], [("pf", 0)], ["cw"])
        MSET(hc[:, 0:HO], 0.0, ["hc0"])
        if stop == "convp":
            return
        for cc in range(4):
            if stop == "conv1" and cc == 1:
                return
            s = load_w(W, [(cc * 128, 128), (512 + cc * 128, 128)])
            for j in range(31):
                TS(dg[:, j, :], ident[:], cw[:, cc, j:j + 1], None, ALU.mult, None, ["const", "cw"], [("dg", j)])
            for tq in range(4):
                bv, bg = (tq % 2) * 2, (tq % 2) * 2 + 1
                for which, bk in ((0, bv), (1, bg)):
                    for k in range(8):
                        mm(pf[bk][:], wbuf[s][:, k, which * 128:(which + 1) * 128], hT[:, k, tq * 512:(tq + 1) * 512], k == 0, k == 7,
                           [("w", s)] + [("hT", 4 * tq + u) for u in range(4)], [("pf", bk)])
                act(T4[tq % 2][:], pf[bg][:], AF.Sigmoid, [("pf", bg)], [("T4", tq % 2)])
                TT(hc[:, HO + tq * 512:HO + (tq + 1) * 512], pf[bv][:], T4[tq % 2][:], ALU.mult, [("pf", bv), ("T4", tq % 2)], [("hc", tq)])
            for tq in range(4):
                pc = pf[4 + tq % 2]
                for j in range(31):
                    o = HO - 30 + j + tq * 512
                    mm(pc[:], dg[:, j, :], hc[:, o:o + 512], j == 0, j == 30,
                       [("dg", j), ("hc", tq)] + ([("hc", tq - 1)] if tq > 0 else ["hc0"]), [("pf", 4 + tq % 2)])
                act(yc[:, cc, tq * 512:(tq + 1) * 512], pc[:], AF.Identity, [("pf", 4 + tq % 2), "cw"], [("yc", cc, tq)],
                    bias=cb[:, cc:cc + 1], scale=1.0)
        if stop == "conv4":
            return
        P.barrier()
        sz_slot = load_w(W, [(1024, 512)])

        def st_a(tq):
            ts = slice(tq * 512, (tq + 1) * 512)
            p2 = tq % 2
            for cc in range(4):
                mm(pf[0][:], onesf[:], yc[:, cc, ts], cc == 0, cc == 3, [("yc", cc, tq), "const"], [("pf", 0)])
            for cc in range(4):
                act(Tsq[cc % 2][:], yc[:, cc, ts], AF.Square, [("yc", cc, tq)], [("Tsq", cc % 2)])
                mm(pf[1][:], onesf[:], Tsq[cc % 2][:], cc == 0, cc == 3, [("Tsq", cc % 2), "const"], [("pf", 1)])
            TS(mu2[p2][:], pf[0][:], 1.0 / 512, None, ALU.mult, None, [("pf", 0)], [("mu", p2)])
            TT(msq[:], mu2[p2][:], mu2[p2][:], ALU.mult, [("mu", p2)], ["msq"])
            STT(rs2[p2][:], pf[1][:], 1.0 / 512, msq[:], ALU.mult, ALU.subtract, [("pf", 1), "msq"], [("rs", p2)])
            TS(rs2[p2][:], rs2[p2][:], EPS, None, ALU.add, None, [("rs", p2)], [("rs", p2)])
            act(rs2[p2][:], rs2[p2][:], AF.Sqrt, [("rs", p2)], [("rs", p2)])
            RCP(rs2[p2][:], rs2[p2][:], [("rs", p2)], [("rs", p2)])

        def st_b(tq):
            ts = slice(tq * 512, (tq + 1) * 512)
            p2 = tq % 2
            for cc in range(4):
                pz = pf[2 + cc % 2]
                for k in range(8):
                    mm(pz[:], wbuf[sz_slot][:, k, cc * 128:(cc + 1) * 128], hT[:, k, ts], k == 0, k == 7,
                       [("w", sz_slot)] + [("hT", 4 * tq + u) for u in range(4)], [("pf", 2 + cc % 2)])
                act(sz4[cc][:], pz[:], AF.Silu, [("pf", 2 + cc % 2)], [("sz", cc)])
            for cc in range(4):
                tt = T4[cc]
                TT(tt[:], yc[:, cc, ts], mu2[p2][:], ALU.subtract, [("yc", cc, tq), ("mu", p2)], [("T4", cc)])
                TT(tt[:], tt[:], rs2[p2][:], ALU.mult, [("T4", cc), ("rs", p2)], [("T4", cc)])
            for cc in range(4):
                tt = T4[cc]
                act(tt[:], tt[:], AF.Silu, [("T4", cc), "cw"], [("T4", cc)], scale=lg[:, cc:cc + 1], bias=lb[:, cc:cc + 1])
            for cc in range(4):
                TT(yaT2[p2][:, cc, :], T4[cc][:], sz4[cc][:], ALU.mult, [("T4", cc), ("sz", cc)], [("yaT", p2, cc)])

        def st_c(tq):
            p2 = tq % 2
            for u in range(4):
                i = 4 * tq + u
                outproj_tile(i, [yaT2[p2][:, cc, u * 128:(u + 1) * 128] for cc in range(4)], [0, 1, 2, 3],
                             [("yaT", p2, cc) for cc in range(4)], (4, 5))

        for step in range(6):
            if step < 4:
                st_a(step)
            if 0 <= step - 1 < 4:
                st_b(step - 1)
            if step - 2 >= 0:
                st_c(step - 2)
        P.barrier()
        if stop == "conv":
            return
        for hh in range(2):
            moba_half(hh, W)
            P.barrier()
            if stop is not None:
                return

    def moba_half(hh, W):
        cv = Carver()
        QT = cv.get([128, 4, S], BF16)
        KT = cv.get([128, 4, S], BF16)
        Vt = cv.get([128, NT, 4, 66], BF16)
        szy = cv.get([128, NT, 256], BF16)
        ptb = [cv.get([128, 512], BF16) for _ in range(3)]
        gq = cv.get([128, 1], F32)
        gk = cv.get([128, 1], F32)
        kmf = cv.get([128, 4, 8], F32)
        kmb = cv.get([128, 4, 8], BF16)
        gs = cv.get([128, 4, 8], F32)
        cmp_ = cv.get([128, 4, 8, 8], F32)
        rank = cv.get([128, 4, 8], F32)
        nm = cv.get([128, 4, 32], BF16)
        rden = [cv.get([128, 1], F32) for _ in range(8)]
        rden4 = [cv.get([128, 4], F32) for _ in range(8)]
        tmpm = [cv.get([128, 4, 64], F32) for _ in range(2)]
        yT = [cv.get([128, 2, 128], BF16) for _ in range(2)]
        tmpf = [cv.get([128, 512], F32) for _ in range(2)]
        s8 = [cv.get([128, 8], F32) for _ in range(3)]
        qa = [cv.get([128, 4, 128], BF16) for _ in range(3)]
        ka = [cv.get([128, 4, 128], BF16) for _ in range(3)]
        c_q = 1536 + hh * 256
        c_k = 2048 + hh * 256
        c_v = 2560 + hh * 256
        c_z = 3072 + hh * 256
        s = load_w(W, [(c_q, 256), (c_k, 256)])
        s2 = load_w(W, [(c_v, 256), (c_z, 256)])
        load_wo(0, 4 + 2 * hh, 2)
        load_gain_col(gq, bq_d, 1.0)
        load_gain_col(gk, bk_d, 8.0)
        for b in range(3):
            MSET(qa[b][:, :, 64:128], 0.0, [("qa", b)])
            MSET(ka[b][:, :, 64:128], 0.0, [("ka", b)])
        MSET(Vt[:, :, :, 64:65], 1.0, ["Vt"])
        MSET(nm[:], 0.0, ["nm"])

        def m0(i):
            r = i % 3
            CPY(qa[r][:, :, 96:100], qal[:, i, hh * 16:hh * 16 + 16].rearrange("p (h c) -> p h c", c=4), ["const"], [("qa", r)])
            CPY(ka[r][:, :, 64:100], kalm[:, i, :].unsqueeze(1).to_broadcast([128, 4, 36]), ["const"], [("ka", r)])
            for k in range(8):
                mm(pf[r][:], hT[:, k, i * 128:(i + 1) * 128], wbuf[s][:, k, :], k == 0, k == 7, [("hT", i), ("w", s)], [("pf", r)])

        def m1(i):
            r = i % 3
            rstd_a(pf[r], ("pf", r), 512, tmpf[i % 2], ("tf", i % 2), s8[r], ("s8", r))

        def m2(i):
            r = i % 3
            rstd_b(512, s8[r], ("s8", r))
            TT(qa[r][:, :, 0:64], pf[r][:, 0:256].rearrange("p (h d) -> p h d", d=64), s8[r][:, 0:4].unsqueeze(2).to_broadcast([128, 4, 64]),
               ALU.mult, [("pf", r), ("s8", r)], [("qa", r)])
            TT(ka[r][:, :, 0:64], pf[r][:, 256:512].rearrange("p (h d) -> p h d", d=64), s8[r][:, 4:8].unsqueeze(2).to_broadcast([128, 4, 64]),
               ALU.mult, [("pf", r), ("s8", r)], [("ka", r)])

        def m3(i):
            r = i % 3
            b2 = i % 2
            pt = pb[b2]
            for h in range(4):
                tr(pt[:, h * 128:(h + 1) * 128], qa[r][:, h, :], [("qa", r)], [("pb", b2)])
            for h in range(4):
                tr(pt[:, (4 + h) * 128:(5 + h) * 128], ka[r][:, h, :], [("ka", r)], [("pb", b2)])
            act(QT[:, :, i * 128:(i + 1) * 128], pt[:, 0:512].rearrange("p (h t) -> p h t", h=4), AF.Copy, [("pb", b2), "gq"], [("QT", i)],
                scale=gq[:, 0:1])
            act(KT[:, :, i * 128:(i + 1) * 128], pt[:, 512:1024].rearrange("p (h t) -> p h t", h=4), AF.Copy, [("pb", b2), "gq"], [("KT", i)],
                scale=gk[:, 0:1])

        pipe4(NT, m0, m1, m2, m3)
        if hh == 1 and 1 in layers:
            prefetch_w1([("qa", r) for r in range(3)] + [("ka", r) for r in range(3)] + [("s8", r) for r in range(3)] + [("tf", 0), ("tf", 1)])
        if stop == "m1":
            return
        for i in range(NT):
            b2 = 2 + i % 2
            pp = pf[b2]
            for k in range(8):
                mm(pp[:], hT[:, k, i * 128:(i + 1) * 128], wbuf[s2][:, k, :], k == 0, k == 7, [("hT", i), ("w", s2)], [("pf", b2)])
            act(Vt[:, i, :, 0:64], pp[:, 0:256].rearrange("p (h d) -> p h d", d=64), AF.Copy, [("pf", b2), "Vt"], [("V", i)])
            act(szy[:, i, :], pp[:, 256:512], AF.Silu, [("pf", b2)], [("szy", i)])
        if stop == "m3":
            return
        for h in range(4):
            RED(kmf[:, h, :], KT[:, h, :].rearrange("p (n t) -> p n t", t=256), ALU.add, [("KT", i) for i in range(NT)], ["kmf"])
        TS(kmb[:], kmf[:], 1.0 / 256, None, ALU.mult, None, ["kmf"], ["kmb"])
        if stop == "mk":
            return
        for i in range(8, NT):
            npast = i // 2
            b2 = 4 + i % 2
            pg = pf[b2]
            for h in range(4):
                mm(pg[:, h * 8:h * 8 + 8], QT[0:64, h, i * 128:(i + 1) * 128], kmb[0:64, h, :], True, True, [("QT", i), "kmb"], [("pf", b2)])
            CPY(gs[:], pg[:, 0:32].rearrange("p (h n) -> p h n", n=8), [("pf", b2)], ["gs"])
            TT(cmp_[:, :, 0:npast, 0:npast], gs[:, :, 0:npast].unsqueeze(2).to_broadcast([128, 4, npast, npast]),
               gs[:, :, 0:npast].unsqueeze(3).to_broadcast([128, 4, npast, npast]), ALU.is_gt, ["gs"], ["cmp"])
            RED(rank[:, :, 0:npast], cmp_[:, :, 0:npast, 0:npast], ALU.add, ["cmp"], ["rank"])
            TS(nm[:, :, 0:npast], rank[:, :, 0:npast], 2.5, NEGM, ALU.is_ge, ALU.mult, ["rank"], ["nm"])
            pt = pb[i % 2]
            tr(pt[:, 0:128], nm[:].rearrange("p h n -> p (h n)"), ["nm"], [("pb", i % 2)])
            for h in range(4):
                act(QT[64:96, h, i * 128:(i + 1) * 128], pt[h * 32:(h + 1) * 32, 0:128], AF.Copy, [("pb", i % 2)], [("QT", i)])

        if stop == "m2":
            return

        def plan(qc):
            items = []
            for kt in range(4 * qc + 4):
                if kt < 4 * qc:
                    items.append((kt, 0, 3, {}))
                else:
                    m = kt - 4 * qc
                    items.append((kt, m, 3, {m: tri[:]}))
            return items

        for h in range(4):
            def finish_chunk(qc, Obank, okey, h=h):
                ri = (qc + 4 * h) % 8
                rd = rden4[ri]
                tb = ri % 2
                Ov = Obank[:].rearrange("p (j c) -> p j c", c=128)
                tl = slice(4 * qc, 4 * qc + 4)
                sk_ = [("szy", i) for i in range(4 * qc, 4 * qc + 4)]
                RCP(rd[:], Ov[:, :, 64:65].rearrange("p j c -> p (j c)"), [okey], [("rden4", ri)])
                TT(tmpm[tb][:], Ov[:, :, 0:64], rd[:].unsqueeze(2).to_broadcast([128, 4, 64]), ALU.mult, [okey, ("rden4", ri)], [("tmpm", tb)])
                dst = szy[:, tl, h * 64:(h + 1) * 64]
                TT(dst, dst, tmpm[tb][:], ALU.mult, [("tmpm", tb)] + sk_, sk_)

            attention(QT[:, h, :], lambda kt, h=h: KT[:, h, kt * 128:(kt + 1) * 128], 128,
                      lambda kt, h=h: Vt[:, kt, h, 0:65], 65, plan, None,
                      lambda qc: [("QT", 4 * qc + u) for u in range(4)], lambda kt: [("KT", kt)], lambda kt: [("V", kt)],
                      ptb, "m", finish_chunk=finish_chunk)
        if stop == "ma":
            return
        outproj_half(szy, yT)

    def rstd_a(pp, pkey, ncol, tf, tfkey, s8i, s8key):
        nh = ncol // 64
        act(tf[:, 0:ncol], pp[:, 0:ncol], AF.Square, [pkey], [tfkey])
        RED(s8i[:, 0:nh], tf[:, 0:ncol].rearrange("p (h d) -> p h d", d=64), ALU.add, [tfkey], [s8key])
        TS(s8i[:, 0:nh], s8i[:, 0:nh], 64.0 * EPS, None, ALU.add, None, [s8key], [s8key])
        act(s8i[:, 0:nh], s8i[:, 0:nh], AF.Sqrt, [s8key], [s8key])

    def rstd_b(ncol, s8i, s8key):
        nh = ncol // 64
        RCP(s8i[:, 0:nh], s8i[:, 0:nh], [s8key], [s8key])

    def head_rstd(pp, pkey, ncol, tf, tfkey, s8i, s8key):
        rstd_a(pp, pkey, ncol, tf, tfkey, s8i, s8key)
        rstd_b(ncol, s8i, s8key)

    def pipe4(n, s0, s1, s2, s3):
        for step in range(n + 2):
            if step < n:
                s0(step)
                s1(step)
            if 1 <= step <= n:
                s2(step - 1)
            if step >= 2:
                s3(step - 2)

    def load_gain_col(dst, gd, mult):
        MSET(dst[:], 1.0, ["gq"])
        dma("sp", dst[0:64, :], gd.rearrange("o d -> d o"), "c", writes=["gq"], slow=True)
        if mult != 1.0:
            TS(dst[0:64, :], dst[0:64, :], mult, None, ALU.mult, None, ["gq"], ["gq"])

    def outproj_half(szy, yT, store=False):
        def oa(i):
            pt = pb[i % 2]
            for c in range(2):
                tr(pt[:, c * 128:(c + 1) * 128], szy[:, i, c * 128:(c + 1) * 128], [("szy", i)], [("pb", i % 2)])
            y = yT[i % 2]
            act(y[:], pt[:, 0:256].rearrange("p (c t) -> p c t", c=2), AF.Copy, [("pb", i % 2)], [("yT", i % 2)])

        def ob(i):
            y = yT[i % 2]
            outproj_tile(i, [y[:, 0, :], y[:, 1, :]], [0, 1], [("yT", i % 2)], (4, 5))
            if store:
                dma("sp", yv[:, i, :], x_sb[:, i, :], "out", reads=[("x", i)])

        lagged(NT, oa, ob)

    def plan_causal(qc):
        items = []
        for kt in range(4 * qc + 4):
            if kt < 4 * qc:
                items.append((kt, 0, 3, {}))
            else:
                m = kt - 4 * qc
                items.append((kt, m, 3, {m: tri[:]}))
        return items

    def plan_band(wt):
        def plan(qc):
            items = []
            for kt in range(max(0, 4 * qc - wt), 4 * qc + 4):
                jlo = max(0, kt - 4 * qc)
                jhi = min(3, kt + wt - 4 * qc)
                if jlo > jhi:
                    continue
                masks = {}
                if 0 <= kt - 4 * qc <= 3:
                    masks[kt - 4 * qc] = tri[:]
                if 0 <= kt + wt - 4 * qc <= 3:
                    masks[kt + wt - 4 * qc] = upm[:]
                items.append((kt, jlo, jhi, masks))
            return items
        return plan

    W1_OFF = 64 * 1024

    def prefetch_w1(war_keys=()):
        cvp = Carver(base=W1_OFF)
        wA = cvp.get([128, 32, 256], BF16)
        for kv, wd in enumerate((ckw1_d, cvw1_d)):
            wv = wd.rearrange("(l d) j -> d l j", d=64)
            for lq in range(4):
                dma("pool", wA[kv * 64:(kv + 1) * 64, lq * 8:(lq + 1) * 8, :], wv[:, lq * 8:(lq + 1) * 8, :], "w1", writes=["w1A"] + list(war_keys))
        return wA

    def layer1():
        W = owin_d
        wA = Carver(base=W1_OFF).get([128, 32, 256], BF16)
        if 0 not in layers:
            prefetch_w1()
        cvc = Carver(base=16 * 1024)
        wB = cvc.get([128, 32, 256], BF16)
        dma("sp", wB[64:128, :, :], wA[0:64, :, :], "c", writes=["w1B"])
        dma("sp", wB[0:64, :, :], wA[64:128, :, :], "c", writes=["w1B"])
        w1 = [[wA, wB], [wB, wA]]
        B = compress_loads(W, cvc)
        norm_phase(1)
        compress_stage(W, B, w1)
        P.barrier()
        if stop == "cmp":
            return
        for g in range(2):
            nsa_half(g, W)
            P.barrier()
            if stop == "nsa0":
                return
        if stop == "nsa":
            return
        for g in range(2):
            swa_half(g, W)
            P.barrier()

    def compress_loads(W, cv):
        s = load_w(W, [(512, 128), (640, 128)])
        B = {}
        B["s"] = s
        B["KVD"] = [cv.get([128, 16, 128], BF16) for _ in range(2)]
        B["w2"] = [cv.get([128, 2, 64], BF16) for _ in range(2)]
        B["posn"] = cv.get([32, 2, 64], F32)
        B["posT"] = cv.get([64, 2, 32], BF16)
        B["stgb"] = cv.get([4, 128], F32)
        B["b1sb"] = cv.get([128, 4], F32)
        B["biasj"] = cv.get([128, 4], F32)
        B["b2bc"] = cv.get([128, 2, 64], F32)
        B["gcm"] = cv.get([128, 64], F32)
        B["kalc"] = cv.get([128, 36], BF16)
        B["hid"] = [cv.get([128, 2, 128], BF16) for _ in range(4)]
        B["kcf"] = cv.get([128, 64], F32)
        B["junk2"] = cv.get([128, 64], F32)
        B["ssc"] = cv.get([128, 1], F32)
        B["kaug"] = cv.get([128, 128], BF16)
        for kv, wd in enumerate((ckw2_d, cvw2_d)):
            dma("pool", B["w2"][kv][:], wd.rearrange("(c p) d -> p c d", p=128), "w2", writes=["w2"])
        dma("sp", B["posn"][:, 0, :], cposk_d, "c", writes=["posn"])
        dma("sp", B["posn"][:, 1, :], cposv_d, "c", writes=["posn"])
        dma("sp", B["stgb"][0:2, :], ckb1_d.rearrange("(a c) -> a c", c=128), "c", writes=["stgb"])
        dma("sp", B["stgb"][2:4, :], cvb1_d.rearrange("(a c) -> a c", c=128), "c", writes=["stgb"])
        dma("sp", B["b2bc"][:, 0, :], ckb2_d.partition_broadcast(128), "c", writes=["b2bc"])
        dma("sp", B["b2bc"][:, 1, :], cvb2_d.partition_broadcast(128), "c", writes=["b2bc"])
        dma("sp", B["gcm"][:], ckc_d.partition_broadcast(128), "c", writes=["gcm"])
        dma("pool", B["kalc"][:], kalc_d, "c", writes=["kalc"])
        for g in range(2):
            dma("pool", Vca[:, g, 65:97], ovl_d, "c", writes=[("Vca", g)])
        return B

    def compress_stage(W, B, w1):
        s = B["s"]
        KVD, w2, posn, posT, stgb, b1sb, biasj = B["KVD"], B["w2"], B["posn"], B["posT"], B["stgb"], B["b1sb"], B["biasj"]
        b2bc, gcm, kalc, hid, kcf, junk2, ssc, kaug = B["b2bc"], B["gcm"], B["kalc"], B["hid"], B["kcf"], B["junk2"], B["ssc"], B["kaug"]
        for g in range(2):
            MSET(Vca[:, g, 64:65], 1.0, [("Vca", g)])
        MSET(kaug[:], 0.0, ["kaug"])
        for kv in range(2):
            tr(pf[0][0:64, kv * 32:(kv + 1) * 32], posn[:, kv, :], ["posn"], [("pf", 0)], idn=identf[0:32, 0:32])
        tr(pf[0][:, 64:68], stgb[:], ["stgb"], [("pf", 0)], idn=identf[0:4, 0:4])
        CPY(posT[:], pf[0][0:64, 0:64].rearrange("p (k l) -> p k l", l=32), [("pf", 0)], ["posT"])
        CPY(b1sb[:], pf[0][:, 64:68], [("pf", 0)], ["b1sb"])
        for kv in range(2):
            for jc in range(2):
                col = kv * 2 + jc
                for l in range(32):
                    mm(pf[1][:, col:col + 1], w1[kv][0][0:64, l, jc * 128:(jc + 1) * 128], posT[0:64, kv, l:l + 1], l == 0, l == 31,
                       ["w1B", "posT"], [("pf", 1)])
        TT(biasj[:], pf[1][:, 0:4], b1sb[:], ALU.add, [("pf", 1), "b1sb"], ["biasj"])
        for tq in range(4):
            for which in range(2):
                pp = pf[2 + which]
                for k in range(8):
                    mm(pp[:], wbuf[s][:, k, which * 128:(which + 1) * 128], hT[:, k, tq * 512:(tq + 1) * 512], k == 0, k == 7,
                       [("w", s)] + [("hT", 4 * tq + u) for u in range(4)], [("pf", 2 + which)])
                act(KVD[which][:, :, tq * 32:(tq + 1) * 32].rearrange("p r m -> p m r"), pp[:].rearrange("p (m r) -> p m r", r=16),
                    AF.Copy, [("pf", 2 + which)], [("KVT", which)])
        n = 0
        for kv in range(2):
            for g in range(2):
                hb_ = hid[kv * 2 + g]
                for jc in range(2):
                    pp = pf[n % 2]
                    for l in range(32):
                        mm(pp[:, 0:127], w1[kv][g][g * 64:(g + 1) * 64, l, jc * 128:(jc + 1) * 128],
                           KVD[kv][g * 64:(g + 1) * 64, l % 16, (l // 16):(l // 16) + 127], l == 0, l == 31,
                           ["w1B", ("KVT", kv)], [("pf", n % 2)])
                    act(hb_[:, jc, 0:127], pp[:, 0:127], AF.Silu, [("pf", n % 2), "biasj"], [("hid", kv * 2 + g)],
                        bias=biasj[:, kv * 2 + jc:kv * 2 + jc + 1], scale=1.0)
                    n += 1
                po = pf[2 + g]
                for jc in range(2):
                    mm(po[0:127, 0:64], hb_[:, jc, 0:127], w2[kv][:, jc, :], jc == 0, jc == 1, [("hid", kv * 2 + g), "w2"], [("pf", 2 + g)])
                if kv == 0:
                    TT(kcf[0:127, :], po[0:127, 0:64], b2bc[0:127, 0, :], ALU.add, [("pf", 2 + g), "b2bc"], ["kcf"])
                    act(junk2[0:127, :], kcf[0:127, :], AF.Square, ["kcf"], ["junk2", "ssc"], accum_out=ssc[0:127, :])
                    TS(ssc[0:127, :], ssc[0:127, :], 1.0 / 64, EPS, ALU.mult, ALU.add, ["ssc"], ["ssc"])
                    act(ssc[0:127, :], ssc[0:127, :], AF.Sqrt, ["ssc"], ["ssc"])
                    RCP(ssc[0:127, :], ssc[0:127, :], ["ssc"], ["ssc"])
                    STT(kaug[0:127, 0:64], kcf[0:127, :], ssc[0:127, :], gcm[0:127, :], ALU.mult, ALU.mult, ["kcf", "ssc", "gcm"], ["kaug"])
                    CPY(kaug[:, 64:100], kalc[:], ["kalc"], ["kaug"])
                    tr(pb[g][:, 0:128], kaug[:], ["kaug"], [("pb", g)])
                    act(KTc[:, g, :], pb[g][:, 0:128], AF.Copy, [("pb", g)], [("KTc", g)])
                else:
                    TT(Vca[0:127, g, 0:64], po[0:127, 0:64], b2bc[0:127, 1, :], ALU.add, [("pf", 2 + g), "b2bc"], [("Vca", g)])

    def nsa_half(g, W):
        cv = Carver()
        QT = cv.get([128, 4, S], BF16)
        KTs = cv.get([128, S], BF16)
        KTw = cv.get([128, S], BF16)
        Vs = cv.get([128, NT, 66], BF16)
        Vw = cv.get([128, NT, 66], BF16)
        szy = cv.get([128, NT, 256], BF16)
        gates = cv.get([128, NT, 12], F32)
        oacc = cv.get([128, NT, 256], F32)
        imp = cv.get([128, 8, 32], F32)
        impc = cv.get([128, 8, 32], F32)
        cmn = cv.get([128, S], BF16)
        qa = [cv.get([128, 4, 128], BF16) for _ in range(3)]
        ksa = [cv.get([128, 128], BF16) for _ in range(3)]
        kwa = [cv.get([128, 128], BF16) for _ in range(3)]
        ptb = [cv.get([128, 512], BF16) for _ in range(3)]
        tmpf = [cv.get([128, 512], F32) for _ in range(2)]
        cmpb = cv.get([128, 32, 32], F32)
        rank = cv.get([128, 32], F32)
        nmt = cv.get([128, 8, 32], BF16)
        s8 = [cv.get([128, 8], F32) for _ in range(3)]
        gq = cv.get([128, 1], F32)
        gks = cv.get([128, 1], F32)
        gkw = cv.get([128, 1], F32)
        rden = [cv.get([128, 1], F32) for _ in range(8)]
        rden4 = [cv.get([128, 4], F32) for _ in range(8)]
        tmp32 = cv.get([128, 4, 32], F32)
        yT = [cv.get([128, 2, 128], BF16) for _ in range(2)]
        s = load_w(W, [(g * 256, 256), (768 + 64 * g, 64), (1024 + 64 * g, 64), (896 + 64 * g, 64), (1152 + 64 * g, 64)])
        s2 = load_w(W, [(1304 + g * 256, 256), (1280 + 4 * g, 4), (1288 + 4 * g, 4), (1296 + 4 * g, 4)])
        load_wo(1, 2 * g, 2)
        load_gain_col(gq, cq_d, 1.0)
        load_gain_col(gks, cks_d, 8.0)
        load_gain_col(gkw, ckw_d, 8.0)
        dma("sp", impc[:], impc_d[:, 8:16, :], "c", writes=["impc"])
        dma("pool", cmn[:], cmn_d, "c", writes=["addmask"])
        for b in range(3):
            MSET(qa[b][:, :, 64:128], 0.0, [("qa", b)])
            MSET(ksa[b][:, 64:128], 0.0, [("ksa", b)])
            MSET(kwa[b][:, 64:128], 0.0, [("kwa", b)])
        MSET(Vs[:, :, 64:65], 1.0, ["Vs"])
        MSET(Vw[:, :, 64:65], 1.0, ["Vw"])
        MSET(nmt[:], 0.0, [("nmt", i) for i in range(8, NT)])
        def pa(i):
            b2 = i % 2
            pp = pf[b2]
            qai, ksi, kwi = qa[b2], ksa[b2], kwa[b2]
            CPY(qai[:, :, 96:100], qal[:, i, g * 16:g * 16 + 16].rearrange("p (h c) -> p h c", c=4), ["const"], [("qa", b2)])
            CPY(ksi[:, 64:100], kals[:, i, :], ["const"], [("ksa", b2)])
            CPY(kwi[:, 64:100], kalp[:, i, :], ["const"], [("kwa", b2)])
            for k in range(8):
                mm(pp[:], hT[:, k, i * 128:(i + 1) * 128], wbuf[s][:, k, :], k == 0, k == 7, [("hT", i), ("w", s)], [("pf", b2)])
            tf = tmpf[b2]
            head_rstd(pp, ("pf", b2), 384, tf, ("tf", b2), s8[b2], ("s8", b2))
            TT(qai[:, :, 0:64], pp[:, 0:256].rearrange("p (h d) -> p h d", d=64), s8[b2][:, 0:4].unsqueeze(2).to_broadcast([128, 4, 64]),
               ALU.mult, [("pf", b2), ("s8", b2)], [("qa", b2)])
            TS(ksi[:, 0:64], pp[:, 256:320], s8[b2][:, 4:5], None, ALU.mult, None, [("pf", b2), ("s8", b2)], [("ksa", b2)])
            TS(kwi[:, 0:64], pp[:, 320:384], s8[b2][:, 5:6], None, ALU.mult, None, [("pf", b2), ("s8", b2)], [("kwa", b2)])
            act(Vs[:, i, 0:64], pp[:, 384:448], AF.Copy, [("pf", b2), "Vs"], [("Vs", i)])
            act(Vw[:, i, 0:64], pp[:, 448:512], AF.Copy, [("pf", b2), "Vw"], [("Vw", i)])

        def pbk(i):
            b2 = i % 2
            qai, ksi, kwi = qa[b2], ksa[b2], kwa[b2]
            pt = pb[b2]
            for h in range(4):
                tr(pt[:, h * 128:(h + 1) * 128], qai[:, h, :], [("qa", b2)], [("pb", b2)])
            tr(pt[:, 512:640], ksi[:], [("ksa", b2)], [("pb", b2)])
            tr(pt[:, 640:768], kwi[:], [("kwa", b2)], [("pb", b2)])
            act(QT[:, :, i * 128:(i + 1) * 128], pt[:, 0:512].rearrange("p (h t) -> p h t", h=4), AF.Copy, [("pb", b2), "gq"], [("QT", i)],
                scale=gq[:, 0:1])
            act(KTs[:, i * 128:(i + 1) * 128], pt[:, 512:640], AF.Copy, [("pb", b2), "gq"], [("KTs", i)], scale=gks[:, 0:1])
            act(KTw[:, i * 128:(i + 1) * 128], pt[:, 640:768], AF.Copy, [("pb", b2), "gq"], [("KTw", i)], scale=gkw[:, 0:1])

        lagged(NT, pa, pbk)
        for i in range(NT):
            b2 = 2 + i % 2
            pp = pf[b2]
            for k in range(8):
                mm(pp[:, 0:256], hT[:, k, i * 128:(i + 1) * 128], wbuf[s2][:, k, 0:256], k == 0, k == 7, [("hT", i), ("w", s2)], [("pf", b2)])
            act(szy[:, i, :], pp[:, 0:256], AF.Silu, [("pf", b2)], [("szy", i)])
        for i in range(NT):
            b2 = 2 + i % 2
            pp = pf[b2]
            for k in range(8):
                mm(pp[:, 0:12], hT[:, k, i * 128:(i + 1) * 128], wbuf[s2][:, k, 256:268], k == 0, k == 7, [("hT", i), ("w", s2)], [("pf", b2)])
            act(gates[:, i, :], pp[:, 0:12], AF.Sigmoid, [("pf", b2)], [("gates", i)])
        if stop == "nsaproj":
            return
        qk = lambda qc: [("QT", 4 * qc + u) for u in range(4)]
        for r in range(4):
            def fin_cmp_chunk(qc, Obank, okey, r=r):
                ri = (qc + 4 * r) % 8
                rd = rden4[ri]
                Ov = Obank[:].rearrange("p (j c) -> p j c", c=128)
                tl = slice(4 * qc, 4 * qc + 4)
                gk_ = [("gates", i) for i in range(4 * qc, 4 * qc + 4)]
                ok_ = [("oacc", i) for i in range(4 * qc, 4 * qc + 4)]
                TS(rd[:], Ov[:, :, 64:65].rearrange("p j c -> p (j c)"), 1e-30, None, ALU.max, None, [okey], [("rden4", ri)])
                RCP(rd[:], rd[:], [("rden4", ri)], [("rden4", ri)])
                if qc >= 2:
                    ik_ = [("imp", i) for i in range(4 * qc, 4 * qc + 4)]
                    dst = imp[:, 4 * qc - 8:4 * qc - 4, :]
                    if r == 0:
                        TT(dst, Ov[:, :, 65:97], rd[:].unsqueeze(2).to_broadcast([128, 4, 32]), ALU.mult, [okey, ("rden4", ri)], ik_)
                    else:
                        TT(tmp32[:], Ov[:, :, 65:97], rd[:].unsqueeze(2).to_broadcast([128, 4, 32]), ALU.mult, [okey, ("rden4", ri)], ["tmp32"])
                        TT(dst, dst, tmp32[:], ALU.add, ["tmp32"] + ik_, ik_)
                TT(rd[:], rd[:], gates[:, tl, r:r + 1].rearrange("p j c -> p (j c)"), ALU.mult, [("rden4", ri)] + gk_, [("rden4", ri)])
                TT(oacc[:, tl, r * 64:(r + 1) * 64], Ov[:, :, 0:64], rd[:].unsqueeze(2).to_broadcast([128, 4, 64]), ALU.mult,
                   [okey, ("rden4", ri)], ok_)

            attention(QT[:, r, :], lambda kt: KTc[:, g, 0:127], 127, lambda kt: Vca[0:127, g, 0:97], 97,
                      lambda qc: [(0, 0, 3, {})], None, qk, lambda kt: [("KTc", g)], lambda kt: [("Vca", g)], ptb, "n", addmask=cmn,
                      finish_chunk=fin_cmp_chunk)
        if stop == "nsacmp":
            return
        def sel(i):
            im = imp[:, i - 8, :]
            TT(im, im, impc[:, i - 8, :], ALU.add, [("imp", i), "impc"], [("imp", i)])
            TT(cmpb[:], im.unsqueeze(1).to_broadcast([128, 32, 32]), im.unsqueeze(2).to_broadcast([128, 32, 32]), ALU.is_gt,
               [("imp", i)], ["cmpb"])
            RED(rank[:], cmpb[:], ALU.add, ["cmpb"], ["rank"])
            TS(nmt[:, i - 8, :], rank[:], 15.5, NEGM, ALU.is_ge, ALU.mult, ["rank"], [("nmt", i)])

        def sel_apply(i):
            pt = pb[i % 2]
            tr(pt[0:32, 0:128], nmt[:, i - 8, :], [("nmt", i)], [("pb", i % 2)])
            for r in range(4):
                act(QT[64:96, r, i * 128:(i + 1) * 128], pt[0:32, 0:128], AF.Copy, [("pb", i % 2)], [("QT", i)])

        def run_branch(KTx, Vx, kname, vname, plan, gcol, r):
            def fin_add_chunk(qc, Obank, okey, r=r, gcol=gcol):
                ri = (qc + 4 * r) % 8
                rd = rden4[ri]
                tb = ri % 2
                tmp = tmpf[tb][:, 0:256].rearrange("p (j c) -> p j c", c=64)
                Ov = Obank[:].rearrange("p (j c) -> p j c", c=128)
                tl = slice(4 * qc, 4 * qc + 4)
                gk_ = [("gates", i) for i in range(4 * qc, 4 * qc + 4)]
                ok_ = [("oacc", i) for i in range(4 * qc, 4 * qc + 4)]
                RCP(rd[:], Ov[:, :, 64:65].rearrange("p j c -> p (j c)"), [okey], [("rden4", ri)])
                TT(rd[:], rd[:], gates[:, tl, gcol + r:gcol + r + 1].rearrange("p j c -> p (j c)"), ALU.mult,
                   [("rden4", ri)] + gk_, [("rden4", ri)])
                TT(tmp, Ov[:, :, 0:64], rd[:].unsqueeze(2).to_broadcast([128, 4, 64]), ALU.mult, [okey, ("rden4", ri)], [("tf", tb)])
                dst = oacc[:, tl, r * 64:(r + 1) * 64]
                TT(dst, dst, tmp, ALU.add, [("tf", tb)] + ok_, ok_)


            attention(QT[:, r, :], lambda kt, KTx=KTx: KTx[:, kt * 128:(kt + 1) * 128], 128,
                      lambda kt, Vx=Vx: Vx[:, kt, 0:65], 65, plan, None, qk,
                      lambda kt, kname=kname: [(kname, kt)], lambda kt, vname=vname: [(vname, kt)], ptb, "n",
                      finish_chunk=fin_add_chunk)

        for r in range(4):
            sel(8 + 2 * r)
            sel(9 + 2 * r)
            run_branch(KTw, Vw, "KTw", "Vw", plan_band(4), 8, r)
        for i in range(8, NT):
            sel_apply(i)
        for r in range(4):
            run_branch(KTs, Vs, "KTs", "Vs", plan_causal, 4, r)
        for i in range(NT):
            TT(szy[:, i, :], oacc[:, i, :], szy[:, i, :], ALU.mult, [("oacc", i), ("szy", i)], [("szy", i)])
        outproj_half(szy, yT)

    def swa_half(g, W):
        cv = Carver()
        QT = cv.get([128, 4, S], BF16)
        KT = cv.get([128, S], BF16)
        Vt = cv.get([128, NT, 66], BF16)
        szy = cv.get([128, NT, 256], BF16)
        qa = [cv.get([128, 4, 128], BF16) for _ in range(3)]
        ka = [cv.get([128, 128], BF16) for _ in range(3)]
        ptb = [cv.get([128, 512], BF16) for _ in range(3)]
        tmpf = [cv.get([128, 512], F32) for _ in range(2)]
        s8 = [cv.get([128, 8], F32) for _ in range(3)]
        gq = cv.get([128, 1], F32)
        gk = cv.get([128, 1], F32)
        esink = cv.get([128, 8], F32)
        rden = [cv.get([128, 1], F32) for _ in range(8)]
        rden4 = [cv.get([128, 4], F32) for _ in range(8)]
        yT = [cv.get([128, 2, 128], BF16) for _ in range(2)]
        load_gain_col(gq, dq_d, 1.0)
        load_gain_col(gk, dk_d, 8.0)
        dma("sp", esink[:], dsink_d.partition_broadcast(128), "c", writes=["esink"])
        act(esink[:], esink[:], AF.Exp, ["esink"], ["esink"])
        for b in range(3):
            MSET(qa[b][:, :, 64:128], 0.0, [("qa", b)])
            MSET(ka[b][:, 64:128], 0.0, [("ka", b)])
        MSET(Vt[:, :, 64:65], 1.0, ["Vt"])
        s = load_w(W, [(1816 + g * 256, 256), (2328 + 64 * g, 64), (2456 + 64 * g, 64)])
        s2 = load_w(W, [(2584 + g * 256, 256)])
        load_wo(1, 4 + 2 * g, 2)
        def pa(i):
            b2 = i % 2
            pp = pf[b2]
            qai, kai = qa[b2], ka[b2]
            CPY(qai[:, :, 96:100], qal[:, i, g * 16:g * 16 + 16].rearrange("p (h c) -> p h c", c=4), ["const"], [("qa", b2)])
            CPY(kai[:, 64:100], kalp[:, i, :], ["const"], [("ka", b2)])
            for k in range(8):
                mm(pp[:, 0:384], hT[:, k, i * 128:(i + 1) * 128], wbuf[s][:, k, 0:384], k == 0, k == 7, [("hT", i), ("w", s)], [("pf", b2)])
            tf = tmpf[b2]
            head_rstd(pp, ("pf", b2), 320, tf, ("tf", b2), s8[b2], ("s8", b2))
            TT(qai[:, :, 0:64], pp[:, 0:256].rearrange("p (h d) -> p h d", d=64), s8[b2][:, 0:4].unsqueeze(2).to_broadcast([128, 4, 64]),
               ALU.mult, [("pf", b2), ("s8", b2)], [("qa", b2)])
            TS(kai[:, 0:64], pp[:, 256:320], s8[b2][:, 4:5], None, ALU.mult, None, [("pf", b2), ("s8", b2)], [("ka", b2)])
            act(Vt[:, i, 0:64], pp[:, 320:384], AF.Copy, [("pf", b2), "Vt"], [("V", i)])

        def pbk(i):
            b2 = i % 2
            qai, kai = qa[b2], ka[b2]
            pt = pb[b2]
            for h in range(4):
                tr(pt[:, h * 128:(h + 1) * 128], qai[:, h, :], [("qa", b2)], [("pb", b2)])
            tr(pt[:, 512:640], kai[:], [("ka", b2)], [("pb", b2)])
            act(QT[:, :, i * 128:(i + 1) * 128], pt[:, 0:512].rearrange("p (h t) -> p h t", h=4), AF.Copy, [("pb", b2), "gq"], [("QT", i)],
                scale=gq[:, 0:1])
            act(KT[:, i * 128:(i + 1) * 128], pt[:, 512:640], AF.Copy, [("pb", b2), "gq"], [("KT", i)], scale=gk[:, 0:1])

        lagged(NT, pa, pbk)
        for i in range(NT):
            b2 = 2 + i % 2
            pp = pf[b2]
            for k in range(8):
                mm(pp[:, 0:256], hT[:, k, i * 128:(i + 1) * 128], wbuf[s2][:, k, 0:256], k == 0, k == 7, [("hT", i), ("w", s2)], [("pf", b2)])
            act(szy[:, i, :], pp[:, 0:256], AF.Silu, [("pf", b2)], [("szy", i)])
        for r in range(4):
            def fin_chunk(qc, Obank, okey, r=r):
                ri = (qc + 4 * r) % 8
                rd = rden4[ri]
                tb = ri % 2
                tmp = tmpf[tb][:, 0:256].rearrange("p (j c) -> p j c", c=64)
                Ov = Obank[:].rearrange("p (j c) -> p j c", c=128)
                tl = slice(4 * qc, 4 * qc + 4)
                sk_ = [("szy", i) for i in range(4 * qc, 4 * qc + 4)]
                TS(rd[:], Ov[:, :, 64:65].rearrange("p j c -> p (j c)"), esink[:, 4 * g + r:4 * g + r + 1], None, ALU.add, None,
                   [okey, "esink"], [("rden4", ri)])
                RCP(rd[:], rd[:], [("rden4", ri)], [("rden4", ri)])
                TT(tmp, Ov[:, :, 0:64], rd[:].unsqueeze(2).to_broadcast([128, 4, 64]), ALU.mult, [okey, ("rden4", ri)], [("tf", tb)])
                dst = szy[:, tl, r * 64:(r + 1) * 64]
                TT(dst, dst, tmp, ALU.mult, [("tf", tb)] + sk_, sk_)

            attention(QT[:, r, :], lambda kt: KT[:, kt * 128:(kt + 1) * 128], 128, lambda kt: Vt[:, kt, 0:65], 65,
                      plan_band(1), None, lambda qc: [("QT", 4 * qc + u) for u in range(4)], lambda kt: [("KT", kt)],
                      lambda kt: [("V", kt)], ptb, "d", finish_chunk=fin_chunk)
        outproj_half(szy, yT, store=(g == 1 and stop is None))

    if 0 in layers:
        layer0()
    if 1 in layers:
        layer1()

    if not (1 in layers and stop is None):
        for i in range(NT):
            dma("sp", yv[:, i, :], x_sb[:, i, :], "out", reads=[("x", i)])
    P.finish("sp", list(P.chan_count.keys()))
    P.emit()
    es.close()
    return nc


_CACHE = {}


def kernel(**inputs):
    consts = _consts()
    shared = {}
    shared["norm_g"] = np.ascontiguousarray(inputs["norm_g"], dtype=np.float32)
    shared["w_out"] = np.ascontiguousarray(inputs["w_out"], dtype=np.float32)
    shared["e_w_in"] = np.ascontiguousarray(inputs["e_w_in"][0], dtype=np.float32)
    shared["a_conv_w"] = np.ascontiguousarray(inputs["a_conv_w"][0], dtype=np.float32)
    shared["a_conv_b"] = np.ascontiguousarray(inputs["a_conv_b"][0], dtype=np.float32)
    shared["a_ln_g"] = np.ascontiguousarray(inputs["a_ln_g"][0], dtype=np.float32)
    shared["a_ln_b"] = np.ascontiguousarray(inputs["a_ln_b"][0], dtype=np.float32)
    shared["b_qnorm_g"] = np.ascontiguousarray(inputs["b_qnorm_g"], dtype=np.float32)
    shared["b_knorm_g"] = np.ascontiguousarray(inputs["b_knorm_g"], dtype=np.float32)
    for k in ("ident", "tri", "up", "qal", "kal_moba", "kal_slc", "kal_plain", "kal_cmp", "cmaskneg", "ovl", "impc"):
        shared[k] = consts[k]
    shared["o_w_in"] = np.ascontiguousarray(inputs["o_w_in"][0], dtype=np.float32)
    for k in ("c_qnorm_g", "c_knorm_cmp_g", "c_knorm_slc_g", "c_knorm_win_g", "c_k_b2", "c_v_b2", "d_qnorm_g", "d_knorm_g", "d_sinks"):
        shared[k] = np.ascontiguousarray(inputs[k], dtype=np.float32).reshape(1, -1)
    for k in ("c_pos_k", "c_pos_v", "c_k_w1", "c_k_b1", "c_k_w2", "c_v_w1", "c_v_b1", "c_v_w2"):
        shared[k] = np.ascontiguousarray(inputs[k][0], dtype=np.float32)
    x = np.ascontiguousarray(inputs["x"], dtype=np.float32)
    nb = x.shape[0]
    layers = inputs.get("_layers", (0, 1))
    nc = build_program(layers, inputs.get("_stop"))
    in_maps = [dict(shared, x=x[b]) for b in range(nb)]
    res = run_bass_kernel_spmd(nc, in_maps, core_ids=list(range(nb)))
    return np.stack([np.asarray(r["y"], dtype=np.float32) for r in res.results], axis=0)
```
